# Optimizing a Trainium2 kernel written in Bass

```python
import math
import jax, jax.numpy as jnp
from jax import lax
import numpy as np

D_MODEL = 1024
BATCH = 8
SEQ = 4096
DEPTH = 1

SSD_HEADS = 32
SSD_HEAD_DIM = 64
D_SSD = SSD_HEADS * SSD_HEAD_DIM
SSD_GROUPS = 8
HEADS_PER_GROUP = SSD_HEADS // SSD_GROUPS
D_STATE = 128
CONV_WIDTH = 4
CHUNK = 128
D_BC = SSD_GROUPS * D_STATE
D_CONV = D_SSD + 2 * D_BC
POOL_WINDOWS = (2, 4, 8, 16)
POOL_GROUPS = len(POOL_WINDOWS)
POOL_CH = 256
D_POOL = POOL_GROUPS * POOL_CH
D_MIX = D_SSD + D_POOL
D_IN = D_SSD + D_CONV + SSD_HEADS + D_POOL
N_EXPERTS = 32
TOP_K = 4
D_FF = 1024
SWIGLU_LIMIT = 7.0
SWIGLU_ALPHA = 1.702
EXPERT_BLOCK = 128
PLE_DIM = 256
EPS = 1e-6

kernel_name = 'hybrid_ssd_pool_moe_ple'


def rms_norm(x, g):
    xf = x.astype(jnp.float32)
    y = xf * lax.rsqrt(jnp.mean(xf * xf, axis=-1, keepdims=True) + EPS)
    return y.astype(x.dtype) * g


def ssd_chunked_scan(xd, a, Bm, Cm):
    b, L = xd.shape[:2]
    nc = L // CHUNK

    def to_chunks(t):
        return jnp.moveaxis(t.reshape((b, nc, CHUNK) + t.shape[2:]), 1, 0)

    causal = jnp.tril(jnp.ones((CHUNK, CHUNK), dtype=bool))[None, :, :, None, None]

    def step(state, inp):
        xd_c, a_c, B_c, C_c = inp
        a_cs = jnp.cumsum(a_c, axis=1)
        seg = a_cs[:, :, None] - a_cs[:, None, :]
        decay = jnp.exp(jnp.where(causal, seg, -jnp.inf))
        cb = jnp.einsum('bign,bjgn->bijg', C_c, B_c)
        y_diag = jnp.einsum('bijg,bijgr,bjgrp->bigrp', cb, decay, xd_c)
        y_off = jnp.einsum('bign,bgrpn,bigr->bigrp', C_c, state, jnp.exp(a_cs))
        a_tot = a_cs[:, -1]
        to_end = jnp.exp(a_tot[:, None] - a_cs)
        new_state = state * jnp.exp(a_tot)[..., None, None] + jnp.einsum(
            'bjgn,bjgr,bjgrp->bgrpn', B_c, to_end, xd_c)
        return new_state, y_diag + y_off

    state0 = jnp.zeros((b, SSD_GROUPS, HEADS_PER_GROUP, SSD_HEAD_DIM, D_STATE), jnp.float32)
    _, ys = lax.scan(step, state0, (to_chunks(xd), to_chunks(a), to_chunks(Bm), to_chunks(Cm)))
    return jnp.moveaxis(ys, 0, 1).reshape(b, L, SSD_GROUPS, HEADS_PER_GROUP, SSD_HEAD_DIM)


def ssd_mixer(z, xbc, dt, conv_w, conv_b, dt_bias, a_log, d_skip, norm_g):
    b, L, _ = z.shape
    f32 = jnp.float32
    xbc = lax.conv_general_dilated(
        xbc, conv_w[:, None, :], window_strides=(1,), padding=[(CONV_WIDTH - 1, 0)],
        dimension_numbers=('NWC', 'WIO', 'NWC'), feature_group_count=D_CONV)
    xbc = jax.nn.silu(xbc + conv_b)
    xs, Bm, Cm = jnp.split(xbc, [D_SSD, D_SSD + D_BC], axis=-1)
    xs = xs.reshape(b, L, SSD_GROUPS, HEADS_PER_GROUP, SSD_HEAD_DIM).astype(f32)
    Bm = Bm.reshape(b, L, SSD_GROUPS, D_STATE).astype(f32)
    Cm = Cm.reshape(b, L, SSD_GROUPS, D_STATE).astype(f32)
    dt = jax.nn.softplus((dt + dt_bias).astype(f32)).reshape(b, L, SSD_GROUPS, HEADS_PER_GROUP)
    A = -jnp.exp(a_log.astype(f32)).reshape(SSD_GROUPS, HEADS_PER_GROUP)
    y = ssd_chunked_scan(xs * dt[..., None], dt * A, Bm, Cm)
    y = y + d_skip.astype(f32).reshape(SSD_GROUPS, HEADS_PER_GROUP)[:, :, None] * xs
    y = y.reshape(b, L, D_SSD).astype(z.dtype) * jax.nn.silu(z)
    y = rms_norm(y.reshape(b, L, SSD_GROUPS, D_SSD // SSD_GROUPS),
                 norm_g.reshape(SSD_GROUPS, D_SSD // SSD_GROUPS))
    return y.reshape(b, L, D_SSD)


def pool_mixer(u, pool_w, pool_scale):
    b, L, _ = u.shape
    ug = u.reshape(b, L, POOL_GROUPS, POOL_CH).astype(jnp.float32)
    cs = jnp.pad(jnp.cumsum(ug, axis=1), ((0, 0), (1, 0), (0, 0), (0, 0)))
    pos = jnp.arange(L)
    means = []
    for gi, w in enumerate(POOL_WINDOWS):
        c = cs[:, :, gi]
        lagged = jnp.pad(c[:, :L + 1 - w], ((0, 0), (w - 1, 0), (0, 0)))
        cnt = jnp.minimum(pos + 1, w).astype(jnp.float32)
        means.append((c[:, 1:] - lagged) / cnt[None, :, None])
    pooled = jnp.stack(means, axis=2)
    y = (pooled - ug).astype(u.dtype)
    y = jnp.einsum('blgc,gcd->blgd', y, pool_w).reshape(b, L, D_POOL)
    return y * pool_scale


def moe(h, router_w, router_b, w_gate_up, b_gate_up, w_down, b_down):
    T = h.shape[0]
    TK = T * TOP_K
    logits = (h @ router_w + router_b).astype(jnp.float32)
    top_vals, top_idx = lax.top_k(logits, TOP_K)
    gates = jax.nn.softmax(top_vals, axis=-1)
    flat_e = top_idx.reshape(-1)
    order = jnp.argsort(flat_e)
    sorted_e = flat_e[order]
    tok = (order // TOP_K).astype(jnp.int32)
    counts = jnp.bincount(flat_e, length=N_EXPERTS)
    padded = ((counts + EXPERT_BLOCK - 1) // EXPERT_BLOCK) * EXPERT_BLOCK
    group_start = jnp.cumsum(counts) - counts
    padded_end = jnp.cumsum(padded)
    padded_start = padded_end - padded
    rank = jnp.arange(TK, dtype=jnp.int32) - group_start[sorted_e]
    dest = padded_start[sorted_e] + rank
    n_blocks = -(-TK // EXPERT_BLOCK) + N_EXPERTS
    n_slots = n_blocks * EXPERT_BLOCK
    slot_token = jnp.full((n_slots,), T, jnp.int32).at[dest].set(tok)
    slot_gate = jnp.zeros((n_slots,), jnp.float32).at[dest].set(gates.reshape(-1)[order])
    block_start = jnp.arange(n_blocks, dtype=jnp.int32) * EXPERT_BLOCK
    block_e = jnp.clip(jnp.searchsorted(padded_end, block_start, side='right'),
                       0, N_EXPERTS - 1).astype(jnp.int32)
    h_pad = jnp.concatenate([h, jnp.zeros((1, h.shape[1]), h.dtype)], axis=0)
    xs = h_pad[slot_token].reshape(n_blocks, EXPERT_BLOCK, h.shape[1])

    def expert_block(args):
        xb, e = args
        gu = xb @ w_gate_up[e] + b_gate_up[e]
        g, up = gu[:, :D_FF], gu[:, D_FF:]
        g = jnp.minimum(g, SWIGLU_LIMIT)
        up = jnp.clip(up, -SWIGLU_LIMIT, SWIGLU_LIMIT)
        act = (up + 1.0) * (g * jax.nn.sigmoid(SWIGLU_ALPHA * g))
        return act @ w_down[e] + b_down[e]

    ys = lax.map(expert_block, (xs, block_e)).reshape(n_slots, h.shape[1])
    ys = ys * slot_gate.astype(ys.dtype)[:, None]
    out = jnp.zeros((T + 1, h.shape[1]), ys.dtype).at[slot_token].add(ys)
    return out[:T]


def setup_inputs(seed: int = 0) -> dict:
    key = jax.random.key(seed)
    ks = jax.random.split(key, 26)
    f32 = jnp.float32

    def nrm(k, shape, scale):
        return jax.random.normal(k, shape, f32) * scale

    def gain(k, shape):
        return 1.0 + 0.05 * jax.random.normal(k, shape, f32)

    dt0 = jnp.exp(jax.random.uniform(ks[6], (DEPTH, SSD_HEADS), f32,
                                     math.log(1e-3), math.log(1e-1)))
    return {
        'x': nrm(ks[0], (BATCH, SEQ, D_MODEL), 1.0),
        'p': nrm(ks[1], (DEPTH, BATCH, SEQ, PLE_DIM), 1.0),
        'mix_norm_g': gain(ks[2], (DEPTH, D_MODEL)),
        'w_in': nrm(ks[3], (DEPTH, D_MODEL, D_IN), D_MODEL ** -0.5),
        'conv_w': nrm(ks[4], (DEPTH, CONV_WIDTH, D_CONV), CONV_WIDTH ** -0.5),
        'conv_b': nrm(ks[5], (DEPTH, D_CONV), 0.01),
        'dt_bias': dt0 + jnp.log(-jnp.expm1(-dt0)),
        'a_log': jnp.log(jax.random.uniform(ks[7], (DEPTH, SSD_HEADS), f32, 1.0, 16.0)),
        'd_skip': gain(ks[8], (DEPTH, SSD_HEADS)),
        'ssd_norm_g': gain(ks[9], (DEPTH, D_SSD)),
        'pool_w': nrm(ks[10], (DEPTH, POOL_GROUPS, POOL_CH, POOL_CH), POOL_CH ** -0.5),
        'pool_scale': gain(ks[11], (DEPTH, D_POOL)),
        'w_out': nrm(ks[12], (DEPTH, D_MIX, D_MODEL), D_MIX ** -0.5),
        'ffn_norm_g': gain(ks[13], (DEPTH, D_MODEL)),
        'router_w': nrm(ks[14], (DEPTH, D_MODEL, N_EXPERTS), D_MODEL ** -0.5),
        'router_b': nrm(ks[15], (DEPTH, N_EXPERTS), 0.01),
        'w_gate_up': nrm(ks[16], (DEPTH, N_EXPERTS, D_MODEL, 2 * D_FF), D_MODEL ** -0.5),
        'b_gate_up': nrm(ks[17], (DEPTH, N_EXPERTS, 2 * D_FF), 0.01),
        'w_down': nrm(ks[18], (DEPTH, N_EXPERTS, D_FF, D_MODEL), D_FF ** -0.5),
        'b_down': nrm(ks[19], (DEPTH, N_EXPERTS, D_MODEL), 0.01),
        'ple_gate_norm_g': gain(ks[20], (DEPTH, D_MODEL)),
        'w_ple_gate': nrm(ks[21], (DEPTH, D_MODEL, D_MODEL), D_MODEL ** -0.5),
        'w_ple_proj': nrm(ks[22], (DEPTH, PLE_DIM, D_MODEL), PLE_DIM ** -0.5),
        'ple_norm_g': gain(ks[23], (DEPTH, D_MODEL)),
        'final_norm_g': gain(ks[24], (D_MODEL,)),
    }


def reference(x, p, mix_norm_g, w_in, conv_w, conv_b, dt_bias, a_log, d_skip, ssd_norm_g,
              pool_w, pool_scale, w_out, ffn_norm_g, router_w, router_b, w_gate_up,
              b_gate_up, w_down, b_down, ple_gate_norm_g, w_ple_gate, w_ple_proj,
              ple_norm_g, final_norm_g):
    b, L, _ = x.shape
    for i in range(DEPTH):
        h = rms_norm(x, mix_norm_g[i])
        u = jnp.einsum('bld,de->ble', h, w_in[i])
        z, xbc, dt, pool_in = jnp.split(
            u, [D_SSD, D_SSD + D_CONV, D_SSD + D_CONV + SSD_HEADS], axis=-1)
        y_ssd = ssd_mixer(z, xbc, dt, conv_w[i], conv_b[i], dt_bias[i], a_log[i],
                          d_skip[i], ssd_norm_g[i])
        y_pool = pool_mixer(pool_in, pool_w[i], pool_scale[i])
        y_mix = jnp.concatenate([y_ssd, y_pool], axis=-1)
        x = x + jnp.einsum('ble,ed->bld', y_mix, w_out[i])
        h = rms_norm(x, ffn_norm_g[i]).reshape(b * L, D_MODEL)
        x = x + moe(h, router_w[i], router_b[i], w_gate_up[i], b_gate_up[i],
                    w_down[i], b_down[i]).reshape(b, L, D_MODEL)
        gate = jax.nn.sigmoid(rms_norm(x, ple_gate_norm_g[i]) @ w_ple_gate[i])
        e = rms_norm(p[i] @ w_ple_proj[i], ple_norm_g[i])
        x = x + e * gate
    return rms_norm(x, final_norm_g)
```

```python
from contextlib import ExitStack
import numpy as np
import concourse.bass as bass
import concourse.mybir as mybir
from concourse.bass_utils import run_bass_kernel_spmd

F32 = mybir.dt.float32
BF16 = mybir.dt.bfloat16
I32 = mybir.dt.int32
AF = mybir.ActivationFunctionType
ALU = mybir.AluOpType
AX = mybir.AxisListType

L = 4096
DM = 1024
NCH = 32
NSC = 8
D_IN = 7200
NE = 32
CAP = 1024
NBLK = CAP // 128
NSLOT = NE * CAP
SLOT_TAB = 128 * 257
EPS = 1e-6


class R:
    __slots__ = ("w", "rs")

    def __init__(self):
        self.w = None
        self.rs = []


class T:
    def __init__(self, t):
        self.t = t
        self.r = R()


class K:
    def __init__(self, nc, sems):
        self.nc = nc
        self.engs = {"pe": nc.tensor, "dve": nc.vector, "act": nc.scalar,
                     "pool": nc.gpsimd, "sp": nc.sync}
        self.psem = sems
        self.cnt = {k: 0 for k in self.engs}
        self.waited = {k: {} for k in self.engs}
        self.dma_cnt = {}
        self.dma_objs = {}
        self.ninst = 0
        self.nwaits = 0

    def wait(self, ek, ev):
        if ev is None:
            return
        sem, val = ev
        sid = id(sem)
        if sid in self.dma_cnt:
            val = max(val, self.dma_cnt[sid])
        w = self.waited[ek]
        if w.get(sid, 0) >= val:
            return
        self.engs[ek].wait_ge(sem, val)
        self.nwaits += 1
        w[sid] = val

    def _deps(self, ek, reads, writes, extra):
        for t in reads:
            self.wait(ek, t.r.w)
        for t in writes:
            self.wait(ek, t.r.w)
            for e in t.r.rs:
                self.wait(ek, e)
        for e in extra:
            self.wait(ek, e)

    def _commit(self, ev, reads, writes):
        for t in reads:
            t.r.rs.append(ev)
        for t in writes:
            t.r.w = ev
            t.r.rs = []

    def barrier(self):
        for ek in self.engs:
            for o in self.engs:
                if self.cnt[o] > 0:
                    self.wait(ek, (self.psem[o], self.cnt[o]))
            for sid_, sem_ in self.dma_objs.items():
                self.wait(ek, (sem_, self.dma_cnt[sid_]))

    def op(self, ek, fn, reads=(), writes=(), extra=()):
        self._deps(ek, reads, writes, extra)
        ins = fn(self.engs[ek])
        self.cnt[ek] += 1
        ins.then_inc(self.psem[ek], 1)
        ev = (self.psem[ek], self.cnt[ek])
        self._commit(ev, reads, writes)
        self.ninst += 1
        return ev

    def ops(self, ek, fns, reads=(), writes=(), extra=()):
        self._deps(ek, reads, writes, extra)
        ins = None
        for fn in fns:
            ins = fn(self.engs[ek])
            self.ninst += 1
        self.cnt[ek] += 1
        ins.then_inc(self.psem[ek], 1)
        ev = (self.psem[ek], self.cnt[ek])
        self._commit(ev, reads, writes)
        return ev

    def dma(self, ek, sem, fns, reads=(), writes=(), extra=()):
        self._deps(ek, reads, writes, extra)
        if not isinstance(fns, (list, tuple)):
            fns = [fns]
        sid = id(sem)
        cur = self.dma_cnt.get(sid, 0)
        for f in fns:
            f(self.engs[ek]).then_inc(sem, 16)
            cur += 16
            self.ninst += 1
        self.dma_cnt[sid] = cur
        self.dma_objs[sid] = sem
        ev = (sem, cur)
        self._commit(ev, reads, writes)
        return ev


def build(debug=None, phases="A0,A1,A1p,A2,B,C"):
    phases = set(phases.split(","))
    debug = debug or ()
    nc = bass.Bass("TRN2", target_bir_lowering=False)

    def din(name, shape, dt=F32):
        return nc.dram_tensor(name, list(shape), dt, kind="ExternalInput").ap()

    x_d = din("x", [L, DM])
    pin_d = din("p", [L, 256])
    w_in_d = din("w_in", [DM, D_IN])
    conv_wT_d = din("conv_wT", [128, 32, 4])
    conv_bT_d = din("conv_bT", [128, 32])
    dtb_d = din("dt_bias_bc", [128, 32])
    alog_d = din("a_log_bc", [128, 32])
    dsk_d = din("d_skip_bc", [128, 32])
    gmixT_d = din("gmixT", [128, 8])
    gssdT_d = din("gssdT", [128, 16])
    pool_w_d = din("pool_w", [4, 256, 256])
    pscT_d = din("pscT", [128, 8])
    invc_d = din("invc", [128, 4, 16])
    w_out_d = din("w_out", [3072, DM])
    gffn_bc_d = din("gffn_bc", [128, DM])
    rw_d = din("router_w", [DM, NE])
    rb_d = din("rb_bc", [128, NE])
    ebase_d = din("ebase_bc", [128, NE])
    wgu_d = din("w_gate_up", [NE, DM, 2048])
    bguT_d = din("bguT", [128, NE, 16])
    wd_d = din("w_down", [NE, DM, DM])
    bd_d = din("b_down", [NE, DM])
    gpgT_d = din("gpgT", [128, 8])
    wpg_d = din("w_ple_gate", [DM, DM])
    wpp_d = din("w_ple_proj", [256, DM])
    gpn_bc_d = din("gpn_bc", [128, DM])
    gfin_bc_d = din("gfin_bc", [128, DM])
    out_d = nc.dram_tensor("out", [L, DM], F32, kind="ExternalOutput").ap()

    def dscr(name, shape, dt, dbg=False):
        kind = "ExternalOutput" if (name in debug) else "Internal"
        return nc.dram_tensor(name, list(shape), dt, kind=kind).ap()

    ymT_d = dscr("ymT", [24, 128, L], BF16)
    x1_d = dscr("x1d", [L, DM], F32)
    h2_d = dscr("h2d", [L + 1, DM], BF16)
    G_d = dscr("Gd", [L + 1, NE], F32)
    slot_d = dscr("slotd", [SLOT_TAB, 2], I32)
    Y_d = dscr("Yd", [NSLOT + 1, DM], F32)

    with ExitStack() as es:
        E = es.enter_context
        sems = {k: E(nc.semaphore("prog_" + k)) for k in ["pe", "dve", "act", "pool", "sp"]}
        k = K(nc, sems)
        nsem = [0]

        def dsem():
            nsem[0] += 1
            return E(nc.semaphore("d%d" % nsem[0]))

        def sb(es_, name, shape, dt=F32):
            return T(es_.enter_context(nc.sbuf_tensor("s_" + name, list(shape), dt)))

        def ps(es_, name, shape, dt=F32):
            esz = 4 if dt == F32 else 2
            n = 1
            for d_ in shape[1:]:
                n *= d_
            per_bank = 2048 // esz
            nb_ = (n + per_bank - 1) // per_bank
            base = es_.enter_context(nc.psum_tensor("ps_" + name, [128, nb_ * per_bank], dt))
            v = base[:, 0:n]
            if len(shape) == 3:
                v = v.rearrange("p (a b) -> p a b", a=shape[1])
            return T(v)

        ident_bf = sb(es, "ident_bf", [128, 128], BF16)
        ident_f = sb(es, "ident_f", [128, 128], F32)
        Mle = sb(es, "Mle", [128, 128], F32)
        Mgt = sb(es, "Mgt", [128, 128], F32)
        Mlt = sb(es, "Mlt", [128, 128], F32)
        ones_f = sb(es, "ones_f", [128, 128], F32)
        dest_all = sb(es, "dest_all", [128, NCH, 4], I32)
        tokid = sb(es, "tokid", [128, NCH, 2], I32)
        zrow = sb(es, "zrow", [1, DM], F32)
        zrow_bf = sb(es, "zrow_bf", [1, DM], BF16)

        def dump(name, t, ap=None):
            if name not in debug:
                return
            a = ap if ap is not None else t.t[:]
            dd = nc.dram_tensor("dbg_" + name, list(a.shape), a.dtype, kind="ExternalOutput").ap()
            k.dma("sp", dsem(), lambda e: e.dma_start(out=dd, in_=a), reads=[t])

        def cst(t, fn):
            k.op("pool", fn, writes=[t])

        cst(ident_bf, lambda e: e.memset(ident_bf.t[:], 1.0))
        cst(ident_bf, lambda e: e.affine_select(out=ident_bf.t[:], in_=ident_bf.t[:], pattern=[[-1, 128]],
                                               compare_op=ALU.is_equal, fill=0.0, base=0, channel_multiplier=1))
        cst(ident_f, lambda e: e.memset(ident_f.t[:], 1.0))
        cst(ident_f, lambda e: e.affine_select(out=ident_f.t[:], in_=ident_f.t[:], pattern=[[-1, 128]],
                                              compare_op=ALU.is_equal, fill=0.0, base=0, channel_multiplier=1))
        cst(Mle, lambda e: e.memset(Mle.t[:], 1.0))
        cst(Mle, lambda e: e.affine_select(out=Mle.t[:], in_=Mle.t[:], pattern=[[1, 128]],
                                          compare_op=ALU.is_ge, fill=0.0, base=0, channel_multiplier=-1))
        cst(Mgt, lambda e: e.memset(Mgt.t[:], 1.0))
        cst(Mgt, lambda e: e.affine_select(out=Mgt.t[:], in_=Mgt.t[:], pattern=[[-1, 128]],
                                          compare_op=ALU.is_gt, fill=0.0, base=0, channel_multiplier=1))
        cst(Mlt, lambda e: e.memset(Mlt.t[:], 1.0))
        cst(Mlt, lambda e: e.affine_select(out=Mlt.t[:], in_=Mlt.t[:], pattern=[[1, 128]],
                                          compare_op=ALU.is_gt, fill=0.0, base=0, channel_multiplier=-1))
        cst(ones_f, lambda e: e.memset(ones_f.t[:], 1.0))
        cst(zrow, lambda e: e.memset(zrow.t[:], 0.0))
        cst(zrow_bf, lambda e: e.memset(zrow_bf.t[:], 0.0))
        cst(tokid, lambda e: e.iota(tokid.t[:], pattern=[[128, NCH], [0, 2]], base=0, channel_multiplier=1))
        cst(dest_all, lambda e: e.memset(dest_all.t[:], 0))

        sem_c = dsem()

        def load_const(es_, name, shape, src, dt=F32, q="sp"):
            t = sb(es_, name, shape, dt)
            k.dma(q, sem_c, lambda e: e.dma_start(out=t.t[:], in_=src), writes=[t])
            return t

        if "A0" in phases:
            with ExitStack() as esA:
                hT = sb(esA, "hT", [128, 8, L], BF16)
                dt_all = sb(esA, "dt_all", [128, NCH, 32], F32)
                a_all = sb(esA, "a_all", [128, NCH, 32], F32)
                e_all = sb(esA, "e_all", [128, NCH, 3, 32], F32)
                gmixT = load_const(esA, "gmixT", [128, 8], gmixT_d)
                gssdT = load_const(esA, "gssdT", [128, 16], gssdT_d)
                conv_wT = load_const(esA, "conv_wT", [128, 32, 4], conv_wT_d)
                conv_bT = load_const(esA, "conv_bT", [128, 32], conv_bT_d)
                dsk = load_const(esA, "dsk", [128, 32], dsk_d)
                pscT = load_const(esA, "pscT", [128, 8], pscT_d)
                invc = load_const(esA, "invc", [128, 4, 16], invc_d)

                with ExitStack() as e0:
                    dtb = load_const(e0, "dtb", [128, 32], dtb_d)
                    alog = load_const(e0, "alog", [128, 32], alog_d)
                    Abc = sb(e0, "Abc", [128, 32], F32)
                    wdt = sb(e0, "wdt", [128, 8, 32], BF16)
                    k.dma("pool", sem_c, lambda e: e.dma_start(
                        out=wdt.t[:], in_=w_in_d[:, 6144:6176].rearrange("(k p) n -> p k n", p=128)), writes=[wdt])
                    k.op("act", lambda e: e.activation(out=Abc.t[:], in_=alog.t[:], func=AF.Exp), reads=[alog], writes=[Abc])
                    k.op("dve", lambda e: e.tensor_scalar(out=Abc.t[:], in0=Abc.t[:], scalar1=-1.0, scalar2=None, op0=ALU.mult),
                         reads=[Abc], writes=[Abc])
                    xt = [sb(e0, "xt%d" % i, [128, DM], F32) for i in range(2)]
                    xsem = [dsem(), dsem()]
                    junk = sb(e0, "junk", [128, DM], F32)
                    ss = sb(e0, "ss", [128, 1], F32)
                    rstd = sb(e0, "rstd", [128, 1], F32)
                    xn = sb(e0, "xn", [128, DM], BF16)
                    tpA = ps(e0, "tpA", [128, 8, 128], BF16)
                    pdt = ps(e0, "pdt", [128, 32], F32)
                    pcs = ps(e0, "pcs", [128, 3, 32], F32)
                    dtr = sb(e0, "dtr", [128, 32], F32)
                    t1 = sb(e0, "t1", [128, 32], F32)
                    t2 = sb(e0, "t2", [128, 32], F32)
                    for c in range(NCH):
                        xc_ = xt[c % 2]
                        cs = slice(c * 128, (c + 1) * 128)
                        k.dma("sp", xsem[c % 2], lambda e: e.dma_start(out=xc_.t[:], in_=x_d[cs, :]), writes=[xc_])
                        k.op("act", lambda e: e.activation(out=junk.t[:], in_=xc_.t[:], func=AF.Square, accum_out=ss.t[:, 0:1]),
                             reads=[xc_], writes=[junk, ss])
                        k.op("act", lambda e: e.activation(out=rstd.t[:], in_=ss.t[:], func=AF.Sqrt, scale=1.0 / DM, bias=EPS),
                             reads=[ss], writes=[rstd])
                        k.op("dve", lambda e: e.reciprocal(out=rstd.t[:], in_=rstd.t[:]), reads=[rstd], writes=[rstd])
                        k.op("act", lambda e: e.activation(out=xn.t[:], in_=xc_.t[:], func=AF.Copy, scale=rstd.t[:, 0:1]),
                             reads=[xc_, rstd], writes=[xn])
                        k.ops("pe", [(lambda e, j=j: e.transpose(out=tpA.t[:, j, :], in_=xn.t[:, j * 128:(j + 1) * 128],
                                                                 identity=ident_bf.t[:])) for j in range(8)],
                              reads=[xn, ident_bf], writes=[tpA])
                        k.op("dve", lambda e: e.tensor_tensor(out=hT.t[:, :, cs], in0=tpA.t[:],
                                                              in1=gmixT.t[:, :, None].to_broadcast([128, 8, 128]), op=ALU.mult),
                             reads=[tpA, gmixT], writes=[hT])
                        k.ops("pe", [(lambda e, j=j: e.matmul(pdt.t[:], lhsT=hT.t[:, j, cs], rhs=wdt.t[:, j, :],
                                                              start=(j == 0), stop=(j == 7))) for j in range(8)],
                              reads=[hT, wdt], writes=[pdt])
                        k.op("dve", lambda e: e.tensor_tensor(out=dtr.t[:], in0=pdt.t[:], in1=dtb.t[:], op=ALU.add),
                             reads=[pdt, dtb], writes=[dtr])
                        k.op("act", lambda e: e.activation(out=t1.t[:], in_=dtr.t[:], func=AF.Abs),
                             reads=[dtr], writes=[t1])
                        k.op("act", lambda e: e.activation(out=t2.t[:], in_=t1.t[:], func=AF.Exp, scale=-1.0), reads=[t1], writes=[t2])
                        k.op("act", lambda e: e.activation(out=t1.t[:], in_=t2.t[:], func=AF.Ln, bias=1.0), reads=[t2], writes=[t1])
                        k.op("dve", lambda e: e.scalar_tensor_tensor(out=dt_all.t[:, c, :], in0=dtr.t[:], scalar=0.0, in1=t1.t[:],
                                                                     op0=ALU.max, op1=ALU.add),
                             reads=[dtr, t1], writes=[dt_all])
                        k.op("dve", lambda e: e.tensor_tensor(out=a_all.t[:, c, :], in0=dt_all.t[:, c, :], in1=Abc.t[:], op=ALU.mult),
                             reads=[dt_all, Abc], writes=[a_all])
                        k.ops("pe", [
                            lambda e: e.matmul(pcs.t[:, 0, :], lhsT=Mle.t[:], rhs=a_all.t[:, c, :], start=True, stop=True),
                            lambda e: e.matmul(pcs.t[:, 1, :], lhsT=Mgt.t[:], rhs=a_all.t[:, c, :], start=True, stop=True),
                            lambda e: e.matmul(pcs.t[:, 2, :], lhsT=ones_f.t[:], rhs=a_all.t[:, c, :], start=True, stop=True),
                        ], reads=[a_all, Mle, Mgt, ones_f], writes=[pcs])
                        k.op("act", lambda e: e.activation(out=e_all.t[:, c, :, :], in_=pcs.t[:], func=AF.Exp),
                             reads=[pcs], writes=[e_all])

                    k.barrier()
                dump("hT", hT); dump("dt_all", dt_all); dump("a_all", a_all); dump("e_all", e_all)
                if "A1" in phases:
                    with ExitStack() as e1:
                        wg = [sb(e1, "wg%d" % i, [128, 8, 768], BF16) for i in range(2)]
                        wsem = [dsem(), dsem()]
                        dg = [sb(e1, "dg%d" % i, [128, 4, 4, 128], BF16) for i in range(2)]
                        ust = [sb(e1, "ust%d" % i, [128, 4, 515], BF16) for i in range(2)]
                        xc = [sb(e1, "xc%d" % i, [128, 4, 512], BF16) for i in range(2)]
                        state = sb(e1, "state", [128, 256], F32)
                        state_bf = sb(e1, "state_bf", [128, 256], BF16)
                        ynT = [sb(e1, "ynT%d" % i, [128, 2, 512], BF16) for i in range(2)]
                        ysem = [dsem(), dsem()]
                        NB = 2
                        sz = [sb(e1, "sz%d" % i, [128, 256], BF16) for i in range(NB)]
                        xbtm = [sb(e1, "xbtm%d" % i, [128, 384], BF16) for i in range(NB)]
                        xd = [sb(e1, "xd%d" % i, [128, 4, 64], BF16) for i in range(NB)]
                        xde = [sb(e1, "xde%d" % i, [128, 4, 64], BF16) for i in range(NB)]
                        cbm = [sb(e1, "cbm%d" % i, [128, 128], F32) for i in range(NB)]
                        lh = [sb(e1, "lh%d" % i, [128, 4, 128], F32) for i in range(NB)]
                        Ex = [sb(e1, "Ex%d" % i, [128, 4, 128], F32) for i in range(NB)]
                        MT = [sb(e1, "MT%d" % i, [128, 4, 128], BF16) for i in range(NB)]
                        y1 = [sb(e1, "y1_%d" % i, [128, 4, 64], F32) for i in range(NB)]
                        tD = [sb(e1, "tD%d" % i, [128, 4, 64], F32) for i in range(NB)]
                        y4 = [sb(e1, "y4_%d" % i, [128, 256], F32) for i in range(NB)]
                        yb = [sb(e1, "yb%d" % i, [128, 256], BF16) for i in range(NB)]
                        junk2 = sb(e1, "junk2", [128, 256], F32)
                        ss2 = [sb(e1, "ss2_%d" % i, [128, 1], F32) for i in range(NB)]
                        rs2 = [sb(e1, "rs2_%d" % i, [128, 1], F32) for i in range(NB)]
                        p_u = [ps(e1, "p_u%d" % i, [128, 512], F32) for i in range(2)]
                        p_z = ps(e1, "p_z", [128, 256], F32)
                        p_tp = ps(e1, "p_tp", [128, 640], BF16)
                        p_cb = ps(e1, "p_cb", [128, 128], F32)
                        p_seg = ps(e1, "p_seg", [128, 4, 128], F32)
                        p_y = ps(e1, "p_y", [128, 512], F32)
                        p_s = ps(e1, "p_s", [128, 256], F32)

                        def load_wg(g):
                            w = wg[g % 2]
                            srcs = [(0, 2048 + g * 256, 256), (256, 4096 + g * 128, 128),
                                    (384, 5120 + g * 128, 128), (512, g * 256, 256)]
                            k.dma("pool", wsem[g % 2],
                                  [(lambda e, o=o, s=s, n=n: e.dma_start(
                                      out=w.t[:, :, o:o + n],
                                      in_=w_in_d[:, s:s + n].rearrange("(k p) n -> p k n", p=128))) for (o, s, n) in srcs],
                                  writes=[w])

                        load_wg(0)
                        for g in range(8):
                            if g + 1 < 8:
                                load_wg(g + 1)
                            w = wg[g % 2]
                            d_ = dg[g % 2]
                            chunks = [2 * g, 2 * g + 1, 16 + g, 24 + g]
                            g4 = slice(g * 4, g * 4 + 4)
                            k.ops("pool", [(lambda e, cc=cc, kk=kk: e.tensor_scalar(
                                out=d_.t[:, cc, kk, :], in0=ident_bf.t[:], scalar1=conv_wT.t[:, chunks[cc], kk:kk + 1],
                                scalar2=None, op0=ALU.mult)) for cc in range(4) for kk in range(4)],
                                reads=[ident_bf, conv_wT], writes=[d_])
                            k.op("pool", lambda e: e.memset(state.t[:], 0.0), writes=[state])
                            k.op("pool", lambda e: e.memset(state_bf.t[:], 0.0), writes=[state_bf])
                            k.op("pool", lambda e: e.memset(ust[0].t[:, :, 0:3], 0.0), writes=[ust[0]])
                            for sc in range(NSC):
                                ts = sc * 512
                                us = ust[sc % 2]
                                un = ust[(sc + 1) % 2]
                                xo = xc[sc % 2]
                                yo = ynT[sc % 2]
                                for cc in range(4):
                                    pu = p_u[cc % 2]
                                    k.ops("pe", [(lambda e, j=j: e.matmul(pu.t[:], lhsT=w.t[:, j, cc * 128:(cc + 1) * 128],
                                                                          rhs=hT.t[:, j, ts:ts + 512], start=(j == 0), stop=(j == 7)))
                                                 for j in range(8)], reads=[w, hT], writes=[pu])
                                    k.op("act", lambda e: e.copy(out=us.t[:, cc, 3:515], in_=pu.t[:]), reads=[pu], writes=[us])
                                if sc + 1 < NSC:
                                    k.op("pool", lambda e: e.tensor_copy(out=un.t[:, :, 0:3], in_=us.t[:, :, 512:515]),
                                         reads=[us], writes=[un])
                                for cc in range(4):
                                    pu = p_u[cc % 2]
                                    k.ops("pe", [(lambda e, kk=kk: e.matmul(pu.t[:], lhsT=d_.t[:, cc, kk, :],
                                                                            rhs=us.t[:, cc, kk:kk + 512], start=(kk == 0), stop=(kk == 3)))
                                                 for kk in range(4)], reads=[d_, us], writes=[pu])
                                    k.op("act", lambda e: e.activation(out=xo.t[:, cc, :], in_=pu.t[:], func=AF.Silu,
                                                                       bias=conv_bT.t[:, chunks[cc]:chunks[cc] + 1]),
                                         reads=[pu, conv_bT], writes=[xo])
                                if g == 0 and sc == 0:
                                    dump("xo", xo); dump("ust", us)
                                for q in range(4):
                                    c = sc * 4 + q
                                    b = c % NB
                                    qs = slice(q * 128, (q + 1) * 128)
                                    cs = slice(c * 128, (c + 1) * 128)
                                    k.ops("pe", [(lambda e, j=j: e.matmul(p_z.t[:], lhsT=hT.t[:, j, cs], rhs=w.t[:, j, 512:768],
                                                                          start=(j == 0), stop=(j == 7))) for j in range(8)],
                                          reads=[hT, w], writes=[p_z])
                                    k.op("act", lambda e: e.activation(out=sz[b].t[:], in_=p_z.t[:], func=AF.Silu),
                                         reads=[p_z], writes=[sz[b]])
                                    k.ops("pe", [(lambda e, cc=cc: e.transpose(out=p_tp.t[:, cc * 128:(cc + 1) * 128],
                                                                               in_=xo.t[:, cc, qs], identity=ident_bf.t[:]))
                                                 for cc in range(3)], reads=[xo, ident_bf], writes=[p_tp])
                                    k.op("act", lambda e: e.copy(out=xbtm[b].t[:], in_=p_tp.t[:, 0:384]),
                                         reads=[p_tp], writes=[xbtm[b]])
                                    k.op("dve", lambda e: e.tensor_tensor(
                                        out=xd[b].t[:], in0=xbtm[b].t[:, 0:256].rearrange("p (r d) -> p r d", r=4),
                                        in1=dt_all.t[:, c, g4, None].to_broadcast([128, 4, 64]), op=ALU.mult),
                                        reads=[xbtm[b], dt_all], writes=[xd[b]])
                                    k.op("pool", lambda e: e.tensor_tensor(
                                        out=xde[b].t[:], in0=xd[b].t[:],
                                        in1=e_all.t[:, c, 1, g4, None].to_broadcast([128, 4, 64]), op=ALU.mult),
                                        reads=[xd[b], e_all], writes=[xde[b]])
                                    k.op("pe", lambda e: e.matmul(p_cb.t[:], lhsT=xo.t[:, 2, qs], rhs=xo.t[:, 3, qs],
                                                                  start=True, stop=True), reads=[xo], writes=[p_cb])
                                    k.op("dve", lambda e: e.tensor_tensor(out=cbm[b].t[:], in0=p_cb.t[:], in1=Mle.t[:], op=ALU.mult),
                                         reads=[p_cb, Mle], writes=[cbm[b]])
                                    k.ops("pool", [(lambda e, r=r: e.tensor_scalar(
                                        out=lh[b].t[:, r, :], in0=Mgt.t[:], scalar1=a_all.t[:, c, g * 4 + r:g * 4 + r + 1],
                                        scalar2=None, op0=ALU.mult)) for r in range(4)],
                                        reads=[Mgt, a_all], writes=[lh[b]])
                                    k.ops("pe", [(lambda e, r=r: e.matmul(p_seg.t[:, r, :], lhsT=lh[b].t[:, r, :], rhs=Mle.t[:],
                                                                          start=True, stop=True)) for r in range(4)],
                                          reads=[lh[b], Mle], writes=[p_seg])
                                    k.op("act", lambda e: e.activation(out=Ex[b].t[:], in_=p_seg.t[:], func=AF.Exp),
                                         reads=[p_seg], writes=[Ex[b]])
                                    k.op("dve", lambda e: e.tensor_tensor(
                                        out=MT[b].t[:], in0=Ex[b].t[:],
                                        in1=cbm[b].t[:, None, :].to_broadcast([128, 4, 128]), op=ALU.mult),
                                        reads=[Ex[b], cbm[b]], writes=[MT[b]])
                                    k.ops("pe", [(lambda e, r=r: e.matmul(p_y.t[:, r * 64:(r + 1) * 64], lhsT=MT[b].t[:, r, :],
                                                                          rhs=xd[b].t[:, r, :], start=True, stop=True))
                                                 for r in range(4)] +
                                          [lambda e: e.matmul(p_y.t[:, 256:512], lhsT=xo.t[:, 3, qs], rhs=state_bf.t[:],
                                                              start=True, stop=True)],
                                          reads=[MT[b], xd[b], xo, state_bf], writes=[p_y])
                                    k.op("dve", lambda e: e.tensor_tensor(
                                        out=y1[b].t[:], in0=p_y.t[:, 256:512].rearrange("p (r d) -> p r d", r=4),
                                        in1=e_all.t[:, c, 0, g4, None].to_broadcast([128, 4, 64]), op=ALU.mult),
                                        reads=[p_y, e_all], writes=[y1[b]])
                                    k.op("pool", lambda e: e.tensor_tensor(
                                        out=tD[b].t[:], in0=xbtm[b].t[:, 0:256].rearrange("p (r d) -> p r d", r=4),
                                        in1=dsk.t[:, g4, None].to_broadcast([128, 4, 64]), op=ALU.mult),
                                        reads=[xbtm[b], dsk], writes=[tD[b]])
                                    k.op("dve", lambda e: e.tensor_tensor(
                                        out=y1[b].t[:], in0=y1[b].t[:],
                                        in1=p_y.t[:, 0:256].rearrange("p (r d) -> p r d", r=4), op=ALU.add),
                                        reads=[y1[b], p_y], writes=[y1[b]])
                                    k.op("dve", lambda e: e.tensor_tensor(out=y1[b].t[:], in0=y1[b].t[:], in1=tD[b].t[:], op=ALU.add),
                                         reads=[y1[b], tD[b]], writes=[y1[b]])
                                    k.op("dve", lambda e: e.tensor_tensor(
                                        out=y4[b].t[:], in0=y1[b].t[:].rearrange("p r d -> p (r d)"), in1=sz[b].t[:], op=ALU.mult),
                                        reads=[y1[b], sz[b]], writes=[y4[b]])
                                    k.op("act", lambda e: e.activation(out=junk2.t[:], in_=y4[b].t[:], func=AF.Square,
                                                                       accum_out=ss2[b].t[:, 0:1]),
                                         reads=[y4[b]], writes=[junk2, ss2[b]])
                                    k.op("act", lambda e: e.activation(out=rs2[b].t[:], in_=ss2[b].t[:], func=AF.Sqrt,
                                                                       scale=1.0 / 256, bias=EPS), reads=[ss2[b]], writes=[rs2[b]])
                                    k.op("dve", lambda e: e.reciprocal(out=rs2[b].t[:], in_=rs2[b].t[:]), reads=[rs2[b]], writes=[rs2[b]])
                                    k.op("dve", lambda e: e.tensor_scalar(out=yb[b].t[:], in0=y4[b].t[:], scalar1=rs2[b].t[:, 0:1],
                                                                          scalar2=None, op0=ALU.mult),
                                         reads=[y4[b], rs2[b]], writes=[yb[b]])
                                    k.ops("pe", [(lambda e, h=h: e.transpose(out=p_tp.t[:, 384 + h * 128:384 + (h + 1) * 128],
                                                                             in_=yb[b].t[:, h * 128:(h + 1) * 128], identity=ident_bf.t[:]))
                                                 for h in range(2)], reads=[yb[b], ident_bf], writes=[p_tp])
                                    k.op("act", lambda e: e.activation(out=yo.t[:, 0, qs], in_=p_tp.t[:, 384:512], func=AF.Copy,
                                                                       scale=gssdT.t[:, 2 * g:2 * g + 1]),
                                         reads=[p_tp, gssdT], writes=[yo])
                                    k.op("act", lambda e: e.activation(out=yo.t[:, 1, qs], in_=p_tp.t[:, 512:640], func=AF.Copy,
                                                                       scale=gssdT.t[:, 2 * g + 1:2 * g + 2]),
                                         reads=[p_tp, gssdT], writes=[yo])
                                    k.op("pe", lambda e: e.matmul(p_s.t[:], lhsT=xbtm[b].t[:, 256:384],
                                                                  rhs=xde[b].t[:].rearrange("p r d -> p (r d)"), start=True, stop=True),
                                         reads=[xbtm[b], xde[b]], writes=[p_s])
                                    k.op("dve", lambda e: e.tensor_tensor(
                                        out=state.t[:].rearrange("p (r d) -> p r d", r=4),
                                        in0=state.t[:].rearrange("p (r d) -> p r d", r=4),
                                        in1=e_all.t[:, c, 2, g4, None].to_broadcast([128, 4, 64]), op=ALU.mult),
                                        reads=[state, e_all], writes=[state])
                                    k.op("dve", lambda e: e.tensor_tensor(out=state.t[:], in0=state.t[:], in1=p_s.t[:], op=ALU.add),
                                         reads=[state, p_s], writes=[state])
                                    k.op("pool", lambda e: e.tensor_copy(out=state_bf.t[:], in_=state.t[:]),
                                         reads=[state], writes=[state_bf])
                                    if g == 0 and c == 0:
                                        for nm, tt_ in [("xbtm", xbtm[b]), ("xd", xd[b]), ("xde", xde[b]), ("cbm", cbm[b]), ("lh", lh[b]),
                                                        ("Ex", Ex[b]), ("MT", MT[b]), ("y1", y1[b]), ("sz", sz[b]), ("y4", y4[b]),
                                                        ("yb", yb[b]), ("state", state)]:
                                            dump(nm, tt_)
                                k.dma("sp", ysem[sc % 2], lambda e: e.dma_start(
                                    out=ymT_d[2 * g:2 * g + 2, :, ts:ts + 512].rearrange("f p t -> p f t"), in_=yo.t[:]),
                                    reads=[yo])

                k.barrier()
                if "A1p" in phases:
                    with ExitStack() as e2:
                        wp = sb(e2, "wp", [128, 8, 1024], BF16)
                        k.dma("pool", sem_c, lambda e: e.dma_start(
                            out=wp.t[:], in_=w_in_d[:, 6176:7200].rearrange("(k p) n -> p k n", p=128)), writes=[wp])
                        pw = sb(e2, "pw", [128, 4, 2, 256], BF16)
                        k.dma("pool", sem_c, lambda e: e.dma_start(
                            out=pw.t[:], in_=pool_w_d.rearrange("g (k p) n -> p g k n", p=128)), writes=[pw])
                        pst = [sb(e2, "pst%d" % i, [128, 8, 527], F32) for i in range(2)]
                        sA = sb(e2, "sA", [128, 2, 527], F32)
                        sB = sb(e2, "sB", [128, 2, 527], F32)
                        ypl = [sb(e2, "ypl%d" % i, [128, 2, 512], BF16) for i in range(2)]
                        tmp16 = sb(e2, "tmp16", [128, 2, 16], F32)
                        ypT = [sb(e2, "ypT%d" % i, [128, 2, 512], BF16) for i in range(2)]
                        psem_ = [dsem(), dsem()]
                        p_u2 = [ps(e2, "p_u2_%d" % i, [128, 512], F32) for i in range(2)]
                        p_p = [ps(e2, "p_p%d" % i, [128, 512], F32) for i in range(2)]
                        k.op("pool", lambda e: e.memset(pst[0].t[:, :, 0:15], 0.0), writes=[pst[0]])
                        it = 0
                        for sc in range(NSC):
                            ts = sc * 512
                            cur = pst[sc % 2]
                            nxt = pst[(sc + 1) % 2]
                            for pc in range(8):
                                pu = p_u2[pc % 2]
                                k.ops("pe", [(lambda e, j=j: e.matmul(pu.t[:], lhsT=wp.t[:, j, pc * 128:(pc + 1) * 128],
                                                                      rhs=hT.t[:, j, ts:ts + 512], start=(j == 0), stop=(j == 7)))
                                             for j in range(8)], reads=[wp, hT], writes=[pu])
                                k.op("act", lambda e: e.copy(out=cur.t[:, pc, 15:527], in_=pu.t[:]), reads=[pu], writes=[cur])
                            if sc + 1 < NSC:
                                k.op("pool", lambda e: e.tensor_copy(out=nxt.t[:, :, 0:15], in_=cur.t[:, :, 512:527]),
                                     reads=[cur], writes=[nxt])
                            for pg in range(4):
                                u = cur.t[:, 2 * pg:2 * pg + 2, :]
                                nlev = pg + 1
                                src = u
                                bufs = [sA, sB]
                                eng = "dve"
                                for lv in range(nlev):
                                    sh = 1 << lv
                                    lo = (1 << (lv + 1)) - 1
                                    dst = bufs[lv % 2]
                                    src_t = cur if lv == 0 else bufs[(lv - 1) % 2]
                                    s_ap = src
                                    k.op(eng, lambda e, dst=dst, s_ap=s_ap, lo=lo, sh=sh: e.tensor_tensor(
                                        out=dst.t[:, :, lo:527], in0=s_ap[:, :, lo:527], in1=s_ap[:, :, lo - sh:527 - sh], op=ALU.add),
                                        reads=[src_t], writes=[dst])
                                    src = dst.t[:, :, :]
                                fin = bufs[(nlev - 1) % 2]
                                wv = 1 << nlev
                                yp = ypl[it % 2]
                                k.op(eng, lambda e: e.scalar_tensor_tensor(
                                    out=yp.t[:], in0=fin.t[:, :, 15:527], scalar=1.0 / wv, in1=u[:, :, 15:527],
                                    op0=ALU.mult, op1=ALU.subtract), reads=[fin, cur], writes=[yp])
                                if sc == 0:
                                    k.op(eng, lambda e: e.tensor_tensor(
                                        out=tmp16.t[:], in0=fin.t[:, :, 15:31],
                                        in1=invc.t[:, pg, None, :].to_broadcast([128, 2, 16]), op=ALU.mult),
                                        reads=[fin, invc], writes=[tmp16])
                                    k.op(eng, lambda e: e.tensor_tensor(out=yp.t[:, :, 0:16], in0=tmp16.t[:], in1=u[:, :, 15:31],
                                                                        op=ALU.subtract), reads=[tmp16, cur], writes=[yp])
                                yT_ = ypT[it % 2]
                                for dc in range(2):
                                    pp = p_p[dc]
                                    k.ops("pe", [(lambda e, kc=kc: e.matmul(pp.t[:], lhsT=pw.t[:, pg, kc, dc * 128:(dc + 1) * 128],
                                                                            rhs=yp.t[:, kc, :], start=(kc == 0), stop=(kc == 1)))
                                                 for kc in range(2)], reads=[pw, yp], writes=[pp])
                                    k.op("act", lambda e: e.activation(out=yT_.t[:, dc, :], in_=pp.t[:], func=AF.Copy,
                                                                       scale=pscT.t[:, 2 * pg + dc:2 * pg + dc + 1]),
                                         reads=[pp, pscT], writes=[yT_])
                                k.dma("sp", psem_[it % 2], lambda e: e.dma_start(
                                    out=ymT_d[16 + 2 * pg:16 + 2 * pg + 2, :, ts:ts + 512].rearrange("f p t -> p f t"), in_=yT_.t[:]),
                                    reads=[yT_])
                                it += 1

        k.barrier()
        if "A2" in phases:
            with ExitStack() as e3:
                wout = sb(e3, "wout", [128, 24, DM], BF16)
                k.dma("pool", sem_c, [(lambda e, i=i: e.dma_start(
                    out=wout.t[:, 6 * i:6 * i + 6, :],
                    in_=w_out_d[768 * i:768 * (i + 1), :].rearrange("(k p) n -> p k n", p=128))) for i in range(4)],
                    writes=[wout])
                gffn_bc = load_const(e3, "gffn_bc", [128, DM], gffn_bc_d)
                rw = load_const(e3, "rw", [128, 8, NE], rw_d.rearrange("(k p) n -> p k n", p=128))
                rb = load_const(e3, "rb", [128, NE], rb_d)
                ebase = load_const(e3, "ebase", [128, NE], ebase_d)
                s4096 = sb(e3, "s4096", [128, 514], I32)
                k.op("pool", lambda e: e.memset(s4096.t[:], L), writes=[s4096])
                ev_init = [
                    k.dma("sp", sem_c, lambda e: e.dma_start(out=slot_d.rearrange("(p f) o -> p (f o)", p=128), in_=s4096.t[:]),
                          reads=[s4096]),
                    k.dma("sp", sem_c, lambda e: e.dma_start(out=h2_d[L:L + 1, :], in_=zrow_bf.t[:]), reads=[zrow_bf]),
                    k.dma("sp", sem_c, lambda e: e.dma_start(out=G_d[L:L + 1, :], in_=zrow.t[:, 0:NE]), reads=[zrow]),
                    k.dma("sp", sem_c, lambda e: e.dma_start(out=Y_d[0:1, :], in_=zrow.t[:]), reads=[zrow]),
                ]
                ym = [sb(e3, "ym%d" % i, [128, 24, 512], BF16) for i in range(2)]
                ymsem = [dsem(), dsem()]
                xt2 = [sb(e3, "xt2_%d" % i, [128, DM], F32) for i in range(2)]
                xsem2 = [dsem(), dsem()]
                x1 = [sb(e3, "x1_%d" % i, [128, DM], F32) for i in range(2)]
                x1sem = [dsem(), dsem()]
                junk3 = sb(e3, "junk3", [128, DM], F32)
                ss3 = sb(e3, "ss3", [128, 1], F32)
                rs3 = sb(e3, "rs3", [128, 1], F32)
                h2f = sb(e3, "h2f", [128, DM], F32)
                h2b = [sb(e3, "h2b%d" % i, [128, DM], BF16) for i in range(2)]
                h2sem = [dsem(), dsem()]
                h2T = sb(e3, "h2T", [128, 8, 128], F32)
                lg = sb(e3, "lg", [128, NE], F32)
                m8 = sb(e3, "m8", [128, 8], F32)
                nv1 = sb(e3, "nv1", [128, 1], F32)
                mask = sb(e3, "mask", [128, NE], F32)
                ex = sb(e3, "ex", [128, NE], F32)
                sm = sb(e3, "sm", [128, 1], F32)
                Gt = [sb(e3, "Gt%d" % i, [128, NE], F32) for i in range(2)]
                gsem = [dsem(), dsem()]
                cnt = sb(e3, "cnt", [128, NE], F32)
                rank = sb(e3, "rank", [128, NE], F32)
                vld = sb(e3, "vld", [128, NE], F32)
                val = sb(e3, "val", [128, NE], F32)
                v8 = sb(e3, "v8", [128, 8], F32)
                scsem = dsem()
                p_o = ps(e3, "p_o", [128, DM], F32)
                p_tf = ps(e3, "p_tf", [128, 8, 128], F32)
                p_l = ps(e3, "p_l", [128, NE], F32)
                p_r = ps(e3, "p_r", [128, 2, NE], F32)
                k.op("pool", lambda e: e.memset(cnt.t[:], 0.0), writes=[cnt])
                scat_evs = []
                for sc in range(NSC):
                    ts = sc * 512
                    ymc = ym[sc % 2]
                    k.dma("sp", ymsem[sc % 2], [(lambda e, i=i: e.dma_start(
                        out=ymc.t[:, 6 * i:6 * i + 6, :],
                        in_=ymT_d[6 * i:6 * i + 6, :, ts:ts + 512].rearrange("f p t -> p f t"))) for i in range(4)],
                        writes=[ymc])
                    for q in range(4):
                        c = sc * 4 + q
                        b = c % 2
                        qs = slice(q * 128, (q + 1) * 128)
                        cs = slice(c * 128, (c + 1) * 128)
                        k.dma("sp", xsem2[b], lambda e: e.dma_start(out=xt2[b].t[:], in_=x_d[cs, :]), writes=[xt2[b]])
                        k.ops("pe", [(lambda e, fc=fc, h=h: e.matmul(p_o.t[:, h * 512:(h + 1) * 512], lhsT=ymc.t[:, fc, qs],
                                                                     rhs=wout.t[:, fc, h * 512:(h + 1) * 512],
                                                                     start=(fc == 0), stop=(fc == 23)))
                                     for h in range(2) for fc in range(24)], reads=[ymc, wout], writes=[p_o])
                        k.op("dve", lambda e: e.tensor_tensor(out=x1[b].t[:], in0=p_o.t[:], in1=xt2[b].t[:], op=ALU.add),
                             reads=[p_o, xt2[b]], writes=[x1[b]])
                        k.dma("sp", x1sem[b], lambda e: e.dma_start(out=x1_d[cs, :], in_=x1[b].t[:]), reads=[x1[b]])
                        k.op("act", lambda e: e.activation(out=junk3.t[:], in_=x1[b].t[:], func=AF.Square, accum_out=ss3.t[:, 0:1]),
                             reads=[x1[b]], writes=[junk3, ss3])
                        k.op("act", lambda e: e.activation(out=rs3.t[:], in_=ss3.t[:], func=AF.Sqrt, scale=1.0 / DM, bias=EPS),
                             reads=[ss3], writes=[rs3])
                        k.op("dve", lambda e: e.reciprocal(out=rs3.t[:], in_=rs3.t[:]), reads=[rs3], writes=[rs3])
                        k.op("dve", lambda e: e.scalar_tensor_tensor(out=h2f.t[:], in0=x1[b].t[:], scalar=rs3.t[:, 0:1],
                                                                     in1=gffn_bc.t[:], op0=ALU.mult, op1=ALU.mult),
                             reads=[x1[b], rs3, gffn_bc], writes=[h2f])
                        k.op("act", lambda e: e.copy(out=h2b[b].t[:], in_=h2f.t[:]), reads=[h2f], writes=[h2b[b]])
                        k.dma("sp", h2sem[b], lambda e: e.dma_start(out=h2_d[cs, :], in_=h2b[b].t[:]), reads=[h2b[b]])
                        k.ops("pe", [(lambda e, j=j: e.transpose(out=p_tf.t[:, j, :], in_=h2f.t[:, j * 128:(j + 1) * 128],
                                                                 identity=ident_f.t[:])) for j in range(8)],
                              reads=[h2f, ident_f], writes=[p_tf])
                        k.op("act", lambda e: e.copy(out=h2T.t[:], in_=p_tf.t[:]), reads=[p_tf], writes=[h2T])
                        k.ops("pe", [(lambda e, j=j: e.matmul(p_l.t[:], lhsT=h2T.t[:, j, :], rhs=rw.t[:, j, :],
                                                              start=(j == 0), stop=(j == 7))) for j in range(8)],
                              reads=[h2T, rw], writes=[p_l])
                        k.op("dve", lambda e: e.tensor_tensor(out=lg.t[:], in0=p_l.t[:], in1=rb.t[:], op=ALU.add),
                             reads=[p_l, rb], writes=[lg])
                        k.op("dve", lambda e: e.max(out=m8.t[:], in_=lg.t[:]), reads=[lg], writes=[m8])
                        k.op("dve", lambda e: e.tensor_scalar(out=mask.t[:], in0=lg.t[:], scalar1=m8.t[:, 3:4], scalar2=None,
                                                              op0=ALU.is_ge), reads=[lg, m8], writes=[mask])
                        k.op("dve", lambda e: e.tensor_scalar(out=nv1.t[:], in0=m8.t[:, 0:1], scalar1=-1.0, scalar2=None,
                                                              op0=ALU.mult), reads=[m8], writes=[nv1])
                        k.op("act", lambda e: e.activation(out=ex.t[:], in_=lg.t[:], func=AF.Exp, bias=nv1.t[:, 0:1]),
                             reads=[lg, nv1], writes=[ex])
                        k.op("dve", lambda e: e.tensor_tensor(out=ex.t[:], in0=ex.t[:], in1=mask.t[:], op=ALU.mult),
                             reads=[ex, mask], writes=[ex])
                        k.op("dve", lambda e: e.reduce_sum(out=sm.t[:], in_=ex.t[:], axis=AX.X), reads=[ex], writes=[sm])
                        k.op("dve", lambda e: e.reciprocal(out=sm.t[:], in_=sm.t[:]), reads=[sm], writes=[sm])
                        k.op("dve", lambda e: e.tensor_scalar(out=Gt[b].t[:], in0=ex.t[:], scalar1=sm.t[:, 0:1], scalar2=None,
                                                              op0=ALU.mult), reads=[ex, sm], writes=[Gt[b]])
                        k.dma("sp", gsem[b], lambda e: e.dma_start(out=G_d[cs, :], in_=Gt[b].t[:]), reads=[Gt[b]])
                        k.ops("pe", [
                            lambda e: e.matmul(p_r.t[:, 0, :], lhsT=Mlt.t[:], rhs=mask.t[:], start=True, stop=True),
                            lambda e: e.matmul(p_r.t[:, 1, :], lhsT=ones_f.t[:], rhs=mask.t[:], start=True, stop=True),
                        ], reads=[Mlt, ones_f, mask], writes=[p_r])
                        k.op("dve", lambda e: e.tensor_tensor(out=rank.t[:], in0=p_r.t[:, 0, :], in1=cnt.t[:], op=ALU.add),
                             reads=[p_r, cnt], writes=[rank])
                        k.op("dve", lambda e: e.tensor_tensor(out=cnt.t[:], in0=p_r.t[:, 1, :], in1=cnt.t[:], op=ALU.add),
                             reads=[p_r, cnt], writes=[cnt])
                        k.op("dve", lambda e: e.tensor_scalar(out=vld.t[:], in0=rank.t[:], scalar1=float(CAP), scalar2=None,
                                                              op0=ALU.is_lt), reads=[rank], writes=[vld])
                        k.op("dve", lambda e: e.tensor_tensor(out=vld.t[:], in0=vld.t[:], in1=mask.t[:], op=ALU.mult),
                             reads=[vld, mask], writes=[vld])
                        k.op("dve", lambda e: e.tensor_tensor(out=val.t[:], in0=rank.t[:], in1=ebase.t[:], op=ALU.add),
                             reads=[rank, ebase], writes=[val])
                        k.op("dve", lambda e: e.tensor_tensor(out=val.t[:], in0=val.t[:], in1=vld.t[:], op=ALU.mult),
                             reads=[val, vld], writes=[val])
                        k.op("dve", lambda e: e.max(out=v8.t[:], in_=val.t[:]), reads=[val], writes=[v8])
                        k.op("dve", lambda e: e.tensor_copy(out=dest_all.t[:, c, :], in_=v8.t[:, 0:4]), reads=[v8], writes=[dest_all])
                        for kk in range(4):
                            scat_evs.append(k.dma("pool", scsem, lambda e: e.indirect_dma_start(
                                out=slot_d[:, :], out_offset=bass.IndirectOffsetOnAxis(ap=dest_all.t[:, c, kk:kk + 1], axis=0),
                                in_=tokid.t[:, c, :], in_offset=None),
                                reads=[dest_all, tokid], extra=ev_init))
                a2_done = [x1[0], x1[1], h2b[0], h2b[1], Gt[0], Gt[1]]
                a2_evs = list(scat_evs[-1:])
                for t in a2_done:
                    a2_evs += t.r.rs
        else:
            a2_evs = []

        k.barrier()
        if "B" in phases:
            with ExitStack() as e4:
                bguT = load_const(e4, "bguT", [128, NE, 16], bguT_d)
                wgu = [sb(e4, "wgu%d" % i, [128, 8, 2048], BF16) for i in range(2)]
                wdn = [sb(e4, "wdn%d" % i, [128, 8, DM], BF16) for i in range(2)]
                bdb = [sb(e4, "bdb%d" % i, [128, DM], F32) for i in range(2)]
                wesem = [dsem(), dsem()]
                idx = [sb(e4, "idx%d" % i, [128, NBLK, 2], I32) for i in range(2)]
                isem = [dsem(), dsem()]
                xg = [sb(e4, "xg%d" % i, [128, DM], BF16) for i in range(3)]
                xgsem = [dsem() for _ in range(3)]
                gg = [sb(e4, "gg%d" % i, [128, NBLK, NE], F32) for i in range(2)]
                ggsem = [dsem(), dsem()]
                xgT = sb(e4, "xgT", [128, 8, CAP], BF16)
                actT = sb(e4, "actT", [128, 8, CAP], BF16)
                HW = CAP // 2
                gm = [sb(e4, "gm%d" % i, [128, HW], F32) for i in range(2)]
                sg = [sb(e4, "sg%d" % i, [128, HW], F32) for i in range(2)]
                u1 = [sb(e4, "u1_%d" % i, [128, HW], F32) for i in range(2)]
                tt = [sb(e4, "tt%d" % i, [128, HW], F32) for i in range(2)]
                yA = [sb(e4, "yA%d" % i, [128, DM], F32) for i in range(2)]
                yB = [sb(e4, "yB%d" % i, [128, DM], F32) for i in range(2)]
                ysem2 = [dsem(), dsem()]
                p_tg = ps(e4, "p_tg", [128, 8, 128], BF16)
                p_g = [ps(e4, "p_g%d" % i, [128, 512], F32) for i in range(2)]
                p_up = [ps(e4, "p_up%d" % i, [128, 512], F32) for i in range(2)]
                p_d = ps(e4, "p_d", [128, DM], F32)

                def load_w(e_):
                    s = e_ % 2
                    k.dma("pool", wesem[s],
                          [(lambda e, i=i: e.dma_start(out=wgu[s].t[:, 2 * i:2 * i + 2, :],
                                                       in_=wgu_d[e_, 256 * i:256 * (i + 1), :].rearrange("(k p) n -> p k n", p=128)))
                           for i in range(4)] +
                          [(lambda e, i=i: e.dma_start(out=wdn[s].t[:, 4 * i:4 * i + 4, :],
                                                       in_=wd_d[e_, 512 * i:512 * (i + 1), :].rearrange("(k p) n -> p k n", p=128)))
                           for i in range(2)],
                          writes=[wgu[s], wdn[s]])
                    k.dma("sp", wesem[s], lambda e: e.dma_start(out=bdb[s].t[:], in_=bd_d[e_:e_ + 1, :].to_broadcast([128, DM])),
                          writes=[bdb[s]])

                load_w(0)
                gi = 0
                for e_ in range(NE):
                    s = e_ % 2
                    if e_ + 1 < NE:
                        load_w(e_ + 1)
                    base = 1 + e_ * CAP
                    k.dma("sp", isem[s], [(lambda e, j=j: e.dma_start(out=idx[s].t[:, j, :],
                                                                      in_=slot_d[base + j * 128:base + (j + 1) * 128, :]))
                                          for j in range(NBLK)], writes=[idx[s]], extra=a2_evs)
                    for j in range(NBLK):
                        xs_ = xg[gi % 3]
                        k.dma("pool", xgsem[gi % 3], lambda e: e.indirect_dma_start(
                            out=xs_.t[:, :], out_offset=None, in_=h2_d[:, :],
                            in_offset=bass.IndirectOffsetOnAxis(ap=idx[s].t[:, j, 0:1], axis=0)),
                            reads=[idx[s]], writes=[xs_], extra=a2_evs)
                        k.ops("pe", [(lambda e, kk=kk: e.transpose(out=p_tg.t[:, kk, :], in_=xs_.t[:, kk * 128:(kk + 1) * 128],
                                                                   identity=ident_bf.t[:])) for kk in range(8)],
                              reads=[xs_, ident_bf], writes=[p_tg])
                        k.op("act", lambda e: e.copy(out=xgT.t[:, :, j * 128:(j + 1) * 128], in_=p_tg.t[:]),
                             reads=[p_tg], writes=[xgT])
                        gi += 1
                    k.dma("pool", ggsem[s], [(lambda e, j=j: e.indirect_dma_start(
                        out=gg[s].t[:, j, :], out_offset=None, in_=G_d[:, :],
                        in_offset=bass.IndirectOffsetOnAxis(ap=idx[s].t[:, j, 0:1], axis=0))) for j in range(NBLK)],
                        reads=[idx[s]], writes=[gg[s]], extra=a2_evs)
                    for fc in range(8):
                        for h in range(2):
                            hs = slice(h * HW, (h + 1) * HW)
                            k.ops("pe", [(lambda e, kk=kk: e.matmul(p_g[h].t[:, 0:HW], lhsT=wgu[s].t[:, kk, fc * 128:(fc + 1) * 128],
                                                                    rhs=xgT.t[:, kk, hs], start=(kk == 0), stop=(kk == 7)))
                                         for kk in range(8)], reads=[wgu[s], xgT], writes=[p_g[h]])
                            k.ops("pe", [(lambda e, kk=kk: e.matmul(p_up[h].t[:, 0:HW],
                                                                    lhsT=wgu[s].t[:, kk, 1024 + fc * 128:1024 + (fc + 1) * 128],
                                                                    rhs=xgT.t[:, kk, hs], start=(kk == 0), stop=(kk == 7)))
                                         for kk in range(8)], reads=[wgu[s], xgT], writes=[p_up[h]])
                            k.op("dve", lambda e: e.tensor_scalar(out=gm[h].t[:], in0=p_g[h].t[:, 0:HW],
                                                                  scalar1=bguT.t[:, e_, fc:fc + 1], scalar2=7.0,
                                                                  op0=ALU.add, op1=ALU.min), reads=[p_g[h], bguT], writes=[gm[h]])
                            k.op("act", lambda e: e.activation(out=sg[h].t[:], in_=gm[h].t[:], func=AF.Sigmoid, scale=1.702),
                                 reads=[gm[h]], writes=[sg[h]])
                            k.op("dve", lambda e: e.tensor_scalar(out=u1[h].t[:], in0=p_up[h].t[:, 0:HW],
                                                                  scalar1=bguT.t[:, e_, 8 + fc:9 + fc], scalar2=7.0,
                                                                  op0=ALU.add, op1=ALU.min), reads=[p_up[h], bguT], writes=[u1[h]])
                            k.op("dve", lambda e: e.tensor_scalar(out=u1[h].t[:], in0=u1[h].t[:], scalar1=-7.0, scalar2=1.0,
                                                                  op0=ALU.max, op1=ALU.add), reads=[u1[h]], writes=[u1[h]])
                            k.op("pool", lambda e: e.tensor_tensor(out=tt[h].t[:], in0=gm[h].t[:], in1=sg[h].t[:], op=ALU.mult),
                                 reads=[gm[h], sg[h]], writes=[tt[h]])
                            k.op("pool", lambda e: e.tensor_tensor(out=actT.t[:, fc, hs], in0=tt[h].t[:], in1=u1[h].t[:], op=ALU.mult),
                                 reads=[tt[h], u1[h]], writes=[actT])
                    for j in range(NBLK):
                        js = slice(j * 128, (j + 1) * 128)
                        b = j % 2
                        k.ops("pe", [(lambda e, kk=kk, h=h: e.matmul(p_d.t[:, h * 512:(h + 1) * 512], lhsT=actT.t[:, kk, js],
                                                                     rhs=wdn[s].t[:, kk, h * 512:(h + 1) * 512],
                                                                     start=(kk == 0), stop=(kk == 7)))
                                     for h in range(2) for kk in range(8)], reads=[actT, wdn[s]], writes=[p_d])
                        k.op("dve", lambda e: e.tensor_tensor(out=yA[b].t[:], in0=p_d.t[:], in1=bdb[s].t[:], op=ALU.add),
                             reads=[p_d, bdb[s]], writes=[yA[b]])
                        k.op("act", lambda e: e.activation(out=yB[b].t[:], in_=yA[b].t[:], func=AF.Copy,
                                                           scale=gg[s].t[:, j, e_:e_ + 1]),
                             reads=[yA[b], gg[s]], writes=[yB[b]])
                        k.dma("sp", ysem2[b], lambda e: e.dma_start(out=Y_d[base + j * 128:base + (j + 1) * 128, :], in_=yB[b].t[:]),
                              reads=[yB[b]])
                b_evs = []
                for t in yB:
                    b_evs += t.r.rs
        else:
            b_evs = []

        k.barrier()
        fin_evs = []
        if "C" in phases:
            with ExitStack() as e5:
                wpg = sb(e5, "wpg", [128, 8, DM], BF16)
                k.dma("pool", sem_c, [(lambda e, i=i: e.dma_start(
                    out=wpg.t[:, 4 * i:4 * i + 4, :],
                    in_=wpg_d[512 * i:512 * (i + 1), :].rearrange("(k p) n -> p k n", p=128))) for i in range(2)], writes=[wpg])
                wpp = sb(e5, "wpp", [128, 2, DM], BF16)
                k.dma("pool", sem_c, lambda e: e.dma_start(out=wpp.t[:], in_=wpp_d.rearrange("(k p) n -> p k n", p=128)),
                      writes=[wpp])
                gpgT = load_const(e5, "gpgT", [128, 8], gpgT_d)
                gpn_bc = load_const(e5, "gpn_bc", [128, DM], gpn_bc_d)
                gfin_bc = load_const(e5, "gfin_bc", [128, DM], gfin_bc_d)
                x1c = [sb(e5, "x1c%d" % i, [128, DM], F32) for i in range(2)]
                x1csem = [dsem(), dsem()]
                yk = [[sb(e5, "yk%d_%d" % (i, kk), [128, DM], F32) for kk in range(4)] for i in range(2)]
                yksem = [[dsem() for kk in range(4)] for i in range(2)]
                pt = [sb(e5, "pt%d" % i, [128, 256], F32) for i in range(2)]
                ptsem = [dsem(), dsem()]
                x2 = sb(e5, "x2", [128, DM], F32)
                junk4 = sb(e5, "junk4", [128, DM], F32)
                ssc = sb(e5, "ssc", [128, 1], F32)
                rsc = sb(e5, "rsc", [128, 1], F32)
                xnb = sb(e5, "xnb", [128, DM], BF16)
                xnT = sb(e5, "xnT", [128, 8, 128], BF16)
                sgate = sb(e5, "sgate", [128, DM], F32)
                pT = sb(e5, "pT", [128, 2, 128], BF16)
                sse = sb(e5, "sse", [128, 1], F32)
                rse = sb(e5, "rse", [128, 1], F32)
                e1_ = sb(e5, "e1_", [128, DM], F32)
                x3 = sb(e5, "x3", [128, DM], F32)
                ssf = sb(e5, "ssf", [128, 1], F32)
                rsf = sb(e5, "rsf", [128, 1], F32)
                ot = [sb(e5, "ot%d" % i, [128, DM], F32) for i in range(2)]
                osem = [dsem(), dsem()]
                p_t3 = ps(e5, "p_t3", [128, 8, 128], BF16)
                p_ga = ps(e5, "p_ga", [128, DM], F32)
                p_pt = ps(e5, "p_pt", [128, 2, 128], F32)
                p_e = ps(e5, "p_e", [128, DM], F32)
                for c in range(NCH):
                    b = c % 2
                    cs = slice(c * 128, (c + 1) * 128)
                    k.dma("sp", x1csem[b], lambda e: e.dma_start(out=x1c[b].t[:], in_=x1_d[cs, :]), writes=[x1c[b]], extra=a2_evs)
                    k.dma("sp", ptsem[b], lambda e: e.dma_start(out=pt[b].t[:], in_=pin_d[cs, :]), writes=[pt[b]])
                    for kk in range(4):
                        k.dma("pool", yksem[b][kk], lambda e: e.indirect_dma_start(
                            out=yk[b][kk].t[:, :], out_offset=None, in_=Y_d[:, :],
                            in_offset=bass.IndirectOffsetOnAxis(ap=dest_all.t[:, c, kk:kk + 1], axis=0)),
                            reads=[dest_all], writes=[yk[b][kk]], extra=b_evs)
                    k.op("dve", lambda e: e.tensor_tensor(out=x2.t[:], in0=x1c[b].t[:], in1=yk[b][0].t[:], op=ALU.add),
                         reads=[x1c[b], yk[b][0]], writes=[x2])
                    k.op("pool", lambda e: e.tensor_tensor(out=yk[b][1].t[:], in0=yk[b][1].t[:], in1=yk[b][2].t[:], op=ALU.add),
                         reads=[yk[b][1], yk[b][2]], writes=[yk[b][1]])
                    k.op("dve", lambda e: e.tensor_tensor(out=x2.t[:], in0=x2.t[:], in1=yk[b][3].t[:], op=ALU.add),
                         reads=[x2, yk[b][3]], writes=[x2])
                    k.op("dve", lambda e: e.tensor_tensor(out=x2.t[:], in0=x2.t[:], in1=yk[b][1].t[:], op=ALU.add),
                         reads=[x2, yk[b][1]], writes=[x2])
                    k.op("act", lambda e: e.activation(out=junk4.t[:], in_=x2.t[:], func=AF.Square, accum_out=ssc.t[:, 0:1]),
                         reads=[x2], writes=[junk4, ssc])
                    k.op("act", lambda e: e.activation(out=rsc.t[:], in_=ssc.t[:], func=AF.Sqrt, scale=1.0 / DM, bias=EPS),
                         reads=[ssc], writes=[rsc])
                    k.op("dve", lambda e: e.reciprocal(out=rsc.t[:], in_=rsc.t[:]), reads=[rsc], writes=[rsc])
                    k.op("act", lambda e: e.activation(out=xnb.t[:], in_=x2.t[:], func=AF.Copy, scale=rsc.t[:, 0:1]),
                         reads=[x2, rsc], writes=[xnb])
                    k.ops("pe", [(lambda e, j=j: e.transpose(out=p_t3.t[:, j, :], in_=xnb.t[:, j * 128:(j + 1) * 128],
                                                             identity=ident_bf.t[:])) for j in range(8)],
                          reads=[xnb, ident_bf], writes=[p_t3])
                    k.op("dve", lambda e: e.tensor_tensor(out=xnT.t[:], in0=p_t3.t[:],
                                                          in1=gpgT.t[:, :, None].to_broadcast([128, 8, 128]), op=ALU.mult),
                         reads=[p_t3, gpgT], writes=[xnT])
                    k.ops("pe", [(lambda e, j=j, h=h: e.matmul(p_ga.t[:, h * 512:(h + 1) * 512], lhsT=xnT.t[:, j, :],
                                                               rhs=wpg.t[:, j, h * 512:(h + 1) * 512], start=(j == 0), stop=(j == 7)))
                                 for h in range(2) for j in range(8)], reads=[xnT, wpg], writes=[p_ga])
                    k.op("act", lambda e: e.activation(out=sgate.t[:], in_=p_ga.t[:], func=AF.Sigmoid), reads=[p_ga], writes=[sgate])
                    k.ops("pe", [(lambda e, j=j: e.transpose(out=p_pt.t[:, j, :], in_=pt[b].t[:, j * 128:(j + 1) * 128],
                                                             identity=ident_f.t[:])) for j in range(2)],
                          reads=[pt[b], ident_f], writes=[p_pt])
                    k.op("act", lambda e: e.copy(out=pT.t[:], in_=p_pt.t[:]), reads=[p_pt], writes=[pT])
                    k.ops("pe", [(lambda e, j=j, h=h: e.matmul(p_e.t[:, h * 512:(h + 1) * 512], lhsT=pT.t[:, j, :],
                                                               rhs=wpp.t[:, j, h * 512:(h + 1) * 512], start=(j == 0), stop=(j == 1)))
                                 for h in range(2) for j in range(2)], reads=[pT, wpp], writes=[p_e])
                    k.op("act", lambda e: e.activation(out=junk4.t[:], in_=p_e.t[:], func=AF.Square, accum_out=sse.t[:, 0:1]),
                         reads=[p_e], writes=[junk4, sse])
                    k.op("act", lambda e: e.activation(out=rse.t[:], in_=sse.t[:], func=AF.Sqrt, scale=1.0 / DM, bias=EPS),
                         reads=[sse], writes=[rse])
                    k.op("dve", lambda e: e.reciprocal(out=rse.t[:], in_=rse.t[:]), reads=[rse], writes=[rse])
                    k.op("dve", lambda e: e.scalar_tensor_tensor(out=e1_.t[:], in0=p_e.t[:], scalar=rse.t[:, 0:1], in1=gpn_bc.t[:],
                                                                 op0=ALU.mult, op1=ALU.mult), reads=[p_e, rse, gpn_bc], writes=[e1_])
                    k.op("pool", lambda e: e.tensor_tensor(out=e1_.t[:], in0=e1_.t[:], in1=sgate.t[:], op=ALU.mult),
                         reads=[e1_, sgate], writes=[e1_])
                    k.op("dve", lambda e: e.tensor_tensor(out=x3.t[:], in0=x2.t[:], in1=e1_.t[:], op=ALU.add),
                         reads=[x2, e1_], writes=[x3])
                    k.op("act", lambda e: e.activation(out=junk4.t[:], in_=x3.t[:], func=AF.Square, accum_out=ssf.t[:, 0:1]),
                         reads=[x3], writes=[junk4, ssf])
                    k.op("act", lambda e: e.activation(out=rsf.t[:], in_=ssf.t[:], func=AF.Sqrt, scale=1.0 / DM, bias=EPS),
                         reads=[ssf], writes=[rsf])
                    k.op("dve", lambda e: e.reciprocal(out=rsf.t[:], in_=rsf.t[:]), reads=[rsf], writes=[rsf])
                    k.op("dve", lambda e: e.scalar_tensor_tensor(out=ot[b].t[:], in0=x3.t[:], scalar=rsf.t[:, 0:1], in1=gfin_bc.t[:],
                                                                 op0=ALU.mult, op1=ALU.mult), reads=[x3, rsf, gfin_bc], writes=[ot[b]])
                    fin_evs.append(k.dma("sp", osem[b], lambda e: e.dma_start(out=out_d[cs, :], in_=ot[b].t[:]), reads=[ot[b]]))

        tail = list(fin_evs[-2:]) + list(a2_evs) + list(b_evs)
        for sid_ev in tail:
            k.wait("sp", sid_ev)
        for sid_, sem_ in k.dma_objs.items():
            k.wait("sp", (sem_, k.dma_cnt[sid_]))
        build.stats = (k.ninst, k.nwaits, dict(k.cnt))
    return nc


def host_layout(inp):
    f = np.float32

    def colT(v, n):
        return np.ascontiguousarray(np.asarray(v, f).reshape(n, 128).T)

    def bc(v):
        v = np.asarray(v, f).reshape(1, -1)
        return np.ascontiguousarray(np.broadcast_to(v, (128, v.shape[1])))

    cw = np.asarray(inp["conv_w"][0], f)
    conv_wT = np.ascontiguousarray(cw.reshape(4, 32, 128).transpose(2, 1, 0))
    bgu = np.asarray(inp["b_gate_up"][0], f)
    bguT = np.ascontiguousarray(bgu.reshape(NE, 16, 128).transpose(2, 0, 1))
    invc = np.zeros((128, 4, 16), f)
    for gi, w in enumerate((2, 4, 8, 16)):
        invc[:, gi, :] = 1.0 / np.minimum(np.arange(16) + 1, w).astype(f)
    ebase = (1 + np.arange(NE) * CAP).astype(f)
    shared = {
        "w_in": np.ascontiguousarray(inp["w_in"][0], f),
        "conv_wT": conv_wT,
        "conv_bT": colT(inp["conv_b"][0], 32),
        "dt_bias_bc": bc(inp["dt_bias"][0]),
        "a_log_bc": bc(inp["a_log"][0]),
        "d_skip_bc": bc(inp["d_skip"][0]),
        "gmixT": colT(inp["mix_norm_g"][0], 8),
        "gssdT": colT(inp["ssd_norm_g"][0], 16),
        "pool_w": np.ascontiguousarray(inp["pool_w"][0], f),
        "pscT": colT(inp["pool_scale"][0], 8),
        "invc": invc,
        "w_out": np.ascontiguousarray(inp["w_out"][0], f),
        "gffn_bc": bc(inp["ffn_norm_g"][0]),
        "router_w": np.ascontiguousarray(inp["router_w"][0], f),
        "rb_bc": bc(inp["router_b"][0]),
        "ebase_bc": bc(ebase),
        "w_gate_up": np.ascontiguousarray(inp["w_gate_up"][0], f),
        "bguT": bguT,
        "w_down": np.ascontiguousarray(inp["w_down"][0], f),
        "b_down": np.ascontiguousarray(inp["b_down"][0], f),
        "gpgT": colT(inp["ple_gate_norm_g"][0], 8),
        "w_ple_gate": np.ascontiguousarray(inp["w_ple_gate"][0], f),
        "w_ple_proj": np.ascontiguousarray(inp["w_ple_proj"][0], f),
        "gpn_bc": bc(inp["ple_norm_g"][0]),
        "gfin_bc": bc(inp["final_norm_g"]),
    }
    return shared


def kernel(**inputs):
    inp = {k_: np.asarray(v) for k_, v in inputs.items()}
    shared = host_layout(inp)
    x = np.asarray(inp["x"], np.float32)
    p = np.asarray(inp["p"], np.float32)[0]
    nb = x.shape[0]
    in_maps = []
    for b in range(nb):
        m = dict(shared)
        m["x"] = np.ascontiguousarray(x[b])
        m["p"] = np.ascontiguousarray(p[b])
        in_maps.append(m)
    nc = build()
    res = run_bass_kernel_spmd(nc, in_maps, core_ids=list(range(nb)))
    return np.stack([np.asarray(r["out"], np.float32) for r in res.results], axis=0)
```

```python
from contextlib import ExitStack
import numpy as np
import concourse.bass as bass
import concourse.mybir as mybir
from concourse.bass_utils import run_bass_kernel_spmd

F32 = mybir.dt.float32
BF16 = mybir.dt.bfloat16
I32 = mybir.dt.int32
AF = mybir.ActivationFunctionType
ALU = mybir.AluOpType
AX = mybir.AxisListType

L = 4096
DM = 1024
NCH = 32
NSC = 8
D_IN = 7200
NE = 32
CAP = 1024
NBLK = CAP // 128
NSLOT = NE * CAP
SLOT_TAB = 128 * 257
EPS = 1e-6


class R:
    __slots__ = ("w", "rs")

    def __init__(self):
        self.w = None
        self.rs = []


class T:
    def __init__(self, t):
        self.t = t
        self.r = R()


class K:
    def __init__(self, nc, sems):
        self.nc = nc
        self.engs = {"pe": nc.tensor, "dve": nc.vector, "act": nc.scalar,
                     "pool": nc.gpsimd, "sp": nc.sync}
        self.psem = sems
        self.cnt = {k: 0 for k in self.engs}
        self.waited = {k: {} for k in self.engs}
        self.dma_cnt = {}
        self.dma_objs = {}
        self.ninst = 0
        self.nwaits = 0

    def wait(self, ek, ev):
        if ev is None:
            return
        sem, val = ev
        sid = id(sem)
        if sid in self.dma_cnt:
            val = max(val, self.dma_cnt[sid])
        w = self.waited[ek]
        if w.get(sid, 0) >= val:
            return
        self.engs[ek].wait_ge(sem, val)
        self.nwaits += 1
        w[sid] = val

    def _deps(self, ek, reads, writes, extra):
        for t in reads:
            self.wait(ek, t.r.w)
        for t in writes:
            self.wait(ek, t.r.w)
            for e in t.r.rs:
                self.wait(ek, e)
        for e in extra:
            self.wait(ek, e)

    def _commit(self, ev, reads, writes):
        for t in reads:
            t.r.rs.append(ev)
        for t in writes:
            t.r.w = ev
            t.r.rs = []

    def barrier(self):
        for ek in self.engs:
            for o in self.engs:
                if self.cnt[o] > 0:
                    self.wait(ek, (self.psem[o], self.cnt[o]))
            for sid_, sem_ in self.dma_objs.items():
                self.wait(ek, (sem_, self.dma_cnt[sid_]))

    def op(self, ek, fn, reads=(), writes=(), extra=()):
        self._deps(ek, reads, writes, extra)
        ins = fn(self.engs[ek])
        self.cnt[ek] += 1
        ins.then_inc(self.psem[ek], 1)
        ev = (self.psem[ek], self.cnt[ek])
        self._commit(ev, reads, writes)
        self.ninst += 1
        return ev

    def ops(self, ek, fns, reads=(), writes=(), extra=()):
        self._deps(ek, reads, writes, extra)
        ins = None
        for fn in fns:
            ins = fn(self.engs[ek])
            self.ninst += 1
        self.cnt[ek] += 1
        ins.then_inc(self.psem[ek], 1)
        ev = (self.psem[ek], self.cnt[ek])
        self._commit(ev, reads, writes)
        return ev

    def dma(self, ek, sem, fns, reads=(), writes=(), extra=()):
        self._deps(ek, reads, writes, extra)
        if not isinstance(fns, (list, tuple)):
            fns = [fns]
        sid = id(sem)
        cur = self.dma_cnt.get(sid, 0)
        for f in fns:
            f(self.engs[ek]).then_inc(sem, 16)
            cur += 16
            self.ninst += 1
        self.dma_cnt[sid] = cur
        self.dma_objs[sid] = sem
        ev = (sem, cur)
        self._commit(ev, reads, writes)
        return ev


def build(debug=None, phases="A0,A1,A1p,A2,B,C"):
    phases = set(phases.split(","))
    debug = debug or ()
    nc = bass.Bass("TRN2", target_bir_lowering=False)

    def din(name, shape, dt=F32):
        return nc.dram_tensor(name, list(shape), dt, kind="ExternalInput").ap()

    x_d = din("x", [L, DM])
    pin_d = din("p", [L, 256])
    w_in_d = din("w_in", [DM, D_IN])
    conv_wT_d = din("conv_wT", [128, 32, 4])
    conv_bT_d = din("conv_bT", [128, 32])
    conv_brow_d = din("conv_brow", [1, 4096])
    dtb_d = din("dt_bias_bc", [128, 32])
    alog_d = din("a_log_bc", [128, 32])
    dsk_d = din("d_skip_bc", [128, 32])
    gmixT_d = din("gmixT", [128, 8])
    gssdT_d = din("gssdT", [128, 16])
    pool_w_d = din("pool_w", [4, 256, 256])
    pscT_d = din("pscT", [128, 8])
    invc_d = din("invc", [128, 4, 16])
    w_out_d = din("w_out", [3072, DM])
    gffn_bc_d = din("gffn_bc", [128, DM])
    rw_d = din("router_w", [DM, NE])
    rb_d = din("rb_bc", [128, NE])
    ebase_d = din("ebase_bc", [128, NE])
    wgu_d = din("w_gate_up", [NE, DM, 2048])
    bguT_d = din("bguT", [128, NE, 16])
    wd_d = din("w_down", [NE, DM, DM])
    bd_d = din("b_down", [NE, DM])
    gpgT_d = din("gpgT", [128, 8])
    wpg_d = din("w_ple_gate", [DM, DM])
    wpp_d = din("w_ple_proj", [256, DM])
    gpn_bc_d = din("gpn_bc", [128, DM])
    gfin_bc_d = din("gfin_bc", [128, DM])
    out_d = nc.dram_tensor("out", [L, DM], F32, kind="ExternalOutput").ap()

    def dscr(name, shape, dt, dbg=False):
        kind = "ExternalOutput" if (name in debug) else "Internal"
        return nc.dram_tensor(name, list(shape), dt, kind=kind).ap()

    ymT_d = dscr("ymT", [24, 128, L], BF16)
    x1_d = dscr("x1d", [L, DM], F32)
    h2_d = dscr("h2d", [L + 1, DM], BF16)
    G_d = dscr("Gd", [L + 1, NE], F32)
    slot_d = dscr("slotd", [SLOT_TAB, 2], I32)
    Y_d = dscr("Yd", [NSLOT + 1, DM], F32)

    with ExitStack() as es:
        E = es.enter_context
        sems = {k: E(nc.semaphore("prog_" + k)) for k in ["pe", "dve", "act", "pool", "sp"]}
        k = K(nc, sems)
        nsem = [0]

        def dsem():
            nsem[0] += 1
            return E(nc.semaphore("d%d" % nsem[0]))

        def sb(es_, name, shape, dt=F32):
            return T(es_.enter_context(nc.sbuf_tensor("s_" + name, list(shape), dt)))

        def ps(es_, name, shape, dt=F32):
            esz = 4 if dt == F32 else 2
            n = 1
            for d_ in shape[1:]:
                n *= d_
            per_bank = 2048 // esz
            nb_ = (n + per_bank - 1) // per_bank
            base = es_.enter_context(nc.psum_tensor("ps_" + name, [128, nb_ * per_bank], dt))
            v = base[:, 0:n]
            if len(shape) == 3:
                v = v.rearrange("p (a b) -> p a b", a=shape[1])
            return T(v)

        def psbank(es_, name, dt=F32):
            per_bank = 2048 // (4 if dt == F32 else 2)
            return es_.enter_context(nc.psum_tensor("ps_" + name, [128, per_bank], dt))

        ident_bf = sb(es, "ident_bf", [128, 128], BF16)
        ident_f = sb(es, "ident_f", [128, 128], F32)
        Mle = sb(es, "Mle", [128, 128], F32)
        Mgt = sb(es, "Mgt", [128, 128], F32)
        Mlt = sb(es, "Mlt", [128, 128], F32)
        ones_f = sb(es, "ones_f", [128, 128], F32)
        dest_all = sb(es, "dest_all", [128, NCH, 4], I32)
        tokid = sb(es, "tokid", [128, NCH, 2], I32)
        zrow = sb(es, "zrow", [1, DM], F32)
        zrow_bf = sb(es, "zrow_bf", [1, DM], BF16)

        def dump(name, t, ap=None):
            if name not in debug:
                return
            a = ap if ap is not None else t.t[:]
            dd = nc.dram_tensor("dbg_" + name, list(a.shape), a.dtype, kind="ExternalOutput").ap()
            k.dma("sp", dsem(), lambda e: e.dma_start(out=dd, in_=a), reads=[t])

        def cst(t, fn):
            k.op("pool", fn, writes=[t])

        cst(ident_bf, lambda e: e.memset(ident_bf.t[:], 1.0))
        cst(ident_bf, lambda e: e.affine_select(out=ident_bf.t[:], in_=ident_bf.t[:], pattern=[[-1, 128]],
                                               compare_op=ALU.is_equal, fill=0.0, base=0, channel_multiplier=1))
        cst(ident_f, lambda e: e.memset(ident_f.t[:], 1.0))
        cst(ident_f, lambda e: e.affine_select(out=ident_f.t[:], in_=ident_f.t[:], pattern=[[-1, 128]],
                                              compare_op=ALU.is_equal, fill=0.0, base=0, channel_multiplier=1))
        cst(Mle, lambda e: e.memset(Mle.t[:], 1.0))
        cst(Mle, lambda e: e.affine_select(out=Mle.t[:], in_=Mle.t[:], pattern=[[1, 128]],
                                          compare_op=ALU.is_ge, fill=0.0, base=0, channel_multiplier=-1))
        cst(Mgt, lambda e: e.memset(Mgt.t[:], 1.0))
        cst(Mgt, lambda e: e.affine_select(out=Mgt.t[:], in_=Mgt.t[:], pattern=[[-1, 128]],
                                          compare_op=ALU.is_gt, fill=0.0, base=0, channel_multiplier=1))
        cst(Mlt, lambda e: e.memset(Mlt.t[:], 1.0))
        cst(Mlt, lambda e: e.affine_select(out=Mlt.t[:], in_=Mlt.t[:], pattern=[[1, 128]],
                                          compare_op=ALU.is_gt, fill=0.0, base=0, channel_multiplier=-1))
        cst(ones_f, lambda e: e.memset(ones_f.t[:], 1.0))
        cst(zrow, lambda e: e.memset(zrow.t[:], 0.0))
        cst(zrow_bf, lambda e: e.memset(zrow_bf.t[:], 0.0))
        cst(tokid, lambda e: e.iota(tokid.t[:], pattern=[[128, NCH], [0, 2]], base=0, channel_multiplier=1))
        cst(dest_all, lambda e: e.memset(dest_all.t[:], 0))

        sem_c = dsem()
        sem_cp = dsem()

        def load_const(es_, name, shape, src, dt=F32, q="sp"):
            t = sb(es_, name, shape, dt)
            k.dma(q, sem_c, lambda e: e.dma_start(out=t.t[:], in_=src), writes=[t])
            return t

        if "A0" in phases:
            with ExitStack() as esA:
                hT = sb(esA, "hT", [128, 8, L], BF16)
                dt_all = sb(esA, "dt_all", [128, NCH, 32], F32)
                a_all = sb(esA, "a_all", [128, NCH, 32], F32)
                e_all = sb(esA, "e_all", [128, NCH, 3, 32], F32)
                gmixT = load_const(esA, "gmixT", [128, 8], gmixT_d)
                gssdT = load_const(esA, "gssdT", [128, 16], gssdT_d)
                conv_wT = load_const(esA, "conv_wT", [128, 32, 4], conv_wT_d)
                conv_bT = load_const(esA, "conv_bT", [128, 32], conv_bT_d)
                dsk = load_const(esA, "dsk", [128, 32], dsk_d)
                pscT = load_const(esA, "pscT", [128, 8], pscT_d)
                invc = load_const(esA, "invc", [128, 4, 16], invc_d)

                with ExitStack() as e0:
                    dtb = load_const(e0, "dtb", [128, 32], dtb_d)
                    alog = load_const(e0, "alog", [128, 32], alog_d)
                    Abc = sb(e0, "Abc", [128, 32], F32)
                    wdt = sb(e0, "wdt", [128, 8, 32], BF16)
                    k.dma("pool", sem_cp, lambda e: e.dma_start(
                        out=wdt.t[:], in_=w_in_d[:, 6144:6176].rearrange("(k p) n -> p k n", p=128)), writes=[wdt])
                    k.op("act", lambda e: e.activation(out=Abc.t[:], in_=alog.t[:], func=AF.Exp), reads=[alog], writes=[Abc])
                    k.op("dve", lambda e: e.tensor_scalar(out=Abc.t[:], in0=Abc.t[:], scalar1=-1.0, scalar2=None, op0=ALU.mult),
                         reads=[Abc], writes=[Abc])
                    xt = [sb(e0, "xt%d" % i, [128, DM], F32) for i in range(2)]
                    xsem = [dsem(), dsem()]
                    junk = sb(e0, "junk", [128, DM], F32)
                    ss = sb(e0, "ss", [128, 1], F32)
                    rstd = sb(e0, "rstd", [128, 1], F32)
                    xn = sb(e0, "xn", [128, DM], BF16)
                    tpA = ps(e0, "tpA", [128, 8, 128], BF16)
                    pdt = ps(e0, "pdt", [128, 32], F32)
                    pcs = ps(e0, "pcs", [128, 3, 32], F32)
                    dtr = sb(e0, "dtr", [128, 32], F32)
                    t1 = sb(e0, "t1", [128, 32], F32)
                    t2 = sb(e0, "t2", [128, 32], F32)
                    for c in range(NCH):
                        xc_ = xt[c % 2]
                        cs = slice(c * 128, (c + 1) * 128)
                        k.dma("sp", xsem[c % 2], lambda e: e.dma_start(out=xc_.t[:], in_=x_d[cs, :]), writes=[xc_])
                        k.op("act", lambda e: e.activation(out=junk.t[:], in_=xc_.t[:], func=AF.Square, accum_out=ss.t[:, 0:1]),
                             reads=[xc_], writes=[junk, ss])
                        k.op("act", lambda e: e.activation(out=rstd.t[:], in_=ss.t[:], func=AF.Sqrt, scale=1.0 / DM, bias=EPS),
                             reads=[ss], writes=[rstd])
                        k.op("dve", lambda e: e.reciprocal(out=rstd.t[:], in_=rstd.t[:]), reads=[rstd], writes=[rstd])
                        k.op("act", lambda e: e.activation(out=xn.t[:], in_=xc_.t[:], func=AF.Copy, scale=rstd.t[:, 0:1]),
                             reads=[xc_, rstd], writes=[xn])
                        k.ops("pe", [(lambda e, j=j: e.transpose(out=tpA.t[:, j, :], in_=xn.t[:, j * 128:(j + 1) * 128],
                                                                 identity=ident_bf.t[:])) for j in range(8)],
                              reads=[xn, ident_bf], writes=[tpA])
                        k.op("dve", lambda e: e.tensor_tensor(out=hT.t[:, :, cs], in0=tpA.t[:],
                                                              in1=gmixT.t[:, :, None].to_broadcast([128, 8, 128]), op=ALU.mult),
                             reads=[tpA, gmixT], writes=[hT])
                        k.ops("pe", [(lambda e, j=j: e.matmul(pdt.t[:], lhsT=hT.t[:, j, cs], rhs=wdt.t[:, j, :],
                                                              start=(j == 0), stop=(j == 7))) for j in range(8)],
                              reads=[hT, wdt], writes=[pdt])
                        k.op("dve", lambda e: e.tensor_tensor(out=dtr.t[:], in0=pdt.t[:], in1=dtb.t[:], op=ALU.add),
                             reads=[pdt, dtb], writes=[dtr])
                        k.op("act", lambda e: e.activation(out=t1.t[:], in_=dtr.t[:], func=AF.Abs),
                             reads=[dtr], writes=[t1])
                        k.op("act", lambda e: e.activation(out=t2.t[:], in_=t1.t[:], func=AF.Exp, scale=-1.0), reads=[t1], writes=[t2])
                        k.op("act", lambda e: e.activation(out=t1.t[:], in_=t2.t[:], func=AF.Ln, bias=1.0), reads=[t2], writes=[t1])
                        k.op("dve", lambda e: e.scalar_tensor_tensor(out=dt_all.t[:, c, :], in0=dtr.t[:], scalar=0.0, in1=t1.t[:],
                                                                     op0=ALU.max, op1=ALU.add),
                             reads=[dtr, t1], writes=[dt_all])
                        k.op("dve", lambda e: e.tensor_tensor(out=a_all.t[:, c, :], in0=dt_all.t[:, c, :], in1=Abc.t[:], op=ALU.mult),
                             reads=[dt_all, Abc], writes=[a_all])
                        k.ops("pe", [
                            lambda e: e.matmul(pcs.t[:, 0, :], lhsT=Mle.t[:], rhs=a_all.t[:, c, :], start=True, stop=True),
                            lambda e: e.matmul(pcs.t[:, 1, :], lhsT=Mgt.t[:], rhs=a_all.t[:, c, :], start=True, stop=True),
                            lambda e: e.matmul(pcs.t[:, 2, :], lhsT=ones_f.t[:], rhs=a_all.t[:, c, :], start=True, stop=True),
                        ], reads=[a_all, Mle, Mgt, ones_f], writes=[pcs])
                        k.op("act", lambda e: e.activation(out=e_all.t[:, c, :, :], in_=pcs.t[:], func=AF.Exp),
                             reads=[pcs], writes=[e_all])

                    k.barrier()
                dump("hT", hT); dump("dt_all", dt_all); dump("a_all", a_all); dump("e_all", e_all)
                if "A1" in phases:
                    with ExitStack() as e1:
                        wg = [sb(e1, "wg%d" % i, [128, 8, 768], BF16) for i in range(2)]
                        wsem = [dsem(), dsem()]
                        dg = [sb(e1, "dg%d" % i, [128, 4, 4, 128], BF16) for i in range(2)]
                        ust = [sb(e1, "ust%d" % i, [128, 4, 515], BF16) for i in range(2)]
                        xc = [sb(e1, "xc%d" % i, [128, 4, 512], BF16) for i in range(2)]
                        state = sb(e1, "state", [128, 256], F32)
                        state_bf = [sb(e1, "state_bf%d" % i, [128, 256], BF16) for i in range(3)]
                        zstate = sb(e1, "zstate", [128, 256], BF16)
                        ynT = [sb(e1, "ynT%d" % i, [128, 2, 512], BF16) for i in range(2)]
                        ysem = [dsem(), dsem()]
                        sz = [sb(e1, "sz%d" % i, [128, 256], BF16) for i in range(3)]
                        xbtm = [sb(e1, "xbtm%d" % i, [128, 384], BF16) for i in range(3)]
                        xd = [sb(e1, "xd%d" % i, [128, 4, 64], BF16) for i in range(3)]
                        y4 = [sb(e1, "y4_%d" % i, [128, 256], F32) for i in range(3)]
                        xde = [sb(e1, "xde%d" % i, [128, 4, 64], BF16) for i in range(2)]
                        cbm = [sb(e1, "cbm%d" % i, [128, 128], F32) for i in range(2)]
                        lh = [sb(e1, "lh%d" % i, [128, 4, 128], F32) for i in range(2)]
                        Ex = [sb(e1, "Ex%d" % i, [128, 4, 128], F32) for i in range(2)]
                        MT = [sb(e1, "MT%d" % i, [128, 4, 128], BF16) for i in range(2)]
                        y1 = [sb(e1, "y1_%d" % i, [128, 4, 64], F32) for i in range(2)]
                        tD = [sb(e1, "tD%d" % i, [128, 4, 64], F32) for i in range(2)]
                        yb = [sb(e1, "yb%d" % i, [128, 256], BF16) for i in range(2)]
                        junk2 = sb(e1, "junk2", [128, 256], F32)
                        ss2 = [sb(e1, "ss2_%d" % i, [128, 1], F32) for i in range(2)]
                        rs2 = [sb(e1, "rs2_%d" % i, [128, 1], F32) for i in range(2)]
                        p_u = [ps(e1, "p_u%d" % i, [128, 512], F32) for i in range(2)]
                        p_z = ps(e1, "p_z", [128, 256], F32)
                        bk1 = psbank(e1, "bk1", F32)
                        p_cb = T(bk1[:, 0:128])
                        p_s = T(bk1[:, 256:512])
                        p_s.r = p_cb.r
                        p_seg = [ps(e1, "p_seg%d" % i, [128, 4, 128], F32) for i in range(2)]
                        bk4 = psbank(e1, "bk4", F32)
                        p_y = T(bk4[:, 0:256])
                        p_yo = T(bk4[:, 256:512])
                        p_yo.r = p_y.r
                        bk6 = psbank(e1, "bk6", BF16)
                        p_tpx = T(bk6[:, 0:384])
                        p_tpy = T(bk6[:, 384:640])
                        p_tpy.r = p_tpx.r
                        k.op("pool", lambda e: e.memset(zstate.t[:], 0.0), writes=[zstate])
                        thz = [sb(e1, "thz%d" % i, [128, 256], F32) for i in range(2)]
                        thc = [sb(e1, "thc%d" % i, [128, 512], F32) for i in range(2)]
                        neghalf = sb(e1, "neghalf", [128, 1], F32)
                        k.op("pool", lambda e: e.memset(neghalf.t[:], -0.5), writes=[neghalf])
                        ones_row = sb(e1, "ones_row", [1, 512], BF16)
                        k.op("pool", lambda e: e.memset(ones_row.t[:], 1.0), writes=[ones_row])
                        cb_row = sb(e1, "cb_row", [1, 4096], F32)
                        k.dma("sp", sem_c, lambda e: e.dma_start(out=cb_row.t[:], in_=conv_brow_d), writes=[cb_row])
                        hb_row = sb(e1, "hb_row", [1, 4096], BF16)
                        k.op("dve", lambda e: e.tensor_scalar(out=hb_row.t[:], in0=cb_row.t[:], scalar1=0.5, scalar2=None, op0=ALU.mult),
                             reads=[cb_row], writes=[hb_row])

                        def group_setup(g):
                            w = wg[g % 2]
                            srcs = [(0, 2048 + g * 256, 256), (256, 4096 + g * 128, 128),
                                    (384, 5120 + g * 128, 128), (512, g * 256, 256)]
                            k.dma("pool", wsem[g % 2],
                                  [(lambda e, o=o, s=s, n=n: e.dma_start(
                                      out=w.t[:, :, o:o + n],
                                      in_=w_in_d[:, s:s + n].rearrange("(k p) n -> p k n", p=128))) for (o, s, n) in srcs],
                                  writes=[w])
                            d_ = dg[g % 2]
                            chunks = [2 * g, 2 * g + 1, 16 + g, 24 + g]
                            k.ops("pool", [(lambda e, cc=cc, kk=kk: e.tensor_scalar(
                                out=d_.t[:, cc, kk, :], in0=ident_bf.t[:], scalar1=conv_wT.t[:, chunks[cc], kk:kk + 1],
                                scalar2=0.5, op0=ALU.mult, op1=ALU.mult)) for cc in range(4) for kk in range(4)],
                                reads=[ident_bf, conv_wT], writes=[d_])
                            k.op("pool", lambda e: e.tensor_scalar(out=w.t[:, :, 512:768], in0=w.t[:, :, 512:768], scalar1=0.5,
                                                                   scalar2=0.0, op0=ALU.mult, op1=ALU.add), reads=[w], writes=[w])

                        NT = 8 * NCH

                        def dec(n):
                            g = n // NCH
                            c = n % NCH
                            return g, c, c // 4, c % 4

                        def S0a(si):
                            g, sc = si // NSC, si % NSC
                            w = wg[g % 2]
                            us = ust[si % 2]
                            ts = sc * 512
                            if sc == 0:
                                k.op("pool", lambda e: e.memset(us.t[:, :, 0:3], 0.0), writes=[us])
                            for cc in range(4):
                                pu = p_u[cc % 2]
                                k.ops("pe", [(lambda e, j=j: e.matmul(pu.t[:], lhsT=w.t[:, j, cc * 128:(cc + 1) * 128],
                                                                      rhs=hT.t[:, j, ts:ts + 512], start=(j == 0), stop=(j == 7)))
                                             for j in range(8)], reads=[w, hT], writes=[pu])
                                k.op("act", lambda e: e.copy(out=us.t[:, cc, 3:515], in_=pu.t[:]), reads=[pu], writes=[us])
                            if sc + 1 < NSC:
                                un = ust[(si + 1) % 2]
                                k.op("pool", lambda e: e.tensor_copy(out=un.t[:, :, 0:3], in_=us.t[:, :, 512:515]),
                                     reads=[us], writes=[un])

                        def S0b(si):
                            g, sc = si // NSC, si % NSC
                            d_ = dg[g % 2]
                            us = ust[si % 2]
                            xo = xc[si % 2]
                            chunks = [2 * g, 2 * g + 1, 16 + g, 24 + g]
                            for cc in range(4):
                                pu = p_u[cc % 2]
                                ch = chunks[cc]
                                k.ops("pe", [(lambda e, kk=kk: e.matmul(pu.t[:], lhsT=d_.t[:, cc, kk, :],
                                                                        rhs=us.t[:, cc, kk:kk + 512], start=(kk == 0), stop=False))
                                             for kk in range(4)] +
                                      [lambda e: e.matmul(pu.t[:], lhsT=hb_row.t[0:1, ch * 128:(ch + 1) * 128],
                                                          rhs=ones_row.t[0:1, :], start=False, stop=True)],
                                      reads=[d_, us, hb_row, ones_row], writes=[pu])
                                tc_ = thc[cc % 2]
                                k.op("act", lambda e: e.activation(out=tc_.t[:], in_=pu.t[:], func=AF.Tanh),
                                     reads=[pu], writes=[tc_])
                                k.op("dve", lambda e: e.scalar_tensor_tensor(out=xo.t[:, cc, :], in0=tc_.t[:], scalar=1.0, in1=pu.t[:],
                                                                             op0=ALU.add, op1=ALU.mult),
                                     reads=[tc_, pu], writes=[xo])

                        def ctx(n):
                            g, c, sc, q = dec(n)
                            si = g * NSC + sc
                            return dict(g=g, c=c, sc=sc, q=q, si=si, w=wg[g % 2], xo=xc[si % 2],
                                        qs=slice(q * 128, (q + 1) * 128), cs=slice(c * 128, (c + 1) * 128),
                                        g4=slice(g * 4, g * 4 + 4), b3=n % 3, b2=n % 2)

                        def A_pe(n):
                            x_ = ctx(n); w = x_["w"]; xo = x_["xo"]; qs = x_["qs"]; cs = x_["cs"]
                            k.ops("pe", [(lambda e, j=j: e.matmul(p_z.t[:], lhsT=hT.t[:, j, cs], rhs=w.t[:, j, 512:768],
                                                                  start=(j == 0), stop=(j == 7))) for j in range(8)],
                                  reads=[hT, w], writes=[p_z])
                            k.ops("pe", [(lambda e, cc=cc: e.transpose(out=p_tpx.t[:, cc * 128:(cc + 1) * 128],
                                                                       in_=xo.t[:, cc, qs], identity=ident_bf.t[:]))
                                         for cc in range(3)], reads=[xo, ident_bf], writes=[p_tpx])
                            k.op("pe", lambda e: e.matmul(p_cb.t[:], lhsT=xo.t[:, 2, qs], rhs=xo.t[:, 3, qs],
                                                          start=True, stop=True), reads=[xo], writes=[p_cb])

                        def A_act(n):
                            x_ = ctx(n); b3 = x_["b3"]; b2 = x_["b2"]; c = x_["c"]; g = x_["g"]
                            k.op("act", lambda e: e.activation(out=thz[b2].t[:], in_=p_z.t[:], func=AF.Tanh),
                                 reads=[p_z], writes=[thz[b2]])
                            k.op("act", lambda e: e.copy(out=xbtm[b3].t[:], in_=p_tpx.t[:]), reads=[p_tpx], writes=[xbtm[b3]])
                            k.ops("act", [(lambda e, r=r: e.activation(out=lh[b2].t[:, r, :], in_=Mgt.t[:], func=AF.Copy,
                                                                       scale=a_all.t[:, c, g * 4 + r:g * 4 + r + 1]))
                                          for r in range(4)], reads=[Mgt, a_all], writes=[lh[b2]])

                        def A_dve(n):
                            x_ = ctx(n); b3 = x_["b3"]; b2 = x_["b2"]; c = x_["c"]; g4 = x_["g4"]
                            k.op("dve", lambda e: e.scalar_tensor_tensor(out=sz[b3].t[:], in0=thz[b2].t[:], scalar=1.0, in1=p_z.t[:],
                                                                         op0=ALU.add, op1=ALU.mult),
                                 reads=[thz[b2], p_z], writes=[sz[b3]])
                            k.op("dve", lambda e: e.tensor_tensor(
                                out=xd[b3].t[:], in0=xbtm[b3].t[:, 0:256].rearrange("p (r d) -> p r d", r=4),
                                in1=dt_all.t[:, c, g4, None].to_broadcast([128, 4, 64]), op=ALU.mult),
                                reads=[xbtm[b3], dt_all], writes=[xd[b3]])
                            k.op("dve", lambda e: e.tensor_tensor(out=cbm[b2].t[:], in0=p_cb.t[:], in1=Mle.t[:], op=ALU.mult),
                                 reads=[p_cb, Mle], writes=[cbm[b2]])

                        def A_pool(n):
                            x_ = ctx(n); b3 = x_["b3"]; b2 = x_["b2"]; c = x_["c"]; g4 = x_["g4"]
                            k.op("pool", lambda e: e.tensor_tensor(
                                out=xde[b2].t[:], in0=xd[b3].t[:],
                                in1=e_all.t[:, c, 1, g4, None].to_broadcast([128, 4, 64]), op=ALU.mult),
                                reads=[xd[b3], e_all], writes=[xde[b2]])

                        def B_pe(n):
                            x_ = ctx(n); b3 = x_["b3"]; b2 = x_["b2"]
                            k.ops("pe", [(lambda e, r=r: e.matmul(p_seg[b2].t[:, r, :], lhsT=lh[b2].t[:, r, :], rhs=Mle.t[:],
                                                                  start=True, stop=True)) for r in range(4)],
                                  reads=[lh[b2], Mle], writes=[p_seg[b2]])
                            k.op("pe", lambda e: e.matmul(p_s.t[:], lhsT=xbtm[b3].t[:, 256:384],
                                                          rhs=xde[b2].t[:].rearrange("p r d -> p (r d)"), start=True, stop=True),
                                 reads=[xbtm[b3], xde[b2]], writes=[p_s])

                        def B_act(n):
                            x_ = ctx(n); b2 = x_["b2"]
                            k.op("act", lambda e: e.activation(out=Ex[b2].t[:], in_=p_seg[b2].t[:], func=AF.Exp),
                                 reads=[p_seg[b2]], writes=[Ex[b2]])

                        def B_dve(n):
                            x_ = ctx(n); b2 = x_["b2"]; c = x_["c"]; g4 = x_["g4"]
                            k.op("dve", lambda e: e.tensor_tensor(
                                out=MT[b2].t[:], in0=Ex[b2].t[:],
                                in1=cbm[b2].t[:, None, :].to_broadcast([128, 4, 128]), op=ALU.mult),
                                reads=[Ex[b2], cbm[b2]], writes=[MT[b2]])
                            if c == 0:
                                k.op("dve", lambda e: e.tensor_copy(out=state.t[:], in_=p_s.t[:]), reads=[p_s], writes=[state])
                            else:
                                k.op("dve", lambda e: e.tensor_tensor(
                                    out=state.t[:].rearrange("p (r d) -> p r d", r=4),
                                    in0=state.t[:].rearrange("p (r d) -> p r d", r=4),
                                    in1=e_all.t[:, c, 2, g4, None].to_broadcast([128, 4, 64]), op=ALU.mult),
                                    reads=[state, e_all], writes=[state])
                                k.op("dve", lambda e: e.tensor_tensor(out=state.t[:], in0=state.t[:], in1=p_s.t[:], op=ALU.add),
                                     reads=[state, p_s], writes=[state])

                        def B_dve2(n):
                            x_ = ctx(n); b3 = x_["b3"]; b2 = x_["b2"]; g4 = x_["g4"]
                            k.op("dve", lambda e: e.tensor_tensor(
                                out=tD[b2].t[:], in0=xbtm[b3].t[:, 0:256].rearrange("p (r d) -> p r d", r=4),
                                in1=dsk.t[:, g4, None].to_broadcast([128, 4, 64]), op=ALU.mult),
                                reads=[xbtm[b3], dsk], writes=[tD[b2]])

                        def C_act(n):
                            x_ = ctx(n); b3 = x_["b3"]
                            k.op("act", lambda e: e.copy(out=state_bf[b3].t[:], in_=state.t[:]),
                                 reads=[state], writes=[state_bf[b3]])

                        def C_pe(n):
                            x_ = ctx(n); b3 = x_["b3"]; b2 = x_["b2"]; xo = x_["xo"]; qs = x_["qs"]; c = x_["c"]
                            k.ops("pe", [(lambda e, r=r: e.matmul(p_y.t[:, r * 64:(r + 1) * 64], lhsT=MT[b2].t[:, r, :],
                                                                  rhs=xd[b3].t[:, r, :], start=True, stop=True))
                                         for r in range(4)], reads=[MT[b2], xd[b3]], writes=[p_y])
                            st_prev = zstate if c == 0 else state_bf[(n - 1) % 3]
                            k.op("pe", lambda e: e.matmul(p_yo.t[:], lhsT=xo.t[:, 3, qs], rhs=st_prev.t[:],
                                                          start=True, stop=True), reads=[xo, st_prev], writes=[p_yo])

                        def C_dve(n):
                            x_ = ctx(n); b3 = x_["b3"]; b2 = x_["b2"]; c = x_["c"]; g4 = x_["g4"]
                            k.op("dve", lambda e: e.tensor_tensor(
                                out=y1[b2].t[:], in0=p_yo.t[:].rearrange("p (r d) -> p r d", r=4),
                                in1=e_all.t[:, c, 0, g4, None].to_broadcast([128, 4, 64]), op=ALU.mult),
                                reads=[p_yo, e_all], writes=[y1[b2]])
                            k.op("dve", lambda e: e.tensor_tensor(
                                out=y1[b2].t[:], in0=y1[b2].t[:],
                                in1=p_y.t[:].rearrange("p (r d) -> p r d", r=4), op=ALU.add),
                                reads=[y1[b2], p_y], writes=[y1[b2]])
                            k.op("dve", lambda e: e.tensor_tensor(out=y1[b2].t[:], in0=y1[b2].t[:], in1=tD[b2].t[:], op=ALU.add),
                                 reads=[y1[b2], tD[b2]], writes=[y1[b2]])
                            k.op("dve", lambda e: e.tensor_tensor(
                                out=y4[b3].t[:], in0=y1[b2].t[:].rearrange("p r d -> p (r d)"), in1=sz[b3].t[:], op=ALU.mult),
                                reads=[y1[b2], sz[b3]], writes=[y4[b3]])

                        def D_act(n):
                            x_ = ctx(n); b3 = x_["b3"]; b2 = x_["b2"]
                            k.op("act", lambda e: e.activation(out=junk2.t[:], in_=y4[b3].t[:], func=AF.Square,
                                                               accum_out=ss2[b2].t[:, 0:1]),
                                 reads=[y4[b3]], writes=[junk2, ss2[b2]])

                        def D_pool(n):
                            x_ = ctx(n); b2 = x_["b2"]
                            k.op("pool", lambda e: e.tensor_scalar(out=rs2[b2].t[:], in0=ss2[b2].t[:], scalar1=1.0 / 256, scalar2=EPS,
                                                                   op0=ALU.mult, op1=ALU.add), reads=[ss2[b2]], writes=[rs2[b2]])
                            k.op("pool", lambda e: e.tensor_tensor(out=rs2[b2].t[:], in0=rs2[b2].t[:], in1=neghalf.t[:], op=ALU.pow),
                                 reads=[rs2[b2], neghalf], writes=[rs2[b2]])

                        def E_dve(n):
                            x_ = ctx(n); b3 = x_["b3"]; b2 = x_["b2"]
                            k.op("dve", lambda e: e.tensor_scalar(out=yb[b2].t[:], in0=y4[b3].t[:], scalar1=rs2[b2].t[:, 0:1],
                                                                  scalar2=None, op0=ALU.mult),
                                 reads=[y4[b3], rs2[b2]], writes=[yb[b2]])

                        def F_pe(n):
                            x_ = ctx(n); b2 = x_["b2"]
                            k.ops("pe", [(lambda e, h=h: e.transpose(out=p_tpy.t[:, h * 128:(h + 1) * 128],
                                                                     in_=yb[b2].t[:, h * 128:(h + 1) * 128], identity=ident_bf.t[:]))
                                         for h in range(2)], reads=[yb[b2], ident_bf], writes=[p_tpy])

                        def F_act(n):
                            x_ = ctx(n); g = x_["g"]; qs = x_["qs"]; si = x_["si"]; q = x_["q"]; sc = x_["sc"]
                            yo = ynT[si % 2]
                            for h in range(2):
                                k.op("act", lambda e: e.activation(out=yo.t[:, h, qs], in_=p_tpy.t[:, h * 128:(h + 1) * 128], func=AF.Copy,
                                                                   scale=gssdT.t[:, 2 * g + h:2 * g + h + 1]),
                                     reads=[p_tpy, gssdT], writes=[yo])
                            if q == 3:
                                ts = sc * 512
                                k.dma("sp", ysem[si % 2], lambda e: e.dma_start(
                                    out=ymT_d[2 * g:2 * g + 2, :, ts:ts + 512].rearrange("f p t -> p f t"), in_=yo.t[:]),
                                    reads=[yo])

                        def ok(n):
                            return 0 <= n < NT

                        import os as _os
                        if _os.environ.get("A1_NOSKEW"):
                            for n in range(NT):
                                g, c, sc, q = dec(n)
                                if c == 0:
                                    group_setup(g)
                                if q == 0:
                                    S0a(g * NSC + sc)
                                    S0b(g * NSC + sc)
                                for fn in (A_pe, A_act, A_dve, A_pool, B_pe, B_act, B_dve, B_dve2, C_pe, C_act, C_dve, D_act, D_pool, E_dve, F_pe, F_act):
                                    fn(n)
                        else:
                            LAGS = dict(A=0, B=1, C=2, D=3, E=4, F=5)
                            group_setup(0)
                            S0a(0)
                            S0b(0)
                            S0a(1)
                            for t in range(NT + 5):
                                for fn, lag in ((F_pe, 5), (C_pe, 2), (B_pe, 1), (A_pe, 0),
                                                (F_act, 5), (C_act, 2), (B_act, 1), (A_act, 0), (D_act, 3),
                                                (E_dve, 4), (B_dve, 1), (B_dve2, 1), (C_dve, 2), (A_dve, 0),
                                                (D_pool, 3), (A_pool, 0)):
                                    if ok(t - lag):
                                        fn(t - lag)
                                if t % 4 == 1 and t // 4 + 1 < 8 * NSC:
                                    S0b(t // 4 + 1)
                                if t % 4 == 2 and t // 4 + 2 < 8 * NSC:
                                    S0a(t // 4 + 2)
                                if t % NCH == 4 and t // NCH + 1 < 8:
                                    group_setup(t // NCH + 1)
                k.barrier()
                if "A1p" in phases:
                    with ExitStack() as e2:
                        wp = sb(e2, "wp", [128, 8, 1024], BF16)
                        k.dma("pool", sem_cp, lambda e: e.dma_start(
                            out=wp.t[:], in_=w_in_d[:, 6176:7200].rearrange("(k p) n -> p k n", p=128)), writes=[wp])
                        pw = sb(e2, "pw", [128, 4, 2, 256], BF16)
                        k.dma("pool", sem_cp, lambda e: e.dma_start(
                            out=pw.t[:], in_=pool_w_d.rearrange("g (k p) n -> p g k n", p=128)), writes=[pw])
                        pst = [sb(e2, "pst%d" % i, [128, 8, 527], F32) for i in range(2)]
                        sA = sb(e2, "sA", [128, 2, 527], F32)
                        sB = sb(e2, "sB", [128, 2, 527], F32)
                        ypl = [sb(e2, "ypl%d" % i, [128, 2, 512], BF16) for i in range(2)]
                        tmp16 = sb(e2, "tmp16", [128, 2, 16], F32)
                        ypT = [sb(e2, "ypT%d" % i, [128, 2, 512], BF16) for i in range(2)]
                        psem_ = [dsem(), dsem()]
                        p_u2 = [ps(e2, "p_u2_%d" % i, [128, 512], F32) for i in range(2)]
                        p_p = [ps(e2, "p_p%d" % i, [128, 512], F32) for i in range(2)]
                        k.op("pool", lambda e: e.memset(pst[0].t[:, :, 0:15], 0.0), writes=[pst[0]])
                        it = 0
                        for sc in range(NSC):
                            ts = sc * 512
                            cur = pst[sc % 2]
                            nxt = pst[(sc + 1) % 2]
                            for pc in range(8):
                                pu = p_u2[pc % 2]
                                k.ops("pe", [(lambda e, j=j: e.matmul(pu.t[:], lhsT=wp.t[:, j, pc * 128:(pc + 1) * 128],
                                                                      rhs=hT.t[:, j, ts:ts + 512], start=(j == 0), stop=(j == 7)))
                                             for j in range(8)], reads=[wp, hT], writes=[pu])
                                k.op("act", lambda e: e.copy(out=cur.t[:, pc, 15:527], in_=pu.t[:]), reads=[pu], writes=[cur])
                            if sc + 1 < NSC:
                                k.op("pool", lambda e: e.tensor_copy(out=nxt.t[:, :, 0:15], in_=cur.t[:, :, 512:527]),
                                     reads=[cur], writes=[nxt])
                            for pg in range(4):
                                u = cur.t[:, 2 * pg:2 * pg + 2, :]
                                nlev = pg + 1
                                src = u
                                bufs = [sA, sB]
                                eng = "dve"
                                for lv in range(nlev):
                                    sh = 1 << lv
                                    lo = (1 << (lv + 1)) - 1
                                    dst = bufs[lv % 2]
                                    src_t = cur if lv == 0 else bufs[(lv - 1) % 2]
                                    s_ap = src
                                    k.op(eng, lambda e, dst=dst, s_ap=s_ap, lo=lo, sh=sh: e.tensor_tensor(
                                        out=dst.t[:, :, lo:527], in0=s_ap[:, :, lo:527], in1=s_ap[:, :, lo - sh:527 - sh], op=ALU.add),
                                        reads=[src_t], writes=[dst])
                                    src = dst.t[:, :, :]
                                fin = bufs[(nlev - 1) % 2]
                                wv = 1 << nlev
                                yp = ypl[it % 2]
                                k.op(eng, lambda e: e.scalar_tensor_tensor(
                                    out=yp.t[:], in0=fin.t[:, :, 15:527], scalar=1.0 / wv, in1=u[:, :, 15:527],
                                    op0=ALU.mult, op1=ALU.subtract), reads=[fin, cur], writes=[yp])
                                if sc == 0:
                                    k.op(eng, lambda e: e.tensor_tensor(
                                        out=tmp16.t[:], in0=fin.t[:, :, 15:31],
                                        in1=invc.t[:, pg, None, :].to_broadcast([128, 2, 16]), op=ALU.mult),
                                        reads=[fin, invc], writes=[tmp16])
                                    k.op(eng, lambda e: e.tensor_tensor(out=yp.t[:, :, 0:16], in0=tmp16.t[:], in1=u[:, :, 15:31],
                                                                        op=ALU.subtract), reads=[tmp16, cur], writes=[yp])
                                yT_ = ypT[it % 2]
                                for dc in range(2):
                                    pp = p_p[dc]
                                    k.ops("pe", [(lambda e, kc=kc: e.matmul(pp.t[:], lhsT=pw.t[:, pg, kc, dc * 128:(dc + 1) * 128],
                                                                            rhs=yp.t[:, kc, :], start=(kc == 0), stop=(kc == 1)))
                                                 for kc in range(2)], reads=[pw, yp], writes=[pp])
                                    k.op("act", lambda e: e.activation(out=yT_.t[:, dc, :], in_=pp.t[:], func=AF.Copy,
                                                                       scale=pscT.t[:, 2 * pg + dc:2 * pg + dc + 1]),
                                         reads=[pp, pscT], writes=[yT_])
                                k.dma("sp", psem_[it % 2], lambda e: e.dma_start(
                                    out=ymT_d[16 + 2 * pg:16 + 2 * pg + 2, :, ts:ts + 512].rearrange("f p t -> p f t"), in_=yT_.t[:]),
                                    reads=[yT_])
                                it += 1

        k.barrier()
        if "A2" in phases:
            with ExitStack() as e3:
                wout = sb(e3, "wout", [128, 24, DM], BF16)
                k.dma("pool", sem_cp, [(lambda e, i=i: e.dma_start(
                    out=wout.t[:, 6 * i:6 * i + 6, :],
                    in_=w_out_d[768 * i:768 * (i + 1), :].rearrange("(k p) n -> p k n", p=128))) for i in range(4)],
                    writes=[wout])
                gffn_bc = load_const(e3, "gffn_bc", [128, DM], gffn_bc_d)
                rw = load_const(e3, "rw", [128, 8, NE], rw_d.rearrange("(k p) n -> p k n", p=128))
                rb = load_const(e3, "rb", [128, NE], rb_d)
                ebase = load_const(e3, "ebase", [128, NE], ebase_d)
                s4096 = sb(e3, "s4096", [128, 514], I32)
                k.op("pool", lambda e: e.memset(s4096.t[:], L), writes=[s4096])
                ev_init = [
                    k.dma("sp", sem_c, lambda e: e.dma_start(out=slot_d.rearrange("(p f) o -> p (f o)", p=128), in_=s4096.t[:]),
                          reads=[s4096]),
                    k.dma("sp", sem_c, lambda e: e.dma_start(out=h2_d[L:L + 1, :], in_=zrow_bf.t[:]), reads=[zrow_bf]),
                    k.dma("sp", sem_c, lambda e: e.dma_start(out=G_d[L:L + 1, :], in_=zrow.t[:, 0:NE]), reads=[zrow]),
                    k.dma("sp", sem_c, lambda e: e.dma_start(out=Y_d[0:1, :], in_=zrow.t[:]), reads=[zrow]),
                ]
                ym = [sb(e3, "ym%d" % i, [128, 24, 512], BF16) for i in range(2)]
                ymsem = [dsem(), dsem()]
                xt2 = [sb(e3, "xt2_%d" % i, [128, DM], F32) for i in range(2)]
                xsem2 = [dsem(), dsem()]
                x1 = [sb(e3, "x1_%d" % i, [128, DM], F32) for i in range(2)]
                x1sem = [dsem(), dsem()]
                junk3 = sb(e3, "junk3", [128, DM], F32)
                ss3 = sb(e3, "ss3", [128, 1], F32)
                rs3 = sb(e3, "rs3", [128, 1], F32)
                h2f = sb(e3, "h2f", [128, DM], F32)
                h2b = [sb(e3, "h2b%d" % i, [128, DM], BF16) for i in range(2)]
                h2sem = [dsem(), dsem()]
                h2T = sb(e3, "h2T", [128, 8, 128], F32)
                lg = sb(e3, "lg", [128, NE], F32)
                m8 = sb(e3, "m8", [128, 8], F32)
                nv1 = sb(e3, "nv1", [128, 1], F32)
                mask = sb(e3, "mask", [128, NE], F32)
                ex = sb(e3, "ex", [128, NE], F32)
                sm = sb(e3, "sm", [128, 1], F32)
                Gt = [sb(e3, "Gt%d" % i, [128, NE], F32) for i in range(2)]
                gsem = [dsem(), dsem()]
                cnt = sb(e3, "cnt", [128, NE], F32)
                rank = sb(e3, "rank", [128, NE], F32)
                vld = sb(e3, "vld", [128, NE], F32)
                val = sb(e3, "val", [128, NE], F32)
                v8 = sb(e3, "v8", [128, 8], F32)
                scsem = dsem()
                p_o = ps(e3, "p_o", [128, DM], F32)
                p_tf = ps(e3, "p_tf", [128, 8, 128], F32)
                p_l = ps(e3, "p_l", [128, NE], F32)
                p_r = ps(e3, "p_r", [128, 2, NE], F32)
                k.op("pool", lambda e: e.memset(cnt.t[:], 0.0), writes=[cnt])
                scat_evs = []
                for sc in range(NSC):
                    ts = sc * 512
                    ymc = ym[sc % 2]
                    k.dma("sp", ymsem[sc % 2], [(lambda e, i=i: e.dma_start(
                        out=ymc.t[:, 6 * i:6 * i + 6, :],
                        in_=ymT_d[6 * i:6 * i + 6, :, ts:ts + 512].rearrange("f p t -> p f t"))) for i in range(4)],
                        writes=[ymc])
                    for q in range(4):
                        c = sc * 4 + q
                        b = c % 2
                        qs = slice(q * 128, (q + 1) * 128)
                        cs = slice(c * 128, (c + 1) * 128)
                        k.dma("sp", xsem2[b], lambda e: e.dma_start(out=xt2[b].t[:], in_=x_d[cs, :]), writes=[xt2[b]])
                        k.ops("pe", [(lambda e, fc=fc, h=h: e.matmul(p_o.t[:, h * 512:(h + 1) * 512], lhsT=ymc.t[:, fc, qs],
                                                                     rhs=wout.t[:, fc, h * 512:(h + 1) * 512],
                                                                     start=(fc == 0), stop=(fc == 23)))
                                     for h in range(2) for fc in range(24)], reads=[ymc, wout], writes=[p_o])
                        k.op("dve", lambda e: e.tensor_tensor(out=x1[b].t[:], in0=p_o.t[:], in1=xt2[b].t[:], op=ALU.add),
                             reads=[p_o, xt2[b]], writes=[x1[b]])
                        k.dma("sp", x1sem[b], lambda e: e.dma_start(out=x1_d[cs, :], in_=x1[b].t[:]), reads=[x1[b]])
                        k.op("act", lambda e: e.activation(out=junk3.t[:], in_=x1[b].t[:], func=AF.Square, accum_out=ss3.t[:, 0:1]),
                             reads=[x1[b]], writes=[junk3, ss3])
                        k.op("act", lambda e: e.activation(out=rs3.t[:], in_=ss3.t[:], func=AF.Sqrt, scale=1.0 / DM, bias=EPS),
                             reads=[ss3], writes=[rs3])
                        k.op("dve", lambda e: e.reciprocal(out=rs3.t[:], in_=rs3.t[:]), reads=[rs3], writes=[rs3])
                        k.op("dve", lambda e: e.scalar_tensor_tensor(out=h2f.t[:], in0=x1[b].t[:], scalar=rs3.t[:, 0:1],
                                                                     in1=gffn_bc.t[:], op0=ALU.mult, op1=ALU.mult),
                             reads=[x1[b], rs3, gffn_bc], writes=[h2f])
                        k.op("act", lambda e: e.copy(out=h2b[b].t[:], in_=h2f.t[:]), reads=[h2f], writes=[h2b[b]])
                        k.dma("sp", h2sem[b], lambda e: e.dma_start(out=h2_d[cs, :], in_=h2b[b].t[:]), reads=[h2b[b]])
                        k.ops("pe", [(lambda e, j=j: e.transpose(out=p_tf.t[:, j, :], in_=h2f.t[:, j * 128:(j + 1) * 128],
                                                                 identity=ident_f.t[:])) for j in range(8)],
                              reads=[h2f, ident_f], writes=[p_tf])
                        k.op("act", lambda e: e.copy(out=h2T.t[:], in_=p_tf.t[:]), reads=[p_tf], writes=[h2T])
                        k.ops("pe", [(lambda e, j=j: e.matmul(p_l.t[:], lhsT=h2T.t[:, j, :], rhs=rw.t[:, j, :],
                                                              start=(j == 0), stop=(j == 7))) for j in range(8)],
                              reads=[h2T, rw], writes=[p_l])
                        k.op("dve", lambda e: e.tensor_tensor(out=lg.t[:], in0=p_l.t[:], in1=rb.t[:], op=ALU.add),
                             reads=[p_l, rb], writes=[lg])
                        k.op("dve", lambda e: e.max(out=m8.t[:], in_=lg.t[:]), reads=[lg], writes=[m8])
                        k.op("dve", lambda e: e.tensor_scalar(out=mask.t[:], in0=lg.t[:], scalar1=m8.t[:, 3:4], scalar2=None,
                                                              op0=ALU.is_ge), reads=[lg, m8], writes=[mask])
                        k.op("dve", lambda e: e.tensor_scalar(out=nv1.t[:], in0=m8.t[:, 0:1], scalar1=-1.0, scalar2=None,
                                                              op0=ALU.mult), reads=[m8], writes=[nv1])
                        k.op("act", lambda e: e.activation(out=ex.t[:], in_=lg.t[:], func=AF.Exp, bias=nv1.t[:, 0:1]),
                             reads=[lg, nv1], writes=[ex])
                        k.op("dve", lambda e: e.tensor_tensor(out=ex.t[:], in0=ex.t[:], in1=mask.t[:], op=ALU.mult),
                             reads=[ex, mask], writes=[ex])
                        k.op("dve", lambda e: e.reduce_sum(out=sm.t[:], in_=ex.t[:], axis=AX.X), reads=[ex], writes=[sm])
                        k.op("dve", lambda e: e.reciprocal(out=sm.t[:], in_=sm.t[:]), reads=[sm], writes=[sm])
                        k.op("dve", lambda e: e.tensor_scalar(out=Gt[b].t[:], in0=ex.t[:], scalar1=sm.t[:, 0:1], scalar2=None,
                                                              op0=ALU.mult), reads=[ex, sm], writes=[Gt[b]])
                        k.dma("sp", gsem[b], lambda e: e.dma_start(out=G_d[cs, :], in_=Gt[b].t[:]), reads=[Gt[b]])
                        k.ops("pe", [
                            lambda e: e.matmul(p_r.t[:, 0, :], lhsT=Mlt.t[:], rhs=mask.t[:], start=True, stop=True),
                            lambda e: e.matmul(p_r.t[:, 1, :], lhsT=ones_f.t[:], rhs=mask.t[:], start=True, stop=True),
                        ], reads=[Mlt, ones_f, mask], writes=[p_r])
                        k.op("dve", lambda e: e.tensor_tensor(out=rank.t[:], in0=p_r.t[:, 0, :], in1=cnt.t[:], op=ALU.add),
                             reads=[p_r, cnt], writes=[rank])
                        k.op("dve", lambda e: e.tensor_tensor(out=cnt.t[:], in0=p_r.t[:, 1, :], in1=cnt.t[:], op=ALU.add),
                             reads=[p_r, cnt], writes=[cnt])
                        k.op("dve", lambda e: e.tensor_scalar(out=vld.t[:], in0=rank.t[:], scalar1=float(CAP), scalar2=None,
                                                              op0=ALU.is_lt), reads=[rank], writes=[vld])
                        k.op("dve", lambda e: e.tensor_tensor(out=vld.t[:], in0=vld.t[:], in1=mask.t[:], op=ALU.mult),
                             reads=[vld, mask], writes=[vld])
                        k.op("dve", lambda e: e.tensor_tensor(out=val.t[:], in0=rank.t[:], in1=ebase.t[:], op=ALU.add),
                             reads=[rank, ebase], writes=[val])
                        k.op("dve", lambda e: e.tensor_tensor(out=val.t[:], in0=val.t[:], in1=vld.t[:], op=ALU.mult),
                             reads=[val, vld], writes=[val])
                        k.op("dve", lambda e: e.max(out=v8.t[:], in_=val.t[:]), reads=[val], writes=[v8])
                        k.op("dve", lambda e: e.tensor_copy(out=dest_all.t[:, c, :], in_=v8.t[:, 0:4]), reads=[v8], writes=[dest_all])
                        for kk in range(4):
                            scat_evs.append(k.dma("pool", scsem, lambda e: e.indirect_dma_start(
                                out=slot_d[:, :], out_offset=bass.IndirectOffsetOnAxis(ap=dest_all.t[:, c, kk:kk + 1], axis=0),
                                in_=tokid.t[:, c, :], in_offset=None),
                                reads=[dest_all, tokid], extra=ev_init))
                a2_done = [x1[0], x1[1], h2b[0], h2b[1], Gt[0], Gt[1]]
                a2_evs = list(scat_evs[-1:])
                for t in a2_done:
                    a2_evs += t.r.rs
        else:
            a2_evs = []

        k.barrier()
        if "B" in phases:
            with ExitStack() as e4:
                bguT = load_const(e4, "bguT", [128, NE, 16], bguT_d)
                wgu = [sb(e4, "wgu%d" % i, [128, 8, 2048], BF16) for i in range(2)]
                wdn = [sb(e4, "wdn%d" % i, [128, 8, DM], BF16) for i in range(2)]
                bdb = [sb(e4, "bdb%d" % i, [128, DM], F32) for i in range(2)]
                wesem = [dsem(), dsem()]
                bdsem = [dsem(), dsem()]
                idx = [sb(e4, "idx%d" % i, [128, NBLK, 2], I32) for i in range(2)]
                isem = [dsem(), dsem()]
                xg = [sb(e4, "xg%d" % i, [128, DM], BF16) for i in range(3)]
                xgsem = [dsem() for _ in range(3)]
                gg = [sb(e4, "gg%d" % i, [128, NBLK, NE], F32) for i in range(2)]
                ggsem = [dsem(), dsem()]
                xgT = sb(e4, "xgT", [128, 8, CAP], BF16)
                actT = sb(e4, "actT", [128, 8, CAP], BF16)
                HW = CAP // 2
                gm = [sb(e4, "gm%d" % i, [128, HW], F32) for i in range(2)]
                sg = [sb(e4, "sg%d" % i, [128, HW], F32) for i in range(2)]
                u1 = [sb(e4, "u1_%d" % i, [128, HW], F32) for i in range(2)]
                tt = [sb(e4, "tt%d" % i, [128, HW], F32) for i in range(2)]
                yA = [sb(e4, "yA%d" % i, [128, DM], F32) for i in range(2)]
                yB = [sb(e4, "yB%d" % i, [128, DM], F32) for i in range(2)]
                ysem2 = [dsem(), dsem()]
                p_tg = ps(e4, "p_tg", [128, 8, 128], BF16)
                p_g = [ps(e4, "p_g%d" % i, [128, 512], F32) for i in range(2)]
                p_up = [ps(e4, "p_up%d" % i, [128, 512], F32) for i in range(2)]
                p_d = ps(e4, "p_d", [128, DM], F32)

                def load_w(e_):
                    s = e_ % 2
                    k.dma("pool", wesem[s],
                          [(lambda e, i=i: e.dma_start(out=wgu[s].t[:, 2 * i:2 * i + 2, :],
                                                       in_=wgu_d[e_, 256 * i:256 * (i + 1), :].rearrange("(k p) n -> p k n", p=128)))
                           for i in range(4)] +
                          [(lambda e, i=i: e.dma_start(out=wdn[s].t[:, 4 * i:4 * i + 4, :],
                                                       in_=wd_d[e_, 512 * i:512 * (i + 1), :].rearrange("(k p) n -> p k n", p=128)))
                           for i in range(2)],
                          writes=[wgu[s], wdn[s]])
                    k.dma("sp", bdsem[s], lambda e: e.dma_start(out=bdb[s].t[:], in_=bd_d[e_:e_ + 1, :].to_broadcast([128, DM])),
                          writes=[bdb[s]])

                load_w(0)
                gi = 0
                for e_ in range(NE):
                    s = e_ % 2
                    if e_ + 1 < NE:
                        load_w(e_ + 1)
                    base = 1 + e_ * CAP
                    k.dma("sp", isem[s], [(lambda e, j=j: e.dma_start(out=idx[s].t[:, j, :],
                                                                      in_=slot_d[base + j * 128:base + (j + 1) * 128, :]))
                                          for j in range(NBLK)], writes=[idx[s]], extra=a2_evs)
                    for j in range(NBLK):
                        xs_ = xg[gi % 3]
                        k.dma("pool", xgsem[gi % 3], lambda e: e.indirect_dma_start(
                            out=xs_.t[:, :], out_offset=None, in_=h2_d[:, :],
                            in_offset=bass.IndirectOffsetOnAxis(ap=idx[s].t[:, j, 0:1], axis=0)),
                            reads=[idx[s]], writes=[xs_], extra=a2_evs)
                        k.ops("pe", [(lambda e, kk=kk: e.transpose(out=p_tg.t[:, kk, :], in_=xs_.t[:, kk * 128:(kk + 1) * 128],
                                                                   identity=ident_bf.t[:])) for kk in range(8)],
                              reads=[xs_, ident_bf], writes=[p_tg])
                        k.op("act", lambda e: e.copy(out=xgT.t[:, :, j * 128:(j + 1) * 128], in_=p_tg.t[:]),
                             reads=[p_tg], writes=[xgT])
                        gi += 1
                    k.dma("pool", ggsem[s], [(lambda e, j=j: e.indirect_dma_start(
                        out=gg[s].t[:, j, :], out_offset=None, in_=G_d[:, :],
                        in_offset=bass.IndirectOffsetOnAxis(ap=idx[s].t[:, j, 0:1], axis=0))) for j in range(NBLK)],
                        reads=[idx[s]], writes=[gg[s]], extra=a2_evs)
                    for fc in range(8):
                        for h in range(2):
                            hs = slice(h * HW, (h + 1) * HW)
                            k.ops("pe", [(lambda e, kk=kk: e.matmul(p_g[h].t[:, 0:HW], lhsT=wgu[s].t[:, kk, fc * 128:(fc + 1) * 128],
                                                                    rhs=xgT.t[:, kk, hs], start=(kk == 0), stop=(kk == 7)))
                                         for kk in range(8)], reads=[wgu[s], xgT], writes=[p_g[h]])
                            k.ops("pe", [(lambda e, kk=kk: e.matmul(p_up[h].t[:, 0:HW],
                                                                    lhsT=wgu[s].t[:, kk, 1024 + fc * 128:1024 + (fc + 1) * 128],
                                                                    rhs=xgT.t[:, kk, hs], start=(kk == 0), stop=(kk == 7)))
                                         for kk in range(8)], reads=[wgu[s], xgT], writes=[p_up[h]])
                            k.op("dve", lambda e: e.tensor_scalar(out=gm[h].t[:], in0=p_g[h].t[:, 0:HW],
                                                                  scalar1=bguT.t[:, e_, fc:fc + 1], scalar2=7.0,
                                                                  op0=ALU.add, op1=ALU.min), reads=[p_g[h], bguT], writes=[gm[h]])
                            k.op("act", lambda e: e.activation(out=sg[h].t[:], in_=gm[h].t[:], func=AF.Sigmoid, scale=1.702),
                                 reads=[gm[h]], writes=[sg[h]])
                            k.op("dve", lambda e: e.tensor_scalar(out=u1[h].t[:], in0=p_up[h].t[:, 0:HW],
                                                                  scalar1=bguT.t[:, e_, 8 + fc:9 + fc], scalar2=7.0,
                                                                  op0=ALU.add, op1=ALU.min), reads=[p_up[h], bguT], writes=[u1[h]])
                            k.op("dve", lambda e: e.tensor_scalar(out=u1[h].t[:], in0=u1[h].t[:], scalar1=-7.0, scalar2=1.0,
                                                                  op0=ALU.max, op1=ALU.add), reads=[u1[h]], writes=[u1[h]])
                            k.op("pool", lambda e: e.tensor_tensor(out=tt[h].t[:], in0=gm[h].t[:], in1=sg[h].t[:], op=ALU.mult),
                                 reads=[gm[h], sg[h]], writes=[tt[h]])
                            k.op("pool", lambda e: e.tensor_tensor(out=actT.t[:, fc, hs], in0=tt[h].t[:], in1=u1[h].t[:], op=ALU.mult),
                                 reads=[tt[h], u1[h]], writes=[actT])
                    for j in range(NBLK):
                        js = slice(j * 128, (j + 1) * 128)
                        b = j % 2
                        k.ops("pe", [(lambda e, kk=kk, h=h: e.matmul(p_d.t[:, h * 512:(h + 1) * 512], lhsT=actT.t[:, kk, js],
                                                                     rhs=wdn[s].t[:, kk, h * 512:(h + 1) * 512],
                                                                     start=(kk == 0), stop=(kk == 7)))
                                     for h in range(2) for kk in range(8)], reads=[actT, wdn[s]], writes=[p_d])
                        k.op("dve", lambda e: e.tensor_tensor(out=yA[b].t[:], in0=p_d.t[:], in1=bdb[s].t[:], op=ALU.add),
                             reads=[p_d, bdb[s]], writes=[yA[b]])
                        k.op("act", lambda e: e.activation(out=yB[b].t[:], in_=yA[b].t[:], func=AF.Copy,
                                                           scale=gg[s].t[:, j, e_:e_ + 1]),
                             reads=[yA[b], gg[s]], writes=[yB[b]])
                        k.dma("sp", ysem2[b], lambda e: e.dma_start(out=Y_d[base + j * 128:base + (j + 1) * 128, :], in_=yB[b].t[:]),
                              reads=[yB[b]])
                b_evs = []
                for t in yB:
                    b_evs += t.r.rs
        else:
            b_evs = []

        k.barrier()
        fin_evs = []
        if "C" in phases:
            with ExitStack() as e5:
                wpg = sb(e5, "wpg", [128, 8, DM], BF16)
                k.dma("pool", sem_cp, [(lambda e, i=i: e.dma_start(
                    out=wpg.t[:, 4 * i:4 * i + 4, :],
                    in_=wpg_d[512 * i:512 * (i + 1), :].rearrange("(k p) n -> p k n", p=128))) for i in range(2)], writes=[wpg])
                wpp = sb(e5, "wpp", [128, 2, DM], BF16)
                k.dma("pool", sem_cp, lambda e: e.dma_start(out=wpp.t[:], in_=wpp_d.rearrange("(k p) n -> p k n", p=128)),
                      writes=[wpp])
                gpgT = load_const(e5, "gpgT", [128, 8], gpgT_d)
                gpn_bc = load_const(e5, "gpn_bc", [128, DM], gpn_bc_d)
                gfin_bc = load_const(e5, "gfin_bc", [128, DM], gfin_bc_d)
                x1c = [sb(e5, "x1c%d" % i, [128, DM], F32) for i in range(2)]
                x1csem = [dsem(), dsem()]
                yk = [[sb(e5, "yk%d_%d" % (i, kk), [128, DM], F32) for kk in range(4)] for i in range(2)]
                yksem = [[dsem() for kk in range(4)] for i in range(2)]
                pt = [sb(e5, "pt%d" % i, [128, 256], F32) for i in range(2)]
                ptsem = [dsem(), dsem()]
                x2 = sb(e5, "x2", [128, DM], F32)
                junk4 = sb(e5, "junk4", [128, DM], F32)
                ssc = sb(e5, "ssc", [128, 1], F32)
                rsc = sb(e5, "rsc", [128, 1], F32)
                xnb = sb(e5, "xnb", [128, DM], BF16)
                xnT = sb(e5, "xnT", [128, 8, 128], BF16)
                sgate = sb(e5, "sgate", [128, DM], F32)
                pT = sb(e5, "pT", [128, 2, 128], BF16)
                sse = sb(e5, "sse", [128, 1], F32)
                rse = sb(e5, "rse", [128, 1], F32)
                e1_ = sb(e5, "e1_", [128, DM], F32)
                x3 = sb(e5, "x3", [128, DM], F32)
                ssf = sb(e5, "ssf", [128, 1], F32)
                rsf = sb(e5, "rsf", [128, 1], F32)
                ot = [sb(e5, "ot%d" % i, [128, DM], F32) for i in range(2)]
                osem = [dsem(), dsem()]
                p_t3 = ps(e5, "p_t3", [128, 8, 128], BF16)
                p_ga = ps(e5, "p_ga", [128, DM], F32)
                p_pt = ps(e5, "p_pt", [128, 2, 128], F32)
                p_e = ps(e5, "p_e", [128, DM], F32)
                for c in range(NCH):
                    b = c % 2
                    cs = slice(c * 128, (c + 1) * 128)
                    k.dma("sp", x1csem[b], lambda e: e.dma_start(out=x1c[b].t[:], in_=x1_d[cs, :]), writes=[x1c[b]], extra=a2_evs)
                    k.dma("sp", ptsem[b], lambda e: e.dma_start(out=pt[b].t[:], in_=pin_d[cs, :]), writes=[pt[b]])
                    for kk in range(4):
                        k.dma("pool", yksem[b][kk], lambda e: e.indirect_dma_start(
                            out=yk[b][kk].t[:, :], out_offset=None, in_=Y_d[:, :],
                            in_offset=bass.IndirectOffsetOnAxis(ap=dest_all.t[:, c, kk:kk + 1], axis=0)),
                            reads=[dest_all], writes=[yk[b][kk]], extra=b_evs)
                    k.op("dve", lambda e: e.tensor_tensor(out=x2.t[:], in0=x1c[b].t[:], in1=yk[b][0].t[:], op=ALU.add),
                         reads=[x1c[b], yk[b][0]], writes=[x2])
                    k.op("pool", lambda e: e.tensor_tensor(out=yk[b][1].t[:], in0=yk[b][1].t[:], in1=yk[b][2].t[:], op=ALU.add),
                         reads=[yk[b][1], yk[b][2]], writes=[yk[b][1]])
                    k.op("dve", lambda e: e.tensor_tensor(out=x2.t[:], in0=x2.t[:], in1=yk[b][3].t[:], op=ALU.add),
                         reads=[x2, yk[b][3]], writes=[x2])
                    k.op("dve", lambda e: e.tensor_tensor(out=x2.t[:], in0=x2.t[:], in1=yk[b][1].t[:], op=ALU.add),
                         reads=[x2, yk[b][1]], writes=[x2])
                    k.op("act", lambda e: e.activation(out=junk4.t[:], in_=x2.t[:], func=AF.Square, accum_out=ssc.t[:, 0:1]),
                         reads=[x2], writes=[junk4, ssc])
                    k.op("act", lambda e: e.activation(out=rsc.t[:], in_=ssc.t[:], func=AF.Sqrt, scale=1.0 / DM, bias=EPS),
                         reads=[ssc], writes=[rsc])
                    k.op("dve", lambda e: e.reciprocal(out=rsc.t[:], in_=rsc.t[:]), reads=[rsc], writes=[rsc])
                    k.op("act", lambda e: e.activation(out=xnb.t[:], in_=x2.t[:], func=AF.Copy, scale=rsc.t[:, 0:1]),
                         reads=[x2, rsc], writes=[xnb])
                    k.ops("pe", [(lambda e, j=j: e.transpose(out=p_t3.t[:, j, :], in_=xnb.t[:, j * 128:(j + 1) * 128],
                                                             identity=ident_bf.t[:])) for j in range(8)],
                          reads=[xnb, ident_bf], writes=[p_t3])
                    k.op("dve", lambda e: e.tensor_tensor(out=xnT.t[:], in0=p_t3.t[:],
                                                          in1=gpgT.t[:, :, None].to_broadcast([128, 8, 128]), op=ALU.mult),
                         reads=[p_t3, gpgT], writes=[xnT])
                    k.ops("pe", [(lambda e, j=j, h=h: e.matmul(p_ga.t[:, h * 512:(h + 1) * 512], lhsT=xnT.t[:, j, :],
                                                               rhs=wpg.t[:, j, h * 512:(h + 1) * 512], start=(j == 0), stop=(j == 7)))
                                 for h in range(2) for j in range(8)], reads=[xnT, wpg], writes=[p_ga])
                    k.op("act", lambda e: e.activation(out=sgate.t[:], in_=p_ga.t[:], func=AF.Sigmoid), reads=[p_ga], writes=[sgate])
                    k.ops("pe", [(lambda e, j=j: e.transpose(out=p_pt.t[:, j, :], in_=pt[b].t[:, j * 128:(j + 1) * 128],
                                                             identity=ident_f.t[:])) for j in range(2)],
                          reads=[pt[b], ident_f], writes=[p_pt])
                    k.op("act", lambda e: e.copy(out=pT.t[:], in_=p_pt.t[:]), reads=[p_pt], writes=[pT])
                    k.ops("pe", [(lambda e, j=j, h=h: e.matmul(p_e.t[:, h * 512:(h + 1) * 512], lhsT=pT.t[:, j, :],
                                                               rhs=wpp.t[:, j, h * 512:(h + 1) * 512], start=(j == 0), stop=(j == 1)))
                                 for h in range(2) for j in range(2)], reads=[pT, wpp], writes=[p_e])
                    k.op("act", lambda e: e.activation(out=junk4.t[:], in_=p_e.t[:], func=AF.Square, accum_out=sse.t[:, 0:1]),
                         reads=[p_e], writes=[junk4, sse])
                    k.op("act", lambda e: e.activation(out=rse.t[:], in_=sse.t[:], func=AF.Sqrt, scale=1.0 / DM, bias=EPS),
                         reads=[sse], writes=[rse])
                    k.op("dve", lambda e: e.reciprocal(out=rse.t[:], in_=rse.t[:]), reads=[rse], writes=[rse])
                    k.op("dve", lambda e: e.scalar_tensor_tensor(out=e1_.t[:], in0=p_e.t[:], scalar=rse.t[:, 0:1], in1=gpn_bc.t[:],
                                                                 op0=ALU.mult, op1=ALU.mult), reads=[p_e, rse, gpn_bc], writes=[e1_])
                    k.op("pool", lambda e: e.tensor_tensor(out=e1_.t[:], in0=e1_.t[:], in1=sgate.t[:], op=ALU.mult),
                         reads=[e1_, sgate], writes=[e1_])
                    k.op("dve", lambda e: e.tensor_tensor(out=x3.t[:], in0=x2.t[:], in1=e1_.t[:], op=ALU.add),
                         reads=[x2, e1_], writes=[x3])
                    k.op("act", lambda e: e.activation(out=junk4.t[:], in_=x3.t[:], func=AF.Square, accum_out=ssf.t[:, 0:1]),
                         reads=[x3], writes=[junk4, ssf])
                    k.op("act", lambda e: e.activation(out=rsf.t[:], in_=ssf.t[:], func=AF.Sqrt, scale=1.0 / DM, bias=EPS),
                         reads=[ssf], writes=[rsf])
                    k.op("dve", lambda e: e.reciprocal(out=rsf.t[:], in_=rsf.t[:]), reads=[rsf], writes=[rsf])
                    k.op("dve", lambda e: e.scalar_tensor_tensor(out=ot[b].t[:], in0=x3.t[:], scalar=rsf.t[:, 0:1], in1=gfin_bc.t[:],
                                                                 op0=ALU.mult, op1=ALU.mult), reads=[x3, rsf, gfin_bc], writes=[ot[b]])
                    fin_evs.append(k.dma("sp", osem[b], lambda e: e.dma_start(out=out_d[cs, :], in_=ot[b].t[:]), reads=[ot[b]]))

        tail = list(fin_evs[-2:]) + list(a2_evs) + list(b_evs)
        for sid_ev in tail:
            k.wait("sp", sid_ev)
        for sid_, sem_ in k.dma_objs.items():
            k.wait("sp", (sem_, k.dma_cnt[sid_]))
        build.stats = (k.ninst, k.nwaits, dict(k.cnt))
    return nc


def host_layout(inp):
    f = np.float32

    def colT(v, n):
        return np.ascontiguousarray(np.asarray(v, f).reshape(n, 128).T)

    def bc(v):
        v = np.asarray(v, f).reshape(1, -1)
        return np.ascontiguousarray(np.broadcast_to(v, (128, v.shape[1])))

    cw = np.asarray(inp["conv_w"][0], f)
    conv_wT = np.ascontiguousarray(cw.reshape(4, 32, 128).transpose(2, 1, 0))
    bgu = np.asarray(inp["b_gate_up"][0], f)
    bguT = np.ascontiguousarray(bgu.reshape(NE, 16, 128).transpose(2, 0, 1))
    invc = np.zeros((128, 4, 16), f)
    for gi, w in enumerate((2, 4, 8, 16)):
        invc[:, gi, :] = 1.0 / np.minimum(np.arange(16) + 1, w).astype(f)
    ebase = (1 + np.arange(NE) * CAP).astype(f)
    shared = {
        "w_in": np.ascontiguousarray(inp["w_in"][0], f),
        "conv_wT": conv_wT,
        "conv_bT": colT(inp["conv_b"][0], 32),
        "conv_brow": np.ascontiguousarray(np.asarray(inp["conv_b"][0], f).reshape(1, 4096)),
        "dt_bias_bc": bc(inp["dt_bias"][0]),
        "a_log_bc": bc(inp["a_log"][0]),
        "d_skip_bc": bc(inp["d_skip"][0]),
        "gmixT": colT(inp["mix_norm_g"][0], 8),
        "gssdT": colT(inp["ssd_norm_g"][0], 16),
        "pool_w": np.ascontiguousarray(inp["pool_w"][0], f),
        "pscT": colT(inp["pool_scale"][0], 8),
        "invc": invc,
        "w_out": np.ascontiguousarray(inp["w_out"][0], f),
        "gffn_bc": bc(inp["ffn_norm_g"][0]),
        "router_w": np.ascontiguousarray(inp["router_w"][0], f),
        "rb_bc": bc(inp["router_b"][0]),
        "ebase_bc": bc(ebase),
        "w_gate_up": np.ascontiguousarray(inp["w_gate_up"][0], f),
        "bguT": bguT,
        "w_down": np.ascontiguousarray(inp["w_down"][0], f),
        "b_down": np.ascontiguousarray(inp["b_down"][0], f),
        "gpgT": colT(inp["ple_gate_norm_g"][0], 8),
        "w_ple_gate": np.ascontiguousarray(inp["w_ple_gate"][0], f),
        "w_ple_proj": np.ascontiguousarray(inp["w_ple_proj"][0], f),
        "gpn_bc": bc(inp["ple_norm_g"][0]),
        "gfin_bc": bc(inp["final_norm_g"]),
    }
    return shared


def kernel(**inputs):
    inp = {k_: np.asarray(v) for k_, v in inputs.items()}
    shared = host_layout(inp)
    x = np.asarray(inp["x"], np.float32)
    p = np.asarray(inp["p"], np.float32)[0]
    nb = x.shape[0]
    in_maps = []
    for b in range(nb):
        m = dict(shared)
        m["x"] = np.ascontiguousarray(x[b])
        m["p"] = np.ascontiguousarray(p[b])
        in_maps.append(m)
    nc = build()
    res = run_bass_kernel_spmd(nc, in_maps, core_ids=list(range(nb)))
    return np.stack([np.asarray(r["out"], np.float32) for r in res.results], axis=0)
```

```python
from contextlib import ExitStack
import numpy as np
import concourse.bass as bass
import concourse.mybir as mybir
from concourse.bass_utils import run_bass_kernel_spmd

F32 = mybir.dt.float32
BF16 = mybir.dt.bfloat16
I32 = mybir.dt.int32
AF = mybir.ActivationFunctionType
ALU = mybir.AluOpType
AX = mybir.AxisListType

L = 4096
DM = 1024
NCH = 32
NSC = 8
D_IN = 7200
NE = 32
CAP = 1024
NBLK = CAP // 128
NSLOT = NE * CAP
SLOT_TAB = 128 * 257
EPS = 1e-6


class R:
    __slots__ = ("w", "rs")

    def __init__(self):
        self.w = None
        self.rs = []


class T:
    def __init__(self, t):
        self.t = t
        self.r = R()


class K:
    def __init__(self, nc, sems):
        self.nc = nc
        self.engs = {"pe": nc.tensor, "dve": nc.vector, "act": nc.scalar,
                     "pool": nc.gpsimd, "sp": nc.sync}
        self.psem = sems
        self.cnt = {k: 0 for k in self.engs}
        self.waited = {k: {} for k in self.engs}
        self.dma_cnt = {}
        self.dma_objs = {}
        self.ninst = 0
        self.nwaits = 0

    def wait(self, ek, ev):
        if ev is None:
            return
        sem, val = ev
        sid = id(sem)
        if sid in self.dma_cnt:
            val = max(val, self.dma_cnt[sid])
        w = self.waited[ek]
        if w.get(sid, 0) >= val:
            return
        self.engs[ek].wait_ge(sem, val)
        self.nwaits += 1
        w[sid] = val

    def _deps(self, ek, reads, writes, extra):
        for t in reads:
            self.wait(ek, t.r.w)
        for t in writes:
            self.wait(ek, t.r.w)
            for e in t.r.rs:
                self.wait(ek, e)
        for e in extra:
            self.wait(ek, e)

    def _commit(self, ev, reads, writes):
        for t in reads:
            t.r.rs.append(ev)
        for t in writes:
            t.r.w = ev
            t.r.rs = []

    def barrier(self):
        for ek in self.engs:
            for o in self.engs:
                if self.cnt[o] > 0:
                    self.wait(ek, (self.psem[o], self.cnt[o]))
            for sid_, sem_ in self.dma_objs.items():
                self.wait(ek, (sem_, self.dma_cnt[sid_]))

    def op(self, ek, fn, reads=(), writes=(), extra=()):
        self._deps(ek, reads, writes, extra)
        ins = fn(self.engs[ek])
        self.cnt[ek] += 1
        ins.then_inc(self.psem[ek], 1)
        ev = (self.psem[ek], self.cnt[ek])
        self._commit(ev, reads, writes)
        self.ninst += 1
        return ev

    def ops(self, ek, fns, reads=(), writes=(), extra=()):
        self._deps(ek, reads, writes, extra)
        ins = None
        for fn in fns:
            ins = fn(self.engs[ek])
            self.ninst += 1
        self.cnt[ek] += 1
        ins.then_inc(self.psem[ek], 1)
        ev = (self.psem[ek], self.cnt[ek])
        self._commit(ev, reads, writes)
        return ev

    def dma(self, ek, sem, fns, reads=(), writes=(), extra=()):
        self._deps(ek, reads, writes, extra)
        if not isinstance(fns, (list, tuple)):
            fns = [fns]
        sid = id(sem)
        cur = self.dma_cnt.get(sid, 0)
        for f in fns:
            f(self.engs[ek]).then_inc(sem, 16)
            cur += 16
            self.ninst += 1
        self.dma_cnt[sid] = cur
        self.dma_objs[sid] = sem
        ev = (sem, cur)
        self._commit(ev, reads, writes)
        return ev


def run_chains(gens, lag=4, width=2):
    pending = list(gens)
    active = []
    while pending or active:
        if len(active) < width and pending and (not active or active[-1][1] >= lag):
            active.append([pending.pop(0), 0])
        for a in list(active):
            try:
                next(a[0])
                a[1] += 1
            except StopIteration:
                active.remove(a)


def build(debug=None, phases="A0,A1,A1p,A2,B,C"):
    phases = set(phases.split(","))
    debug = debug or ()
    nc = bass.Bass("TRN2", target_bir_lowering=False)

    def din(name, shape, dt=F32):
        return nc.dram_tensor(name, list(shape), dt, kind="ExternalInput").ap()

    x_d = din("x", [L, DM])
    pin_d = din("p", [L, 256])
    w_in_d = din("w_in", [DM, D_IN])
    conv_wT_d = din("conv_wT", [128, 32, 4])
    conv_bT_d = din("conv_bT", [128, 32])
    conv_brow_d = din("conv_brow", [1, 4096])
    dtb_d = din("dt_bias_bc", [128, 32])
    alog_d = din("a_log_bc", [128, 32])
    dsk_d = din("d_skip_bc", [128, 32])
    gmixT_d = din("gmixT", [128, 8])
    gssdT_d = din("gssdT", [128, 16])
    pool_w_d = din("pool_w", [4, 256, 256])
    pscT_d = din("pscT", [128, 8])
    invc_d = din("invc", [128, 4, 16])
    w_out_d = din("w_out", [3072, DM])
    gffn_bc_d = din("gffn_bc", [128, DM])
    rw_d = din("router_w", [DM, NE])
    rb_d = din("rb_bc", [128, NE])
    ebase_d = din("ebase_bc", [128, NE])
    wgu_d = din("w_gate_up", [NE, DM, 2048])
    bguT_d = din("bguT", [128, NE, 16])
    wd_d = din("w_down", [NE, DM, DM])
    bd_d = din("b_down", [NE, DM])
    gpgT_d = din("gpgT", [128, 8])
    wpg_d = din("w_ple_gate", [DM, DM])
    wpp_d = din("w_ple_proj", [256, DM])
    gpn_bc_d = din("gpn_bc", [128, DM])
    gfin_bc_d = din("gfin_bc", [128, DM])
    out_d = nc.dram_tensor("out", [L, DM], F32, kind="ExternalOutput").ap()

    def dscr(name, shape, dt, dbg=False):
        kind = "ExternalOutput" if (name in debug) else "Internal"
        return nc.dram_tensor(name, list(shape), dt, kind=kind).ap()

    ymT_d = dscr("ymT", [24, 128, L], BF16)
    x1_d = dscr("x1d", [L, DM], F32)
    h2_d = dscr("h2d", [L + 1, DM], BF16)
    G_d = dscr("Gd", [L + 1, NE], F32)
    slot_d = dscr("slotd", [SLOT_TAB, 2], I32)
    Y_d = dscr("Yd", [NSLOT + 1, DM], F32)

    with ExitStack() as es:
        E = es.enter_context
        sems = {k: E(nc.semaphore("prog_" + k)) for k in ["pe", "dve", "act", "pool", "sp"]}
        k = K(nc, sems)
        nsem = [0]

        def dsem():
            nsem[0] += 1
            return E(nc.semaphore("d%d" % nsem[0]))

        def sb(es_, name, shape, dt=F32):
            return T(es_.enter_context(nc.sbuf_tensor("s_" + name, list(shape), dt)))

        def ps(es_, name, shape, dt=F32):
            esz = 4 if dt == F32 else 2
            n = 1
            for d_ in shape[1:]:
                n *= d_
            per_bank = 2048 // esz
            nb_ = (n + per_bank - 1) // per_bank
            base = es_.enter_context(nc.psum_tensor("ps_" + name, [128, nb_ * per_bank], dt))
            v = base[:, 0:n]
            if len(shape) == 3:
                v = v.rearrange("p (a b) -> p a b", a=shape[1])
            return T(v)

        def psbank(es_, name, dt=F32):
            per_bank = 2048 // (4 if dt == F32 else 2)
            return es_.enter_context(nc.psum_tensor("ps_" + name, [128, per_bank], dt))

        ident_bf = sb(es, "ident_bf", [128, 128], BF16)
        ident_f = sb(es, "ident_f", [128, 128], F32)
        Mle = sb(es, "Mle", [128, 128], F32)
        Mgt = sb(es, "Mgt", [128, 128], F32)
        Mlt = sb(es, "Mlt", [128, 128], F32)
        ones_f = sb(es, "ones_f", [128, 128], F32)
        dest_all = sb(es, "dest_all", [128, NCH, 4], I32)
        tokid = sb(es, "tokid", [128, NCH, 2], I32)
        zrow = sb(es, "zrow", [1, DM], F32)
        zrow_bf = sb(es, "zrow_bf", [1, DM], BF16)

        def dump(name, t, ap=None):
            if name not in debug:
                return
            a = ap if ap is not None else t.t[:]
            dd = nc.dram_tensor("dbg_" + name, list(a.shape), a.dtype, kind="ExternalOutput").ap()
            k.dma("sp", dsem(), lambda e: e.dma_start(out=dd, in_=a), reads=[t])

        def cst(t, fn):
            k.op("pool", fn, writes=[t])

        cst(ident_bf, lambda e: e.memset(ident_bf.t[:], 1.0))
        cst(ident_bf, lambda e: e.affine_select(out=ident_bf.t[:], in_=ident_bf.t[:], pattern=[[-1, 128]],
                                               compare_op=ALU.is_equal, fill=0.0, base=0, channel_multiplier=1))
        cst(ident_f, lambda e: e.memset(ident_f.t[:], 1.0))
        cst(ident_f, lambda e: e.affine_select(out=ident_f.t[:], in_=ident_f.t[:], pattern=[[-1, 128]],
                                              compare_op=ALU.is_equal, fill=0.0, base=0, channel_multiplier=1))
        cst(Mle, lambda e: e.memset(Mle.t[:], 1.0))
        cst(Mle, lambda e: e.affine_select(out=Mle.t[:], in_=Mle.t[:], pattern=[[1, 128]],
                                          compare_op=ALU.is_ge, fill=0.0, base=0, channel_multiplier=-1))
        cst(Mgt, lambda e: e.memset(Mgt.t[:], 1.0))
        cst(Mgt, lambda e: e.affine_select(out=Mgt.t[:], in_=Mgt.t[:], pattern=[[-1, 128]],
                                          compare_op=ALU.is_gt, fill=0.0, base=0, channel_multiplier=1))
        cst(Mlt, lambda e: e.memset(Mlt.t[:], 1.0))
        cst(Mlt, lambda e: e.affine_select(out=Mlt.t[:], in_=Mlt.t[:], pattern=[[1, 128]],
                                          compare_op=ALU.is_gt, fill=0.0, base=0, channel_multiplier=-1))
        cst(ones_f, lambda e: e.memset(ones_f.t[:], 1.0))
        cst(zrow, lambda e: e.memset(zrow.t[:], 0.0))
        cst(zrow_bf, lambda e: e.memset(zrow_bf.t[:], 0.0))
        cst(tokid, lambda e: e.iota(tokid.t[:], pattern=[[128, NCH], [0, 2]], base=0, channel_multiplier=1))
        cst(dest_all, lambda e: e.memset(dest_all.t[:], 0))

        sem_c = dsem()
        sem_cp = dsem()

        def load_const(es_, name, shape, src, dt=F32, q="sp"):
            t = sb(es_, name, shape, dt)
            k.dma(q, sem_c, lambda e: e.dma_start(out=t.t[:], in_=src), writes=[t])
            return t

        if "A0" in phases:
            with ExitStack() as esA:
                hT = sb(esA, "hT", [128, 8, L], BF16)
                dt_all = sb(esA, "dt_all", [128, NCH, 32], F32)
                a_all = sb(esA, "a_all", [128, NCH, 32], F32)
                e_all = sb(esA, "e_all", [128, NCH, 3, 32], F32)
                gmixT = load_const(esA, "gmixT", [128, 8], gmixT_d)
                gssdT = load_const(esA, "gssdT", [128, 16], gssdT_d)
                conv_wT = load_const(esA, "conv_wT", [128, 32, 4], conv_wT_d)
                conv_bT = load_const(esA, "conv_bT", [128, 32], conv_bT_d)
                dsk = load_const(esA, "dsk", [128, 32], dsk_d)
                pscT = load_const(esA, "pscT", [128, 8], pscT_d)
                invc = load_const(esA, "invc", [128, 4, 16], invc_d)

                with ExitStack() as e0:
                    dtb = load_const(e0, "dtb", [128, 32], dtb_d)
                    alog = load_const(e0, "alog", [128, 32], alog_d)
                    Abc = sb(e0, "Abc", [128, 32], F32)
                    wdt = sb(e0, "wdt", [128, 8, 32], BF16)
                    k.dma("pool", sem_cp, lambda e: e.dma_start(
                        out=wdt.t[:], in_=w_in_d[:, 6144:6176].rearrange("(k p) n -> p k n", p=128)), writes=[wdt])
                    k.op("act", lambda e: e.activation(out=Abc.t[:], in_=alog.t[:], func=AF.Exp), reads=[alog], writes=[Abc])
                    k.op("dve", lambda e: e.tensor_scalar(out=Abc.t[:], in0=Abc.t[:], scalar1=-1.0, scalar2=None, op0=ALU.mult),
                         reads=[Abc], writes=[Abc])
                    xt = [sb(e0, "xt%d" % i, [128, DM], F32) for i in range(2)]
                    xsem = [dsem(), dsem()]
                    junk = sb(e0, "junk", [128, DM], F32)
                    ss = sb(e0, "ss", [128, 1], F32)
                    rstd = sb(e0, "rstd", [128, 1], F32)
                    xn = sb(e0, "xn", [128, DM], BF16)
                    tpA = ps(e0, "tpA", [128, 8, 128], BF16)
                    pdt = ps(e0, "pdt", [128, 32], F32)
                    pcs = ps(e0, "pcs", [128, 3, 32], F32)
                    dtr = sb(e0, "dtr", [128, 32], F32)
                    t1 = sb(e0, "t1", [128, 32], F32)
                    t2 = sb(e0, "t2", [128, 32], F32)
                    for c in range(NCH):
                        xc_ = xt[c % 2]
                        cs = slice(c * 128, (c + 1) * 128)
                        k.dma("sp", xsem[c % 2], lambda e: e.dma_start(out=xc_.t[:], in_=x_d[cs, :]), writes=[xc_])
                        k.op("act", lambda e: e.activation(out=junk.t[:], in_=xc_.t[:], func=AF.Square, accum_out=ss.t[:, 0:1]),
                             reads=[xc_], writes=[junk, ss])
                        k.op("act", lambda e: e.activation(out=rstd.t[:], in_=ss.t[:], func=AF.Sqrt, scale=1.0 / DM, bias=EPS),
                             reads=[ss], writes=[rstd])
                        k.op("dve", lambda e: e.reciprocal(out=rstd.t[:], in_=rstd.t[:]), reads=[rstd], writes=[rstd])
                        k.op("act", lambda e: e.activation(out=xn.t[:], in_=xc_.t[:], func=AF.Copy, scale=rstd.t[:, 0:1]),
                             reads=[xc_, rstd], writes=[xn])
                        k.ops("pe", [(lambda e, j=j: e.transpose(out=tpA.t[:, j, :], in_=xn.t[:, j * 128:(j + 1) * 128],
                                                                 identity=ident_bf.t[:])) for j in range(8)],
                              reads=[xn, ident_bf], writes=[tpA])
                        k.op("dve", lambda e: e.tensor_tensor(out=hT.t[:, :, cs], in0=tpA.t[:],
                                                              in1=gmixT.t[:, :, None].to_broadcast([128, 8, 128]), op=ALU.mult),
                             reads=[tpA, gmixT], writes=[hT])
                        k.ops("pe", [(lambda e, j=j: e.matmul(pdt.t[:], lhsT=hT.t[:, j, cs], rhs=wdt.t[:, j, :],
                                                              start=(j == 0), stop=(j == 7))) for j in range(8)],
                              reads=[hT, wdt], writes=[pdt])
                        k.op("dve", lambda e: e.tensor_tensor(out=dtr.t[:], in0=pdt.t[:], in1=dtb.t[:], op=ALU.add),
                             reads=[pdt, dtb], writes=[dtr])
                        k.op("act", lambda e: e.activation(out=t1.t[:], in_=dtr.t[:], func=AF.Abs),
                             reads=[dtr], writes=[t1])
                        k.op("act", lambda e: e.activation(out=t2.t[:], in_=t1.t[:], func=AF.Exp, scale=-1.0), reads=[t1], writes=[t2])
                        k.op("act", lambda e: e.activation(out=t1.t[:], in_=t2.t[:], func=AF.Ln, bias=1.0), reads=[t2], writes=[t1])
                        k.op("dve", lambda e: e.scalar_tensor_tensor(out=dt_all.t[:, c, :], in0=dtr.t[:], scalar=0.0, in1=t1.t[:],
                                                                     op0=ALU.max, op1=ALU.add),
                             reads=[dtr, t1], writes=[dt_all])
                        k.op("dve", lambda e: e.tensor_tensor(out=a_all.t[:, c, :], in0=dt_all.t[:, c, :], in1=Abc.t[:], op=ALU.mult),
                             reads=[dt_all, Abc], writes=[a_all])
                        k.ops("pe", [
                            lambda e: e.matmul(pcs.t[:, 0, :], lhsT=Mle.t[:], rhs=a_all.t[:, c, :], start=True, stop=True),
                            lambda e: e.matmul(pcs.t[:, 1, :], lhsT=Mgt.t[:], rhs=a_all.t[:, c, :], start=True, stop=True),
                            lambda e: e.matmul(pcs.t[:, 2, :], lhsT=ones_f.t[:], rhs=a_all.t[:, c, :], start=True, stop=True),
                        ], reads=[a_all, Mle, Mgt, ones_f], writes=[pcs])
                        k.op("act", lambda e: e.activation(out=e_all.t[:, c, :, :], in_=pcs.t[:], func=AF.Exp),
                             reads=[pcs], writes=[e_all])

                    k.barrier()
                dump("hT", hT); dump("dt_all", dt_all); dump("a_all", a_all); dump("e_all", e_all)
                if "A1" in phases:
                    with ExitStack() as e1:
                        wg = [sb(e1, "wg%d" % i, [128, 8, 768], BF16) for i in range(2)]
                        wsem = [dsem(), dsem()]
                        dg = [sb(e1, "dg%d" % i, [128, 4, 4, 128], BF16) for i in range(2)]
                        ust = [sb(e1, "ust%d" % i, [128, 4, 515], BF16) for i in range(2)]
                        xc = [sb(e1, "xc%d" % i, [128, 4, 512], BF16) for i in range(2)]
                        state = sb(e1, "state", [128, 256], F32)
                        state_bf = [sb(e1, "state_bf%d" % i, [128, 256], BF16) for i in range(3)]
                        zstate = sb(e1, "zstate", [128, 256], BF16)
                        ynT = [sb(e1, "ynT%d" % i, [128, 2, 512], BF16) for i in range(2)]
                        ysem = [dsem(), dsem()]
                        sz = [sb(e1, "sz%d" % i, [128, 256], BF16) for i in range(3)]
                        xbtm = [sb(e1, "xbtm%d" % i, [128, 384], BF16) for i in range(3)]
                        xd = [sb(e1, "xd%d" % i, [128, 4, 64], BF16) for i in range(3)]
                        y4 = [sb(e1, "y4_%d" % i, [128, 256], F32) for i in range(3)]
                        xde = [sb(e1, "xde%d" % i, [128, 4, 64], BF16) for i in range(2)]
                        cbm = [sb(e1, "cbm%d" % i, [128, 128], F32) for i in range(2)]
                        lh = [sb(e1, "lh%d" % i, [128, 4, 128], F32) for i in range(2)]
                        Ex = [sb(e1, "Ex%d" % i, [128, 4, 128], F32) for i in range(2)]
                        MT = [sb(e1, "MT%d" % i, [128, 4, 128], BF16) for i in range(2)]
                        y1 = [sb(e1, "y1_%d" % i, [128, 4, 64], F32) for i in range(2)]
                        tD = [sb(e1, "tD%d" % i, [128, 4, 64], F32) for i in range(2)]
                        yb = [sb(e1, "yb%d" % i, [128, 256], BF16) for i in range(2)]
                        junk2 = sb(e1, "junk2", [128, 256], F32)
                        ss2 = [sb(e1, "ss2_%d" % i, [128, 1], F32) for i in range(2)]
                        rs2 = [sb(e1, "rs2_%d" % i, [128, 1], F32) for i in range(2)]
                        p_u = [ps(e1, "p_u%d" % i, [128, 512], F32) for i in range(2)]
                        p_z = ps(e1, "p_z", [128, 256], F32)
                        bk1 = psbank(e1, "bk1", F32)
                        p_cb = T(bk1[:, 0:128])
                        p_s = T(bk1[:, 256:512])
                        p_s.r = p_cb.r
                        p_seg = [ps(e1, "p_seg%d" % i, [128, 4, 128], F32) for i in range(2)]
                        bk4 = psbank(e1, "bk4", F32)
                        p_y = T(bk4[:, 0:256])
                        p_yo = T(bk4[:, 256:512])
                        p_yo.r = p_y.r
                        bk6 = psbank(e1, "bk6", BF16)
                        p_tpx = T(bk6[:, 0:384])
                        p_tpy = T(bk6[:, 384:640])
                        p_tpy.r = p_tpx.r
                        k.op("pool", lambda e: e.memset(zstate.t[:], 0.0), writes=[zstate])
                        thz = [sb(e1, "thz%d" % i, [128, 256], F32) for i in range(2)]
                        thc = [sb(e1, "thc%d" % i, [128, 512], F32) for i in range(2)]
                        neghalf = sb(e1, "neghalf", [128, 1], F32)
                        k.op("pool", lambda e: e.memset(neghalf.t[:], -0.5), writes=[neghalf])
                        ones_row = sb(e1, "ones_row", [1, 512], BF16)
                        k.op("pool", lambda e: e.memset(ones_row.t[:], 1.0), writes=[ones_row])
                        cb_row = sb(e1, "cb_row", [1, 4096], F32)
                        k.dma("sp", sem_c, lambda e: e.dma_start(out=cb_row.t[:], in_=conv_brow_d), writes=[cb_row])
                        hb_row = sb(e1, "hb_row", [1, 4096], BF16)
                        k.op("dve", lambda e: e.tensor_scalar(out=hb_row.t[:], in0=cb_row.t[:], scalar1=0.5, scalar2=None, op0=ALU.mult),
                             reads=[cb_row], writes=[hb_row])

                        def group_setup(g):
                            w = wg[g % 2]
                            srcs = [(0, 2048 + g * 256, 256), (256, 4096 + g * 128, 128),
                                    (384, 5120 + g * 128, 128), (512, g * 256, 256)]
                            k.dma("pool", wsem[g % 2],
                                  [(lambda e, o=o, s=s, n=n: e.dma_start(
                                      out=w.t[:, :, o:o + n],
                                      in_=w_in_d[:, s:s + n].rearrange("(k p) n -> p k n", p=128))) for (o, s, n) in srcs],
                                  writes=[w])
                            d_ = dg[g % 2]
                            chunks = [2 * g, 2 * g + 1, 16 + g, 24 + g]
                            k.ops("pool", [(lambda e, cc=cc, kk=kk: e.tensor_scalar(
                                out=d_.t[:, cc, kk, :], in0=ident_bf.t[:], scalar1=conv_wT.t[:, chunks[cc], kk:kk + 1],
                                scalar2=0.5, op0=ALU.mult, op1=ALU.mult)) for cc in range(4) for kk in range(4)],
                                reads=[ident_bf, conv_wT], writes=[d_])
                            k.op("pool", lambda e: e.tensor_scalar(out=w.t[:, :, 512:768], in0=w.t[:, :, 512:768], scalar1=0.5,
                                                                   scalar2=0.0, op0=ALU.mult, op1=ALU.add), reads=[w], writes=[w])

                        NT = 8 * NCH

                        def dec(n):
                            g = n // NCH
                            c = n % NCH
                            return g, c, c // 4, c % 4

                        def S0a(si):
                            g, sc = si // NSC, si % NSC
                            w = wg[g % 2]
                            us = ust[si % 2]
                            ts = sc * 512
                            if sc == 0:
                                k.op("pool", lambda e: e.memset(us.t[:, :, 0:3], 0.0), writes=[us])
                            for cc in range(4):
                                pu = p_u[cc % 2]
                                k.ops("pe", [(lambda e, j=j: e.matmul(pu.t[:], lhsT=w.t[:, j, cc * 128:(cc + 1) * 128],
                                                                      rhs=hT.t[:, j, ts:ts + 512], start=(j == 0), stop=(j == 7)))
                                             for j in range(8)], reads=[w, hT], writes=[pu])
                                k.op("act", lambda e: e.copy(out=us.t[:, cc, 3:515], in_=pu.t[:]), reads=[pu], writes=[us])
                            if sc + 1 < NSC:
                                un = ust[(si + 1) % 2]
                                k.op("pool", lambda e: e.tensor_copy(out=un.t[:, :, 0:3], in_=us.t[:, :, 512:515]),
                                     reads=[us], writes=[un])

                        def S0b(si):
                            g, sc = si // NSC, si % NSC
                            d_ = dg[g % 2]
                            us = ust[si % 2]
                            xo = xc[si % 2]
                            chunks = [2 * g, 2 * g + 1, 16 + g, 24 + g]
                            for cc in range(4):
                                pu = p_u[cc % 2]
                                ch = chunks[cc]
                                k.ops("pe", [(lambda e, kk=kk: e.matmul(pu.t[:], lhsT=d_.t[:, cc, kk, :],
                                                                        rhs=us.t[:, cc, kk:kk + 512], start=(kk == 0), stop=False))
                                             for kk in range(4)] +
                                      [lambda e: e.matmul(pu.t[:], lhsT=hb_row.t[0:1, ch * 128:(ch + 1) * 128],
                                                          rhs=ones_row.t[0:1, :], start=False, stop=True)],
                                      reads=[d_, us, hb_row, ones_row], writes=[pu])
                                tc_ = thc[cc % 2]
                                k.op("act", lambda e: e.activation(out=tc_.t[:], in_=pu.t[:], func=AF.Tanh),
                                     reads=[pu], writes=[tc_])
                                k.op("dve", lambda e: e.scalar_tensor_tensor(out=xo.t[:, cc, :], in0=tc_.t[:], scalar=1.0, in1=pu.t[:],
                                                                             op0=ALU.add, op1=ALU.mult),
                                     reads=[tc_, pu], writes=[xo])

                        def ctx(n):
                            g, c, sc, q = dec(n)
                            si = g * NSC + sc
                            return dict(g=g, c=c, sc=sc, q=q, si=si, w=wg[g % 2], xo=xc[si % 2],
                                        qs=slice(q * 128, (q + 1) * 128), cs=slice(c * 128, (c + 1) * 128),
                                        g4=slice(g * 4, g * 4 + 4), b3=n % 3, b2=n % 2)

                        def A_pe(n):
                            x_ = ctx(n); w = x_["w"]; xo = x_["xo"]; qs = x_["qs"]; cs = x_["cs"]
                            k.ops("pe", [(lambda e, j=j: e.matmul(p_z.t[:], lhsT=hT.t[:, j, cs], rhs=w.t[:, j, 512:768],
                                                                  start=(j == 0), stop=(j == 7))) for j in range(8)],
                                  reads=[hT, w], writes=[p_z])
                            k.ops("pe", [(lambda e, cc=cc: e.transpose(out=p_tpx.t[:, cc * 128:(cc + 1) * 128],
                                                                       in_=xo.t[:, cc, qs], identity=ident_bf.t[:]))
                                         for cc in range(3)], reads=[xo, ident_bf], writes=[p_tpx])
                            k.op("pe", lambda e: e.matmul(p_cb.t[:], lhsT=xo.t[:, 2, qs], rhs=xo.t[:, 3, qs],
                                                          start=True, stop=True), reads=[xo], writes=[p_cb])

                        def A_act(n):
                            x_ = ctx(n); b3 = x_["b3"]; b2 = x_["b2"]; c = x_["c"]; g = x_["g"]
                            k.op("act", lambda e: e.activation(out=thz[b2].t[:], in_=p_z.t[:], func=AF.Tanh),
                                 reads=[p_z], writes=[thz[b2]])
                            k.op("act", lambda e: e.copy(out=xbtm[b3].t[:], in_=p_tpx.t[:]), reads=[p_tpx], writes=[xbtm[b3]])
                            k.ops("act", [(lambda e, r=r: e.activation(out=lh[b2].t[:, r, :], in_=Mgt.t[:], func=AF.Copy,
                                                                       scale=a_all.t[:, c, g * 4 + r:g * 4 + r + 1]))
                                          for r in range(4)], reads=[Mgt, a_all], writes=[lh[b2]])

                        def A_dve(n):
                            x_ = ctx(n); b3 = x_["b3"]; b2 = x_["b2"]; c = x_["c"]; g4 = x_["g4"]
                            k.op("dve", lambda e: e.scalar_tensor_tensor(out=sz[b3].t[:], in0=thz[b2].t[:], scalar=1.0, in1=p_z.t[:],
                                                                         op0=ALU.add, op1=ALU.mult),
                                 reads=[thz[b2], p_z], writes=[sz[b3]])
                            k.op("dve", lambda e: e.tensor_tensor(
                                out=xd[b3].t[:], in0=xbtm[b3].t[:, 0:256].rearrange("p (r d) -> p r d", r=4),
                                in1=dt_all.t[:, c, g4, None].to_broadcast([128, 4, 64]), op=ALU.mult),
                                reads=[xbtm[b3], dt_all], writes=[xd[b3]])
                            k.op("dve", lambda e: e.tensor_tensor(out=cbm[b2].t[:], in0=p_cb.t[:], in1=Mle.t[:], op=ALU.mult),
                                 reads=[p_cb, Mle], writes=[cbm[b2]])

                        def A_pool(n):
                            x_ = ctx(n); b3 = x_["b3"]; b2 = x_["b2"]; c = x_["c"]; g4 = x_["g4"]
                            k.op("pool", lambda e: e.tensor_tensor(
                                out=xde[b2].t[:], in0=xd[b3].t[:],
                                in1=e_all.t[:, c, 1, g4, None].to_broadcast([128, 4, 64]), op=ALU.mult),
                                reads=[xd[b3], e_all], writes=[xde[b2]])

                        def B_pe(n):
                            x_ = ctx(n); b3 = x_["b3"]; b2 = x_["b2"]
                            k.ops("pe", [(lambda e, r=r: e.matmul(p_seg[b2].t[:, r, :], lhsT=lh[b2].t[:, r, :], rhs=Mle.t[:],
                                                                  start=True, stop=True)) for r in range(4)],
                                  reads=[lh[b2], Mle], writes=[p_seg[b2]])
                            k.op("pe", lambda e: e.matmul(p_s.t[:], lhsT=xbtm[b3].t[:, 256:384],
                                                          rhs=xde[b2].t[:].rearrange("p r d -> p (r d)"), start=True, stop=True),
                                 reads=[xbtm[b3], xde[b2]], writes=[p_s])

                        def B_act(n):
                            x_ = ctx(n); b2 = x_["b2"]
                            k.op("act", lambda e: e.activation(out=Ex[b2].t[:], in_=p_seg[b2].t[:], func=AF.Exp),
                                 reads=[p_seg[b2]], writes=[Ex[b2]])

                        def B_dve(n):
                            x_ = ctx(n); b2 = x_["b2"]; c = x_["c"]; g4 = x_["g4"]
                            k.op("dve", lambda e: e.tensor_tensor(
                                out=MT[b2].t[:], in0=Ex[b2].t[:],
                                in1=cbm[b2].t[:, None, :].to_broadcast([128, 4, 128]), op=ALU.mult),
                                reads=[Ex[b2], cbm[b2]], writes=[MT[b2]])
                            if c == 0:
                                k.op("dve", lambda e: e.tensor_copy(out=state.t[:], in_=p_s.t[:]), reads=[p_s], writes=[state])
                            else:
                                k.op("dve", lambda e: e.tensor_tensor(
                                    out=state.t[:].rearrange("p (r d) -> p r d", r=4),
                                    in0=state.t[:].rearrange("p (r d) -> p r d", r=4),
                                    in1=e_all.t[:, c, 2, g4, None].to_broadcast([128, 4, 64]), op=ALU.mult),
                                    reads=[state, e_all], writes=[state])
                                k.op("dve", lambda e: e.tensor_tensor(out=state.t[:], in0=state.t[:], in1=p_s.t[:], op=ALU.add),
                                     reads=[state, p_s], writes=[state])

                        def B_dve2(n):
                            x_ = ctx(n); b3 = x_["b3"]; b2 = x_["b2"]; g4 = x_["g4"]
                            k.op("dve", lambda e: e.tensor_tensor(
                                out=tD[b2].t[:], in0=xbtm[b3].t[:, 0:256].rearrange("p (r d) -> p r d", r=4),
                                in1=dsk.t[:, g4, None].to_broadcast([128, 4, 64]), op=ALU.mult),
                                reads=[xbtm[b3], dsk], writes=[tD[b2]])

                        def C_act(n):
                            x_ = ctx(n); b3 = x_["b3"]
                            k.op("act", lambda e: e.copy(out=state_bf[b3].t[:], in_=state.t[:]),
                                 reads=[state], writes=[state_bf[b3]])

                        def C_pe(n):
                            x_ = ctx(n); b3 = x_["b3"]; b2 = x_["b2"]; xo = x_["xo"]; qs = x_["qs"]; c = x_["c"]
                            k.ops("pe", [(lambda e, r=r: e.matmul(p_y.t[:, r * 64:(r + 1) * 64], lhsT=MT[b2].t[:, r, :],
                                                                  rhs=xd[b3].t[:, r, :], start=True, stop=True))
                                         for r in range(4)], reads=[MT[b2], xd[b3]], writes=[p_y])
                            st_prev = zstate if c == 0 else state_bf[(n - 1) % 3]
                            k.op("pe", lambda e: e.matmul(p_yo.t[:], lhsT=xo.t[:, 3, qs], rhs=st_prev.t[:],
                                                          start=True, stop=True), reads=[xo, st_prev], writes=[p_yo])

                        def C_dve(n):
                            x_ = ctx(n); b3 = x_["b3"]; b2 = x_["b2"]; c = x_["c"]; g4 = x_["g4"]
                            k.op("dve", lambda e: e.tensor_tensor(
                                out=y1[b2].t[:], in0=p_yo.t[:].rearrange("p (r d) -> p r d", r=4),
                                in1=e_all.t[:, c, 0, g4, None].to_broadcast([128, 4, 64]), op=ALU.mult),
                                reads=[p_yo, e_all], writes=[y1[b2]])
                            k.op("dve", lambda e: e.tensor_tensor(
                                out=y1[b2].t[:], in0=y1[b2].t[:],
                                in1=p_y.t[:].rearrange("p (r d) -> p r d", r=4), op=ALU.add),
                                reads=[y1[b2], p_y], writes=[y1[b2]])
                            k.op("dve", lambda e: e.tensor_tensor(out=y1[b2].t[:], in0=y1[b2].t[:], in1=tD[b2].t[:], op=ALU.add),
                                 reads=[y1[b2], tD[b2]], writes=[y1[b2]])
                            k.op("dve", lambda e: e.tensor_tensor(
                                out=y4[b3].t[:], in0=y1[b2].t[:].rearrange("p r d -> p (r d)"), in1=sz[b3].t[:], op=ALU.mult),
                                reads=[y1[b2], sz[b3]], writes=[y4[b3]])

                        def D_act(n):
                            x_ = ctx(n); b3 = x_["b3"]; b2 = x_["b2"]
                            k.op("act", lambda e: e.activation(out=junk2.t[:], in_=y4[b3].t[:], func=AF.Square,
                                                               accum_out=ss2[b2].t[:, 0:1]),
                                 reads=[y4[b3]], writes=[junk2, ss2[b2]])

                        def D_pool(n):
                            x_ = ctx(n); b2 = x_["b2"]
                            k.op("pool", lambda e: e.tensor_scalar(out=rs2[b2].t[:], in0=ss2[b2].t[:], scalar1=1.0 / 256, scalar2=EPS,
                                                                   op0=ALU.mult, op1=ALU.add), reads=[ss2[b2]], writes=[rs2[b2]])
                            k.op("pool", lambda e: e.tensor_tensor(out=rs2[b2].t[:], in0=rs2[b2].t[:], in1=neghalf.t[:], op=ALU.pow),
                                 reads=[rs2[b2], neghalf], writes=[rs2[b2]])

                        def E_dve(n):
                            x_ = ctx(n); b3 = x_["b3"]; b2 = x_["b2"]
                            k.op("dve", lambda e: e.tensor_scalar(out=yb[b2].t[:], in0=y4[b3].t[:], scalar1=rs2[b2].t[:, 0:1],
                                                                  scalar2=None, op0=ALU.mult),
                                 reads=[y4[b3], rs2[b2]], writes=[yb[b2]])

                        def F_pe(n):
                            x_ = ctx(n); b2 = x_["b2"]
                            k.ops("pe", [(lambda e, h=h: e.transpose(out=p_tpy.t[:, h * 128:(h + 1) * 128],
                                                                     in_=yb[b2].t[:, h * 128:(h + 1) * 128], identity=ident_bf.t[:]))
                                         for h in range(2)], reads=[yb[b2], ident_bf], writes=[p_tpy])

                        def F_act(n):
                            x_ = ctx(n); g = x_["g"]; qs = x_["qs"]; si = x_["si"]; q = x_["q"]; sc = x_["sc"]
                            yo = ynT[si % 2]
                            for h in range(2):
                                k.op("act", lambda e: e.activation(out=yo.t[:, h, qs], in_=p_tpy.t[:, h * 128:(h + 1) * 128], func=AF.Copy,
                                                                   scale=gssdT.t[:, 2 * g + h:2 * g + h + 1]),
                                     reads=[p_tpy, gssdT], writes=[yo])
                            if q == 3:
                                ts = sc * 512
                                k.dma("sp", ysem[si % 2], lambda e: e.dma_start(
                                    out=ymT_d[2 * g:2 * g + 2, :, ts:ts + 512].rearrange("f p t -> p f t"), in_=yo.t[:]),
                                    reads=[yo])

                        def ok(n):
                            return 0 <= n < NT

                        import os as _os
                        if _os.environ.get("A1_NOSKEW"):
                            for n in range(NT):
                                g, c, sc, q = dec(n)
                                if c == 0:
                                    group_setup(g)
                                if q == 0:
                                    S0a(g * NSC + sc)
                                    S0b(g * NSC + sc)
                                for fn in (A_pe, A_act, A_dve, A_pool, B_pe, B_act, B_dve, B_dve2, C_pe, C_act, C_dve, D_act, D_pool, E_dve, F_pe, F_act):
                                    fn(n)
                        else:
                            LAGS = dict(A=0, B=1, C=2, D=3, E=4, F=5)
                            group_setup(0)
                            S0a(0)
                            S0b(0)
                            S0a(1)
                            for t in range(NT + 5):
                                for fn, lag in ((F_pe, 5), (C_pe, 2), (B_pe, 1), (A_pe, 0),
                                                (F_act, 5), (C_act, 2), (B_act, 1), (A_act, 0), (D_act, 3),
                                                (E_dve, 4), (B_dve, 1), (B_dve2, 1), (C_dve, 2), (A_dve, 0),
                                                (D_pool, 3), (A_pool, 0)):
                                    if ok(t - lag):
                                        fn(t - lag)
                                if t % 4 == 1 and t // 4 + 1 < 8 * NSC:
                                    S0b(t // 4 + 1)
                                if t % 4 == 2 and t // 4 + 2 < 8 * NSC:
                                    S0a(t // 4 + 2)
                                if t % NCH == 4 and t // NCH + 1 < 8:
                                    group_setup(t // NCH + 1)
                k.barrier()
                if "A1p" in phases:
                    with ExitStack() as e2:
                        wp = sb(e2, "wp", [128, 8, 1024], BF16)
                        k.dma("pool", sem_cp, lambda e: e.dma_start(
                            out=wp.t[:], in_=w_in_d[:, 6176:7200].rearrange("(k p) n -> p k n", p=128)), writes=[wp])
                        pw = sb(e2, "pw", [128, 4, 2, 256], BF16)
                        k.dma("pool", sem_cp, lambda e: e.dma_start(
                            out=pw.t[:], in_=pool_w_d.rearrange("g (k p) n -> p g k n", p=128)), writes=[pw])
                        pst = [sb(e2, "pst%d" % i, [128, 8, 527], F32) for i in range(2)]
                        sA = sb(e2, "sA", [128, 2, 527], F32)
                        sB = sb(e2, "sB", [128, 2, 527], F32)
                        ypl = [sb(e2, "ypl%d" % i, [128, 2, 512], BF16) for i in range(2)]
                        tmp16 = sb(e2, "tmp16", [128, 2, 16], F32)
                        ypT = [sb(e2, "ypT%d" % i, [128, 2, 512], BF16) for i in range(2)]
                        psem_ = [dsem(), dsem()]
                        p_u2 = [ps(e2, "p_u2_%d" % i, [128, 512], F32) for i in range(2)]
                        p_p = [ps(e2, "p_p%d" % i, [128, 512], F32) for i in range(2)]
                        k.op("pool", lambda e: e.memset(pst[0].t[:, :, 0:15], 0.0), writes=[pst[0]])
                        it = 0
                        for sc in range(NSC):
                            ts = sc * 512
                            cur = pst[sc % 2]
                            nxt = pst[(sc + 1) % 2]
                            for pc in range(8):
                                pu = p_u2[pc % 2]
                                k.ops("pe", [(lambda e, j=j: e.matmul(pu.t[:], lhsT=wp.t[:, j, pc * 128:(pc + 1) * 128],
                                                                      rhs=hT.t[:, j, ts:ts + 512], start=(j == 0), stop=(j == 7)))
                                             for j in range(8)], reads=[wp, hT], writes=[pu])
                                k.op("act", lambda e: e.copy(out=cur.t[:, pc, 15:527], in_=pu.t[:]), reads=[pu], writes=[cur])
                            if sc + 1 < NSC:
                                k.op("pool", lambda e: e.tensor_copy(out=nxt.t[:, :, 0:15], in_=cur.t[:, :, 512:527]),
                                     reads=[cur], writes=[nxt])
                            for pg in range(4):
                                u = cur.t[:, 2 * pg:2 * pg + 2, :]
                                nlev = pg + 1
                                src = u
                                bufs = [sA, sB]
                                eng = "dve"
                                for lv in range(nlev):
                                    sh = 1 << lv
                                    lo = (1 << (lv + 1)) - 1
                                    dst = bufs[lv % 2]
                                    src_t = cur if lv == 0 else bufs[(lv - 1) % 2]
                                    s_ap = src
                                    k.op(eng, lambda e, dst=dst, s_ap=s_ap, lo=lo, sh=sh: e.tensor_tensor(
                                        out=dst.t[:, :, lo:527], in0=s_ap[:, :, lo:527], in1=s_ap[:, :, lo - sh:527 - sh], op=ALU.add),
                                        reads=[src_t], writes=[dst])
                                    src = dst.t[:, :, :]
                                fin = bufs[(nlev - 1) % 2]
                                wv = 1 << nlev
                                yp = ypl[it % 2]
                                k.op(eng, lambda e: e.scalar_tensor_tensor(
                                    out=yp.t[:], in0=fin.t[:, :, 15:527], scalar=1.0 / wv, in1=u[:, :, 15:527],
                                    op0=ALU.mult, op1=ALU.subtract), reads=[fin, cur], writes=[yp])
                                if sc == 0:
                                    k.op(eng, lambda e: e.tensor_tensor(
                                        out=tmp16.t[:], in0=fin.t[:, :, 15:31],
                                        in1=invc.t[:, pg, None, :].to_broadcast([128, 2, 16]), op=ALU.mult),
                                        reads=[fin, invc], writes=[tmp16])
                                    k.op(eng, lambda e: e.tensor_tensor(out=yp.t[:, :, 0:16], in0=tmp16.t[:], in1=u[:, :, 15:31],
                                                                        op=ALU.subtract), reads=[tmp16, cur], writes=[yp])
                                yT_ = ypT[it % 2]
                                for dc in range(2):
                                    pp = p_p[dc]
                                    k.ops("pe", [(lambda e, kc=kc: e.matmul(pp.t[:], lhsT=pw.t[:, pg, kc, dc * 128:(dc + 1) * 128],
                                                                            rhs=yp.t[:, kc, :], start=(kc == 0), stop=(kc == 1)))
                                                 for kc in range(2)], reads=[pw, yp], writes=[pp])
                                    k.op("act", lambda e: e.activation(out=yT_.t[:, dc, :], in_=pp.t[:], func=AF.Copy,
                                                                       scale=pscT.t[:, 2 * pg + dc:2 * pg + dc + 1]),
                                         reads=[pp, pscT], writes=[yT_])
                                k.dma("sp", psem_[it % 2], lambda e: e.dma_start(
                                    out=ymT_d[16 + 2 * pg:16 + 2 * pg + 2, :, ts:ts + 512].rearrange("f p t -> p f t"), in_=yT_.t[:]),
                                    reads=[yT_])
                                it += 1

        k.barrier()
        if "A2" in phases:
            with ExitStack() as e3:
                wout = sb(e3, "wout", [128, 24, DM], BF16)
                k.dma("pool", sem_cp, [(lambda e, i=i: e.dma_start(
                    out=wout.t[:, 6 * i:6 * i + 6, :],
                    in_=w_out_d[768 * i:768 * (i + 1), :].rearrange("(k p) n -> p k n", p=128))) for i in range(4)],
                    writes=[wout])
                gffn_bc = load_const(e3, "gffn_bc", [128, DM], gffn_bc_d)
                rw = load_const(e3, "rw", [128, 8, NE], rw_d.rearrange("(k p) n -> p k n", p=128))
                rb = load_const(e3, "rb", [128, NE], rb_d)
                ebase = load_const(e3, "ebase", [128, NE], ebase_d)
                s4096 = sb(e3, "s4096", [128, 514], I32)
                k.op("pool", lambda e: e.memset(s4096.t[:], L), writes=[s4096])
                ev_init = [
                    k.dma("sp", sem_c, lambda e: e.dma_start(out=slot_d.rearrange("(p f) o -> p (f o)", p=128), in_=s4096.t[:]),
                          reads=[s4096]),
                    k.dma("sp", sem_c, lambda e: e.dma_start(out=h2_d[L:L + 1, :], in_=zrow_bf.t[:]), reads=[zrow_bf]),
                    k.dma("sp", sem_c, lambda e: e.dma_start(out=G_d[L:L + 1, :], in_=zrow.t[:, 0:NE]), reads=[zrow]),
                    k.dma("sp", sem_c, lambda e: e.dma_start(out=Y_d[0:1, :], in_=zrow.t[:]), reads=[zrow]),
                ]
                ym = [sb(e3, "ym%d" % i, [128, 24, 512], BF16) for i in range(2)]
                ymsem = [dsem(), dsem()]
                xt2 = [sb(e3, "xt2_%d" % i, [128, DM], F32) for i in range(2)]
                xsem2 = [dsem(), dsem()]
                x1 = [sb(e3, "x1_%d" % i, [128, DM], F32) for i in range(2)]
                x1sem = [dsem(), dsem()]
                junk3 = sb(e3, "junk3", [128, DM], F32)
                ss3 = sb(e3, "ss3", [128, 1], F32)
                rs3 = sb(e3, "rs3", [128, 1], F32)
                h2f = sb(e3, "h2f", [128, DM], F32)
                h2b = [sb(e3, "h2b%d" % i, [128, DM], BF16) for i in range(2)]
                h2sem = [dsem(), dsem()]
                h2T = sb(e3, "h2T", [128, 8, 128], F32)
                lg = sb(e3, "lg", [128, NE], F32)
                m8 = sb(e3, "m8", [128, 8], F32)
                nv1 = sb(e3, "nv1", [128, 1], F32)
                mask = sb(e3, "mask", [128, NE], F32)
                ex = sb(e3, "ex", [128, NE], F32)
                sm = sb(e3, "sm", [128, 1], F32)
                Gt = [sb(e3, "Gt%d" % i, [128, NE], F32) for i in range(2)]
                gsem = [dsem(), dsem()]
                cnt = sb(e3, "cnt", [128, NE], F32)
                rank = sb(e3, "rank", [128, NE], F32)
                vld = sb(e3, "vld", [128, NE], F32)
                val = sb(e3, "val", [128, NE], F32)
                v8 = sb(e3, "v8", [128, 8], F32)
                scsem = dsem()
                p_o = ps(e3, "p_o", [128, DM], F32)
                p_tf = ps(e3, "p_tf", [128, 8, 128], F32)
                p_l = ps(e3, "p_l", [128, NE], F32)
                p_r = ps(e3, "p_r", [128, 2, NE], F32)
                k.op("pool", lambda e: e.memset(cnt.t[:], 0.0), writes=[cnt])
                scat_evs = []
                for sc in range(NSC):
                    ts = sc * 512
                    ymc = ym[sc % 2]
                    k.dma("sp", ymsem[sc % 2], [(lambda e, i=i: e.dma_start(
                        out=ymc.t[:, 6 * i:6 * i + 6, :],
                        in_=ymT_d[6 * i:6 * i + 6, :, ts:ts + 512].rearrange("f p t -> p f t"))) for i in range(4)],
                        writes=[ymc])
                    for q in range(4):
                        c = sc * 4 + q
                        b = c % 2
                        qs = slice(q * 128, (q + 1) * 128)
                        cs = slice(c * 128, (c + 1) * 128)
                        k.dma("sp", xsem2[b], lambda e: e.dma_start(out=xt2[b].t[:], in_=x_d[cs, :]), writes=[xt2[b]])
                        k.ops("pe", [(lambda e, fc=fc, h=h: e.matmul(p_o.t[:, h * 512:(h + 1) * 512], lhsT=ymc.t[:, fc, qs],
                                                                     rhs=wout.t[:, fc, h * 512:(h + 1) * 512],
                                                                     start=(fc == 0), stop=(fc == 23)))
                                     for h in range(2) for fc in range(24)], reads=[ymc, wout], writes=[p_o])
                        k.op("dve", lambda e: e.tensor_tensor(out=x1[b].t[:], in0=p_o.t[:], in1=xt2[b].t[:], op=ALU.add),
                             reads=[p_o, xt2[b]], writes=[x1[b]])
                        k.dma("sp", x1sem[b], lambda e: e.dma_start(out=x1_d[cs, :], in_=x1[b].t[:]), reads=[x1[b]])
                        k.op("act", lambda e: e.activation(out=junk3.t[:], in_=x1[b].t[:], func=AF.Square, accum_out=ss3.t[:, 0:1]),
                             reads=[x1[b]], writes=[junk3, ss3])
                        k.op("act", lambda e: e.activation(out=rs3.t[:], in_=ss3.t[:], func=AF.Sqrt, scale=1.0 / DM, bias=EPS),
                             reads=[ss3], writes=[rs3])
                        k.op("dve", lambda e: e.reciprocal(out=rs3.t[:], in_=rs3.t[:]), reads=[rs3], writes=[rs3])
                        k.op("dve", lambda e: e.scalar_tensor_tensor(out=h2f.t[:], in0=x1[b].t[:], scalar=rs3.t[:, 0:1],
                                                                     in1=gffn_bc.t[:], op0=ALU.mult, op1=ALU.mult),
                             reads=[x1[b], rs3, gffn_bc], writes=[h2f])
                        k.op("act", lambda e: e.copy(out=h2b[b].t[:], in_=h2f.t[:]), reads=[h2f], writes=[h2b[b]])
                        k.dma("sp", h2sem[b], lambda e: e.dma_start(out=h2_d[cs, :], in_=h2b[b].t[:]), reads=[h2b[b]])
                        k.ops("pe", [(lambda e, j=j: e.transpose(out=p_tf.t[:, j, :], in_=h2f.t[:, j * 128:(j + 1) * 128],
                                                                 identity=ident_f.t[:])) for j in range(8)],
                              reads=[h2f, ident_f], writes=[p_tf])
                        k.op("act", lambda e: e.copy(out=h2T.t[:], in_=p_tf.t[:]), reads=[p_tf], writes=[h2T])
                        k.ops("pe", [(lambda e, j=j: e.matmul(p_l.t[:], lhsT=h2T.t[:, j, :], rhs=rw.t[:, j, :],
                                                              start=(j == 0), stop=(j == 7))) for j in range(8)],
                              reads=[h2T, rw], writes=[p_l])
                        k.op("dve", lambda e: e.tensor_tensor(out=lg.t[:], in0=p_l.t[:], in1=rb.t[:], op=ALU.add),
                             reads=[p_l, rb], writes=[lg])
                        k.op("dve", lambda e: e.max(out=m8.t[:], in_=lg.t[:]), reads=[lg], writes=[m8])
                        k.op("dve", lambda e: e.tensor_scalar(out=mask.t[:], in0=lg.t[:], scalar1=m8.t[:, 3:4], scalar2=None,
                                                              op0=ALU.is_ge), reads=[lg, m8], writes=[mask])
                        k.op("dve", lambda e: e.tensor_scalar(out=nv1.t[:], in0=m8.t[:, 0:1], scalar1=-1.0, scalar2=None,
                                                              op0=ALU.mult), reads=[m8], writes=[nv1])
                        k.op("act", lambda e: e.activation(out=ex.t[:], in_=lg.t[:], func=AF.Exp, bias=nv1.t[:, 0:1]),
                             reads=[lg, nv1], writes=[ex])
                        k.op("dve", lambda e: e.tensor_tensor(out=ex.t[:], in0=ex.t[:], in1=mask.t[:], op=ALU.mult),
                             reads=[ex, mask], writes=[ex])
                        k.op("dve", lambda e: e.reduce_sum(out=sm.t[:], in_=ex.t[:], axis=AX.X), reads=[ex], writes=[sm])
                        k.op("dve", lambda e: e.reciprocal(out=sm.t[:], in_=sm.t[:]), reads=[sm], writes=[sm])
                        k.op("dve", lambda e: e.tensor_scalar(out=Gt[b].t[:], in0=ex.t[:], scalar1=sm.t[:, 0:1], scalar2=None,
                                                              op0=ALU.mult), reads=[ex, sm], writes=[Gt[b]])
                        k.dma("sp", gsem[b], lambda e: e.dma_start(out=G_d[cs, :], in_=Gt[b].t[:]), reads=[Gt[b]])
                        k.ops("pe", [
                            lambda e: e.matmul(p_r.t[:, 0, :], lhsT=Mlt.t[:], rhs=mask.t[:], start=True, stop=True),
                            lambda e: e.matmul(p_r.t[:, 1, :], lhsT=ones_f.t[:], rhs=mask.t[:], start=True, stop=True),
                        ], reads=[Mlt, ones_f, mask], writes=[p_r])
                        k.op("dve", lambda e: e.tensor_tensor(out=rank.t[:], in0=p_r.t[:, 0, :], in1=cnt.t[:], op=ALU.add),
                             reads=[p_r, cnt], writes=[rank])
                        k.op("dve", lambda e: e.tensor_tensor(out=cnt.t[:], in0=p_r.t[:, 1, :], in1=cnt.t[:], op=ALU.add),
                             reads=[p_r, cnt], writes=[cnt])
                        k.op("dve", lambda e: e.tensor_scalar(out=vld.t[:], in0=rank.t[:], scalar1=float(CAP), scalar2=None,
                                                              op0=ALU.is_lt), reads=[rank], writes=[vld])
                        k.op("dve", lambda e: e.tensor_tensor(out=vld.t[:], in0=vld.t[:], in1=mask.t[:], op=ALU.mult),
                             reads=[vld, mask], writes=[vld])
                        k.op("dve", lambda e: e.tensor_tensor(out=val.t[:], in0=rank.t[:], in1=ebase.t[:], op=ALU.add),
                             reads=[rank, ebase], writes=[val])
                        k.op("dve", lambda e: e.tensor_tensor(out=val.t[:], in0=val.t[:], in1=vld.t[:], op=ALU.mult),
                             reads=[val, vld], writes=[val])
                        k.op("dve", lambda e: e.max(out=v8.t[:], in_=val.t[:]), reads=[val], writes=[v8])
                        k.op("dve", lambda e: e.tensor_copy(out=dest_all.t[:, c, :], in_=v8.t[:, 0:4]), reads=[v8], writes=[dest_all])
                        for kk in range(4):
                            scat_evs.append(k.dma("pool", scsem, lambda e: e.indirect_dma_start(
                                out=slot_d[:, :], out_offset=bass.IndirectOffsetOnAxis(ap=dest_all.t[:, c, kk:kk + 1], axis=0),
                                in_=tokid.t[:, c, :], in_offset=None),
                                reads=[dest_all, tokid], extra=ev_init))
                a2_done = [x1[0], x1[1], h2b[0], h2b[1], Gt[0], Gt[1]]
                a2_evs = list(scat_evs[-1:])
                for t in a2_done:
                    a2_evs += t.r.rs
        else:
            a2_evs = []

        k.barrier()
        if "B" in phases:
            with ExitStack() as e4:
                bguT = load_const(e4, "bguT", [128, NE, 16], bguT_d)
                wgu = [sb(e4, "wgu%d" % i, [128, 8, 2048], BF16) for i in range(2)]
                wdn = [sb(e4, "wdn%d" % i, [128, 8, DM], BF16) for i in range(2)]
                bdb = [sb(e4, "bdb%d" % i, [128, DM], F32) for i in range(2)]
                wesem = [dsem(), dsem()]
                bdsem = [dsem(), dsem()]
                idx = [sb(e4, "idx%d" % i, [128, NBLK, 2], I32) for i in range(2)]
                isem = [dsem(), dsem()]
                xg = [sb(e4, "xg%d" % i, [128, DM], BF16) for i in range(NBLK)]
                xgsem = [dsem() for _ in range(NBLK)]
                gg = [sb(e4, "gg%d" % i, [128, NBLK, NE], F32) for i in range(2)]
                ggsem = [dsem(), dsem()]
                xgT = sb(e4, "xgT", [128, 8, CAP], BF16)
                actT = sb(e4, "actT", [128, 8, CAP], BF16)
                HW = CAP // 2
                gm = [sb(e4, "gm%d" % i, [128, HW], F32) for i in range(2)]
                sg = [sb(e4, "sg%d" % i, [128, HW], F32) for i in range(2)]
                u1 = [sb(e4, "u1_%d" % i, [128, HW], F32) for i in range(2)]
                yA = [sb(e4, "yA%d" % i, [128, DM], F32) for i in range(2)]
                yB = [sb(e4, "yB%d" % i, [128, DM], F32) for i in range(2)]
                ysem2 = [dsem(), dsem()]
                p_tg2 = [ps(e4, "p_tg%d" % i, [128, 8, 128], BF16) for i in range(2)]
                p_g = [ps(e4, "p_g%d" % i, [128, 512], F32) for i in range(2)]
                p_up = [ps(e4, "p_up%d" % i, [128, 512], F32) for i in range(2)]
                p_dh = [ps(e4, "p_dh%d" % i, [128, 512], F32) for i in range(2)]

                def load_w(e_):
                    s = e_ % 2
                    k.dma("pool", wesem[s],
                          [(lambda e, i=i: e.dma_start(out=wgu[s].t[:, 2 * i:2 * i + 2, :],
                                                       in_=wgu_d[e_, 256 * i:256 * (i + 1), :].rearrange("(k p) n -> p k n", p=128)))
                           for i in range(4)] +
                          [(lambda e, i=i: e.dma_start(out=wdn[s].t[:, 4 * i:4 * i + 4, :],
                                                       in_=wd_d[e_, 512 * i:512 * (i + 1), :].rearrange("(k p) n -> p k n", p=128)))
                           for i in range(2)],
                          writes=[wgu[s], wdn[s]])
                    k.dma("sp", bdsem[s], lambda e: e.dma_start(out=bdb[s].t[:], in_=bd_d[e_:e_ + 1, :].to_broadcast([128, DM])),
                          writes=[bdb[s]])

                def prefetch(e_):
                    s_ = e_ % 2
                    base_ = 1 + e_ * CAP
                    k.dma("sp", isem[s_], [(lambda e, j=j: e.dma_start(out=idx[s_].t[:, j, :],
                                                                       in_=slot_d[base_ + j * 128:base_ + (j + 1) * 128, :]))
                                           for j in range(NBLK)], writes=[idx[s_]], extra=a2_evs)
                    for j in range(NBLK):
                        k.dma("pool", xgsem[j], lambda e: e.indirect_dma_start(
                            out=xg[j].t[:, :], out_offset=None, in_=h2_d[:, :],
                            in_offset=bass.IndirectOffsetOnAxis(ap=idx[s_].t[:, j, 0:1], axis=0)),
                            reads=[idx[s_]], writes=[xg[j]], extra=a2_evs)
                    k.dma("pool", ggsem[s_], [(lambda e, j=j: e.indirect_dma_start(
                        out=gg[s_].t[:, j, :], out_offset=None, in_=G_d[:, :],
                        in_offset=bass.IndirectOffsetOnAxis(ap=idx[s_].t[:, j, 0:1], axis=0))) for j in range(NBLK)],
                        reads=[idx[s_]], writes=[gg[s_]], extra=a2_evs)

                def transposes():
                    for j in range(NBLK):
                        p_tg = p_tg2[j % 2]
                        k.ops("pe", [(lambda e, kk=kk: e.transpose(out=p_tg.t[:, kk, :], in_=xg[j].t[:, kk * 128:(kk + 1) * 128],
                                                                   identity=ident_bf.t[:])) for kk in range(8)],
                              reads=[xg[j], ident_bf], writes=[p_tg])
                        k.op("act", lambda e: e.copy(out=xgT.t[:, :, j * 128:(j + 1) * 128], in_=p_tg.t[:]),
                             reads=[p_tg], writes=[xgT])

                load_w(0)
                prefetch(0)
                transposes()
                for e_ in range(NE):
                    s = e_ % 2
                    base = 1 + e_ * CAP
                    if e_ + 1 < NE:
                        load_w(e_ + 1)
                        prefetch(e_ + 1)
                    for fc in range(8):
                        for h in range(2):
                            hs = slice(h * HW, (h + 1) * HW)
                            k.ops("pe", [(lambda e, kk=kk: e.matmul(p_g[h].t[:, 0:HW], lhsT=wgu[s].t[:, kk, fc * 128:(fc + 1) * 128],
                                                                    rhs=xgT.t[:, kk, hs], start=(kk == 0), stop=(kk == 7)))
                                         for kk in range(8)], reads=[wgu[s], xgT], writes=[p_g[h]])
                            k.ops("pe", [(lambda e, kk=kk: e.matmul(p_up[h].t[:, 0:HW],
                                                                    lhsT=wgu[s].t[:, kk, 1024 + fc * 128:1024 + (fc + 1) * 128],
                                                                    rhs=xgT.t[:, kk, hs], start=(kk == 0), stop=(kk == 7)))
                                         for kk in range(8)], reads=[wgu[s], xgT], writes=[p_up[h]])
                            k.op("dve", lambda e: e.tensor_scalar(out=gm[h].t[:], in0=p_g[h].t[:, 0:HW],
                                                                  scalar1=bguT.t[:, e_, fc:fc + 1], scalar2=7.0,
                                                                  op0=ALU.add, op1=ALU.min), reads=[p_g[h], bguT], writes=[gm[h]])
                            k.op("act", lambda e: e.activation(out=sg[h].t[:], in_=gm[h].t[:], func=AF.Sigmoid, scale=1.702),
                                 reads=[gm[h]], writes=[sg[h]])
                            k.op("dve", lambda e: e.tensor_scalar(out=u1[h].t[:], in0=p_up[h].t[:, 0:HW],
                                                                  scalar1=bguT.t[:, e_, 8 + fc:9 + fc], scalar2=7.0,
                                                                  op0=ALU.add, op1=ALU.min), reads=[p_up[h], bguT], writes=[u1[h]])
                            k.op("dve", lambda e: e.tensor_scalar(out=u1[h].t[:], in0=u1[h].t[:], scalar1=-7.0, scalar2=1.0,
                                                                  op0=ALU.max, op1=ALU.add), reads=[u1[h]], writes=[u1[h]])
                            k.op("pool", lambda e: e.tensor_tensor(out=sg[h].t[:], in0=gm[h].t[:], in1=sg[h].t[:], op=ALU.mult),
                                 reads=[gm[h], sg[h]], writes=[sg[h]])
                            k.op("pool", lambda e: e.tensor_tensor(out=actT.t[:, fc, hs], in0=sg[h].t[:], in1=u1[h].t[:], op=ALU.mult),
                                 reads=[sg[h], u1[h]], writes=[actT])
                    if e_ + 1 < NE:
                        transposes()
                    for j in range(NBLK):
                        js = slice(j * 128, (j + 1) * 128)
                        b = j % 2
                        for h in range(2):
                            k.ops("pe", [(lambda e, kk=kk: e.matmul(p_dh[h].t[:], lhsT=actT.t[:, kk, js],
                                                                    rhs=wdn[s].t[:, kk, h * 512:(h + 1) * 512],
                                                                    start=(kk == 0), stop=(kk == 7)))
                                         for kk in range(8)], reads=[actT, wdn[s]], writes=[p_dh[h]])
                            k.op("dve", lambda e: e.tensor_tensor(out=yA[b].t[:, h * 512:(h + 1) * 512], in0=p_dh[h].t[:],
                                                                  in1=bdb[s].t[:, h * 512:(h + 1) * 512], op=ALU.add),
                                 reads=[p_dh[h], bdb[s]], writes=[yA[b]])
                        k.op("act", lambda e: e.activation(out=yB[b].t[:], in_=yA[b].t[:], func=AF.Copy,
                                                           scale=gg[s].t[:, j, e_:e_ + 1]),
                             reads=[yA[b], gg[s]], writes=[yB[b]])
                        k.dma("sp", ysem2[b], lambda e: e.dma_start(out=Y_d[base + j * 128:base + (j + 1) * 128, :], in_=yB[b].t[:]),
                              reads=[yB[b]])
                b_evs = []
                for t in yB:
                    b_evs += t.r.rs
        else:
            b_evs = []

        k.barrier()
        fin_evs = []
        if "C" in phases:
            with ExitStack() as e5:
                wpg = sb(e5, "wpg", [128, 8, DM], BF16)
                k.dma("pool", sem_cp, [(lambda e, i=i: e.dma_start(
                    out=wpg.t[:, 4 * i:4 * i + 4, :],
                    in_=wpg_d[512 * i:512 * (i + 1), :].rearrange("(k p) n -> p k n", p=128))) for i in range(2)], writes=[wpg])
                wpp = sb(e5, "wpp", [128, 2, DM], BF16)
                k.dma("pool", sem_cp, lambda e: e.dma_start(out=wpp.t[:], in_=wpp_d.rearrange("(k p) n -> p k n", p=128)),
                      writes=[wpp])
                gpgT = load_const(e5, "gpgT", [128, 8], gpgT_d)
                gpn_bc = load_const(e5, "gpn_bc", [128, DM], gpn_bc_d)
                gfin_bc = load_const(e5, "gfin_bc", [128, DM], gfin_bc_d)
                x1c = [sb(e5, "x1c%d" % i, [128, DM], F32) for i in range(2)]
                x1csem = [dsem(), dsem()]
                yk = [[sb(e5, "yk%d_%d" % (i, kk), [128, DM], F32) for kk in range(4)] for i in range(2)]
                yksem = [[dsem() for kk in range(4)] for i in range(2)]
                pt = [sb(e5, "pt%d" % i, [128, 256], F32) for i in range(2)]
                ptsem = [dsem(), dsem()]
                x2_l = [sb(e5, "x2%d" % i_, [128, DM], F32) for i_ in range(2)]
                junk4_l = [sb(e5, "junk4%d" % i_, [128, DM], F32) for i_ in range(2)]
                ssc_l = [sb(e5, "ssc%d" % i_, [128, 1], F32) for i_ in range(2)]
                rsc_l = [sb(e5, "rsc%d" % i_, [128, 1], F32) for i_ in range(2)]
                xnb_l = [sb(e5, "xnb%d" % i_, [128, DM], BF16) for i_ in range(2)]
                xnT_l = [sb(e5, "xnT%d" % i_, [128, 8, 128], BF16) for i_ in range(2)]
                sgate_l = [sb(e5, "sgate%d" % i_, [128, DM], F32) for i_ in range(2)]
                pT_l = [sb(e5, "pT%d" % i_, [128, 2, 128], BF16) for i_ in range(2)]
                sse_l = [sb(e5, "sse%d" % i_, [128, 1], F32) for i_ in range(2)]
                rse_l = [sb(e5, "rse%d" % i_, [128, 1], F32) for i_ in range(2)]
                e1__l = [sb(e5, "e1_%d" % i_, [128, DM], F32) for i_ in range(2)]
                x3_l = [sb(e5, "x3%d" % i_, [128, DM], F32) for i_ in range(2)]
                ssf_l = [sb(e5, "ssf%d" % i_, [128, 1], F32) for i_ in range(2)]
                rsf_l = [sb(e5, "rsf%d" % i_, [128, 1], F32) for i_ in range(2)]
                ot = [sb(e5, "ot%d" % i, [128, DM], F32) for i in range(2)]
                osem = [dsem(), dsem()]
                p_t3 = ps(e5, "p_t3", [128, 8, 128], BF16)
                p_ga = ps(e5, "p_ga", [128, DM], F32)
                p_pt = ps(e5, "p_pt", [128, 2, 128], F32)
                p_e = ps(e5, "p_e", [128, DM], F32)
                def cbody(c):
                    b = c % 2
                    cs = slice(c * 128, (c + 1) * 128)
                    x2 = x2_l[b]; junk4 = junk4_l[b]; ssc = ssc_l[b]; rsc = rsc_l[b]; xnb = xnb_l[b]; xnT = xnT_l[b]; sgate = sgate_l[b]; pT = pT_l[b]; sse = sse_l[b]; rse = rse_l[b]; e1_ = e1__l[b]; x3 = x3_l[b]; ssf = ssf_l[b]; rsf = rsf_l[b]
                    yield
                    k.dma("sp", x1csem[b], lambda e: e.dma_start(out=x1c[b].t[:], in_=x1_d[cs, :]), writes=[x1c[b]], extra=a2_evs)
                    yield
                    k.dma("sp", ptsem[b], lambda e: e.dma_start(out=pt[b].t[:], in_=pin_d[cs, :]), writes=[pt[b]])
                    yield
                    for kk in range(4):
                        k.dma("pool", yksem[b][kk], lambda e: e.indirect_dma_start(
                            out=yk[b][kk].t[:, :], out_offset=None, in_=Y_d[:, :],
                            in_offset=bass.IndirectOffsetOnAxis(ap=dest_all.t[:, c, kk:kk + 1], axis=0)),
                            reads=[dest_all], writes=[yk[b][kk]], extra=b_evs)
                    yield
                    k.op("dve", lambda e: e.tensor_tensor(out=x2.t[:], in0=x1c[b].t[:], in1=yk[b][0].t[:], op=ALU.add),
                         reads=[x1c[b], yk[b][0]], writes=[x2])
                    yield
                    k.op("pool", lambda e: e.tensor_tensor(out=yk[b][1].t[:], in0=yk[b][1].t[:], in1=yk[b][2].t[:], op=ALU.add),
                         reads=[yk[b][1], yk[b][2]], writes=[yk[b][1]])
                    yield
                    k.op("dve", lambda e: e.tensor_tensor(out=x2.t[:], in0=x2.t[:], in1=yk[b][3].t[:], op=ALU.add),
                         reads=[x2, yk[b][3]], writes=[x2])
                    yield
                    k.op("dve", lambda e: e.tensor_tensor(out=x2.t[:], in0=x2.t[:], in1=yk[b][1].t[:], op=ALU.add),
                         reads=[x2, yk[b][1]], writes=[x2])
                    yield
                    k.op("act", lambda e: e.activation(out=junk4.t[:], in_=x2.t[:], func=AF.Square, accum_out=ssc.t[:, 0:1]),
                         reads=[x2], writes=[junk4, ssc])
                    yield
                    k.op("act", lambda e: e.activation(out=rsc.t[:], in_=ssc.t[:], func=AF.Sqrt, scale=1.0 / DM, bias=EPS),
                         reads=[ssc], writes=[rsc])
                    yield
                    k.op("dve", lambda e: e.reciprocal(out=rsc.t[:], in_=rsc.t[:]), reads=[rsc], writes=[rsc])
                    yield
                    k.op("act", lambda e: e.activation(out=xnb.t[:], in_=x2.t[:], func=AF.Copy, scale=rsc.t[:, 0:1]),
                         reads=[x2, rsc], writes=[xnb])
                    yield
                    k.ops("pe", [(lambda e, j=j: e.transpose(out=p_t3.t[:, j, :], in_=xnb.t[:, j * 128:(j + 1) * 128],
                                                             identity=ident_bf.t[:])) for j in range(8)],
                          reads=[xnb, ident_bf], writes=[p_t3])
                    yield
                    k.op("dve", lambda e: e.tensor_tensor(out=xnT.t[:], in0=p_t3.t[:],
                                                          in1=gpgT.t[:, :, None].to_broadcast([128, 8, 128]), op=ALU.mult),
                         reads=[p_t3, gpgT], writes=[xnT])
                    yield
                    k.ops("pe", [(lambda e, j=j, h=h: e.matmul(p_ga.t[:, h * 512:(h + 1) * 512], lhsT=xnT.t[:, j, :],
                                                               rhs=wpg.t[:, j, h * 512:(h + 1) * 512], start=(j == 0), stop=(j == 7)))
                                 for h in range(2) for j in range(8)], reads=[xnT, wpg], writes=[p_ga])
                    yield
                    k.op("act", lambda e: e.activation(out=sgate.t[:], in_=p_ga.t[:], func=AF.Sigmoid), reads=[p_ga], writes=[sgate])
                    yield
                    k.ops("pe", [(lambda e, j=j: e.transpose(out=p_pt.t[:, j, :], in_=pt[b].t[:, j * 128:(j + 1) * 128],
                                                             identity=ident_f.t[:])) for j in range(2)],
                          reads=[pt[b], ident_f], writes=[p_pt])
                    yield
                    k.op("act", lambda e: e.copy(out=pT.t[:], in_=p_pt.t[:]), reads=[p_pt], writes=[pT])
                    yield
                    k.ops("pe", [(lambda e, j=j, h=h: e.matmul(p_e.t[:, h * 512:(h + 1) * 512], lhsT=pT.t[:, j, :],
                                                               rhs=wpp.t[:, j, h * 512:(h + 1) * 512], start=(j == 0), stop=(j == 1)))
                                 for h in range(2) for j in range(2)], reads=[pT, wpp], writes=[p_e])
                    yield
                    k.op("act", lambda e: e.activation(out=junk4.t[:], in_=p_e.t[:], func=AF.Square, accum_out=sse.t[:, 0:1]),
                         reads=[p_e], writes=[junk4, sse])
                    yield
                    k.op("act", lambda e: e.activation(out=rse.t[:], in_=sse.t[:], func=AF.Sqrt, scale=1.0 / DM, bias=EPS),
                         reads=[sse], writes=[rse])
                    yield
                    k.op("dve", lambda e: e.reciprocal(out=rse.t[:], in_=rse.t[:]), reads=[rse], writes=[rse])
                    yield
                    k.op("dve", lambda e: e.scalar_tensor_tensor(out=e1_.t[:], in0=p_e.t[:], scalar=rse.t[:, 0:1], in1=gpn_bc.t[:],
                                                                 op0=ALU.mult, op1=ALU.mult), reads=[p_e, rse, gpn_bc], writes=[e1_])
                    yield
                    k.op("pool", lambda e: e.tensor_tensor(out=e1_.t[:], in0=e1_.t[:], in1=sgate.t[:], op=ALU.mult),
                         reads=[e1_, sgate], writes=[e1_])
                    yield
                    k.op("dve", lambda e: e.tensor_tensor(out=x3.t[:], in0=x2.t[:], in1=e1_.t[:], op=ALU.add),
                         reads=[x2, e1_], writes=[x3])
                    yield
                    k.op("act", lambda e: e.activation(out=junk4.t[:], in_=x3.t[:], func=AF.Square, accum_out=ssf.t[:, 0:1]),
                         reads=[x3], writes=[junk4, ssf])
                    yield
                    k.op("act", lambda e: e.activation(out=rsf.t[:], in_=ssf.t[:], func=AF.Sqrt, scale=1.0 / DM, bias=EPS),
                         reads=[ssf], writes=[rsf])
                    yield
                    k.op("dve", lambda e: e.reciprocal(out=rsf.t[:], in_=rsf.t[:]), reads=[rsf], writes=[rsf])
                    yield
                    k.op("dve", lambda e: e.scalar_tensor_tensor(out=ot[b].t[:], in0=x3.t[:], scalar=rsf.t[:, 0:1], in1=gfin_bc.t[:],
                                                                 op0=ALU.mult, op1=ALU.mult), reads=[x3, rsf, gfin_bc], writes=[ot[b]])
                    yield
                    fin_evs.append(k.dma("sp", osem[b], lambda e: e.dma_start(out=out_d[cs, :], in_=ot[b].t[:]), reads=[ot[b]]))

                    yield
                run_chains([cbody(c) for c in range(NCH)], lag=6)

        tail = list(fin_evs[-2:]) + list(a2_evs) + list(b_evs)
        for sid_ev in tail:
            k.wait("sp", sid_ev)
        for sid_, sem_ in k.dma_objs.items():
            k.wait("sp", (sem_, k.dma_cnt[sid_]))
        build.stats = (k.ninst, k.nwaits, dict(k.cnt))
    return nc


def host_layout(inp):
    f = np.float32

    def colT(v, n):
        return np.ascontiguousarray(np.asarray(v, f).reshape(n, 128).T)

    def bc(v):
        v = np.asarray(v, f).reshape(1, -1)
        return np.ascontiguousarray(np.broadcast_to(v, (128, v.shape[1])))

    cw = np.asarray(inp["conv_w"][0], f)
    conv_wT = np.ascontiguousarray(cw.reshape(4, 32, 128).transpose(2, 1, 0))
    bgu = np.asarray(inp["b_gate_up"][0], f)
    bguT = np.ascontiguousarray(bgu.reshape(NE, 16, 128).transpose(2, 0, 1))
    invc = np.zeros((128, 4, 16), f)
    for gi, w in enumerate((2, 4, 8, 16)):
        invc[:, gi, :] = 1.0 / np.minimum(np.arange(16) + 1, w).astype(f)
    ebase = (1 + np.arange(NE) * CAP).astype(f)
    shared = {
        "w_in": np.ascontiguousarray(inp["w_in"][0], f),
        "conv_wT": conv_wT,
        "conv_bT": colT(inp["conv_b"][0], 32),
        "conv_brow": np.ascontiguousarray(np.asarray(inp["conv_b"][0], f).reshape(1, 4096)),
        "dt_bias_bc": bc(inp["dt_bias"][0]),
        "a_log_bc": bc(inp["a_log"][0]),
        "d_skip_bc": bc(inp["d_skip"][0]),
        "gmixT": colT(inp["mix_norm_g"][0], 8),
        "gssdT": colT(inp["ssd_norm_g"][0], 16),
        "pool_w": np.ascontiguousarray(inp["pool_w"][0], f),
        "pscT": colT(inp["pool_scale"][0], 8),
        "invc": invc,
        "w_out": np.ascontiguousarray(inp["w_out"][0], f),
        "gffn_bc": bc(inp["ffn_norm_g"][0]),
        "router_w": np.ascontiguousarray(inp["router_w"][0], f),
        "rb_bc": bc(inp["router_b"][0]),
        "ebase_bc": bc(ebase),
        "w_gate_up": np.ascontiguousarray(inp["w_gate_up"][0], f),
        "bguT": bguT,
        "w_down": np.ascontiguousarray(inp["w_down"][0], f),
        "b_down": np.ascontiguousarray(inp["b_down"][0], f),
        "gpgT": colT(inp["ple_gate_norm_g"][0], 8),
        "w_ple_gate": np.ascontiguousarray(inp["w_ple_gate"][0], f),
        "w_ple_proj": np.ascontiguousarray(inp["w_ple_proj"][0], f),
        "gpn_bc": bc(inp["ple_norm_g"][0]),
        "gfin_bc": bc(inp["final_norm_g"]),
    }
    return shared


def kernel(**inputs):
    inp = {k_: np.asarray(v) for k_, v in inputs.items()}
    shared = host_layout(inp)
    x = np.asarray(inp["x"], np.float32)
    p = np.asarray(inp["p"], np.float32)[0]
    nb = x.shape[0]
    in_maps = []
    for b in range(nb):
        m = dict(shared)
        m["x"] = np.ascontiguousarray(x[b])
        m["p"] = np.ascontiguousarray(p[b])
        in_maps.append(m)
    nc = build()
    res = run_bass_kernel_spmd(nc, in_maps, core_ids=list(range(nb)))
    return np.stack([np.asarray(r["out"], np.float32) for r in res.results], axis=0)
```

```python
from contextlib import ExitStack
import numpy as np
import concourse.bass as bass
import concourse.mybir as mybir
from concourse.bass_utils import run_bass_kernel_spmd

F32 = mybir.dt.float32
BF16 = mybir.dt.bfloat16
I32 = mybir.dt.int32
AF = mybir.ActivationFunctionType
ALU = mybir.AluOpType
AX = mybir.AxisListType

L = 4096
DM = 1024
NCH = 32
NSC = 8
D_IN = 7200
NE = 32
CAP = 1024
NBLK = CAP // 128
NSLOT = NE * CAP
SLOT_TAB = 128 * 257
EPS = 1e-6


class R:
    __slots__ = ("w", "rs")

    def __init__(self):
        self.w = None
        self.rs = []


class T:
    def __init__(self, t):
        self.t = t
        self.r = R()


class K:
    def __init__(self, nc, sems):
        self.nc = nc
        self.engs = {"pe": nc.tensor, "dve": nc.vector, "act": nc.scalar,
                     "pool": nc.gpsimd, "sp": nc.sync}
        self.psem = sems
        self.cnt = {k: 0 for k in self.engs}
        self.waited = {k: {} for k in self.engs}
        self.dma_cnt = {}
        self.dma_objs = {}
        self.ninst = 0
        self.nwaits = 0

    def wait(self, ek, ev):
        if ev is None:
            return
        sem, val = ev
        sid = id(sem)
        if sid in self.dma_cnt:
            val = max(val, self.dma_cnt[sid])
        w = self.waited[ek]
        if w.get(sid, 0) >= val:
            return
        self.engs[ek].wait_ge(sem, val)
        self.nwaits += 1
        w[sid] = val

    def _deps(self, ek, reads, writes, extra):
        for t in reads:
            self.wait(ek, t.r.w)
        for t in writes:
            self.wait(ek, t.r.w)
            for e in t.r.rs:
                self.wait(ek, e)
        for e in extra:
            self.wait(ek, e)

    def _commit(self, ev, reads, writes):
        for t in reads:
            t.r.rs.append(ev)
        for t in writes:
            t.r.w = ev
            t.r.rs = []

    def barrier(self):
        for ek in self.engs:
            for o in self.engs:
                if self.cnt[o] > 0:
                    self.wait(ek, (self.psem[o], self.cnt[o]))
            for sid_, sem_ in self.dma_objs.items():
                self.wait(ek, (sem_, self.dma_cnt[sid_]))

    def op(self, ek, fn, reads=(), writes=(), extra=()):
        self._deps(ek, reads, writes, extra)
        ins = fn(self.engs[ek])
        self.cnt[ek] += 1
        ins.then_inc(self.psem[ek], 1)
        ev = (self.psem[ek], self.cnt[ek])
        self._commit(ev, reads, writes)
        self.ninst += 1
        return ev

    def ops(self, ek, fns, reads=(), writes=(), extra=()):
        self._deps(ek, reads, writes, extra)
        ins = None
        for fn in fns:
            ins = fn(self.engs[ek])
            self.ninst += 1
        self.cnt[ek] += 1
        ins.then_inc(self.psem[ek], 1)
        ev = (self.psem[ek], self.cnt[ek])
        self._commit(ev, reads, writes)
        return ev

    def dma(self, ek, sem, fns, reads=(), writes=(), extra=()):
        self._deps(ek, reads, writes, extra)
        if not isinstance(fns, (list, tuple)):
            fns = [fns]
        sid = id(sem)
        cur = self.dma_cnt.get(sid, 0)
        for f in fns:
            f(self.engs[ek]).then_inc(sem, 16)
            cur += 16
            self.ninst += 1
        self.dma_cnt[sid] = cur
        self.dma_objs[sid] = sem
        ev = (sem, cur)
        self._commit(ev, reads, writes)
        return ev


def run_chains(gens, lag=4, width=2):
    pending = list(gens)
    active = []
    while pending or active:
        if len(active) < width and pending and (not active or active[-1][1] >= lag):
            active.append([pending.pop(0), 0])
        for a in list(active):
            try:
                next(a[0])
                a[1] += 1
            except StopIteration:
                active.remove(a)


def build(debug=None, phases="A0,A1,A1p,A2,B,C"):
    phases = set(phases.split(","))
    debug = debug or ()
    nc = bass.Bass("TRN2", target_bir_lowering=False)

    def din(name, shape, dt=F32):
        return nc.dram_tensor(name, list(shape), dt, kind="ExternalInput").ap()

    x_d = din("x", [L, DM])
    pin_d = din("p", [L, 256])
    w_in_d = din("w_in", [DM, D_IN])
    conv_wT_d = din("conv_wT", [128, 32, 4])
    conv_bT_d = din("conv_bT", [128, 32])
    conv_brow_d = din("conv_brow", [1, 4096])
    dtb_d = din("dt_bias_bc", [128, 32])
    alog_d = din("a_log_bc", [128, 32])
    dsk_d = din("d_skip_bc", [128, 32])
    gmixT_d = din("gmixT", [128, 8])
    gssdT_d = din("gssdT", [128, 16])
    pool_w_d = din("pool_w", [4, 256, 256])
    pscT_d = din("pscT", [128, 8])
    invc_d = din("invc", [128, 4, 16])
    w_out_d = din("w_out", [3072, DM])
    gffn_bc_d = din("gffn_bc", [128, DM])
    rw_d = din("router_w", [DM, NE])
    rb_d = din("rb_bc", [128, NE])
    ebase_d = din("ebase_bc", [128, NE])
    wgu_d = din("w_gate_up", [NE, DM, 2048])
    bguT_d = din("bguT", [128, NE, 16])
    wd_d = din("w_down", [NE, DM, DM])
    bd_d = din("b_down", [NE, DM])
    gpgT_d = din("gpgT", [128, 8])
    wpg_d = din("w_ple_gate", [DM, DM])
    wpp_d = din("w_ple_proj", [256, DM])
    gpn_bc_d = din("gpn_bc", [128, DM])
    gfin_bc_d = din("gfin_bc", [128, DM])
    out_d = nc.dram_tensor("out", [L, DM], F32, kind="ExternalOutput").ap()

    def dscr(name, shape, dt, dbg=False):
        kind = "ExternalOutput" if (name in debug) else "Internal"
        return nc.dram_tensor(name, list(shape), dt, kind=kind).ap()

    ymT_d = dscr("ymT", [24, 128, L], BF16)
    x1_d = dscr("x1d", [L, DM], F32)
    h2_d = dscr("h2d", [L + 1, DM], BF16)
    G_d = dscr("Gd", [L + 1, NE], F32)
    slot_d = dscr("slotd", [SLOT_TAB, 2], I32)
    Y_d = dscr("Yd", [NSLOT + 1, DM], F32)

    with ExitStack() as es:
        E = es.enter_context
        sems = {k: E(nc.semaphore("prog_" + k)) for k in ["pe", "dve", "act", "pool", "sp"]}
        k = K(nc, sems)
        nsem = [0]

        def dsem():
            nsem[0] += 1
            return E(nc.semaphore("d%d" % nsem[0]))

        def sb(es_, name, shape, dt=F32):
            return T(es_.enter_context(nc.sbuf_tensor("s_" + name, list(shape), dt)))

        def ps(es_, name, shape, dt=F32):
            esz = 4 if dt == F32 else 2
            n = 1
            for d_ in shape[1:]:
                n *= d_
            per_bank = 2048 // esz
            nb_ = (n + per_bank - 1) // per_bank
            base = es_.enter_context(nc.psum_tensor("ps_" + name, [128, nb_ * per_bank], dt))
            v = base[:, 0:n]
            if len(shape) == 3:
                v = v.rearrange("p (a b) -> p a b", a=shape[1])
            return T(v)

        def psbank(es_, name, dt=F32):
            per_bank = 2048 // (4 if dt == F32 else 2)
            return es_.enter_context(nc.psum_tensor("ps_" + name, [128, per_bank], dt))

        ident_bf = sb(es, "ident_bf", [128, 128], BF16)
        ident_f = sb(es, "ident_f", [128, 128], F32)
        Mle = sb(es, "Mle", [128, 128], F32)
        Mgt = sb(es, "Mgt", [128, 128], F32)
        Mlt = sb(es, "Mlt", [128, 128], F32)
        ones_f = sb(es, "ones_f", [128, 128], F32)
        dest_all = sb(es, "dest_all", [128, NCH, 4], I32)
        tokid = sb(es, "tokid", [128, NCH, 2], I32)
        zrow = sb(es, "zrow", [1, DM], F32)
        zrow_bf = sb(es, "zrow_bf", [1, DM], BF16)

        def dump(name, t, ap=None):
            if name not in debug:
                return
            a = ap if ap is not None else t.t[:]
            dd = nc.dram_tensor("dbg_" + name, list(a.shape), a.dtype, kind="ExternalOutput").ap()
            k.dma("sp", dsem(), lambda e: e.dma_start(out=dd, in_=a), reads=[t])

        def cst(t, fn):
            k.op("pool", fn, writes=[t])

        cst(ident_bf, lambda e: e.memset(ident_bf.t[:], 1.0))
        cst(ident_bf, lambda e: e.affine_select(out=ident_bf.t[:], in_=ident_bf.t[:], pattern=[[-1, 128]],
                                               compare_op=ALU.is_equal, fill=0.0, base=0, channel_multiplier=1))
        cst(ident_f, lambda e: e.memset(ident_f.t[:], 1.0))
        cst(ident_f, lambda e: e.affine_select(out=ident_f.t[:], in_=ident_f.t[:], pattern=[[-1, 128]],
                                              compare_op=ALU.is_equal, fill=0.0, base=0, channel_multiplier=1))
        cst(Mle, lambda e: e.memset(Mle.t[:], 1.0))
        cst(Mle, lambda e: e.affine_select(out=Mle.t[:], in_=Mle.t[:], pattern=[[1, 128]],
                                          compare_op=ALU.is_ge, fill=0.0, base=0, channel_multiplier=-1))
        cst(Mgt, lambda e: e.memset(Mgt.t[:], 1.0))
        cst(Mgt, lambda e: e.affine_select(out=Mgt.t[:], in_=Mgt.t[:], pattern=[[-1, 128]],
                                          compare_op=ALU.is_gt, fill=0.0, base=0, channel_multiplier=1))
        cst(Mlt, lambda e: e.memset(Mlt.t[:], 1.0))
        cst(Mlt, lambda e: e.affine_select(out=Mlt.t[:], in_=Mlt.t[:], pattern=[[1, 128]],
                                          compare_op=ALU.is_gt, fill=0.0, base=0, channel_multiplier=-1))
        cst(ones_f, lambda e: e.memset(ones_f.t[:], 1.0))
        cst(zrow, lambda e: e.memset(zrow.t[:], 0.0))
        cst(zrow_bf, lambda e: e.memset(zrow_bf.t[:], 0.0))
        cst(tokid, lambda e: e.iota(tokid.t[:], pattern=[[128, NCH], [0, 2]], base=0, channel_multiplier=1))
        cst(dest_all, lambda e: e.memset(dest_all.t[:], 0))

        sem_c = dsem()
        sem_cp = dsem()

        def load_const(es_, name, shape, src, dt=F32, q="sp"):
            t = sb(es_, name, shape, dt)
            k.dma(q, sem_c, lambda e: e.dma_start(out=t.t[:], in_=src), writes=[t])
            return t

        if "A0" in phases:
            with ExitStack() as esA:
                hT = sb(esA, "hT", [128, 8, L], BF16)
                dt_all = sb(esA, "dt_all", [128, NCH, 32], F32)
                a_all = sb(esA, "a_all", [128, NCH, 32], F32)
                e_all = sb(esA, "e_all", [128, NCH, 3, 32], F32)
                gmixT = load_const(esA, "gmixT", [128, 8], gmixT_d)
                gssdT = load_const(esA, "gssdT", [128, 16], gssdT_d)
                conv_wT = load_const(esA, "conv_wT", [128, 32, 4], conv_wT_d)
                conv_bT = load_const(esA, "conv_bT", [128, 32], conv_bT_d)
                dsk = load_const(esA, "dsk", [128, 32], dsk_d)
                pscT = load_const(esA, "pscT", [128, 8], pscT_d)
                invc = load_const(esA, "invc", [128, 4, 16], invc_d)

                with ExitStack() as e0:
                    dtb = load_const(e0, "dtb", [128, 32], dtb_d)
                    alog = load_const(e0, "alog", [128, 32], alog_d)
                    Abc = sb(e0, "Abc", [128, 32], F32)
                    wdt = sb(e0, "wdt", [128, 8, 32], BF16)
                    k.dma("pool", sem_cp, lambda e: e.dma_start(
                        out=wdt.t[:], in_=w_in_d[:, 6144:6176].rearrange("(k p) n -> p k n", p=128)), writes=[wdt])
                    k.op("act", lambda e: e.activation(out=Abc.t[:], in_=alog.t[:], func=AF.Exp), reads=[alog], writes=[Abc])
                    k.op("dve", lambda e: e.tensor_scalar(out=Abc.t[:], in0=Abc.t[:], scalar1=-1.0, scalar2=None, op0=ALU.mult),
                         reads=[Abc], writes=[Abc])
                    xt = [sb(e0, "xt%d" % i, [128, DM], F32) for i in range(2)]
                    xsem = [dsem(), dsem()]
                    junk = sb(e0, "junk", [128, DM], F32)
                    ss = sb(e0, "ss", [128, 1], F32)
                    rstd = sb(e0, "rstd", [128, 1], F32)
                    xn = sb(e0, "xn", [128, DM], BF16)
                    tpA = ps(e0, "tpA", [128, 8, 128], BF16)
                    pdt = ps(e0, "pdt", [128, 32], F32)
                    pcs = ps(e0, "pcs", [128, 3, 32], F32)
                    dtr = sb(e0, "dtr", [128, 32], F32)
                    t1 = sb(e0, "t1", [128, 32], F32)
                    t2 = sb(e0, "t2", [128, 32], F32)
                    for c in range(NCH):
                        xc_ = xt[c % 2]
                        cs = slice(c * 128, (c + 1) * 128)
                        k.dma("sp", xsem[c % 2], lambda e: e.dma_start(out=xc_.t[:], in_=x_d[cs, :]), writes=[xc_])
                        k.op("act", lambda e: e.activation(out=junk.t[:], in_=xc_.t[:], func=AF.Square, accum_out=ss.t[:, 0:1]),
                             reads=[xc_], writes=[junk, ss])
                        k.op("act", lambda e: e.activation(out=rstd.t[:], in_=ss.t[:], func=AF.Sqrt, scale=1.0 / DM, bias=EPS),
                             reads=[ss], writes=[rstd])
                        k.op("dve", lambda e: e.reciprocal(out=rstd.t[:], in_=rstd.t[:]), reads=[rstd], writes=[rstd])
                        k.op("act", lambda e: e.activation(out=xn.t[:], in_=xc_.t[:], func=AF.Copy, scale=rstd.t[:, 0:1]),
                             reads=[xc_, rstd], writes=[xn])
                        k.ops("pe", [(lambda e, j=j: e.transpose(out=tpA.t[:, j, :], in_=xn.t[:, j * 128:(j + 1) * 128],
                                                                 identity=ident_bf.t[:])) for j in range(8)],
                              reads=[xn, ident_bf], writes=[tpA])
                        k.op("dve", lambda e: e.tensor_tensor(out=hT.t[:, :, cs], in0=tpA.t[:],
                                                              in1=gmixT.t[:, :, None].to_broadcast([128, 8, 128]), op=ALU.mult),
                             reads=[tpA, gmixT], writes=[hT])
                        k.ops("pe", [(lambda e, j=j: e.matmul(pdt.t[:], lhsT=hT.t[:, j, cs], rhs=wdt.t[:, j, :],
                                                              start=(j == 0), stop=(j == 7))) for j in range(8)],
                              reads=[hT, wdt], writes=[pdt])
                        k.op("dve", lambda e: e.tensor_tensor(out=dtr.t[:], in0=pdt.t[:], in1=dtb.t[:], op=ALU.add),
                             reads=[pdt, dtb], writes=[dtr])
                        k.op("act", lambda e: e.activation(out=t1.t[:], in_=dtr.t[:], func=AF.Abs),
                             reads=[dtr], writes=[t1])
                        k.op("act", lambda e: e.activation(out=t2.t[:], in_=t1.t[:], func=AF.Exp, scale=-1.0), reads=[t1], writes=[t2])
                        k.op("act", lambda e: e.activation(out=t1.t[:], in_=t2.t[:], func=AF.Ln, bias=1.0), reads=[t2], writes=[t1])
                        k.op("dve", lambda e: e.scalar_tensor_tensor(out=dt_all.t[:, c, :], in0=dtr.t[:], scalar=0.0, in1=t1.t[:],
                                                                     op0=ALU.max, op1=ALU.add),
                             reads=[dtr, t1], writes=[dt_all])
                        k.op("dve", lambda e: e.tensor_tensor(out=a_all.t[:, c, :], in0=dt_all.t[:, c, :], in1=Abc.t[:], op=ALU.mult),
                             reads=[dt_all, Abc], writes=[a_all])
                        k.ops("pe", [
                            lambda e: e.matmul(pcs.t[:, 0, :], lhsT=Mle.t[:], rhs=a_all.t[:, c, :], start=True, stop=True),
                            lambda e: e.matmul(pcs.t[:, 1, :], lhsT=Mgt.t[:], rhs=a_all.t[:, c, :], start=True, stop=True),
                            lambda e: e.matmul(pcs.t[:, 2, :], lhsT=ones_f.t[:], rhs=a_all.t[:, c, :], start=True, stop=True),
                        ], reads=[a_all, Mle, Mgt, ones_f], writes=[pcs])
                        k.op("act", lambda e: e.activation(out=e_all.t[:, c, :, :], in_=pcs.t[:], func=AF.Exp),
                             reads=[pcs], writes=[e_all])

                    k.barrier()
                dump("hT", hT); dump("dt_all", dt_all); dump("a_all", a_all); dump("e_all", e_all)
                if "A1" in phases:
                    with ExitStack() as e1:
                        wg = [sb(e1, "wg%d" % i, [128, 8, 768], BF16) for i in range(2)]
                        wsem = [dsem(), dsem()]
                        dg = [sb(e1, "dg%d" % i, [128, 4, 4, 128], BF16) for i in range(2)]
                        ust = [sb(e1, "ust%d" % i, [128, 4, 515], BF16) for i in range(2)]
                        xc = [sb(e1, "xc%d" % i, [128, 4, 512], BF16) for i in range(2)]
                        state = sb(e1, "state", [128, 256], F32)
                        state_bf = [sb(e1, "state_bf%d" % i, [128, 256], BF16) for i in range(3)]
                        zstate = sb(e1, "zstate", [128, 256], BF16)
                        ynT = [sb(e1, "ynT%d" % i, [128, 2, 512], BF16) for i in range(2)]
                        ysem = [dsem(), dsem()]
                        sz = [sb(e1, "sz%d" % i, [128, 256], BF16) for i in range(3)]
                        xbtm = [sb(e1, "xbtm%d" % i, [128, 384], BF16) for i in range(3)]
                        xd = [sb(e1, "xd%d" % i, [128, 4, 64], BF16) for i in range(3)]
                        y4 = [sb(e1, "y4_%d" % i, [128, 256], F32) for i in range(3)]
                        xde = [sb(e1, "xde%d" % i, [128, 4, 64], BF16) for i in range(2)]
                        cbm = [sb(e1, "cbm%d" % i, [128, 128], F32) for i in range(2)]
                        lh = [sb(e1, "lh%d" % i, [128, 4, 128], F32) for i in range(2)]
                        Ex = [sb(e1, "Ex%d" % i, [128, 4, 128], F32) for i in range(2)]
                        MT = [sb(e1, "MT%d" % i, [128, 4, 128], BF16) for i in range(2)]
                        y1 = [sb(e1, "y1_%d" % i, [128, 4, 64], F32) for i in range(2)]
                        tD = [sb(e1, "tD%d" % i, [128, 4, 64], F32) for i in range(2)]
                        yb = [sb(e1, "yb%d" % i, [128, 256], BF16) for i in range(2)]
                        junk2 = sb(e1, "junk2", [128, 256], F32)
                        ss2 = [sb(e1, "ss2_%d" % i, [128, 1], F32) for i in range(2)]
                        rs2 = [sb(e1, "rs2_%d" % i, [128, 1], F32) for i in range(2)]
                        p_u = [ps(e1, "p_u%d" % i, [128, 512], F32) for i in range(2)]
                        p_z = ps(e1, "p_z", [128, 256], F32)
                        bk1 = psbank(e1, "bk1", F32)
                        p_cb = T(bk1[:, 0:128])
                        p_s = T(bk1[:, 256:512])
                        p_s.r = p_cb.r
                        p_seg = [ps(e1, "p_seg%d" % i, [128, 4, 128], F32) for i in range(2)]
                        bk4 = psbank(e1, "bk4", F32)
                        p_y = T(bk4[:, 0:256])
                        p_yo = T(bk4[:, 256:512])
                        p_yo.r = p_y.r
                        bk6 = psbank(e1, "bk6", BF16)
                        p_tpx = T(bk6[:, 0:384])
                        p_tpy = T(bk6[:, 384:640])
                        p_tpy.r = p_tpx.r
                        k.op("pool", lambda e: e.memset(zstate.t[:], 0.0), writes=[zstate])
                        thz = [sb(e1, "thz%d" % i, [128, 256], F32) for i in range(2)]
                        thc = [sb(e1, "thc%d" % i, [128, 512], F32) for i in range(2)]
                        neghalf = sb(e1, "neghalf", [128, 1], F32)
                        k.op("pool", lambda e: e.memset(neghalf.t[:], -0.5), writes=[neghalf])
                        ones_row = sb(e1, "ones_row", [1, 512], BF16)
                        k.op("pool", lambda e: e.memset(ones_row.t[:], 1.0), writes=[ones_row])
                        cb_row = sb(e1, "cb_row", [1, 4096], F32)
                        k.dma("sp", sem_c, lambda e: e.dma_start(out=cb_row.t[:], in_=conv_brow_d), writes=[cb_row])
                        hb_row = sb(e1, "hb_row", [1, 4096], BF16)
                        k.op("dve", lambda e: e.tensor_scalar(out=hb_row.t[:], in0=cb_row.t[:], scalar1=0.5, scalar2=None, op0=ALU.mult),
                             reads=[cb_row], writes=[hb_row])

                        def group_setup(g):
                            w = wg[g % 2]
                            srcs = [(0, 2048 + g * 256, 256), (256, 4096 + g * 128, 128),
                                    (384, 5120 + g * 128, 128), (512, g * 256, 256)]
                            k.dma("pool", wsem[g % 2],
                                  [(lambda e, o=o, s=s, n=n: e.dma_start(
                                      out=w.t[:, :, o:o + n],
                                      in_=w_in_d[:, s:s + n].rearrange("(k p) n -> p k n", p=128))) for (o, s, n) in srcs],
                                  writes=[w])
                            d_ = dg[g % 2]
                            chunks = [2 * g, 2 * g + 1, 16 + g, 24 + g]
                            k.ops("pool", [(lambda e, cc=cc, kk=kk: e.tensor_scalar(
                                out=d_.t[:, cc, kk, :], in0=ident_bf.t[:], scalar1=conv_wT.t[:, chunks[cc], kk:kk + 1],
                                scalar2=0.5, op0=ALU.mult, op1=ALU.mult)) for cc in range(4) for kk in range(4)],
                                reads=[ident_bf, conv_wT], writes=[d_])
                            k.op("pool", lambda e: e.tensor_scalar(out=w.t[:, :, 512:768], in0=w.t[:, :, 512:768], scalar1=0.5,
                                                                   scalar2=0.0, op0=ALU.mult, op1=ALU.add), reads=[w], writes=[w])

                        NT = 8 * NCH

                        def dec(n):
                            g = n // NCH
                            c = n % NCH
                            return g, c, c // 4, c % 4

                        def S0a(si):
                            g, sc = si // NSC, si % NSC
                            w = wg[g % 2]
                            us = ust[si % 2]
                            ts = sc * 512
                            if sc == 0:
                                k.op("pool", lambda e: e.memset(us.t[:, :, 0:3], 0.0), writes=[us])
                            for cc in range(4):
                                pu = p_u[cc % 2]
                                k.ops("pe", [(lambda e, j=j: e.matmul(pu.t[:], lhsT=w.t[:, j, cc * 128:(cc + 1) * 128],
                                                                      rhs=hT.t[:, j, ts:ts + 512], start=(j == 0), stop=(j == 7)))
                                             for j in range(8)], reads=[w, hT], writes=[pu])
                                k.op("act", lambda e: e.copy(out=us.t[:, cc, 3:515], in_=pu.t[:]), reads=[pu], writes=[us])
                            if sc + 1 < NSC:
                                un = ust[(si + 1) % 2]
                                k.op("pool", lambda e: e.tensor_copy(out=un.t[:, :, 0:3], in_=us.t[:, :, 512:515]),
                                     reads=[us], writes=[un])

                        def S0b(si):
                            g, sc = si // NSC, si % NSC
                            d_ = dg[g % 2]
                            us = ust[si % 2]
                            xo = xc[si % 2]
                            chunks = [2 * g, 2 * g + 1, 16 + g, 24 + g]
                            for cc in range(4):
                                pu = p_u[cc % 2]
                                ch = chunks[cc]
                                k.ops("pe", [(lambda e, kk=kk: e.matmul(pu.t[:], lhsT=d_.t[:, cc, kk, :],
                                                                        rhs=us.t[:, cc, kk:kk + 512], start=(kk == 0), stop=False))
                                             for kk in range(4)] +
                                      [lambda e: e.matmul(pu.t[:], lhsT=hb_row.t[0:1, ch * 128:(ch + 1) * 128],
                                                          rhs=ones_row.t[0:1, :], start=False, stop=True)],
                                      reads=[d_, us, hb_row, ones_row], writes=[pu])
                                tc_ = thc[cc % 2]
                                k.op("act", lambda e: e.activation(out=tc_.t[:], in_=pu.t[:], func=AF.Tanh),
                                     reads=[pu], writes=[tc_])
                                k.op("dve", lambda e: e.scalar_tensor_tensor(out=xo.t[:, cc, :], in0=tc_.t[:], scalar=1.0, in1=pu.t[:],
                                                                             op0=ALU.add, op1=ALU.mult),
                                     reads=[tc_, pu], writes=[xo])

                        def ctx(n):
                            g, c, sc, q = dec(n)
                            si = g * NSC + sc
                            return dict(g=g, c=c, sc=sc, q=q, si=si, w=wg[g % 2], xo=xc[si % 2],
                                        qs=slice(q * 128, (q + 1) * 128), cs=slice(c * 128, (c + 1) * 128),
                                        g4=slice(g * 4, g * 4 + 4), b3=n % 3, b2=n % 2)

                        def A_pe(n):
                            x_ = ctx(n); w = x_["w"]; xo = x_["xo"]; qs = x_["qs"]; cs = x_["cs"]
                            k.ops("pe", [(lambda e, j=j: e.matmul(p_z.t[:], lhsT=hT.t[:, j, cs], rhs=w.t[:, j, 512:768],
                                                                  start=(j == 0), stop=(j == 7))) for j in range(8)],
                                  reads=[hT, w], writes=[p_z])
                            k.ops("pe", [(lambda e, cc=cc: e.transpose(out=p_tpx.t[:, cc * 128:(cc + 1) * 128],
                                                                       in_=xo.t[:, cc, qs], identity=ident_bf.t[:]))
                                         for cc in range(3)], reads=[xo, ident_bf], writes=[p_tpx])
                            k.op("pe", lambda e: e.matmul(p_cb.t[:], lhsT=xo.t[:, 2, qs], rhs=xo.t[:, 3, qs],
                                                          start=True, stop=True), reads=[xo], writes=[p_cb])

                        def A_act(n):
                            x_ = ctx(n); b3 = x_["b3"]; b2 = x_["b2"]; c = x_["c"]; g = x_["g"]
                            k.op("act", lambda e: e.activation(out=thz[b2].t[:], in_=p_z.t[:], func=AF.Tanh),
                                 reads=[p_z], writes=[thz[b2]])
                            k.op("act", lambda e: e.copy(out=xbtm[b3].t[:], in_=p_tpx.t[:]), reads=[p_tpx], writes=[xbtm[b3]])
                            k.ops("act", [(lambda e, r=r: e.activation(out=lh[b2].t[:, r, :], in_=Mgt.t[:], func=AF.Copy,
                                                                       scale=a_all.t[:, c, g * 4 + r:g * 4 + r + 1]))
                                          for r in range(4)], reads=[Mgt, a_all], writes=[lh[b2]])

                        def A_dve(n):
                            x_ = ctx(n); b3 = x_["b3"]; b2 = x_["b2"]; c = x_["c"]; g4 = x_["g4"]
                            k.op("dve", lambda e: e.scalar_tensor_tensor(out=sz[b3].t[:], in0=thz[b2].t[:], scalar=1.0, in1=p_z.t[:],
                                                                         op0=ALU.add, op1=ALU.mult),
                                 reads=[thz[b2], p_z], writes=[sz[b3]])
                            k.op("dve", lambda e: e.tensor_tensor(
                                out=xd[b3].t[:], in0=xbtm[b3].t[:, 0:256].rearrange("p (r d) -> p r d", r=4),
                                in1=dt_all.t[:, c, g4, None].to_broadcast([128, 4, 64]), op=ALU.mult),
                                reads=[xbtm[b3], dt_all], writes=[xd[b3]])
                            k.op("dve", lambda e: e.tensor_tensor(out=cbm[b2].t[:], in0=p_cb.t[:], in1=Mle.t[:], op=ALU.mult),
                                 reads=[p_cb, Mle], writes=[cbm[b2]])

                        def A_pool(n):
                            x_ = ctx(n); b3 = x_["b3"]; b2 = x_["b2"]; c = x_["c"]; g4 = x_["g4"]
                            k.op("pool", lambda e: e.tensor_tensor(
                                out=xde[b2].t[:], in0=xd[b3].t[:],
                                in1=e_all.t[:, c, 1, g4, None].to_broadcast([128, 4, 64]), op=ALU.mult),
                                reads=[xd[b3], e_all], writes=[xde[b2]])

                        def B_pe(n):
                            x_ = ctx(n); b3 = x_["b3"]; b2 = x_["b2"]
                            k.ops("pe", [(lambda e, r=r: e.matmul(p_seg[b2].t[:, r, :], lhsT=lh[b2].t[:, r, :], rhs=Mle.t[:],
                                                                  start=True, stop=True)) for r in range(4)],
                                  reads=[lh[b2], Mle], writes=[p_seg[b2]])
                            k.op("pe", lambda e: e.matmul(p_s.t[:], lhsT=xbtm[b3].t[:, 256:384],
                                                          rhs=xde[b2].t[:].rearrange("p r d -> p (r d)"), start=True, stop=True),
                                 reads=[xbtm[b3], xde[b2]], writes=[p_s])

                        def B_act(n):
                            x_ = ctx(n); b2 = x_["b2"]
                            k.op("act", lambda e: e.activation(out=Ex[b2].t[:], in_=p_seg[b2].t[:], func=AF.Exp),
                                 reads=[p_seg[b2]], writes=[Ex[b2]])

                        def B_dve(n):
                            x_ = ctx(n); b2 = x_["b2"]; c = x_["c"]; g4 = x_["g4"]
                            k.op("dve", lambda e: e.tensor_tensor(
                                out=MT[b2].t[:], in0=Ex[b2].t[:],
                                in1=cbm[b2].t[:, None, :].to_broadcast([128, 4, 128]), op=ALU.mult),
                                reads=[Ex[b2], cbm[b2]], writes=[MT[b2]])
                            if c == 0:
                                k.op("dve", lambda e: e.tensor_copy(out=state.t[:], in_=p_s.t[:]), reads=[p_s], writes=[state])
                            else:
                                k.op("dve", lambda e: e.tensor_tensor(
                                    out=state.t[:].rearrange("p (r d) -> p r d", r=4),
                                    in0=state.t[:].rearrange("p (r d) -> p r d", r=4),
                                    in1=e_all.t[:, c, 2, g4, None].to_broadcast([128, 4, 64]), op=ALU.mult),
                                    reads=[state, e_all], writes=[state])
                                k.op("dve", lambda e: e.tensor_tensor(out=state.t[:], in0=state.t[:], in1=p_s.t[:], op=ALU.add),
                                     reads=[state, p_s], writes=[state])

                        def B_dve2(n):
                            x_ = ctx(n); b3 = x_["b3"]; b2 = x_["b2"]; g4 = x_["g4"]
                            k.op("dve", lambda e: e.tensor_tensor(
                                out=tD[b2].t[:], in0=xbtm[b3].t[:, 0:256].rearrange("p (r d) -> p r d", r=4),
                                in1=dsk.t[:, g4, None].to_broadcast([128, 4, 64]), op=ALU.mult),
                                reads=[xbtm[b3], dsk], writes=[tD[b2]])

                        def C_act(n):
                            x_ = ctx(n); b3 = x_["b3"]
                            k.op("act", lambda e: e.copy(out=state_bf[b3].t[:], in_=state.t[:]),
                                 reads=[state], writes=[state_bf[b3]])

                        def C_pe(n):
                            x_ = ctx(n); b3 = x_["b3"]; b2 = x_["b2"]; xo = x_["xo"]; qs = x_["qs"]; c = x_["c"]
                            k.ops("pe", [(lambda e, r=r: e.matmul(p_y.t[:, r * 64:(r + 1) * 64], lhsT=MT[b2].t[:, r, :],
                                                                  rhs=xd[b3].t[:, r, :], start=True, stop=True))
                                         for r in range(4)], reads=[MT[b2], xd[b3]], writes=[p_y])
                            st_prev = zstate if c == 0 else state_bf[(n - 1) % 3]
                            k.op("pe", lambda e: e.matmul(p_yo.t[:], lhsT=xo.t[:, 3, qs], rhs=st_prev.t[:],
                                                          start=True, stop=True), reads=[xo, st_prev], writes=[p_yo])

                        def C_dve(n):
                            x_ = ctx(n); b3 = x_["b3"]; b2 = x_["b2"]; c = x_["c"]; g4 = x_["g4"]
                            k.op("dve", lambda e: e.tensor_tensor(
                                out=y1[b2].t[:], in0=p_yo.t[:].rearrange("p (r d) -> p r d", r=4),
                                in1=e_all.t[:, c, 0, g4, None].to_broadcast([128, 4, 64]), op=ALU.mult),
                                reads=[p_yo, e_all], writes=[y1[b2]])
                            k.op("dve", lambda e: e.tensor_tensor(
                                out=y1[b2].t[:], in0=y1[b2].t[:],
                                in1=p_y.t[:].rearrange("p (r d) -> p r d", r=4), op=ALU.add),
                                reads=[y1[b2], p_y], writes=[y1[b2]])
                            k.op("dve", lambda e: e.tensor_tensor(out=y1[b2].t[:], in0=y1[b2].t[:], in1=tD[b2].t[:], op=ALU.add),
                                 reads=[y1[b2], tD[b2]], writes=[y1[b2]])
                            k.op("dve", lambda e: e.tensor_tensor(
                                out=y4[b3].t[:], in0=y1[b2].t[:].rearrange("p r d -> p (r d)"), in1=sz[b3].t[:], op=ALU.mult),
                                reads=[y1[b2], sz[b3]], writes=[y4[b3]])

                        def D_act(n):
                            x_ = ctx(n); b3 = x_["b3"]; b2 = x_["b2"]
                            k.op("act", lambda e: e.activation(out=junk2.t[:], in_=y4[b3].t[:], func=AF.Square,
                                                               accum_out=ss2[b2].t[:, 0:1]),
                                 reads=[y4[b3]], writes=[junk2, ss2[b2]])

                        def D_pool(n):
                            x_ = ctx(n); b2 = x_["b2"]
                            k.op("pool", lambda e: e.tensor_scalar(out=rs2[b2].t[:], in0=ss2[b2].t[:], scalar1=1.0 / 256, scalar2=EPS,
                                                                   op0=ALU.mult, op1=ALU.add), reads=[ss2[b2]], writes=[rs2[b2]])
                            k.op("pool", lambda e: e.tensor_tensor(out=rs2[b2].t[:], in0=rs2[b2].t[:], in1=neghalf.t[:], op=ALU.pow),
                                 reads=[rs2[b2], neghalf], writes=[rs2[b2]])

                        def E_dve(n):
                            x_ = ctx(n); b3 = x_["b3"]; b2 = x_["b2"]
                            k.op("dve", lambda e: e.tensor_scalar(out=yb[b2].t[:], in0=y4[b3].t[:], scalar1=rs2[b2].t[:, 0:1],
                                                                  scalar2=None, op0=ALU.mult),
                                 reads=[y4[b3], rs2[b2]], writes=[yb[b2]])

                        def F_pe(n):
                            x_ = ctx(n); b2 = x_["b2"]
                            k.ops("pe", [(lambda e, h=h: e.transpose(out=p_tpy.t[:, h * 128:(h + 1) * 128],
                                                                     in_=yb[b2].t[:, h * 128:(h + 1) * 128], identity=ident_bf.t[:]))
                                         for h in range(2)], reads=[yb[b2], ident_bf], writes=[p_tpy])

                        def F_act(n):
                            x_ = ctx(n); g = x_["g"]; qs = x_["qs"]; si = x_["si"]; q = x_["q"]; sc = x_["sc"]
                            yo = ynT[si % 2]
                            for h in range(2):
                                k.op("act", lambda e: e.activation(out=yo.t[:, h, qs], in_=p_tpy.t[:, h * 128:(h + 1) * 128], func=AF.Copy,
                                                                   scale=gssdT.t[:, 2 * g + h:2 * g + h + 1]),
                                     reads=[p_tpy, gssdT], writes=[yo])
                            if q == 3:
                                ts = sc * 512
                                k.dma("sp", ysem[si % 2], lambda e: e.dma_start(
                                    out=ymT_d[2 * g:2 * g + 2, :, ts:ts + 512].rearrange("f p t -> p f t"), in_=yo.t[:]),
                                    reads=[yo])

                        def ok(n):
                            return 0 <= n < NT

                        import os as _os
                        if _os.environ.get("A1_NOSKEW"):
                            for n in range(NT):
                                g, c, sc, q = dec(n)
                                if c == 0:
                                    group_setup(g)
                                if q == 0:
                                    S0a(g * NSC + sc)
                                    S0b(g * NSC + sc)
                                for fn in (A_pe, A_act, A_dve, A_pool, B_pe, B_act, B_dve, B_dve2, C_pe, C_act, C_dve, D_act, D_pool, E_dve, F_pe, F_act):
                                    fn(n)
                        else:
                            LAGS = dict(A=0, B=1, C=2, D=3, E=4, F=5)
                            group_setup(0)
                            S0a(0)
                            S0b(0)
                            S0a(1)
                            for t in range(NT + 5):
                                for fn, lag in ((F_pe, 5), (C_pe, 2), (B_pe, 1), (A_pe, 0),
                                                (F_act, 5), (C_act, 2), (B_act, 1), (A_act, 0), (D_act, 3),
                                                (E_dve, 4), (B_dve, 1), (B_dve2, 1), (C_dve, 2), (A_dve, 0),
                                                (D_pool, 3), (A_pool, 0)):
                                    if ok(t - lag):
                                        fn(t - lag)
                                if t % 4 == 1 and t // 4 + 1 < 8 * NSC:
                                    S0b(t // 4 + 1)
                                if t % 4 == 2 and t // 4 + 2 < 8 * NSC:
                                    S0a(t // 4 + 2)
                                if t % NCH == 4 and t // NCH + 1 < 8:
                                    group_setup(t // NCH + 1)
                k.barrier()
                if "A1p" in phases:
                    with ExitStack() as e2:
                        wp = sb(e2, "wp", [128, 8, 1024], BF16)
                        k.dma("pool", sem_cp, lambda e: e.dma_start(
                            out=wp.t[:], in_=w_in_d[:, 6176:7200].rearrange("(k p) n -> p k n", p=128)), writes=[wp])
                        pw = sb(e2, "pw", [128, 4, 2, 256], BF16)
                        k.dma("pool", sem_cp, lambda e: e.dma_start(
                            out=pw.t[:], in_=pool_w_d.rearrange("g (k p) n -> p g k n", p=128)), writes=[pw])
                        pst = [sb(e2, "pst%d" % i, [128, 8, 527], F32) for i in range(2)]
                        sA = sb(e2, "sA", [128, 2, 527], F32)
                        sB = sb(e2, "sB", [128, 2, 527], F32)
                        ypl = [sb(e2, "ypl%d" % i, [128, 2, 512], BF16) for i in range(2)]
                        tmp16 = sb(e2, "tmp16", [128, 2, 16], F32)
                        ypT = [sb(e2, "ypT%d" % i, [128, 2, 512], BF16) for i in range(2)]
                        psem_ = [dsem(), dsem()]
                        p_u2 = [ps(e2, "p_u2_%d" % i, [128, 512], F32) for i in range(2)]
                        p_p = [ps(e2, "p_p%d" % i, [128, 512], F32) for i in range(2)]
                        k.op("pool", lambda e: e.memset(pst[0].t[:, :, 0:15], 0.0), writes=[pst[0]])
                        it = 0
                        for sc in range(NSC):
                            ts = sc * 512
                            cur = pst[sc % 2]
                            nxt = pst[(sc + 1) % 2]
                            for pc in range(8):
                                pu = p_u2[pc % 2]
                                k.ops("pe", [(lambda e, j=j: e.matmul(pu.t[:], lhsT=wp.t[:, j, pc * 128:(pc + 1) * 128],
                                                                      rhs=hT.t[:, j, ts:ts + 512], start=(j == 0), stop=(j == 7)))
                                             for j in range(8)], reads=[wp, hT], writes=[pu])
                                k.op("act", lambda e: e.copy(out=cur.t[:, pc, 15:527], in_=pu.t[:]), reads=[pu], writes=[cur])
                            if sc + 1 < NSC:
                                k.op("pool", lambda e: e.tensor_copy(out=nxt.t[:, :, 0:15], in_=cur.t[:, :, 512:527]),
                                     reads=[cur], writes=[nxt])
                            for pg in range(4):
                                u = cur.t[:, 2 * pg:2 * pg + 2, :]
                                nlev = pg + 1
                                src = u
                                bufs = [sA, sB]
                                eng = "dve"
                                for lv in range(nlev):
                                    sh = 1 << lv
                                    lo = (1 << (lv + 1)) - 1
                                    dst = bufs[lv % 2]
                                    src_t = cur if lv == 0 else bufs[(lv - 1) % 2]
                                    s_ap = src
                                    k.op(eng, lambda e, dst=dst, s_ap=s_ap, lo=lo, sh=sh: e.tensor_tensor(
                                        out=dst.t[:, :, lo:527], in0=s_ap[:, :, lo:527], in1=s_ap[:, :, lo - sh:527 - sh], op=ALU.add),
                                        reads=[src_t], writes=[dst])
                                    src = dst.t[:, :, :]
                                fin = bufs[(nlev - 1) % 2]
                                wv = 1 << nlev
                                yp = ypl[it % 2]
                                k.op(eng, lambda e: e.scalar_tensor_tensor(
                                    out=yp.t[:], in0=fin.t[:, :, 15:527], scalar=1.0 / wv, in1=u[:, :, 15:527],
                                    op0=ALU.mult, op1=ALU.subtract), reads=[fin, cur], writes=[yp])
                                if sc == 0:
                                    k.op(eng, lambda e: e.tensor_tensor(
                                        out=tmp16.t[:], in0=fin.t[:, :, 15:31],
                                        in1=invc.t[:, pg, None, :].to_broadcast([128, 2, 16]), op=ALU.mult),
                                        reads=[fin, invc], writes=[tmp16])
                                    k.op(eng, lambda e: e.tensor_tensor(out=yp.t[:, :, 0:16], in0=tmp16.t[:], in1=u[:, :, 15:31],
                                                                        op=ALU.subtract), reads=[tmp16, cur], writes=[yp])
                                yT_ = ypT[it % 2]
                                for dc in range(2):
                                    pp = p_p[dc]
                                    k.ops("pe", [(lambda e, kc=kc: e.matmul(pp.t[:], lhsT=pw.t[:, pg, kc, dc * 128:(dc + 1) * 128],
                                                                            rhs=yp.t[:, kc, :], start=(kc == 0), stop=(kc == 1)))
                                                 for kc in range(2)], reads=[pw, yp], writes=[pp])
                                    k.op("act", lambda e: e.activation(out=yT_.t[:, dc, :], in_=pp.t[:], func=AF.Copy,
                                                                       scale=pscT.t[:, 2 * pg + dc:2 * pg + dc + 1]),
                                         reads=[pp, pscT], writes=[yT_])
                                k.dma("sp", psem_[it % 2], lambda e: e.dma_start(
                                    out=ymT_d[16 + 2 * pg:16 + 2 * pg + 2, :, ts:ts + 512].rearrange("f p t -> p f t"), in_=yT_.t[:]),
                                    reads=[yT_])
                                it += 1

        k.barrier()
        if "A2" in phases:
            with ExitStack() as e3:
                wout = sb(e3, "wout", [128, 24, DM], BF16)
                k.dma("pool", sem_cp, [(lambda e, i=i: e.dma_start(
                    out=wout.t[:, 6 * i:6 * i + 6, :],
                    in_=w_out_d[768 * i:768 * (i + 1), :].rearrange("(k p) n -> p k n", p=128))) for i in range(4)],
                    writes=[wout])
                gffn_bc = load_const(e3, "gffn_bc", [128, DM], gffn_bc_d)
                rw = load_const(e3, "rw", [128, 8, NE], rw_d.rearrange("(k p) n -> p k n", p=128))
                rb = load_const(e3, "rb", [128, NE], rb_d)
                ebase = load_const(e3, "ebase", [128, NE], ebase_d)
                s4096 = sb(e3, "s4096", [128, 514], I32)
                k.op("pool", lambda e: e.memset(s4096.t[:], L), writes=[s4096])
                ev_init = [
                    k.dma("sp", sem_c, lambda e: e.dma_start(out=slot_d.rearrange("(p f) o -> p (f o)", p=128), in_=s4096.t[:]),
                          reads=[s4096]),
                    k.dma("sp", sem_c, lambda e: e.dma_start(out=h2_d[L:L + 1, :], in_=zrow_bf.t[:]), reads=[zrow_bf]),
                    k.dma("sp", sem_c, lambda e: e.dma_start(out=G_d[L:L + 1, :], in_=zrow.t[:, 0:NE]), reads=[zrow]),
                    k.dma("sp", sem_c, lambda e: e.dma_start(out=Y_d[0:1, :], in_=zrow.t[:]), reads=[zrow]),
                ]
                ym = [sb(e3, "ym%d" % i, [128, 24, 512], BF16) for i in range(2)]
                ymsem = [dsem(), dsem()]
                xt2 = [sb(e3, "xt2_%d" % i, [128, DM], F32) for i in range(2)]
                xsem2 = [dsem(), dsem()]
                x1 = [sb(e3, "x1_%d" % i, [128, DM], F32) for i in range(2)]
                x1sem = [dsem(), dsem()]
                junk3_l = [sb(e3, "junk3%d" % i_, [128, DM], F32) for i_ in range(2)]
                ss3_l = [sb(e3, "ss3%d" % i_, [128, 1], F32) for i_ in range(2)]
                rs3_l = [sb(e3, "rs3%d" % i_, [128, 1], F32) for i_ in range(2)]
                h2f_l = [sb(e3, "h2f%d" % i_, [128, DM], F32) for i_ in range(2)]
                h2b = [sb(e3, "h2b%d" % i, [128, DM], BF16) for i in range(2)]
                h2sem = [dsem(), dsem()]
                h2T_l = [sb(e3, "h2T%d" % i_, [128, 8, 128], F32) for i_ in range(2)]
                lg_l = [sb(e3, "lg%d" % i_, [128, NE], F32) for i_ in range(2)]
                m8_l = [sb(e3, "m8%d" % i_, [128, 8], F32) for i_ in range(2)]
                nv1_l = [sb(e3, "nv1%d" % i_, [128, 1], F32) for i_ in range(2)]
                mask_l = [sb(e3, "mask%d" % i_, [128, NE], F32) for i_ in range(2)]
                ex_l = [sb(e3, "ex%d" % i_, [128, NE], F32) for i_ in range(2)]
                sm_l = [sb(e3, "sm%d" % i_, [128, 1], F32) for i_ in range(2)]
                Gt = [sb(e3, "Gt%d" % i, [128, NE], F32) for i in range(2)]
                gsem = [dsem(), dsem()]
                cnt = sb(e3, "cnt", [128, NE], F32)
                rank_l = [sb(e3, "rank%d" % i_, [128, NE], F32) for i_ in range(2)]
                vld_l = [sb(e3, "vld%d" % i_, [128, NE], F32) for i_ in range(2)]
                val_l = [sb(e3, "val%d" % i_, [128, NE], F32) for i_ in range(2)]
                v8_l = [sb(e3, "v8%d" % i_, [128, 8], F32) for i_ in range(2)]
                scsem = dsem()
                p_o_l = [ps(e3, "p_o%d" % i_, [128, DM], F32) for i_ in range(2)]
                p_tf = ps(e3, "p_tf", [128, 8, 128], F32)
                p_l = ps(e3, "p_l", [128, NE], F32)
                p_r = ps(e3, "p_r", [128, 2, NE], F32)
                k.op("pool", lambda e: e.memset(cnt.t[:], 0.0), writes=[cnt])
                scat_evs = []
                def ym_load(sc):
                    ts = sc * 512
                    ymc = ym[sc % 2]
                    k.dma("sp", ymsem[sc % 2], [(lambda e, i=i: e.dma_start(
                        out=ymc.t[:, 6 * i:6 * i + 6, :],
                        in_=ymT_d[6 * i:6 * i + 6, :, ts:ts + 512].rearrange("f p t -> p f t"))) for i in range(4)],
                        writes=[ymc])

                def a2body(sc, q):
                    ts = sc * 512
                    ymc = ym[sc % 2]
                    if q == 1 and sc + 1 < NSC:
                        ym_load(sc + 1)
                    c = sc * 4 + q
                    b = c % 2
                    qs = slice(q * 128, (q + 1) * 128)
                    cs = slice(c * 128, (c + 1) * 128)
                    p_o = p_o_l[b]
                    junk3 = junk3_l[b]; ss3 = ss3_l[b]; rs3 = rs3_l[b]; h2f = h2f_l[b]; h2T = h2T_l[b]; lg = lg_l[b]; m8 = m8_l[b]; nv1 = nv1_l[b]; mask = mask_l[b]; ex = ex_l[b]; sm = sm_l[b]; rank = rank_l[b]; vld = vld_l[b]; val = val_l[b]; v8 = v8_l[b]
                    yield
                    k.dma("sp", xsem2[b], lambda e: e.dma_start(out=xt2[b].t[:], in_=x_d[cs, :]), writes=[xt2[b]])
                    yield
                    k.ops("pe", [(lambda e, fc=fc, h=h: e.matmul(p_o.t[:, h * 512:(h + 1) * 512], lhsT=ymc.t[:, fc, qs],
                                                                 rhs=wout.t[:, fc, h * 512:(h + 1) * 512],
                                                                 start=(fc == 0), stop=(fc == 23)))
                                 for h in range(2) for fc in range(24)], reads=[ymc, wout], writes=[p_o])
                    yield
                    k.op("dve", lambda e: e.tensor_tensor(out=x1[b].t[:], in0=p_o.t[:], in1=xt2[b].t[:], op=ALU.add),
                         reads=[p_o, xt2[b]], writes=[x1[b]])
                    yield
                    k.dma("sp", x1sem[b], lambda e: e.dma_start(out=x1_d[cs, :], in_=x1[b].t[:]), reads=[x1[b]])
                    yield
                    k.op("act", lambda e: e.activation(out=junk3.t[:], in_=x1[b].t[:], func=AF.Square, accum_out=ss3.t[:, 0:1]),
                         reads=[x1[b]], writes=[junk3, ss3])
                    yield
                    k.op("act", lambda e: e.activation(out=rs3.t[:], in_=ss3.t[:], func=AF.Sqrt, scale=1.0 / DM, bias=EPS),
                         reads=[ss3], writes=[rs3])
                    yield
                    k.op("dve", lambda e: e.reciprocal(out=rs3.t[:], in_=rs3.t[:]), reads=[rs3], writes=[rs3])
                    yield
                    k.op("dve", lambda e: e.scalar_tensor_tensor(out=h2f.t[:], in0=x1[b].t[:], scalar=rs3.t[:, 0:1],
                                                                 in1=gffn_bc.t[:], op0=ALU.mult, op1=ALU.mult),
                         reads=[x1[b], rs3, gffn_bc], writes=[h2f])
                    yield
                    k.op("act", lambda e: e.copy(out=h2b[b].t[:], in_=h2f.t[:]), reads=[h2f], writes=[h2b[b]])
                    yield
                    k.dma("sp", h2sem[b], lambda e: e.dma_start(out=h2_d[cs, :], in_=h2b[b].t[:]), reads=[h2b[b]])
                    yield
                    k.ops("pe", [(lambda e, j=j: e.transpose(out=p_tf.t[:, j, :], in_=h2f.t[:, j * 128:(j + 1) * 128],
                                                             identity=ident_f.t[:])) for j in range(8)],
                          reads=[h2f, ident_f], writes=[p_tf])
                    yield
                    k.op("act", lambda e: e.copy(out=h2T.t[:], in_=p_tf.t[:]), reads=[p_tf], writes=[h2T])
                    yield
                    k.ops("pe", [(lambda e, j=j: e.matmul(p_l.t[:], lhsT=h2T.t[:, j, :], rhs=rw.t[:, j, :],
                                                          start=(j == 0), stop=(j == 7))) for j in range(8)],
                          reads=[h2T, rw], writes=[p_l])
                    yield
                    k.op("dve", lambda e: e.tensor_tensor(out=lg.t[:], in0=p_l.t[:], in1=rb.t[:], op=ALU.add),
                         reads=[p_l, rb], writes=[lg])
                    yield
                    k.op("dve", lambda e: e.max(out=m8.t[:], in_=lg.t[:]), reads=[lg], writes=[m8])
                    yield
                    k.op("dve", lambda e: e.tensor_scalar(out=mask.t[:], in0=lg.t[:], scalar1=m8.t[:, 3:4], scalar2=None,
                                                          op0=ALU.is_ge), reads=[lg, m8], writes=[mask])
                    yield
                    k.op("dve", lambda e: e.tensor_scalar(out=nv1.t[:], in0=m8.t[:, 0:1], scalar1=-1.0, scalar2=None,
                                                          op0=ALU.mult), reads=[m8], writes=[nv1])
                    yield
                    k.op("act", lambda e: e.activation(out=ex.t[:], in_=lg.t[:], func=AF.Exp, bias=nv1.t[:, 0:1]),
                         reads=[lg, nv1], writes=[ex])
                    yield
                    k.op("dve", lambda e: e.tensor_tensor(out=ex.t[:], in0=ex.t[:], in1=mask.t[:], op=ALU.mult),
                         reads=[ex, mask], writes=[ex])
                    yield
                    k.op("dve", lambda e: e.reduce_sum(out=sm.t[:], in_=ex.t[:], axis=AX.X), reads=[ex], writes=[sm])
                    yield
                    k.op("dve", lambda e: e.reciprocal(out=sm.t[:], in_=sm.t[:]), reads=[sm], writes=[sm])
                    yield
                    k.op("dve", lambda e: e.tensor_scalar(out=Gt[b].t[:], in0=ex.t[:], scalar1=sm.t[:, 0:1], scalar2=None,
                                                          op0=ALU.mult), reads=[ex, sm], writes=[Gt[b]])
                    yield
                    k.dma("sp", gsem[b], lambda e: e.dma_start(out=G_d[cs, :], in_=Gt[b].t[:]), reads=[Gt[b]])
                    yield
                    k.ops("pe", [
                        lambda e: e.matmul(p_r.t[:, 0, :], lhsT=Mlt.t[:], rhs=mask.t[:], start=True, stop=True),
                        lambda e: e.matmul(p_r.t[:, 1, :], lhsT=ones_f.t[:], rhs=mask.t[:], start=True, stop=True),
                    ], reads=[Mlt, ones_f, mask], writes=[p_r])
                    yield
                    k.op("dve", lambda e: e.tensor_tensor(out=rank.t[:], in0=p_r.t[:, 0, :], in1=cnt.t[:], op=ALU.add),
                         reads=[p_r, cnt], writes=[rank])
                    yield
                    k.op("dve", lambda e: e.tensor_tensor(out=cnt.t[:], in0=p_r.t[:, 1, :], in1=cnt.t[:], op=ALU.add),
                         reads=[p_r, cnt], writes=[cnt])
                    yield
                    k.op("dve", lambda e: e.tensor_scalar(out=vld.t[:], in0=rank.t[:], scalar1=float(CAP), scalar2=None,
                                                          op0=ALU.is_lt), reads=[rank], writes=[vld])
                    yield
                    k.op("dve", lambda e: e.tensor_tensor(out=vld.t[:], in0=vld.t[:], in1=mask.t[:], op=ALU.mult),
                         reads=[vld, mask], writes=[vld])
                    yield
                    k.op("dve", lambda e: e.tensor_tensor(out=val.t[:], in0=rank.t[:], in1=ebase.t[:], op=ALU.add),
                         reads=[rank, ebase], writes=[val])
                    yield
                    k.op("dve", lambda e: e.tensor_tensor(out=val.t[:], in0=val.t[:], in1=vld.t[:], op=ALU.mult),
                         reads=[val, vld], writes=[val])
                    yield
                    k.op("dve", lambda e: e.max(out=v8.t[:], in_=val.t[:]), reads=[val], writes=[v8])
                    yield
                    k.op("dve", lambda e: e.tensor_copy(out=dest_all.t[:, c, :], in_=v8.t[:, 0:4]), reads=[v8], writes=[dest_all])
                    yield
                    for kk in range(4):
                        scat_evs.append(k.dma("pool", scsem, lambda e: e.indirect_dma_start(
                            out=slot_d[:, :], out_offset=bass.IndirectOffsetOnAxis(ap=dest_all.t[:, c, kk:kk + 1], axis=0),
                            in_=tokid.t[:, c, :], in_offset=None),
                            reads=[dest_all, tokid], extra=ev_init))
                    yield
                ym_load(0)
                run_chains([a2body(sc, q) for sc in range(NSC) for q in range(4)], lag=8)
                a2_done = [x1[0], x1[1], h2b[0], h2b[1], Gt[0], Gt[1]]
                a2_evs = list(scat_evs[-1:])
                for t in a2_done:
                    a2_evs += t.r.rs
        else:
            a2_evs = []

        k.barrier()
        if "B" in phases:
            with ExitStack() as e4:
                bguT = load_const(e4, "bguT", [128, NE, 16], bguT_d)
                wgu = [sb(e4, "wgu%d" % i, [128, 8, 2048], BF16) for i in range(2)]
                wdn = [sb(e4, "wdn%d" % i, [128, 8, DM], BF16) for i in range(2)]
                bdb = [sb(e4, "bdb%d" % i, [128, DM], F32) for i in range(2)]
                wesem = [dsem(), dsem()]
                bdsem = [dsem(), dsem()]
                idx = [sb(e4, "idx%d" % i, [128, NBLK, 2], I32) for i in range(2)]
                isem = [dsem(), dsem()]
                xg = [sb(e4, "xg%d" % i, [128, DM], BF16) for i in range(NBLK)]
                xgsem = [dsem() for _ in range(NBLK)]
                gg = [sb(e4, "gg%d" % i, [128, NBLK, NE], F32) for i in range(2)]
                ggsem = [dsem(), dsem()]
                xgT = sb(e4, "xgT", [128, 8, CAP], BF16)
                actT = sb(e4, "actT", [128, 8, CAP], BF16)
                HW = CAP // 2
                gm = [sb(e4, "gm%d" % i, [128, HW], F32) for i in range(2)]
                sg = [sb(e4, "sg%d" % i, [128, HW], F32) for i in range(2)]
                u1 = [sb(e4, "u1_%d" % i, [128, HW], F32) for i in range(2)]
                yA = [sb(e4, "yA%d" % i, [128, DM], F32) for i in range(2)]
                yB = [sb(e4, "yB%d" % i, [128, DM], F32) for i in range(2)]
                ysem2 = [dsem(), dsem()]
                p_tg2 = [ps(e4, "p_tg%d" % i, [128, 8, 128], BF16) for i in range(2)]
                p_g = [ps(e4, "p_g%d" % i, [128, 512], F32) for i in range(2)]
                p_up = [ps(e4, "p_up%d" % i, [128, 512], F32) for i in range(2)]
                p_dh = [ps(e4, "p_dh%d" % i, [128, 512], F32) for i in range(2)]

                def load_w(e_):
                    s = e_ % 2
                    k.dma("pool", wesem[s],
                          [(lambda e, i=i: e.dma_start(out=wgu[s].t[:, 2 * i:2 * i + 2, :],
                                                       in_=wgu_d[e_, 256 * i:256 * (i + 1), :].rearrange("(k p) n -> p k n", p=128)))
                           for i in range(4)] +
                          [(lambda e, i=i: e.dma_start(out=wdn[s].t[:, 4 * i:4 * i + 4, :],
                                                       in_=wd_d[e_, 512 * i:512 * (i + 1), :].rearrange("(k p) n -> p k n", p=128)))
                           for i in range(2)],
                          writes=[wgu[s], wdn[s]])
                    k.dma("sp", bdsem[s], lambda e: e.dma_start(out=bdb[s].t[:], in_=bd_d[e_:e_ + 1, :].to_broadcast([128, DM])),
                          writes=[bdb[s]])

                def prefetch(e_):
                    s_ = e_ % 2
                    base_ = 1 + e_ * CAP
                    k.dma("sp", isem[s_], [(lambda e, j=j: e.dma_start(out=idx[s_].t[:, j, :],
                                                                       in_=slot_d[base_ + j * 128:base_ + (j + 1) * 128, :]))
                                           for j in range(NBLK)], writes=[idx[s_]], extra=a2_evs)
                    for j in range(NBLK):
                        k.dma("pool", xgsem[j], lambda e: e.indirect_dma_start(
                            out=xg[j].t[:, :], out_offset=None, in_=h2_d[:, :],
                            in_offset=bass.IndirectOffsetOnAxis(ap=idx[s_].t[:, j, 0:1], axis=0)),
                            reads=[idx[s_]], writes=[xg[j]], extra=a2_evs)
                    k.dma("pool", ggsem[s_], [(lambda e, j=j: e.indirect_dma_start(
                        out=gg[s_].t[:, j, :], out_offset=None, in_=G_d[:, :],
                        in_offset=bass.IndirectOffsetOnAxis(ap=idx[s_].t[:, j, 0:1], axis=0))) for j in range(NBLK)],
                        reads=[idx[s_]], writes=[gg[s_]], extra=a2_evs)

                def transposes():
                    for j in range(NBLK):
                        p_tg = p_tg2[j % 2]
                        k.ops("pe", [(lambda e, kk=kk: e.transpose(out=p_tg.t[:, kk, :], in_=xg[j].t[:, kk * 128:(kk + 1) * 128],
                                                                   identity=ident_bf.t[:])) for kk in range(8)],
                              reads=[xg[j], ident_bf], writes=[p_tg])
                        k.op("act", lambda e: e.copy(out=xgT.t[:, :, j * 128:(j + 1) * 128], in_=p_tg.t[:]),
                             reads=[p_tg], writes=[xgT])

                load_w(0)
                prefetch(0)
                transposes()
                for e_ in range(NE):
                    s = e_ % 2
                    base = 1 + e_ * CAP
                    if e_ + 1 < NE:
                        prefetch(e_ + 1)
                        load_w(e_ + 1)
                    for fc in range(8):
                        for h in range(2):
                            hs = slice(h * HW, (h + 1) * HW)
                            k.ops("pe", [(lambda e, kk=kk: e.matmul(p_g[h].t[:, 0:HW], lhsT=wgu[s].t[:, kk, fc * 128:(fc + 1) * 128],
                                                                    rhs=xgT.t[:, kk, hs], start=(kk == 0), stop=(kk == 7)))
                                         for kk in range(8)], reads=[wgu[s], xgT], writes=[p_g[h]])
                            k.ops("pe", [(lambda e, kk=kk: e.matmul(p_up[h].t[:, 0:HW],
                                                                    lhsT=wgu[s].t[:, kk, 1024 + fc * 128:1024 + (fc + 1) * 128],
                                                                    rhs=xgT.t[:, kk, hs], start=(kk == 0), stop=(kk == 7)))
                                         for kk in range(8)], reads=[wgu[s], xgT], writes=[p_up[h]])
                            k.op("dve", lambda e: e.tensor_scalar(out=gm[h].t[:], in0=p_g[h].t[:, 0:HW],
                                                                  scalar1=bguT.t[:, e_, fc:fc + 1], scalar2=7.0,
                                                                  op0=ALU.add, op1=ALU.min), reads=[p_g[h], bguT], writes=[gm[h]])
                            k.op("act", lambda e: e.activation(out=sg[h].t[:], in_=gm[h].t[:], func=AF.Sigmoid, scale=1.702),
                                 reads=[gm[h]], writes=[sg[h]])
                            k.op("dve", lambda e: e.tensor_scalar(out=u1[h].t[:], in0=p_up[h].t[:, 0:HW],
                                                                  scalar1=bguT.t[:, e_, 8 + fc:9 + fc], scalar2=7.0,
                                                                  op0=ALU.add, op1=ALU.min), reads=[p_up[h], bguT], writes=[u1[h]])
                            k.op("dve", lambda e: e.tensor_scalar(out=u1[h].t[:], in0=u1[h].t[:], scalar1=-7.0, scalar2=1.0,
                                                                  op0=ALU.max, op1=ALU.add), reads=[u1[h]], writes=[u1[h]])
                            k.op("dve", lambda e: e.tensor_tensor(out=sg[h].t[:], in0=gm[h].t[:], in1=sg[h].t[:], op=ALU.mult),
                                 reads=[gm[h], sg[h]], writes=[sg[h]])
                            k.op("dve", lambda e: e.tensor_tensor(out=actT.t[:, fc, hs], in0=sg[h].t[:], in1=u1[h].t[:], op=ALU.mult),
                                 reads=[sg[h], u1[h]], writes=[actT])
                    if e_ + 1 < NE:
                        transposes()
                    for j in range(NBLK):
                        js = slice(j * 128, (j + 1) * 128)
                        b = j % 2
                        for h in range(2):
                            k.ops("pe", [(lambda e, kk=kk: e.matmul(p_dh[h].t[:], lhsT=actT.t[:, kk, js],
                                                                    rhs=wdn[s].t[:, kk, h * 512:(h + 1) * 512],
                                                                    start=(kk == 0), stop=(kk == 7)))
                                         for kk in range(8)], reads=[actT, wdn[s]], writes=[p_dh[h]])
                            k.op("dve", lambda e: e.tensor_tensor(out=yA[b].t[:, h * 512:(h + 1) * 512], in0=p_dh[h].t[:],
                                                                  in1=bdb[s].t[:, h * 512:(h + 1) * 512], op=ALU.add),
                                 reads=[p_dh[h], bdb[s]], writes=[yA[b]])
                        k.op("act", lambda e: e.activation(out=yB[b].t[:], in_=yA[b].t[:], func=AF.Copy,
                                                           scale=gg[s].t[:, j, e_:e_ + 1]),
                             reads=[yA[b], gg[s]], writes=[yB[b]])
                        k.dma("sp", ysem2[b], lambda e: e.dma_start(out=Y_d[base + j * 128:base + (j + 1) * 128, :], in_=yB[b].t[:]),
                              reads=[yB[b]])
                b_evs = []
                for t in yB:
                    b_evs += t.r.rs
        else:
            b_evs = []

        k.barrier()
        fin_evs = []
        if "C" in phases:
            with ExitStack() as e5:
                wpg = sb(e5, "wpg", [128, 8, DM], BF16)
                k.dma("pool", sem_cp, [(lambda e, i=i: e.dma_start(
                    out=wpg.t[:, 4 * i:4 * i + 4, :],
                    in_=wpg_d[512 * i:512 * (i + 1), :].rearrange("(k p) n -> p k n", p=128))) for i in range(2)], writes=[wpg])
                wpp = sb(e5, "wpp", [128, 2, DM], BF16)
                k.dma("pool", sem_cp, lambda e: e.dma_start(out=wpp.t[:], in_=wpp_d.rearrange("(k p) n -> p k n", p=128)),
                      writes=[wpp])
                gpgT = load_const(e5, "gpgT", [128, 8], gpgT_d)
                gpn_bc = load_const(e5, "gpn_bc", [128, DM], gpn_bc_d)
                gfin_bc = load_const(e5, "gfin_bc", [128, DM], gfin_bc_d)
                x1c = [sb(e5, "x1c%d" % i, [128, DM], F32) for i in range(2)]
                x1csem = [dsem(), dsem()]
                yk = [[sb(e5, "yk%d_%d" % (i, kk), [128, DM], F32) for kk in range(4)] for i in range(2)]
                yksem = [[dsem() for kk in range(4)] for i in range(2)]
                pt = [sb(e5, "pt%d" % i, [128, 256], F32) for i in range(2)]
                ptsem = [dsem(), dsem()]
                x2_l = [sb(e5, "x2%d" % i_, [128, DM], F32) for i_ in range(2)]
                junk4_l = [sb(e5, "junk4%d" % i_, [128, DM], F32) for i_ in range(2)]
                ssc_l = [sb(e5, "ssc%d" % i_, [128, 1], F32) for i_ in range(2)]
                rsc_l = [sb(e5, "rsc%d" % i_, [128, 1], F32) for i_ in range(2)]
                xnb_l = [sb(e5, "xnb%d" % i_, [128, DM], BF16) for i_ in range(2)]
                xnT_l = [sb(e5, "xnT%d" % i_, [128, 8, 128], BF16) for i_ in range(2)]
                sgate_l = [sb(e5, "sgate%d" % i_, [128, DM], F32) for i_ in range(2)]
                pT_l = [sb(e5, "pT%d" % i_, [128, 2, 128], BF16) for i_ in range(2)]
                sse_l = [sb(e5, "sse%d" % i_, [128, 1], F32) for i_ in range(2)]
                rse_l = [sb(e5, "rse%d" % i_, [128, 1], F32) for i_ in range(2)]
                e1__l = [sb(e5, "e1_%d" % i_, [128, DM], F32) for i_ in range(2)]
                x3_l = [sb(e5, "x3%d" % i_, [128, DM], F32) for i_ in range(2)]
                ssf_l = [sb(e5, "ssf%d" % i_, [128, 1], F32) for i_ in range(2)]
                rsf_l = [sb(e5, "rsf%d" % i_, [128, 1], F32) for i_ in range(2)]
                ot = [sb(e5, "ot%d" % i, [128, DM], F32) for i in range(2)]
                osem = [dsem(), dsem()]
                p_t3 = ps(e5, "p_t3", [128, 8, 128], BF16)
                p_ga = ps(e5, "p_ga", [128, DM], F32)
                p_pt = ps(e5, "p_pt", [128, 2, 128], F32)
                p_e = ps(e5, "p_e", [128, DM], F32)
                def cbody(c):
                    b = c % 2
                    cs = slice(c * 128, (c + 1) * 128)
                    x2 = x2_l[b]; junk4 = junk4_l[b]; ssc = ssc_l[b]; rsc = rsc_l[b]; xnb = xnb_l[b]; xnT = xnT_l[b]; sgate = sgate_l[b]; pT = pT_l[b]; sse = sse_l[b]; rse = rse_l[b]; e1_ = e1__l[b]; x3 = x3_l[b]; ssf = ssf_l[b]; rsf = rsf_l[b]
                    yield
                    k.dma("sp", x1csem[b], lambda e: e.dma_start(out=x1c[b].t[:], in_=x1_d[cs, :]), writes=[x1c[b]], extra=a2_evs)
                    yield
                    k.dma("sp", ptsem[b], lambda e: e.dma_start(out=pt[b].t[:], in_=pin_d[cs, :]), writes=[pt[b]])
                    yield
                    for kk in range(4):
                        k.dma("pool", yksem[b][kk], lambda e: e.indirect_dma_start(
                            out=yk[b][kk].t[:, :], out_offset=None, in_=Y_d[:, :],
                            in_offset=bass.IndirectOffsetOnAxis(ap=dest_all.t[:, c, kk:kk + 1], axis=0)),
                            reads=[dest_all], writes=[yk[b][kk]], extra=b_evs)
                    yield
                    k.op("dve", lambda e: e.tensor_tensor(out=x2.t[:], in0=x1c[b].t[:], in1=yk[b][0].t[:], op=ALU.add),
                         reads=[x1c[b], yk[b][0]], writes=[x2])
                    yield
                    k.op("pool", lambda e: e.tensor_tensor(out=yk[b][1].t[:], in0=yk[b][1].t[:], in1=yk[b][2].t[:], op=ALU.add),
                         reads=[yk[b][1], yk[b][2]], writes=[yk[b][1]])
                    yield
                    k.op("dve", lambda e: e.tensor_tensor(out=x2.t[:], in0=x2.t[:], in1=yk[b][3].t[:], op=ALU.add),
                         reads=[x2, yk[b][3]], writes=[x2])
                    yield
                    k.op("dve", lambda e: e.tensor_tensor(out=x2.t[:], in0=x2.t[:], in1=yk[b][1].t[:], op=ALU.add),
                         reads=[x2, yk[b][1]], writes=[x2])
                    yield
                    k.op("act", lambda e: e.activation(out=junk4.t[:], in_=x2.t[:], func=AF.Square, accum_out=ssc.t[:, 0:1]),
                         reads=[x2], writes=[junk4, ssc])
                    yield
                    k.op("act", lambda e: e.activation(out=rsc.t[:], in_=ssc.t[:], func=AF.Sqrt, scale=1.0 / DM, bias=EPS),
                         reads=[ssc], writes=[rsc])
                    yield
                    k.op("dve", lambda e: e.reciprocal(out=rsc.t[:], in_=rsc.t[:]), reads=[rsc], writes=[rsc])
                    yield
                    k.op("act", lambda e: e.activation(out=xnb.t[:], in_=x2.t[:], func=AF.Copy, scale=rsc.t[:, 0:1]),
                         reads=[x2, rsc], writes=[xnb])
                    yield
                    k.ops("pe", [(lambda e, j=j: e.transpose(out=p_t3.t[:, j, :], in_=xnb.t[:, j * 128:(j + 1) * 128],
                                                             identity=ident_bf.t[:])) for j in range(8)],
                          reads=[xnb, ident_bf], writes=[p_t3])
                    yield
                    k.op("dve", lambda e: e.tensor_tensor(out=xnT.t[:], in0=p_t3.t[:],
                                                          in1=gpgT.t[:, :, None].to_broadcast([128, 8, 128]), op=ALU.mult),
                         reads=[p_t3, gpgT], writes=[xnT])
                    yield
                    k.ops("pe", [(lambda e, j=j, h=h: e.matmul(p_ga.t[:, h * 512:(h + 1) * 512], lhsT=xnT.t[:, j, :],
                                                               rhs=wpg.t[:, j, h * 512:(h + 1) * 512], start=(j == 0), stop=(j == 7)))
                                 for h in range(2) for j in range(8)], reads=[xnT, wpg], writes=[p_ga])
                    yield
                    k.op("act", lambda e: e.activation(out=sgate.t[:], in_=p_ga.t[:], func=AF.Sigmoid), reads=[p_ga], writes=[sgate])
                    yield
                    k.ops("pe", [(lambda e, j=j: e.transpose(out=p_pt.t[:, j, :], in_=pt[b].t[:, j * 128:(j + 1) * 128],
                                                             identity=ident_f.t[:])) for j in range(2)],
                          reads=[pt[b], ident_f], writes=[p_pt])
                    yield
                    k.op("act", lambda e: e.copy(out=pT.t[:], in_=p_pt.t[:]), reads=[p_pt], writes=[pT])
                    yield
                    k.ops("pe", [(lambda e, j=j, h=h: e.matmul(p_e.t[:, h * 512:(h + 1) * 512], lhsT=pT.t[:, j, :],
                                                               rhs=wpp.t[:, j, h * 512:(h + 1) * 512], start=(j == 0), stop=(j == 1)))
                                 for h in range(2) for j in range(2)], reads=[pT, wpp], writes=[p_e])
                    yield
                    k.op("act", lambda e: e.activation(out=junk4.t[:], in_=p_e.t[:], func=AF.Square, accum_out=sse.t[:, 0:1]),
                         reads=[p_e], writes=[junk4, sse])
                    yield
                    k.op("act", lambda e: e.activation(out=rse.t[:], in_=sse.t[:], func=AF.Sqrt, scale=1.0 / DM, bias=EPS),
                         reads=[sse], writes=[rse])
                    yield
                    k.op("dve", lambda e: e.reciprocal(out=rse.t[:], in_=rse.t[:]), reads=[rse], writes=[rse])
                    yield
                    k.op("dve", lambda e: e.scalar_tensor_tensor(out=e1_.t[:], in0=p_e.t[:], scalar=rse.t[:, 0:1], in1=gpn_bc.t[:],
                                                                 op0=ALU.mult, op1=ALU.mult), reads=[p_e, rse, gpn_bc], writes=[e1_])
                    yield
                    k.op("pool", lambda e: e.tensor_tensor(out=e1_.t[:], in0=e1_.t[:], in1=sgate.t[:], op=ALU.mult),
                         reads=[e1_, sgate], writes=[e1_])
                    yield
                    k.op("dve", lambda e: e.tensor_tensor(out=x3.t[:], in0=x2.t[:], in1=e1_.t[:], op=ALU.add),
                         reads=[x2, e1_], writes=[x3])
                    yield
                    k.op("act", lambda e: e.activation(out=junk4.t[:], in_=x3.t[:], func=AF.Square, accum_out=ssf.t[:, 0:1]),
                         reads=[x3], writes=[junk4, ssf])
                    yield
                    k.op("act", lambda e: e.activation(out=rsf.t[:], in_=ssf.t[:], func=AF.Sqrt, scale=1.0 / DM, bias=EPS),
                         reads=[ssf], writes=[rsf])
                    yield
                    k.op("dve", lambda e: e.reciprocal(out=rsf.t[:], in_=rsf.t[:]), reads=[rsf], writes=[rsf])
                    yield
                    k.op("dve", lambda e: e.scalar_tensor_tensor(out=ot[b].t[:], in0=x3.t[:], scalar=rsf.t[:, 0:1], in1=gfin_bc.t[:],
                                                                 op0=ALU.mult, op1=ALU.mult), reads=[x3, rsf, gfin_bc], writes=[ot[b]])
                    yield
                    fin_evs.append(k.dma("sp", osem[b], lambda e: e.dma_start(out=out_d[cs, :], in_=ot[b].t[:]), reads=[ot[b]]))

                    yield
                run_chains([cbody(c) for c in range(NCH)], lag=6)

        tail = list(fin_evs[-2:]) + list(a2_evs) + list(b_evs)
        for sid_ev in tail:
            k.wait("sp", sid_ev)
        for sid_, sem_ in k.dma_objs.items():
            k.wait("sp", (sem_, k.dma_cnt[sid_]))
        build.stats = (k.ninst, k.nwaits, dict(k.cnt))
    return nc


def host_layout(inp):
    f = np.float32

    def colT(v, n):
        return np.ascontiguousarray(np.asarray(v, f).reshape(n, 128).T)

    def bc(v):
        v = np.asarray(v, f).reshape(1, -1)
        return np.ascontiguousarray(np.broadcast_to(v, (128, v.shape[1])))

    cw = np.asarray(inp["conv_w"][0], f)
    conv_wT = np.ascontiguousarray(cw.reshape(4, 32, 128).transpose(2, 1, 0))
    bgu = np.asarray(inp["b_gate_up"][0], f)
    bguT = np.ascontiguousarray(bgu.reshape(NE, 16, 128).transpose(2, 0, 1))
    invc = np.zeros((128, 4, 16), f)
    for gi, w in enumerate((2, 4, 8, 16)):
        invc[:, gi, :] = 1.0 / np.minimum(np.arange(16) + 1, w).astype(f)
    ebase = (1 + np.arange(NE) * CAP).astype(f)
    shared = {
        "w_in": np.ascontiguousarray(inp["w_in"][0], f),
        "conv_wT": conv_wT,
        "conv_bT": colT(inp["conv_b"][0], 32),
        "conv_brow": np.ascontiguousarray(np.asarray(inp["conv_b"][0], f).reshape(1, 4096)),
        "dt_bias_bc": bc(inp["dt_bias"][0]),
        "a_log_bc": bc(inp["a_log"][0]),
        "d_skip_bc": bc(inp["d_skip"][0]),
        "gmixT": colT(inp["mix_norm_g"][0], 8),
        "gssdT": colT(inp["ssd_norm_g"][0], 16),
        "pool_w": np.ascontiguousarray(inp["pool_w"][0], f),
        "pscT": colT(inp["pool_scale"][0], 8),
        "invc": invc,
        "w_out": np.ascontiguousarray(inp["w_out"][0], f),
        "gffn_bc": bc(inp["ffn_norm_g"][0]),
        "router_w": np.ascontiguousarray(inp["router_w"][0], f),
        "rb_bc": bc(inp["router_b"][0]),
        "ebase_bc": bc(ebase),
        "w_gate_up": np.ascontiguousarray(inp["w_gate_up"][0], f),
        "bguT": bguT,
        "w_down": np.ascontiguousarray(inp["w_down"][0], f),
        "b_down": np.ascontiguousarray(inp["b_down"][0], f),
        "gpgT": colT(inp["ple_gate_norm_g"][0], 8),
        "w_ple_gate": np.ascontiguousarray(inp["w_ple_gate"][0], f),
        "w_ple_proj": np.ascontiguousarray(inp["w_ple_proj"][0], f),
        "gpn_bc": bc(inp["ple_norm_g"][0]),
        "gfin_bc": bc(inp["final_norm_g"]),
    }
    return shared


def kernel(**inputs):
    inp = {k_: np.asarray(v) for k_, v in inputs.items()}
    shared = host_layout(inp)
    x = np.asarray(inp["x"], np.float32)
    p = np.asarray(inp["p"], np.float32)[0]
    nb = x.shape[0]
    in_maps = []
    for b in range(nb):
        m = dict(shared)
        m["x"] = np.ascontiguousarray(x[b])
        m["p"] = np.ascontiguousarray(p[b])
        in_maps.append(m)
    nc = build()
    res = run_bass_kernel_spmd(nc, in_maps, core_ids=list(range(nb)))
    return np.stack([np.asarray(r["out"], np.float32) for r in res.results], axis=0)
```

```python
from contextlib import ExitStack
import numpy as np
import concourse.bass as bass
import concourse.mybir as mybir
from concourse.bass_utils import run_bass_kernel_spmd

F32 = mybir.dt.float32
BF16 = mybir.dt.bfloat16
I32 = mybir.dt.int32
AF = mybir.ActivationFunctionType
ALU = mybir.AluOpType
AX = mybir.AxisListType

L = 4096
DM = 1024
NCH = 32
NSC = 8
D_IN = 7200
NE = 32
CAP = 1024
NBLK = CAP // 128
NSLOT = NE * CAP
SLOT_TAB = 128 * 257
EPS = 1e-6


class R:
    __slots__ = ("w", "rs")

    def __init__(self):
        self.w = None
        self.rs = []


class T:
    def __init__(self, t):
        self.t = t
        self.r = R()


class K:
    def __init__(self, nc, sems):
        self.nc = nc
        self.engs = {"pe": nc.tensor, "dve": nc.vector, "act": nc.scalar,
                     "pool": nc.gpsimd, "sp": nc.sync}
        self.psem = sems
        self.cnt = {k: 0 for k in self.engs}
        self.waited = {k: {} for k in self.engs}
        self.dma_cnt = {}
        self.dma_objs = {}
        self.ninst = 0
        self.nwaits = 0

    def wait(self, ek, ev):
        if ev is None:
            return
        sem, val = ev
        sid = id(sem)
        if sid in self.dma_cnt:
            val = max(val, self.dma_cnt[sid])
        w = self.waited[ek]
        if w.get(sid, 0) >= val:
            return
        self.engs[ek].wait_ge(sem, val)
        self.nwaits += 1
        w[sid] = val

    def _deps(self, ek, reads, writes, extra):
        for t in reads:
            self.wait(ek, t.r.w)
        for t in writes:
            self.wait(ek, t.r.w)
            for e in t.r.rs:
                self.wait(ek, e)
        for e in extra:
            self.wait(ek, e)

    def _commit(self, ev, reads, writes):
        for t in reads:
            t.r.rs.append(ev)
        for t in writes:
            t.r.w = ev
            t.r.rs = []

    def barrier(self):
        for ek in self.engs:
            for o in self.engs:
                if self.cnt[o] > 0:
                    self.wait(ek, (self.psem[o], self.cnt[o]))
            for sid_, sem_ in self.dma_objs.items():
                self.wait(ek, (sem_, self.dma_cnt[sid_]))

    def op(self, ek, fn, reads=(), writes=(), extra=()):
        self._deps(ek, reads, writes, extra)
        ins = fn(self.engs[ek])
        self.cnt[ek] += 1
        ins.then_inc(self.psem[ek], 1)
        ev = (self.psem[ek], self.cnt[ek])
        self._commit(ev, reads, writes)
        self.ninst += 1
        return ev

    def ops(self, ek, fns, reads=(), writes=(), extra=()):
        self._deps(ek, reads, writes, extra)
        ins = None
        for fn in fns:
            ins = fn(self.engs[ek])
            self.ninst += 1
        self.cnt[ek] += 1
        ins.then_inc(self.psem[ek], 1)
        ev = (self.psem[ek], self.cnt[ek])
        self._commit(ev, reads, writes)
        return ev

    def dma(self, ek, sem, fns, reads=(), writes=(), extra=()):
        self._deps(ek, reads, writes, extra)
        if not isinstance(fns, (list, tuple)):
            fns = [fns]
        sid = id(sem)
        cur = self.dma_cnt.get(sid, 0)
        for f in fns:
            f(self.engs[ek]).then_inc(sem, 16)
            cur += 16
            self.ninst += 1
        self.dma_cnt[sid] = cur
        self.dma_objs[sid] = sem
        ev = (sem, cur)
        self._commit(ev, reads, writes)
        return ev


def run_chains(gens, lag=4, width=2):
    pending = list(gens)
    active = []
    while pending or active:
        if len(active) < width and pending and (not active or active[-1][1] >= lag):
            active.append([pending.pop(0), 0])
        for a in list(active):
            try:
                next(a[0])
                a[1] += 1
            except StopIteration:
                active.remove(a)


def build(debug=None, phases="A0,A1,A1p,A2,B,C"):
    phases = set(phases.split(","))
    debug = debug or ()
    nc = bass.Bass("TRN2", target_bir_lowering=False)

    def din(name, shape, dt=F32):
        return nc.dram_tensor(name, list(shape), dt, kind="ExternalInput").ap()

    x_d = din("x", [L, DM])
    pin_d = din("p", [L, 256])
    w_in_d = din("w_in", [DM, D_IN])
    conv_wT_d = din("conv_wT", [128, 32, 4])
    conv_bT_d = din("conv_bT", [128, 32])
    conv_brow_d = din("conv_brow", [1, 4096])
    dtb_d = din("dt_bias_bc", [128, 32])
    alog_d = din("a_log_bc", [128, 32])
    dsk_d = din("d_skip_bc", [128, 32])
    gmixT_d = din("gmixT", [128, 8])
    gssdT_d = din("gssdT", [128, 16])
    pool_w_d = din("pool_w", [4, 256, 256])
    pscT_d = din("pscT", [128, 8])
    invc_d = din("invc", [128, 4, 16])
    w_out_d = din("w_out", [3072, DM])
    gffn_bc_d = din("gffn_bc", [128, DM])
    rw_d = din("router_w", [DM, NE])
    rb_d = din("rb_bc", [128, NE])
    ebase_d = din("ebase_bc", [128, NE])
    wgu_d = din("w_gate_up", [NE, DM, 2048])
    bguT_d = din("bguT", [128, NE, 16])
    wd_d = din("w_down", [NE, DM, DM])
    bd_d = din("b_down", [NE, DM])
    gpgT_d = din("gpgT", [128, 8])
    wpg_d = din("w_ple_gate", [DM, DM])
    wpp_d = din("w_ple_proj", [256, DM])
    gpn_bc_d = din("gpn_bc", [128, DM])
    gfin_bc_d = din("gfin_bc", [128, DM])
    out_d = nc.dram_tensor("out", [L, DM], F32, kind="ExternalOutput").ap()

    def dscr(name, shape, dt, dbg=False):
        kind = "ExternalOutput" if (name in debug) else "Internal"
        return nc.dram_tensor(name, list(shape), dt, kind=kind).ap()

    ymT_d = dscr("ymT", [24, 128, L], BF16)
    x1_d = dscr("x1d", [L, DM], F32)
    h2_d = dscr("h2d", [L + 1, DM], BF16)
    G_d = dscr("Gd", [L + 1, NE], F32)
    slot_d = dscr("slotd", [SLOT_TAB, 2], I32)
    Y_d = dscr("Yd", [NSLOT + 1, DM], F32)

    with ExitStack() as es:
        E = es.enter_context
        sems = {k: E(nc.semaphore("prog_" + k)) for k in ["pe", "dve", "act", "pool", "sp"]}
        k = K(nc, sems)
        nsem = [0]

        def dsem():
            nsem[0] += 1
            return E(nc.semaphore("d%d" % nsem[0]))

        def sb(es_, name, shape, dt=F32):
            return T(es_.enter_context(nc.sbuf_tensor("s_" + name, list(shape), dt)))

        def ps(es_, name, shape, dt=F32):
            esz = 4 if dt == F32 else 2
            n = 1
            for d_ in shape[1:]:
                n *= d_
            per_bank = 2048 // esz
            nb_ = (n + per_bank - 1) // per_bank
            base = es_.enter_context(nc.psum_tensor("ps_" + name, [128, nb_ * per_bank], dt))
            v = base[:, 0:n]
            if len(shape) == 3:
                v = v.rearrange("p (a b) -> p a b", a=shape[1])
            return T(v)

        def psbank(es_, name, dt=F32):
            per_bank = 2048 // (4 if dt == F32 else 2)
            return es_.enter_context(nc.psum_tensor("ps_" + name, [128, per_bank], dt))

        ident_bf = sb(es, "ident_bf", [128, 128], BF16)
        ident_f = sb(es, "ident_f", [128, 128], F32)
        Mle = sb(es, "Mle", [128, 128], F32)
        Mgt = sb(es, "Mgt", [128, 128], F32)
        Mlt = sb(es, "Mlt", [128, 128], F32)
        ones_f = sb(es, "ones_f", [128, 128], F32)
        dest_all = sb(es, "dest_all", [128, NCH, 4], I32)
        tokid = sb(es, "tokid", [128, NCH, 2], I32)
        zrow = sb(es, "zrow", [1, DM], F32)
        zrow_bf = sb(es, "zrow_bf", [1, DM], BF16)

        def dump(name, t, ap=None):
            if name not in debug:
                return
            a = ap if ap is not None else t.t[:]
            dd = nc.dram_tensor("dbg_" + name, list(a.shape), a.dtype, kind="ExternalOutput").ap()
            k.dma("sp", dsem(), lambda e: e.dma_start(out=dd, in_=a), reads=[t])

        def cst(t, fn):
            k.op("pool", fn, writes=[t])

        cst(ident_bf, lambda e: e.memset(ident_bf.t[:], 1.0))
        cst(ident_bf, lambda e: e.affine_select(out=ident_bf.t[:], in_=ident_bf.t[:], pattern=[[-1, 128]],
                                               compare_op=ALU.is_equal, fill=0.0, base=0, channel_multiplier=1))
        cst(ident_f, lambda e: e.memset(ident_f.t[:], 1.0))
        cst(ident_f, lambda e: e.affine_select(out=ident_f.t[:], in_=ident_f.t[:], pattern=[[-1, 128]],
                                              compare_op=ALU.is_equal, fill=0.0, base=0, channel_multiplier=1))
        cst(Mle, lambda e: e.memset(Mle.t[:], 1.0))
        cst(Mle, lambda e: e.affine_select(out=Mle.t[:], in_=Mle.t[:], pattern=[[1, 128]],
                                          compare_op=ALU.is_ge, fill=0.0, base=0, channel_multiplier=-1))
        cst(Mgt, lambda e: e.memset(Mgt.t[:], 1.0))
        cst(Mgt, lambda e: e.affine_select(out=Mgt.t[:], in_=Mgt.t[:], pattern=[[-1, 128]],
                                          compare_op=ALU.is_gt, fill=0.0, base=0, channel_multiplier=1))
        cst(Mlt, lambda e: e.memset(Mlt.t[:], 1.0))
        cst(Mlt, lambda e: e.affine_select(out=Mlt.t[:], in_=Mlt.t[:], pattern=[[1, 128]],
                                          compare_op=ALU.is_gt, fill=0.0, base=0, channel_multiplier=-1))
        cst(ones_f, lambda e: e.memset(ones_f.t[:], 1.0))
        cst(zrow, lambda e: e.memset(zrow.t[:], 0.0))
        cst(zrow_bf, lambda e: e.memset(zrow_bf.t[:], 0.0))
        cst(tokid, lambda e: e.iota(tokid.t[:], pattern=[[128, NCH], [0, 2]], base=0, channel_multiplier=1))
        cst(dest_all, lambda e: e.memset(dest_all.t[:], 0))

        sem_c = dsem()
        sem_cp = dsem()

        def load_const(es_, name, shape, src, dt=F32, q="sp"):
            t = sb(es_, name, shape, dt)
            k.dma(q, sem_c, lambda e: e.dma_start(out=t.t[:], in_=src), writes=[t])
            return t

        if "A0" in phases:
            with ExitStack() as esA:
                hT = sb(esA, "hT", [128, 8, L], BF16)
                dt_all = sb(esA, "dt_all", [128, NCH, 32], F32)
                a_all = sb(esA, "a_all", [128, NCH, 32], F32)
                e_all = sb(esA, "e_all", [128, NCH, 3, 32], F32)
                gmixT = load_const(esA, "gmixT", [128, 8], gmixT_d)
                gssdT = load_const(esA, "gssdT", [128, 16], gssdT_d)
                conv_wT = load_const(esA, "conv_wT", [128, 32, 4], conv_wT_d)
                conv_bT = load_const(esA, "conv_bT", [128, 32], conv_bT_d)
                dsk = load_const(esA, "dsk", [128, 32], dsk_d)
                pscT = load_const(esA, "pscT", [128, 8], pscT_d)
                invc = load_const(esA, "invc", [128, 4, 16], invc_d)

                with ExitStack() as e0:
                    dtb = load_const(e0, "dtb", [128, 32], dtb_d)
                    alog = load_const(e0, "alog", [128, 32], alog_d)
                    Abc = sb(e0, "Abc", [128, 32], F32)
                    wdt = sb(e0, "wdt", [128, 8, 32], BF16)
                    k.dma("pool", sem_cp, lambda e: e.dma_start(
                        out=wdt.t[:], in_=w_in_d[:, 6144:6176].rearrange("(k p) n -> p k n", p=128)), writes=[wdt])
                    k.op("act", lambda e: e.activation(out=Abc.t[:], in_=alog.t[:], func=AF.Exp), reads=[alog], writes=[Abc])
                    k.op("dve", lambda e: e.tensor_scalar(out=Abc.t[:], in0=Abc.t[:], scalar1=-1.0, scalar2=None, op0=ALU.mult),
                         reads=[Abc], writes=[Abc])
                    xt = [sb(e0, "xt%d" % i, [128, DM], F32) for i in range(2)]
                    xsem = [dsem(), dsem()]
                    junk = sb(e0, "junk", [128, DM], F32)
                    ss = sb(e0, "ss", [128, 1], F32)
                    rstd = sb(e0, "rstd", [128, 1], F32)
                    xn = sb(e0, "xn", [128, DM], BF16)
                    tpA = ps(e0, "tpA", [128, 8, 128], BF16)
                    pdt = ps(e0, "pdt", [128, 32], F32)
                    pcs = ps(e0, "pcs", [128, 3, 32], F32)
                    dtr = sb(e0, "dtr", [128, 32], F32)
                    t1 = sb(e0, "t1", [128, 32], F32)
                    t2 = sb(e0, "t2", [128, 32], F32)
                    for c in range(NCH):
                        xc_ = xt[c % 2]
                        cs = slice(c * 128, (c + 1) * 128)
                        k.dma("sp", xsem[c % 2], lambda e: e.dma_start(out=xc_.t[:], in_=x_d[cs, :]), writes=[xc_])
                        k.op("act", lambda e: e.activation(out=junk.t[:], in_=xc_.t[:], func=AF.Square, accum_out=ss.t[:, 0:1]),
                             reads=[xc_], writes=[junk, ss])
                        k.op("act", lambda e: e.activation(out=rstd.t[:], in_=ss.t[:], func=AF.Sqrt, scale=1.0 / DM, bias=EPS),
                             reads=[ss], writes=[rstd])
                        k.op("dve", lambda e: e.reciprocal(out=rstd.t[:], in_=rstd.t[:]), reads=[rstd], writes=[rstd])
                        k.op("act", lambda e: e.activation(out=xn.t[:], in_=xc_.t[:], func=AF.Copy, scale=rstd.t[:, 0:1]),
                             reads=[xc_, rstd], writes=[xn])
                        k.ops("pe", [(lambda e, j=j: e.transpose(out=tpA.t[:, j, :], in_=xn.t[:, j * 128:(j + 1) * 128],
                                                                 identity=ident_bf.t[:])) for j in range(8)],
                              reads=[xn, ident_bf], writes=[tpA])
                        k.op("dve", lambda e: e.tensor_tensor(out=hT.t[:, :, cs], in0=tpA.t[:],
                                                              in1=gmixT.t[:, :, None].to_broadcast([128, 8, 128]), op=ALU.mult),
                             reads=[tpA, gmixT], writes=[hT])
                        k.ops("pe", [(lambda e, j=j: e.matmul(pdt.t[:], lhsT=hT.t[:, j, cs], rhs=wdt.t[:, j, :],
                                                              start=(j == 0), stop=(j == 7))) for j in range(8)],
                              reads=[hT, wdt], writes=[pdt])
                        k.op("dve", lambda e: e.tensor_tensor(out=dtr.t[:], in0=pdt.t[:], in1=dtb.t[:], op=ALU.add),
                             reads=[pdt, dtb], writes=[dtr])
                        k.op("act", lambda e: e.activation(out=t1.t[:], in_=dtr.t[:], func=AF.Abs),
                             reads=[dtr], writes=[t1])
                        k.op("act", lambda e: e.activation(out=t2.t[:], in_=t1.t[:], func=AF.Exp, scale=-1.0), reads=[t1], writes=[t2])
                        k.op("act", lambda e: e.activation(out=t1.t[:], in_=t2.t[:], func=AF.Ln, bias=1.0), reads=[t2], writes=[t1])
                        k.op("dve", lambda e: e.scalar_tensor_tensor(out=dt_all.t[:, c, :], in0=dtr.t[:], scalar=0.0, in1=t1.t[:],
                                                                     op0=ALU.max, op1=ALU.add),
                             reads=[dtr, t1], writes=[dt_all])
                        k.op("dve", lambda e: e.tensor_tensor(out=a_all.t[:, c, :], in0=dt_all.t[:, c, :], in1=Abc.t[:], op=ALU.mult),
                             reads=[dt_all, Abc], writes=[a_all])
                        k.ops("pe", [
                            lambda e: e.matmul(pcs.t[:, 0, :], lhsT=Mle.t[:], rhs=a_all.t[:, c, :], start=True, stop=True),
                            lambda e: e.matmul(pcs.t[:, 1, :], lhsT=Mgt.t[:], rhs=a_all.t[:, c, :], start=True, stop=True),
                            lambda e: e.matmul(pcs.t[:, 2, :], lhsT=ones_f.t[:], rhs=a_all.t[:, c, :], start=True, stop=True),
                        ], reads=[a_all, Mle, Mgt, ones_f], writes=[pcs])
                        k.op("act", lambda e: e.activation(out=e_all.t[:, c, :, :], in_=pcs.t[:], func=AF.Exp),
                             reads=[pcs], writes=[e_all])

                    k.barrier()
                dump("hT", hT); dump("dt_all", dt_all); dump("a_all", a_all); dump("e_all", e_all)
                if "A1" in phases:
                    with ExitStack() as e1:
                        wg = [sb(e1, "wg%d" % i, [128, 8, 768], BF16) for i in range(2)]
                        wsem = [dsem(), dsem()]
                        dg = [sb(e1, "dg%d" % i, [128, 4, 4, 128], BF16) for i in range(2)]
                        ust = [sb(e1, "ust%d" % i, [128, 4, 515], BF16) for i in range(2)]
                        xc = [sb(e1, "xc%d" % i, [128, 4, 512], BF16) for i in range(2)]
                        state = sb(e1, "state", [128, 256], F32)
                        state_bf = [sb(e1, "state_bf%d" % i, [128, 256], BF16) for i in range(3)]
                        zstate = sb(e1, "zstate", [128, 256], BF16)
                        ynT = [sb(e1, "ynT%d" % i, [128, 2, 512], BF16) for i in range(2)]
                        ysem = [dsem(), dsem()]
                        sz = [sb(e1, "sz%d" % i, [128, 256], BF16) for i in range(3)]
                        xbtm = [sb(e1, "xbtm%d" % i, [128, 384], BF16) for i in range(3)]
                        xd = [sb(e1, "xd%d" % i, [128, 4, 64], BF16) for i in range(3)]
                        y4 = [sb(e1, "y4_%d" % i, [128, 256], F32) for i in range(3)]
                        xde = [sb(e1, "xde%d" % i, [128, 4, 64], BF16) for i in range(2)]
                        cbm = [sb(e1, "cbm%d" % i, [128, 128], F32) for i in range(2)]
                        lh = [sb(e1, "lh%d" % i, [128, 4, 128], F32) for i in range(2)]
                        Ex = [sb(e1, "Ex%d" % i, [128, 4, 128], F32) for i in range(2)]
                        MT = [sb(e1, "MT%d" % i, [128, 4, 128], BF16) for i in range(2)]
                        y1 = [sb(e1, "y1_%d" % i, [128, 4, 64], F32) for i in range(2)]
                        tD = [sb(e1, "tD%d" % i, [128, 4, 64], F32) for i in range(2)]
                        yb = [sb(e1, "yb%d" % i, [128, 256], BF16) for i in range(2)]
                        junk2 = sb(e1, "junk2", [128, 256], F32)
                        ss2 = [sb(e1, "ss2_%d" % i, [128, 1], F32) for i in range(2)]
                        rs2 = [sb(e1, "rs2_%d" % i, [128, 1], F32) for i in range(2)]
                        p_u = [ps(e1, "p_u%d" % i, [128, 512], F32) for i in range(2)]
                        p_z = ps(e1, "p_z", [128, 256], F32)
                        bk1 = psbank(e1, "bk1", F32)
                        p_cb = T(bk1[:, 0:128])
                        p_s = T(bk1[:, 256:512])
                        p_s.r = p_cb.r
                        p_seg1 = ps(e1, "p_seg", [128, 4, 128], F32)
                        p_seg = [p_seg1, p_seg1]
                        bk4 = psbank(e1, "bk4", F32)
                        p_y = T(bk4[:, 0:256])
                        p_yo = T(bk4[:, 256:512])
                        p_yo.r = p_y.r
                        bk6 = psbank(e1, "bk6", BF16)
                        p_tpx = T(bk6[:, 0:384])
                        bk7 = psbank(e1, "bk7", BF16)
                        p_tpy = T(bk7[:, 0:256])
                        k.op("pool", lambda e: e.memset(zstate.t[:], 0.0), writes=[zstate])
                        thz = [sb(e1, "thz%d" % i, [128, 256], F32) for i in range(2)]
                        thc = [sb(e1, "thc%d" % i, [128, 512], F32) for i in range(2)]
                        neghalf = sb(e1, "neghalf", [128, 1], F32)
                        k.op("pool", lambda e: e.memset(neghalf.t[:], -0.5), writes=[neghalf])
                        ones_row = sb(e1, "ones_row", [1, 512], BF16)
                        k.op("pool", lambda e: e.memset(ones_row.t[:], 1.0), writes=[ones_row])
                        cb_row = sb(e1, "cb_row", [1, 4096], F32)
                        k.dma("sp", sem_c, lambda e: e.dma_start(out=cb_row.t[:], in_=conv_brow_d), writes=[cb_row])
                        hb_row = sb(e1, "hb_row", [1, 4096], BF16)
                        k.op("dve", lambda e: e.tensor_scalar(out=hb_row.t[:], in0=cb_row.t[:], scalar1=0.5, scalar2=None, op0=ALU.mult),
                             reads=[cb_row], writes=[hb_row])

                        def group_setup(g):
                            w = wg[g % 2]
                            srcs = [(0, 2048 + g * 256, 256), (256, 4096 + g * 128, 128),
                                    (384, 5120 + g * 128, 128), (512, g * 256, 256)]
                            k.dma("pool", wsem[g % 2],
                                  [(lambda e, o=o, s=s, n=n: e.dma_start(
                                      out=w.t[:, :, o:o + n],
                                      in_=w_in_d[:, s:s + n].rearrange("(k p) n -> p k n", p=128))) for (o, s, n) in srcs],
                                  writes=[w])
                            d_ = dg[g % 2]
                            chunks = [2 * g, 2 * g + 1, 16 + g, 24 + g]
                            k.ops("pool", [(lambda e, cc=cc, kk=kk: e.tensor_scalar(
                                out=d_.t[:, cc, kk, :], in0=ident_bf.t[:], scalar1=conv_wT.t[:, chunks[cc], kk:kk + 1],
                                scalar2=0.5, op0=ALU.mult, op1=ALU.mult)) for cc in range(4) for kk in range(4)],
                                reads=[ident_bf, conv_wT], writes=[d_])
                            k.op("pool", lambda e: e.tensor_scalar(out=w.t[:, :, 512:768], in0=w.t[:, :, 512:768], scalar1=0.5,
                                                                   scalar2=0.0, op0=ALU.mult, op1=ALU.add), reads=[w], writes=[w])

                        NT = 8 * NCH

                        def dec(n):
                            g = n // NCH
                            c = n % NCH
                            return g, c, c // 4, c % 4

                        def S0a(si):
                            g, sc = si // NSC, si % NSC
                            w = wg[g % 2]
                            us = ust[si % 2]
                            ts = sc * 512
                            if sc == 0:
                                k.op("pool", lambda e: e.memset(us.t[:, :, 0:3], 0.0), writes=[us])
                            for cc in range(4):
                                pu = p_u[cc % 2]
                                k.ops("pe", [(lambda e, j=j: e.matmul(pu.t[:], lhsT=w.t[:, j, cc * 128:(cc + 1) * 128],
                                                                      rhs=hT.t[:, j, ts:ts + 512], start=(j == 0), stop=(j == 7)))
                                             for j in range(8)], reads=[w, hT], writes=[pu])
                                k.op("act", lambda e: e.copy(out=us.t[:, cc, 3:515], in_=pu.t[:]), reads=[pu], writes=[us])
                            if sc + 1 < NSC:
                                un = ust[(si + 1) % 2]
                                k.op("pool", lambda e: e.tensor_copy(out=un.t[:, :, 0:3], in_=us.t[:, :, 512:515]),
                                     reads=[us], writes=[un])

                        def S0b(si):
                            g, sc = si // NSC, si % NSC
                            d_ = dg[g % 2]
                            us = ust[si % 2]
                            xo = xc[si % 2]
                            chunks = [2 * g, 2 * g + 1, 16 + g, 24 + g]
                            for cc in range(4):
                                pu = p_u[cc % 2]
                                ch = chunks[cc]
                                k.ops("pe", [(lambda e, kk=kk: e.matmul(pu.t[:], lhsT=d_.t[:, cc, kk, :],
                                                                        rhs=us.t[:, cc, kk:kk + 512], start=(kk == 0), stop=False))
                                             for kk in range(4)] +
                                      [lambda e: e.matmul(pu.t[:], lhsT=hb_row.t[0:1, ch * 128:(ch + 1) * 128],
                                                          rhs=ones_row.t[0:1, :], start=False, stop=True)],
                                      reads=[d_, us, hb_row, ones_row], writes=[pu])
                                tc_ = thc[cc % 2]
                                k.op("act", lambda e: e.activation(out=tc_.t[:], in_=pu.t[:], func=AF.Tanh),
                                     reads=[pu], writes=[tc_])
                                k.op("dve", lambda e: e.scalar_tensor_tensor(out=xo.t[:, cc, :], in0=tc_.t[:], scalar=1.0, in1=pu.t[:],
                                                                             op0=ALU.add, op1=ALU.mult),
                                     reads=[tc_, pu], writes=[xo])

                        def ctx(n):
                            g, c, sc, q = dec(n)
                            si = g * NSC + sc
                            return dict(g=g, c=c, sc=sc, q=q, si=si, w=wg[g % 2], xo=xc[si % 2],
                                        qs=slice(q * 128, (q + 1) * 128), cs=slice(c * 128, (c + 1) * 128),
                                        g4=slice(g * 4, g * 4 + 4), b3=n % 3, b2=n % 2)

                        def A_pe(n):
                            x_ = ctx(n); w = x_["w"]; xo = x_["xo"]; qs = x_["qs"]; cs = x_["cs"]
                            k.ops("pe", [(lambda e, j=j: e.matmul(p_z.t[:], lhsT=hT.t[:, j, cs], rhs=w.t[:, j, 512:768],
                                                                  start=(j == 0), stop=(j == 7))) for j in range(8)],
                                  reads=[hT, w], writes=[p_z])
                            k.ops("pe", [(lambda e, cc=cc: e.transpose(out=p_tpx.t[:, cc * 128:(cc + 1) * 128],
                                                                       in_=xo.t[:, cc, qs], identity=ident_bf.t[:]))
                                         for cc in range(3)], reads=[xo, ident_bf], writes=[p_tpx])
                            k.op("pe", lambda e: e.matmul(p_cb.t[:], lhsT=xo.t[:, 2, qs], rhs=xo.t[:, 3, qs],
                                                          start=True, stop=True), reads=[xo], writes=[p_cb])

                        def A_act(n):
                            x_ = ctx(n); b3 = x_["b3"]; b2 = x_["b2"]; c = x_["c"]; g = x_["g"]
                            k.op("act", lambda e: e.activation(out=thz[b2].t[:], in_=p_z.t[:], func=AF.Tanh),
                                 reads=[p_z], writes=[thz[b2]])
                            k.op("act", lambda e: e.copy(out=xbtm[b3].t[:], in_=p_tpx.t[:]), reads=[p_tpx], writes=[xbtm[b3]])
                            k.ops("act", [(lambda e, r=r: e.activation(out=lh[b2].t[:, r, :], in_=Mgt.t[:], func=AF.Copy,
                                                                       scale=a_all.t[:, c, g * 4 + r:g * 4 + r + 1]))
                                          for r in range(4)], reads=[Mgt, a_all], writes=[lh[b2]])

                        def A_dve(n):
                            x_ = ctx(n); b3 = x_["b3"]; b2 = x_["b2"]; c = x_["c"]; g4 = x_["g4"]
                            k.op("dve", lambda e: e.scalar_tensor_tensor(out=sz[b3].t[:], in0=thz[b2].t[:], scalar=1.0, in1=p_z.t[:],
                                                                         op0=ALU.add, op1=ALU.mult),
                                 reads=[thz[b2], p_z], writes=[sz[b3]])
                            k.op("dve", lambda e: e.tensor_tensor(
                                out=xd[b3].t[:], in0=xbtm[b3].t[:, 0:256].rearrange("p (r d) -> p r d", r=4),
                                in1=dt_all.t[:, c, g4, None].to_broadcast([128, 4, 64]), op=ALU.mult),
                                reads=[xbtm[b3], dt_all], writes=[xd[b3]])
                            k.op("dve", lambda e: e.tensor_tensor(out=cbm[b2].t[:], in0=p_cb.t[:], in1=Mle.t[:], op=ALU.mult),
                                 reads=[p_cb, Mle], writes=[cbm[b2]])

                        def A_pool(n):
                            x_ = ctx(n); b3 = x_["b3"]; b2 = x_["b2"]; c = x_["c"]; g4 = x_["g4"]
                            k.op("pool", lambda e: e.tensor_tensor(
                                out=xde[b2].t[:], in0=xd[b3].t[:],
                                in1=e_all.t[:, c, 1, g4, None].to_broadcast([128, 4, 64]), op=ALU.mult),
                                reads=[xd[b3], e_all], writes=[xde[b2]])

                        def B_pe(n):
                            x_ = ctx(n); b3 = x_["b3"]; b2 = x_["b2"]
                            k.ops("pe", [(lambda e, r=r: e.matmul(p_seg[b2].t[:, r, :], lhsT=lh[b2].t[:, r, :], rhs=Mle.t[:],
                                                                  start=True, stop=True)) for r in range(4)],
                                  reads=[lh[b2], Mle], writes=[p_seg[b2]])
                            k.op("pe", lambda e: e.matmul(p_s.t[:], lhsT=xbtm[b3].t[:, 256:384],
                                                          rhs=xde[b2].t[:].rearrange("p r d -> p (r d)"), start=True, stop=True),
                                 reads=[xbtm[b3], xde[b2]], writes=[p_s])

                        def B_act(n):
                            x_ = ctx(n); b2 = x_["b2"]
                            k.op("act", lambda e: e.activation(out=Ex[b2].t[:], in_=p_seg[b2].t[:], func=AF.Exp),
                                 reads=[p_seg[b2]], writes=[Ex[b2]])

                        def B_dve(n):
                            x_ = ctx(n); b2 = x_["b2"]; c = x_["c"]; g4 = x_["g4"]
                            k.op("dve", lambda e: e.tensor_tensor(
                                out=MT[b2].t[:], in0=Ex[b2].t[:],
                                in1=cbm[b2].t[:, None, :].to_broadcast([128, 4, 128]), op=ALU.mult),
                                reads=[Ex[b2], cbm[b2]], writes=[MT[b2]])
                            if c == 0:
                                k.op("dve", lambda e: e.tensor_copy(out=state.t[:], in_=p_s.t[:]), reads=[p_s], writes=[state])
                            else:
                                k.op("dve", lambda e: e.tensor_tensor(
                                    out=state.t[:].rearrange("p (r d) -> p r d", r=4),
                                    in0=state.t[:].rearrange("p (r d) -> p r d", r=4),
                                    in1=e_all.t[:, c, 2, g4, None].to_broadcast([128, 4, 64]), op=ALU.mult),
                                    reads=[state, e_all], writes=[state])
                                k.op("dve", lambda e: e.tensor_tensor(out=state.t[:], in0=state.t[:], in1=p_s.t[:], op=ALU.add),
                                     reads=[state, p_s], writes=[state])

                        def B_dve2(n):
                            x_ = ctx(n); b3 = x_["b3"]; b2 = x_["b2"]; g4 = x_["g4"]
                            k.op("dve", lambda e: e.tensor_tensor(
                                out=tD[b2].t[:], in0=xbtm[b3].t[:, 0:256].rearrange("p (r d) -> p r d", r=4),
                                in1=dsk.t[:, g4, None].to_broadcast([128, 4, 64]), op=ALU.mult),
                                reads=[xbtm[b3], dsk], writes=[tD[b2]])

                        def C_act(n):
                            x_ = ctx(n); b3 = x_["b3"]
                            k.op("act", lambda e: e.copy(out=state_bf[b3].t[:], in_=state.t[:]),
                                 reads=[state], writes=[state_bf[b3]])

                        def C_pe(n):
                            x_ = ctx(n); b3 = x_["b3"]; b2 = x_["b2"]; xo = x_["xo"]; qs = x_["qs"]; c = x_["c"]
                            k.ops("pe", [(lambda e, r=r: e.matmul(p_y.t[:, r * 64:(r + 1) * 64], lhsT=MT[b2].t[:, r, :],
                                                                  rhs=xd[b3].t[:, r, :], start=True, stop=True))
                                         for r in range(4)], reads=[MT[b2], xd[b3]], writes=[p_y])
                            st_prev = zstate if c == 0 else state_bf[(n - 1) % 3]
                            k.op("pe", lambda e: e.matmul(p_yo.t[:], lhsT=xo.t[:, 3, qs], rhs=st_prev.t[:],
                                                          start=True, stop=True), reads=[xo, st_prev], writes=[p_yo])

                        def C_dve(n):
                            x_ = ctx(n); b3 = x_["b3"]; b2 = x_["b2"]; c = x_["c"]; g4 = x_["g4"]
                            k.op("dve", lambda e: e.tensor_tensor(
                                out=y1[b2].t[:], in0=p_yo.t[:].rearrange("p (r d) -> p r d", r=4),
                                in1=e_all.t[:, c, 0, g4, None].to_broadcast([128, 4, 64]), op=ALU.mult),
                                reads=[p_yo, e_all], writes=[y1[b2]])
                            k.op("dve", lambda e: e.tensor_tensor(
                                out=y1[b2].t[:], in0=y1[b2].t[:],
                                in1=p_y.t[:].rearrange("p (r d) -> p r d", r=4), op=ALU.add),
                                reads=[y1[b2], p_y], writes=[y1[b2]])
                            k.op("dve", lambda e: e.tensor_tensor(out=y1[b2].t[:], in0=y1[b2].t[:], in1=tD[b2].t[:], op=ALU.add),
                                 reads=[y1[b2], tD[b2]], writes=[y1[b2]])
                            k.op("dve", lambda e: e.tensor_tensor(
                                out=y4[b3].t[:], in0=y1[b2].t[:].rearrange("p r d -> p (r d)"), in1=sz[b3].t[:], op=ALU.mult),
                                reads=[y1[b2], sz[b3]], writes=[y4[b3]])

                        def D_act(n):
                            x_ = ctx(n); b3 = x_["b3"]; b2 = x_["b2"]
                            k.op("act", lambda e: e.activation(out=junk2.t[:], in_=y4[b3].t[:], func=AF.Square,
                                                               accum_out=ss2[b2].t[:, 0:1]),
                                 reads=[y4[b3]], writes=[junk2, ss2[b2]])

                        def D_pool(n):
                            x_ = ctx(n); b2 = x_["b2"]
                            k.op("pool", lambda e: e.tensor_scalar(out=rs2[b2].t[:], in0=ss2[b2].t[:], scalar1=1.0 / 256, scalar2=EPS,
                                                                   op0=ALU.mult, op1=ALU.add), reads=[ss2[b2]], writes=[rs2[b2]])
                            k.op("pool", lambda e: e.tensor_tensor(out=rs2[b2].t[:], in0=rs2[b2].t[:], in1=neghalf.t[:], op=ALU.pow),
                                 reads=[rs2[b2], neghalf], writes=[rs2[b2]])

                        def E_dve(n):
                            x_ = ctx(n); b3 = x_["b3"]; b2 = x_["b2"]
                            k.op("dve", lambda e: e.tensor_scalar(out=yb[b2].t[:], in0=y4[b3].t[:], scalar1=rs2[b2].t[:, 0:1],
                                                                  scalar2=None, op0=ALU.mult),
                                 reads=[y4[b3], rs2[b2]], writes=[yb[b2]])

                        def F_pe(n):
                            x_ = ctx(n); b2 = x_["b2"]
                            k.ops("pe", [(lambda e, h=h: e.transpose(out=p_tpy.t[:, h * 128:(h + 1) * 128],
                                                                     in_=yb[b2].t[:, h * 128:(h + 1) * 128], identity=ident_bf.t[:]))
                                         for h in range(2)], reads=[yb[b2], ident_bf], writes=[p_tpy])

                        def F_act(n):
                            x_ = ctx(n); g = x_["g"]; qs = x_["qs"]; si = x_["si"]; q = x_["q"]; sc = x_["sc"]
                            yo = ynT[si % 2]
                            for h in range(2):
                                k.op("act", lambda e: e.activation(out=yo.t[:, h, qs], in_=p_tpy.t[:, h * 128:(h + 1) * 128], func=AF.Copy,
                                                                   scale=gssdT.t[:, 2 * g + h:2 * g + h + 1]),
                                     reads=[p_tpy, gssdT], writes=[yo])
                            if q == 3:
                                ts = sc * 512
                                k.dma("sp", ysem[si % 2], lambda e: e.dma_start(
                                    out=ymT_d[2 * g:2 * g + 2, :, ts:ts + 512].rearrange("f p t -> p f t"), in_=yo.t[:]),
                                    reads=[yo])

                        def ok(n):
                            return 0 <= n < NT

                        import os as _os
                        if _os.environ.get("A1_NOSKEW"):
                            for n in range(NT):
                                g, c, sc, q = dec(n)
                                if c == 0:
                                    group_setup(g)
                                if q == 0:
                                    S0a(g * NSC + sc)
                                    S0b(g * NSC + sc)
                                for fn in (A_pe, A_act, A_dve, A_pool, B_pe, B_act, B_dve, B_dve2, C_pe, C_act, C_dve, D_act, D_pool, E_dve, F_pe, F_act):
                                    fn(n)
                        else:
                            LAGS = dict(A=0, B=1, C=2, D=3, E=4, F=5)
                            group_setup(0)
                            S0a(0)
                            S0b(0)
                            S0a(1)
                            for t in range(NT + 5):
                                for fn, lag in ((F_pe, 5), (C_pe, 2), (B_pe, 1), (A_pe, 0),
                                                (F_act, 5), (C_act, 2), (B_act, 1), (A_act, 0), (D_act, 3),
                                                (E_dve, 4), (B_dve, 1), (B_dve2, 1), (C_dve, 2), (A_dve, 0),
                                                (D_pool, 3), (A_pool, 0)):
                                    if ok(t - lag):
                                        fn(t - lag)
                                if t % 4 == 1 and t // 4 + 1 < 8 * NSC:
                                    S0b(t // 4 + 1)
                                if t % 4 == 2 and t // 4 + 2 < 8 * NSC:
                                    S0a(t // 4 + 2)
                                if t % NCH == 4 and t // NCH + 1 < 8:
                                    group_setup(t // NCH + 1)
                k.barrier()
                if "A1p" in phases:
                    with ExitStack() as e2:
                        wp = sb(e2, "wp", [128, 8, 1024], BF16)
                        k.dma("pool", sem_cp, lambda e: e.dma_start(
                            out=wp.t[:], in_=w_in_d[:, 6176:7200].rearrange("(k p) n -> p k n", p=128)), writes=[wp])
                        pw = sb(e2, "pw", [128, 4, 2, 256], BF16)
                        k.dma("pool", sem_cp, lambda e: e.dma_start(
                            out=pw.t[:], in_=pool_w_d.rearrange("g (k p) n -> p g k n", p=128)), writes=[pw])
                        pst = [sb(e2, "pst%d" % i, [128, 8, 527], F32) for i in range(2)]
                        sA = sb(e2, "sA", [128, 2, 527], F32)
                        sB = sb(e2, "sB", [128, 2, 527], F32)
                        ypl = [sb(e2, "ypl%d" % i, [128, 2, 512], BF16) for i in range(2)]
                        tmp16 = sb(e2, "tmp16", [128, 2, 16], F32)
                        ypT = [sb(e2, "ypT%d" % i, [128, 2, 512], BF16) for i in range(2)]
                        psem_ = [dsem(), dsem()]
                        p_u2 = [ps(e2, "p_u2_%d" % i, [128, 512], F32) for i in range(2)]
                        p_p = [ps(e2, "p_p%d" % i, [128, 512], F32) for i in range(2)]
                        k.op("pool", lambda e: e.memset(pst[0].t[:, :, 0:15], 0.0), writes=[pst[0]])
                        it = 0
                        for sc in range(NSC):
                            ts = sc * 512
                            cur = pst[sc % 2]
                            nxt = pst[(sc + 1) % 2]
                            for pc in range(8):
                                pu = p_u2[pc % 2]
                                k.ops("pe", [(lambda e, j=j: e.matmul(pu.t[:], lhsT=wp.t[:, j, pc * 128:(pc + 1) * 128],
                                                                      rhs=hT.t[:, j, ts:ts + 512], start=(j == 0), stop=(j == 7)))
                                             for j in range(8)], reads=[wp, hT], writes=[pu])
                                k.op("act", lambda e: e.copy(out=cur.t[:, pc, 15:527], in_=pu.t[:]), reads=[pu], writes=[cur])
                            if sc + 1 < NSC:
                                k.op("pool", lambda e: e.tensor_copy(out=nxt.t[:, :, 0:15], in_=cur.t[:, :, 512:527]),
                                     reads=[cur], writes=[nxt])
                            for pg in range(4):
                                u = cur.t[:, 2 * pg:2 * pg + 2, :]
                                nlev = pg + 1
                                src = u
                                bufs = [sA, sB]
                                eng = "dve"
                                for lv in range(nlev):
                                    sh = 1 << lv
                                    lo = (1 << (lv + 1)) - 1
                                    dst = bufs[lv % 2]
                                    src_t = cur if lv == 0 else bufs[(lv - 1) % 2]
                                    s_ap = src
                                    k.op(eng, lambda e, dst=dst, s_ap=s_ap, lo=lo, sh=sh: e.tensor_tensor(
                                        out=dst.t[:, :, lo:527], in0=s_ap[:, :, lo:527], in1=s_ap[:, :, lo - sh:527 - sh], op=ALU.add),
                                        reads=[src_t], writes=[dst])
                                    src = dst.t[:, :, :]
                                fin = bufs[(nlev - 1) % 2]
                                wv = 1 << nlev
                                yp = ypl[it % 2]
                                k.op(eng, lambda e: e.scalar_tensor_tensor(
                                    out=yp.t[:], in0=fin.t[:, :, 15:527], scalar=1.0 / wv, in1=u[:, :, 15:527],
                                    op0=ALU.mult, op1=ALU.subtract), reads=[fin, cur], writes=[yp])
                                if sc == 0:
                                    k.op(eng, lambda e: e.tensor_tensor(
                                        out=tmp16.t[:], in0=fin.t[:, :, 15:31],
                                        in1=invc.t[:, pg, None, :].to_broadcast([128, 2, 16]), op=ALU.mult),
                                        reads=[fin, invc], writes=[tmp16])
                                    k.op(eng, lambda e: e.tensor_tensor(out=yp.t[:, :, 0:16], in0=tmp16.t[:], in1=u[:, :, 15:31],
                                                                        op=ALU.subtract), reads=[tmp16, cur], writes=[yp])
                                yT_ = ypT[it % 2]
                                for dc in range(2):
                                    pp = p_p[dc]
                                    k.ops("pe", [(lambda e, kc=kc: e.matmul(pp.t[:], lhsT=pw.t[:, pg, kc, dc * 128:(dc + 1) * 128],
                                                                            rhs=yp.t[:, kc, :], start=(kc == 0), stop=(kc == 1)))
                                                 for kc in range(2)], reads=[pw, yp], writes=[pp])
                                    k.op("act", lambda e: e.activation(out=yT_.t[:, dc, :], in_=pp.t[:], func=AF.Copy,
                                                                       scale=pscT.t[:, 2 * pg + dc:2 * pg + dc + 1]),
                                         reads=[pp, pscT], writes=[yT_])
                                k.dma("sp", psem_[it % 2], lambda e: e.dma_start(
                                    out=ymT_d[16 + 2 * pg:16 + 2 * pg + 2, :, ts:ts + 512].rearrange("f p t -> p f t"), in_=yT_.t[:]),
                                    reads=[yT_])
                                it += 1

        k.barrier()
        if "A2" in phases:
            with ExitStack() as e3:
                wout = sb(e3, "wout", [128, 24, DM], BF16)
                k.dma("pool", sem_cp, [(lambda e, i=i: e.dma_start(
                    out=wout.t[:, 6 * i:6 * i + 6, :],
                    in_=w_out_d[768 * i:768 * (i + 1), :].rearrange("(k p) n -> p k n", p=128))) for i in range(4)],
                    writes=[wout])
                gffn_bc = load_const(e3, "gffn_bc", [128, DM], gffn_bc_d)
                rw = load_const(e3, "rw", [128, 8, NE], rw_d.rearrange("(k p) n -> p k n", p=128))
                rb = load_const(e3, "rb", [128, NE], rb_d)
                ebase = load_const(e3, "ebase", [128, NE], ebase_d)
                s4096 = sb(e3, "s4096", [128, 514], I32)
                k.op("pool", lambda e: e.memset(s4096.t[:], L), writes=[s4096])
                ev_init = [
                    k.dma("sp", sem_c, lambda e: e.dma_start(out=slot_d.rearrange("(p f) o -> p (f o)", p=128), in_=s4096.t[:]),
                          reads=[s4096]),
                    k.dma("sp", sem_c, lambda e: e.dma_start(out=h2_d[L:L + 1, :], in_=zrow_bf.t[:]), reads=[zrow_bf]),
                    k.dma("sp", sem_c, lambda e: e.dma_start(out=G_d[L:L + 1, :], in_=zrow.t[:, 0:NE]), reads=[zrow]),
                    k.dma("sp", sem_c, lambda e: e.dma_start(out=Y_d[0:1, :], in_=zrow.t[:]), reads=[zrow]),
                ]
                ym = [sb(e3, "ym%d" % i, [128, 24, 512], BF16) for i in range(2)]
                ymsem = [dsem(), dsem()]
                xt2 = [sb(e3, "xt2_%d" % i, [128, DM], F32) for i in range(2)]
                xsem2 = [dsem(), dsem()]
                x1 = [sb(e3, "x1_%d" % i, [128, DM], F32) for i in range(2)]
                x1sem = [dsem(), dsem()]
                junk3_l = [sb(e3, "junk3%d" % i_, [128, DM], F32) for i_ in range(2)]
                ss3_l = [sb(e3, "ss3%d" % i_, [128, 1], F32) for i_ in range(2)]
                rs3_l = [sb(e3, "rs3%d" % i_, [128, 1], F32) for i_ in range(2)]
                h2f_l = [sb(e3, "h2f%d" % i_, [128, DM], F32) for i_ in range(2)]
                h2b = [sb(e3, "h2b%d" % i, [128, DM], BF16) for i in range(2)]
                h2sem = [dsem(), dsem()]
                h2T_l = [sb(e3, "h2T%d" % i_, [128, 8, 128], F32) for i_ in range(2)]
                lg_l = [sb(e3, "lg%d" % i_, [128, NE], F32) for i_ in range(2)]
                m8_l = [sb(e3, "m8%d" % i_, [128, 8], F32) for i_ in range(2)]
                nv1_l = [sb(e3, "nv1%d" % i_, [128, 1], F32) for i_ in range(2)]
                mask_l = [sb(e3, "mask%d" % i_, [128, NE], F32) for i_ in range(2)]
                ex_l = [sb(e3, "ex%d" % i_, [128, NE], F32) for i_ in range(2)]
                sm_l = [sb(e3, "sm%d" % i_, [128, 1], F32) for i_ in range(2)]
                Gt = [sb(e3, "Gt%d" % i, [128, NE], F32) for i in range(2)]
                gsem = [dsem(), dsem()]
                cnt = sb(e3, "cnt", [128, NE], F32)
                rank_l = [sb(e3, "rank%d" % i_, [128, NE], F32) for i_ in range(2)]
                vld_l = [sb(e3, "vld%d" % i_, [128, NE], F32) for i_ in range(2)]
                val_l = [sb(e3, "val%d" % i_, [128, NE], F32) for i_ in range(2)]
                v8_l = [sb(e3, "v8%d" % i_, [128, 8], F32) for i_ in range(2)]
                scsem = dsem()
                p_o_l = [ps(e3, "p_o%d" % i_, [128, DM], F32) for i_ in range(2)]
                p_tf = ps(e3, "p_tf", [128, 8, 128], F32)
                p_l = ps(e3, "p_l", [128, NE], F32)
                p_r = ps(e3, "p_r", [128, 2, NE], F32)
                k.op("pool", lambda e: e.memset(cnt.t[:], 0.0), writes=[cnt])
                scat_evs = []
                def ym_load(sc):
                    ts = sc * 512
                    ymc = ym[sc % 2]
                    k.dma("sp", ymsem[sc % 2], [(lambda e, i=i: e.dma_start(
                        out=ymc.t[:, 6 * i:6 * i + 6, :],
                        in_=ymT_d[6 * i:6 * i + 6, :, ts:ts + 512].rearrange("f p t -> p f t"))) for i in range(4)],
                        writes=[ymc])

                def a2body(sc, q):
                    ts = sc * 512
                    ymc = ym[sc % 2]
                    if q == 1 and sc + 1 < NSC:
                        ym_load(sc + 1)
                    c = sc * 4 + q
                    b = c % 2
                    qs = slice(q * 128, (q + 1) * 128)
                    cs = slice(c * 128, (c + 1) * 128)
                    p_o = p_o_l[b]
                    junk3 = junk3_l[b]; ss3 = ss3_l[b]; rs3 = rs3_l[b]; h2f = h2f_l[b]; h2T = h2T_l[b]; lg = lg_l[b]; m8 = m8_l[b]; nv1 = nv1_l[b]; mask = mask_l[b]; ex = ex_l[b]; sm = sm_l[b]; rank = rank_l[b]; vld = vld_l[b]; val = val_l[b]; v8 = v8_l[b]
                    yield
                    k.dma("sp", xsem2[b], lambda e: e.dma_start(out=xt2[b].t[:], in_=x_d[cs, :]), writes=[xt2[b]])
                    yield
                    k.ops("pe", [(lambda e, fc=fc, h=h: e.matmul(p_o.t[:, h * 512:(h + 1) * 512], lhsT=ymc.t[:, fc, qs],
                                                                 rhs=wout.t[:, fc, h * 512:(h + 1) * 512],
                                                                 start=(fc == 0), stop=(fc == 23)))
                                 for h in range(2) for fc in range(24)], reads=[ymc, wout], writes=[p_o])
                    yield
                    k.op("dve", lambda e: e.tensor_tensor(out=x1[b].t[:], in0=p_o.t[:], in1=xt2[b].t[:], op=ALU.add),
                         reads=[p_o, xt2[b]], writes=[x1[b]])
                    yield
                    k.dma("sp", x1sem[b], lambda e: e.dma_start(out=x1_d[cs, :], in_=x1[b].t[:]), reads=[x1[b]])
                    yield
                    k.op("act", lambda e: e.activation(out=junk3.t[:], in_=x1[b].t[:], func=AF.Square, accum_out=ss3.t[:, 0:1]),
                         reads=[x1[b]], writes=[junk3, ss3])
                    yield
                    k.op("act", lambda e: e.activation(out=rs3.t[:], in_=ss3.t[:], func=AF.Sqrt, scale=1.0 / DM, bias=EPS),
                         reads=[ss3], writes=[rs3])
                    yield
                    k.op("dve", lambda e: e.reciprocal(out=rs3.t[:], in_=rs3.t[:]), reads=[rs3], writes=[rs3])
                    yield
                    k.op("dve", lambda e: e.scalar_tensor_tensor(out=h2f.t[:], in0=x1[b].t[:], scalar=rs3.t[:, 0:1],
                                                                 in1=gffn_bc.t[:], op0=ALU.mult, op1=ALU.mult),
                         reads=[x1[b], rs3, gffn_bc], writes=[h2f])
                    yield
                    k.op("act", lambda e: e.copy(out=h2b[b].t[:], in_=h2f.t[:]), reads=[h2f], writes=[h2b[b]])
                    yield
                    k.dma("sp", h2sem[b], lambda e: e.dma_start(out=h2_d[cs, :], in_=h2b[b].t[:]), reads=[h2b[b]])
                    yield
                    k.ops("pe", [(lambda e, j=j: e.transpose(out=p_tf.t[:, j, :], in_=h2f.t[:, j * 128:(j + 1) * 128],
                                                             identity=ident_f.t[:])) for j in range(8)],
                          reads=[h2f, ident_f], writes=[p_tf])
                    yield
                    k.op("act", lambda e: e.copy(out=h2T.t[:], in_=p_tf.t[:]), reads=[p_tf], writes=[h2T])
                    yield
                    k.ops("pe", [(lambda e, j=j: e.matmul(p_l.t[:], lhsT=h2T.t[:, j, :], rhs=rw.t[:, j, :],
                                                          start=(j == 0), stop=(j == 7))) for j in range(8)],
                          reads=[h2T, rw], writes=[p_l])
                    yield
                    k.op("dve", lambda e: e.tensor_tensor(out=lg.t[:], in0=p_l.t[:], in1=rb.t[:], op=ALU.add),
                         reads=[p_l, rb], writes=[lg])
                    yield
                    k.op("dve", lambda e: e.max(out=m8.t[:], in_=lg.t[:]), reads=[lg], writes=[m8])
                    yield
                    k.op("dve", lambda e: e.tensor_scalar(out=mask.t[:], in0=lg.t[:], scalar1=m8.t[:, 3:4], scalar2=None,
                                                          op0=ALU.is_ge), reads=[lg, m8], writes=[mask])
                    yield
                    k.op("dve", lambda e: e.tensor_scalar(out=nv1.t[:], in0=m8.t[:, 0:1], scalar1=-1.0, scalar2=None,
                                                          op0=ALU.mult), reads=[m8], writes=[nv1])
                    yield
                    k.op("act", lambda e: e.activation(out=ex.t[:], in_=lg.t[:], func=AF.Exp, bias=nv1.t[:, 0:1]),
                         reads=[lg, nv1], writes=[ex])
                    yield
                    k.op("dve", lambda e: e.tensor_tensor(out=ex.t[:], in0=ex.t[:], in1=mask.t[:], op=ALU.mult),
                         reads=[ex, mask], writes=[ex])
                    yield
                    k.op("dve", lambda e: e.reduce_sum(out=sm.t[:], in_=ex.t[:], axis=AX.X), reads=[ex], writes=[sm])
                    yield
                    k.op("dve", lambda e: e.reciprocal(out=sm.t[:], in_=sm.t[:]), reads=[sm], writes=[sm])
                    yield
                    k.op("dve", lambda e: e.tensor_scalar(out=Gt[b].t[:], in0=ex.t[:], scalar1=sm.t[:, 0:1], scalar2=None,
                                                          op0=ALU.mult), reads=[ex, sm], writes=[Gt[b]])
                    yield
                    k.dma("sp", gsem[b], lambda e: e.dma_start(out=G_d[cs, :], in_=Gt[b].t[:]), reads=[Gt[b]])
                    yield
                    k.ops("pe", [
                        lambda e: e.matmul(p_r.t[:, 0, :], lhsT=Mlt.t[:], rhs=mask.t[:], start=True, stop=True),
                        lambda e: e.matmul(p_r.t[:, 1, :], lhsT=ones_f.t[:], rhs=mask.t[:], start=True, stop=True),
                    ], reads=[Mlt, ones_f, mask], writes=[p_r])
                    yield
                    k.op("dve", lambda e: e.tensor_tensor(out=rank.t[:], in0=p_r.t[:, 0, :], in1=cnt.t[:], op=ALU.add),
                         reads=[p_r, cnt], writes=[rank])
                    yield
                    k.op("dve", lambda e: e.tensor_tensor(out=cnt.t[:], in0=p_r.t[:, 1, :], in1=cnt.t[:], op=ALU.add),
                         reads=[p_r, cnt], writes=[cnt])
                    yield
                    k.op("dve", lambda e: e.tensor_scalar(out=vld.t[:], in0=rank.t[:], scalar1=float(CAP), scalar2=None,
                                                          op0=ALU.is_lt), reads=[rank], writes=[vld])
                    yield
                    k.op("dve", lambda e: e.tensor_tensor(out=vld.t[:], in0=vld.t[:], in1=mask.t[:], op=ALU.mult),
                         reads=[vld, mask], writes=[vld])
                    yield
                    k.op("dve", lambda e: e.tensor_tensor(out=val.t[:], in0=rank.t[:], in1=ebase.t[:], op=ALU.add),
                         reads=[rank, ebase], writes=[val])
                    yield
                    k.op("dve", lambda e: e.tensor_tensor(out=val.t[:], in0=val.t[:], in1=vld.t[:], op=ALU.mult),
                         reads=[val, vld], writes=[val])
                    yield
                    k.op("dve", lambda e: e.max(out=v8.t[:], in_=val.t[:]), reads=[val], writes=[v8])
                    yield
                    k.op("dve", lambda e: e.tensor_copy(out=dest_all.t[:, c, :], in_=v8.t[:, 0:4]), reads=[v8], writes=[dest_all])
                    yield
                    for kk in range(4):
                        scat_evs.append(k.dma("pool", scsem, lambda e: e.indirect_dma_start(
                            out=slot_d[:, :], out_offset=bass.IndirectOffsetOnAxis(ap=dest_all.t[:, c, kk:kk + 1], axis=0),
                            in_=tokid.t[:, c, :], in_offset=None),
                            reads=[dest_all, tokid], extra=ev_init))
                    yield
                ym_load(0)
                run_chains([a2body(sc, q) for sc in range(NSC) for q in range(4)], lag=8)
                a2_done = [x1[0], x1[1], h2b[0], h2b[1], Gt[0], Gt[1]]
                a2_evs = list(scat_evs[-1:])
                for t in a2_done:
                    a2_evs += t.r.rs
        else:
            a2_evs = []

        k.barrier()
        if "B" in phases:
            with ExitStack() as e4:
                bguT = load_const(e4, "bguT", [128, NE, 16], bguT_d)
                wgu = [sb(e4, "wgu%d" % i, [128, 8, 2048], BF16) for i in range(2)]
                wdn = [sb(e4, "wdn%d" % i, [128, 8, DM], BF16) for i in range(2)]
                bdb = [sb(e4, "bdb%d" % i, [128, DM], F32) for i in range(2)]
                wesem = [dsem(), dsem()]
                bdsem = [dsem(), dsem()]
                idx = [sb(e4, "idx%d" % i, [128, NBLK, 2], I32) for i in range(2)]
                isem = [dsem(), dsem()]
                xg = [sb(e4, "xg%d" % i, [128, DM], BF16) for i in range(NBLK)]
                xgsem = [dsem() for _ in range(NBLK)]
                gg = [sb(e4, "gg%d" % i, [128, NBLK, NE], F32) for i in range(2)]
                ggsem = [dsem(), dsem()]
                xgT = sb(e4, "xgT", [128, 8, CAP], BF16)
                actT = sb(e4, "actT", [128, 8, CAP], BF16)
                HW = CAP // 2
                gm = [sb(e4, "gm%d" % i, [128, HW], F32) for i in range(2)]
                sg = [sb(e4, "sg%d" % i, [128, HW], F32) for i in range(2)]
                u1 = [sb(e4, "u1_%d" % i, [128, HW], F32) for i in range(2)]
                yA = [sb(e4, "yA%d" % i, [128, DM], F32) for i in range(2)]
                yB = [sb(e4, "yB%d" % i, [128, DM], F32) for i in range(2)]
                ysem2 = [dsem(), dsem()]
                p_tg2 = [ps(e4, "p_tg%d" % i, [128, 8, 128], BF16) for i in range(2)]
                p_g = [ps(e4, "p_g%d" % i, [128, 512], F32) for i in range(2)]
                p_up = [ps(e4, "p_up%d" % i, [128, 512], F32) for i in range(2)]
                p_dh = [ps(e4, "p_dh%d" % i, [128, 512], F32) for i in range(2)]

                def load_w(e_):
                    s = e_ % 2
                    k.dma("pool", wesem[s],
                          [(lambda e, i=i: e.dma_start(out=wgu[s].t[:, 2 * i:2 * i + 2, :],
                                                       in_=wgu_d[e_, 256 * i:256 * (i + 1), :].rearrange("(k p) n -> p k n", p=128)))
                           for i in range(4)] +
                          [(lambda e, i=i: e.dma_start(out=wdn[s].t[:, 4 * i:4 * i + 4, :],
                                                       in_=wd_d[e_, 512 * i:512 * (i + 1), :].rearrange("(k p) n -> p k n", p=128)))
                           for i in range(2)],
                          writes=[wgu[s], wdn[s]])
                    k.dma("sp", bdsem[s], lambda e: e.dma_start(out=bdb[s].t[:], in_=bd_d[e_:e_ + 1, :].to_broadcast([128, DM])),
                          writes=[bdb[s]])

                def prefetch(e_):
                    s_ = e_ % 2
                    base_ = 1 + e_ * CAP
                    k.dma("sp", isem[s_], [(lambda e, j=j: e.dma_start(out=idx[s_].t[:, j, :],
                                                                       in_=slot_d[base_ + j * 128:base_ + (j + 1) * 128, :]))
                                           for j in range(NBLK)], writes=[idx[s_]], extra=a2_evs)
                    for j in range(NBLK):
                        k.dma("pool", xgsem[j], lambda e: e.indirect_dma_start(
                            out=xg[j].t[:, :], out_offset=None, in_=h2_d[:, :],
                            in_offset=bass.IndirectOffsetOnAxis(ap=idx[s_].t[:, j, 0:1], axis=0)),
                            reads=[idx[s_]], writes=[xg[j]], extra=a2_evs)
                    k.dma("pool", ggsem[s_], [(lambda e, j=j: e.indirect_dma_start(
                        out=gg[s_].t[:, j, :], out_offset=None, in_=G_d[:, :],
                        in_offset=bass.IndirectOffsetOnAxis(ap=idx[s_].t[:, j, 0:1], axis=0))) for j in range(NBLK)],
                        reads=[idx[s_]], writes=[gg[s_]], extra=a2_evs)

                def transposes():
                    for j in range(NBLK):
                        p_tg = p_tg2[j % 2]
                        k.ops("pe", [(lambda e, kk=kk: e.transpose(out=p_tg.t[:, kk, :], in_=xg[j].t[:, kk * 128:(kk + 1) * 128],
                                                                   identity=ident_bf.t[:])) for kk in range(8)],
                              reads=[xg[j], ident_bf], writes=[p_tg])
                        k.op("act", lambda e: e.copy(out=xgT.t[:, :, j * 128:(j + 1) * 128], in_=p_tg.t[:]),
                             reads=[p_tg], writes=[xgT])

                load_w(0)
                prefetch(0)
                transposes()
                for e_ in range(NE):
                    s = e_ % 2
                    base = 1 + e_ * CAP
                    if e_ + 1 < NE:
                        prefetch(e_ + 1)
                        load_w(e_ + 1)
                    for fc in range(8):
                        for h in range(2):
                            hs = slice(h * HW, (h + 1) * HW)
                            k.ops("pe", [(lambda e, kk=kk: e.matmul(p_g[h].t[:, 0:HW], lhsT=wgu[s].t[:, kk, fc * 128:(fc + 1) * 128],
                                                                    rhs=xgT.t[:, kk, hs], start=(kk == 0), stop=(kk == 7)))
                                         for kk in range(8)], reads=[wgu[s], xgT], writes=[p_g[h]])
                            k.ops("pe", [(lambda e, kk=kk: e.matmul(p_up[h].t[:, 0:HW],
                                                                    lhsT=wgu[s].t[:, kk, 1024 + fc * 128:1024 + (fc + 1) * 128],
                                                                    rhs=xgT.t[:, kk, hs], start=(kk == 0), stop=(kk == 7)))
                                         for kk in range(8)], reads=[wgu[s], xgT], writes=[p_up[h]])
                            k.op("dve", lambda e: e.tensor_scalar(out=gm[h].t[:], in0=p_g[h].t[:, 0:HW],
                                                                  scalar1=bguT.t[:, e_, fc:fc + 1], scalar2=7.0,
                                                                  op0=ALU.add, op1=ALU.min), reads=[p_g[h], bguT], writes=[gm[h]])
                            k.op("act", lambda e: e.activation(out=sg[h].t[:], in_=gm[h].t[:], func=AF.Sigmoid, scale=1.702),
                                 reads=[gm[h]], writes=[sg[h]])
                            k.op("dve", lambda e: e.tensor_scalar(out=u1[h].t[:], in0=p_up[h].t[:, 0:HW],
                                                                  scalar1=bguT.t[:, e_, 8 + fc:9 + fc], scalar2=7.0,
                                                                  op0=ALU.add, op1=ALU.min), reads=[p_up[h], bguT], writes=[u1[h]])
                            k.op("dve", lambda e: e.tensor_scalar(out=u1[h].t[:], in0=u1[h].t[:], scalar1=-7.0, scalar2=1.0,
                                                                  op0=ALU.max, op1=ALU.add), reads=[u1[h]], writes=[u1[h]])
                            k.op("dve", lambda e: e.tensor_tensor(out=sg[h].t[:], in0=gm[h].t[:], in1=sg[h].t[:], op=ALU.mult),
                                 reads=[gm[h], sg[h]], writes=[sg[h]])
                            k.op("dve", lambda e: e.tensor_tensor(out=actT.t[:, fc, hs], in0=sg[h].t[:], in1=u1[h].t[:], op=ALU.mult),
                                 reads=[sg[h], u1[h]], writes=[actT])
                    if e_ + 1 < NE:
                        transposes()
                    for j in range(NBLK):
                        js = slice(j * 128, (j + 1) * 128)
                        b = j % 2
                        for h in range(2):
                            k.ops("pe", [(lambda e, kk=kk: e.matmul(p_dh[h].t[:], lhsT=actT.t[:, kk, js],
                                                                    rhs=wdn[s].t[:, kk, h * 512:(h + 1) * 512],
                                                                    start=(kk == 0), stop=(kk == 7)))
                                         for kk in range(8)], reads=[actT, wdn[s]], writes=[p_dh[h]])
                            k.op("dve", lambda e: e.tensor_tensor(out=yA[b].t[:, h * 512:(h + 1) * 512], in0=p_dh[h].t[:],
                                                                  in1=bdb[s].t[:, h * 512:(h + 1) * 512], op=ALU.add),
                                 reads=[p_dh[h], bdb[s]], writes=[yA[b]])
                        k.op("act", lambda e: e.activation(out=yB[b].t[:], in_=yA[b].t[:], func=AF.Copy,
                                                           scale=gg[s].t[:, j, e_:e_ + 1]),
                             reads=[yA[b], gg[s]], writes=[yB[b]])
                        k.dma("sp", ysem2[b], lambda e: e.dma_start(out=Y_d[base + j * 128:base + (j + 1) * 128, :], in_=yB[b].t[:]),
                              reads=[yB[b]])
                b_evs = []
                for t in yB:
                    b_evs += t.r.rs
        else:
            b_evs = []

        k.barrier()
        fin_evs = []
        if "C" in phases:
            with ExitStack() as e5:
                wpg = sb(e5, "wpg", [128, 8, DM], BF16)
                k.dma("pool", sem_cp, [(lambda e, i=i: e.dma_start(
                    out=wpg.t[:, 4 * i:4 * i + 4, :],
                    in_=wpg_d[512 * i:512 * (i + 1), :].rearrange("(k p) n -> p k n", p=128))) for i in range(2)], writes=[wpg])
                wpp = sb(e5, "wpp", [128, 2, DM], BF16)
                k.dma("pool", sem_cp, lambda e: e.dma_start(out=wpp.t[:], in_=wpp_d.rearrange("(k p) n -> p k n", p=128)),
                      writes=[wpp])
                gpgT = load_const(e5, "gpgT", [128, 8], gpgT_d)
                gpn_bc = load_const(e5, "gpn_bc", [128, DM], gpn_bc_d)
                gfin_bc = load_const(e5, "gfin_bc", [128, DM], gfin_bc_d)
                x1c = [sb(e5, "x1c%d" % i, [128, DM], F32) for i in range(2)]
                x1csem = [dsem(), dsem()]
                yk = [[sb(e5, "yk%d_%d" % (i, kk), [128, DM], F32) for kk in range(4)] for i in range(2)]
                yksem = [[dsem() for kk in range(4)] for i in range(2)]
                pt = [sb(e5, "pt%d" % i, [128, 256], F32) for i in range(2)]
                ptsem = [dsem(), dsem()]
                x2_l = [sb(e5, "x2%d" % i_, [128, DM], F32) for i_ in range(2)]
                junk4_l = [sb(e5, "junk4%d" % i_, [128, DM], F32) for i_ in range(2)]
                ssc_l = [sb(e5, "ssc%d" % i_, [128, 1], F32) for i_ in range(2)]
                rsc_l = [sb(e5, "rsc%d" % i_, [128, 1], F32) for i_ in range(2)]
                xnb_l = [sb(e5, "xnb%d" % i_, [128, DM], BF16) for i_ in range(2)]
                xnT_l = [sb(e5, "xnT%d" % i_, [128, 8, 128], BF16) for i_ in range(2)]
                sgate_l = [sb(e5, "sgate%d" % i_, [128, DM], F32) for i_ in range(2)]
                pT_l = [sb(e5, "pT%d" % i_, [128, 2, 128], BF16) for i_ in range(2)]
                sse_l = [sb(e5, "sse%d" % i_, [128, 1], F32) for i_ in range(2)]
                rse_l = [sb(e5, "rse%d" % i_, [128, 1], F32) for i_ in range(2)]
                e1__l = [sb(e5, "e1_%d" % i_, [128, DM], F32) for i_ in range(2)]
                x3_l = [sb(e5, "x3%d" % i_, [128, DM], F32) for i_ in range(2)]
                ssf_l = [sb(e5, "ssf%d" % i_, [128, 1], F32) for i_ in range(2)]
                rsf_l = [sb(e5, "rsf%d" % i_, [128, 1], F32) for i_ in range(2)]
                ot = [sb(e5, "ot%d" % i, [128, DM], F32) for i in range(2)]
                osem = [dsem(), dsem()]
                p_t3 = ps(e5, "p_t3", [128, 8, 128], BF16)
                p_ga = ps(e5, "p_ga", [128, DM], F32)
                p_pt = ps(e5, "p_pt", [128, 2, 128], F32)
                p_e = ps(e5, "p_e", [128, DM], F32)
                def cbody(c):
                    b = c % 2
                    cs = slice(c * 128, (c + 1) * 128)
                    x2 = x2_l[b]; junk4 = junk4_l[b]; ssc = ssc_l[b]; rsc = rsc_l[b]; xnb = xnb_l[b]; xnT = xnT_l[b]; sgate = sgate_l[b]; pT = pT_l[b]; sse = sse_l[b]; rse = rse_l[b]; e1_ = e1__l[b]; x3 = x3_l[b]; ssf = ssf_l[b]; rsf = rsf_l[b]
                    yield
                    k.dma("sp", x1csem[b], lambda e: e.dma_start(out=x1c[b].t[:], in_=x1_d[cs, :]), writes=[x1c[b]], extra=a2_evs)
                    yield
                    k.dma("sp", ptsem[b], lambda e: e.dma_start(out=pt[b].t[:], in_=pin_d[cs, :]), writes=[pt[b]])
                    yield
                    for kk in range(4):
                        k.dma("pool", yksem[b][kk], lambda e: e.indirect_dma_start(
                            out=yk[b][kk].t[:, :], out_offset=None, in_=Y_d[:, :],
                            in_offset=bass.IndirectOffsetOnAxis(ap=dest_all.t[:, c, kk:kk + 1], axis=0)),
                            reads=[dest_all], writes=[yk[b][kk]], extra=b_evs)
                    yield
                    k.op("dve", lambda e: e.tensor_tensor(out=x2.t[:], in0=x1c[b].t[:], in1=yk[b][0].t[:], op=ALU.add),
                         reads=[x1c[b], yk[b][0]], writes=[x2])
                    yield
                    k.op("pool", lambda e: e.tensor_tensor(out=yk[b][1].t[:], in0=yk[b][1].t[:], in1=yk[b][2].t[:], op=ALU.add),
                         reads=[yk[b][1], yk[b][2]], writes=[yk[b][1]])
                    yield
                    k.op("dve", lambda e: e.tensor_tensor(out=x2.t[:], in0=x2.t[:], in1=yk[b][3].t[:], op=ALU.add),
                         reads=[x2, yk[b][3]], writes=[x2])
                    yield
                    k.op("dve", lambda e: e.tensor_tensor(out=x2.t[:], in0=x2.t[:], in1=yk[b][1].t[:], op=ALU.add),
                         reads=[x2, yk[b][1]], writes=[x2])
                    yield
                    k.op("act", lambda e: e.activation(out=junk4.t[:], in_=x2.t[:], func=AF.Square, accum_out=ssc.t[:, 0:1]),
                         reads=[x2], writes=[junk4, ssc])
                    yield
                    k.op("act", lambda e: e.activation(out=rsc.t[:], in_=ssc.t[:], func=AF.Sqrt, scale=1.0 / DM, bias=EPS),
                         reads=[ssc], writes=[rsc])
                    yield
                    k.op("dve", lambda e: e.reciprocal(out=rsc.t[:], in_=rsc.t[:]), reads=[rsc], writes=[rsc])
                    yield
                    k.op("act", lambda e: e.activation(out=xnb.t[:], in_=x2.t[:], func=AF.Copy, scale=rsc.t[:, 0:1]),
                         reads=[x2, rsc], writes=[xnb])
                    yield
                    k.ops("pe", [(lambda e, j=j: e.transpose(out=p_t3.t[:, j, :], in_=xnb.t[:, j * 128:(j + 1) * 128],
                                                             identity=ident_bf.t[:])) for j in range(8)],
                          reads=[xnb, ident_bf], writes=[p_t3])
                    yield
                    k.op("dve", lambda e: e.tensor_tensor(out=xnT.t[:], in0=p_t3.t[:],
                                                          in1=gpgT.t[:, :, None].to_broadcast([128, 8, 128]), op=ALU.mult),
                         reads=[p_t3, gpgT], writes=[xnT])
                    yield
                    k.ops("pe", [(lambda e, j=j, h=h: e.matmul(p_ga.t[:, h * 512:(h + 1) * 512], lhsT=xnT.t[:, j, :],
                                                               rhs=wpg.t[:, j, h * 512:(h + 1) * 512], start=(j == 0), stop=(j == 7)))
                                 for h in range(2) for j in range(8)], reads=[xnT, wpg], writes=[p_ga])
                    yield
                    k.op("act", lambda e: e.activation(out=sgate.t[:], in_=p_ga.t[:], func=AF.Sigmoid), reads=[p_ga], writes=[sgate])
                    yield
                    k.ops("pe", [(lambda e, j=j: e.transpose(out=p_pt.t[:, j, :], in_=pt[b].t[:, j * 128:(j + 1) * 128],
                                                             identity=ident_f.t[:])) for j in range(2)],
                          reads=[pt[b], ident_f], writes=[p_pt])
                    yield
                    k.op("act", lambda e: e.copy(out=pT.t[:], in_=p_pt.t[:]), reads=[p_pt], writes=[pT])
                    yield
                    k.ops("pe", [(lambda e, j=j, h=h: e.matmul(p_e.t[:, h * 512:(h + 1) * 512], lhsT=pT.t[:, j, :],
                                                               rhs=wpp.t[:, j, h * 512:(h + 1) * 512], start=(j == 0), stop=(j == 1)))
                                 for h in range(2) for j in range(2)], reads=[pT, wpp], writes=[p_e])
                    yield
                    k.op("act", lambda e: e.activation(out=junk4.t[:], in_=p_e.t[:], func=AF.Square, accum_out=sse.t[:, 0:1]),
                         reads=[p_e], writes=[junk4, sse])
                    yield
                    k.op("act", lambda e: e.activation(out=rse.t[:], in_=sse.t[:], func=AF.Sqrt, scale=1.0 / DM, bias=EPS),
                         reads=[sse], writes=[rse])
                    yield
                    k.op("dve", lambda e: e.reciprocal(out=rse.t[:], in_=rse.t[:]), reads=[rse], writes=[rse])
                    yield
                    k.op("dve", lambda e: e.scalar_tensor_tensor(out=e1_.t[:], in0=p_e.t[:], scalar=rse.t[:, 0:1], in1=gpn_bc.t[:],
                                                                 op0=ALU.mult, op1=ALU.mult), reads=[p_e, rse, gpn_bc], writes=[e1_])
                    yield
                    k.op("pool", lambda e: e.tensor_tensor(out=e1_.t[:], in0=e1_.t[:], in1=sgate.t[:], op=ALU.mult),
                         reads=[e1_, sgate], writes=[e1_])
                    yield
                    k.op("dve", lambda e: e.tensor_tensor(out=x3.t[:], in0=x2.t[:], in1=e1_.t[:], op=ALU.add),
                         reads=[x2, e1_], writes=[x3])
                    yield
                    k.op("act", lambda e: e.activation(out=junk4.t[:], in_=x3.t[:], func=AF.Square, accum_out=ssf.t[:, 0:1]),
                         reads=[x3], writes=[junk4, ssf])
                    yield
                    k.op("act", lambda e: e.activation(out=rsf.t[:], in_=ssf.t[:], func=AF.Sqrt, scale=1.0 / DM, bias=EPS),
                         reads=[ssf], writes=[rsf])
                    yield
                    k.op("dve", lambda e: e.reciprocal(out=rsf.t[:], in_=rsf.t[:]), reads=[rsf], writes=[rsf])
                    yield
                    k.op("dve", lambda e: e.scalar_tensor_tensor(out=ot[b].t[:], in0=x3.t[:], scalar=rsf.t[:, 0:1], in1=gfin_bc.t[:],
                                                                 op0=ALU.mult, op1=ALU.mult), reads=[x3, rsf, gfin_bc], writes=[ot[b]])
                    yield
                    fin_evs.append(k.dma("sp", osem[b], lambda e: e.dma_start(out=out_d[cs, :], in_=ot[b].t[:]), reads=[ot[b]]))

                    yield
                run_chains([cbody(c) for c in range(NCH)], lag=6)

        tail = list(fin_evs[-2:]) + list(a2_evs) + list(b_evs)
        for sid_ev in tail:
            k.wait("sp", sid_ev)
        for sid_, sem_ in k.dma_objs.items():
            k.wait("sp", (sem_, k.dma_cnt[sid_]))
        build.stats = (k.ninst, k.nwaits, dict(k.cnt))
    return nc


def host_layout(inp):
    f = np.float32

    def colT(v, n):
        return np.ascontiguousarray(np.asarray(v, f).reshape(n, 128).T)

    def bc(v):
        v = np.asarray(v, f).reshape(1, -1)
        return np.ascontiguousarray(np.broadcast_to(v, (128, v.shape[1])))

    cw = np.asarray(inp["conv_w"][0], f)
    conv_wT = np.ascontiguousarray(cw.reshape(4, 32, 128).transpose(2, 1, 0))
    bgu = np.asarray(inp["b_gate_up"][0], f)
    bguT = np.ascontiguousarray(bgu.reshape(NE, 16, 128).transpose(2, 0, 1))
    invc = np.zeros((128, 4, 16), f)
    for gi, w in enumerate((2, 4, 8, 16)):
        invc[:, gi, :] = 1.0 / np.minimum(np.arange(16) + 1, w).astype(f)
    ebase = (1 + np.arange(NE) * CAP).astype(f)
    shared = {
        "w_in": np.ascontiguousarray(inp["w_in"][0], f),
        "conv_wT": conv_wT,
        "conv_bT": colT(inp["conv_b"][0], 32),
        "conv_brow": np.ascontiguousarray(np.asarray(inp["conv_b"][0], f).reshape(1, 4096)),
        "dt_bias_bc": bc(inp["dt_bias"][0]),
        "a_log_bc": bc(inp["a_log"][0]),
        "d_skip_bc": bc(inp["d_skip"][0]),
        "gmixT": colT(inp["mix_norm_g"][0], 8),
        "gssdT": colT(inp["ssd_norm_g"][0], 16),
        "pool_w": np.ascontiguousarray(inp["pool_w"][0], f),
        "pscT": colT(inp["pool_scale"][0], 8),
        "invc": invc,
        "w_out": np.ascontiguousarray(inp["w_out"][0], f),
        "gffn_bc": bc(inp["ffn_norm_g"][0]),
        "router_w": np.ascontiguousarray(inp["router_w"][0], f),
        "rb_bc": bc(inp["router_b"][0]),
        "ebase_bc": bc(ebase),
        "w_gate_up": np.ascontiguousarray(inp["w_gate_up"][0], f),
        "bguT": bguT,
        "w_down": np.ascontiguousarray(inp["w_down"][0], f),
        "b_down": np.ascontiguousarray(inp["b_down"][0], f),
        "gpgT": colT(inp["ple_gate_norm_g"][0], 8),
        "w_ple_gate": np.ascontiguousarray(inp["w_ple_gate"][0], f),
        "w_ple_proj": np.ascontiguousarray(inp["w_ple_proj"][0], f),
        "gpn_bc": bc(inp["ple_norm_g"][0]),
        "gfin_bc": bc(inp["final_norm_g"]),
    }
    return shared


def kernel(**inputs):
    inp = {k_: np.asarray(v) for k_, v in inputs.items()}
    shared = host_layout(inp)
    x = np.asarray(inp["x"], np.float32)
    p = np.asarray(inp["p"], np.float32)[0]
    nb = x.shape[0]
    in_maps = []
    for b in range(nb):
        m = dict(shared)
        m["x"] = np.ascontiguousarray(x[b])
        m["p"] = np.ascontiguousarray(p[b])
        in_maps.append(m)
    nc = build()
    res = run_bass_kernel_spmd(nc, in_maps, core_ids=list(range(nb)))
    return np.stack([np.asarray(r["out"], np.float32) for r in res.results], axis=0)
```

```python
from contextlib import ExitStack
import numpy as np
import concourse.bass as bass
import concourse.mybir as mybir
from concourse.bass_utils import run_bass_kernel_spmd

F32 = mybir.dt.float32
BF16 = mybir.dt.bfloat16
I32 = mybir.dt.int32
AF = mybir.ActivationFunctionType
ALU = mybir.AluOpType
AX = mybir.AxisListType

L = 4096
DM = 1024
NCH = 32
NSC = 8
D_IN = 7200
NE = 32
CAP = 1024
NBLK = CAP // 128
NSLOT = NE * CAP
SLOT_TAB = 128 * 257
EPS = 1e-6


class R:
    __slots__ = ("w", "rs")

    def __init__(self):
        self.w = None
        self.rs = []


class T:
    def __init__(self, t):
        self.t = t
        self.r = R()


class K:
    def __init__(self, nc, sems):
        self.nc = nc
        self.engs = {"pe": nc.tensor, "dve": nc.vector, "act": nc.scalar,
                     "pool": nc.gpsimd, "sp": nc.sync}
        self.psem = sems
        self.cnt = {k: 0 for k in self.engs}
        self.waited = {k: {} for k in self.engs}
        self.dma_cnt = {}
        self.dma_objs = {}
        self.ninst = 0
        self.nwaits = 0

    def wait(self, ek, ev):
        if ev is None:
            return
        sem, val = ev
        sid = id(sem)
        if sid in self.dma_cnt:
            val = max(val, self.dma_cnt[sid])
        w = self.waited[ek]
        if w.get(sid, 0) >= val:
            return
        self.engs[ek].wait_ge(sem, val)
        self.nwaits += 1
        w[sid] = val

    def _deps(self, ek, reads, writes, extra):
        for t in reads:
            self.wait(ek, t.r.w)
        for t in writes:
            self.wait(ek, t.r.w)
            for e in t.r.rs:
                self.wait(ek, e)
        for e in extra:
            self.wait(ek, e)

    def _commit(self, ev, reads, writes):
        for t in reads:
            t.r.rs.append(ev)
        for t in writes:
            t.r.w = ev
            t.r.rs = []

    def barrier(self):
        for ek in self.engs:
            for o in self.engs:
                if self.cnt[o] > 0:
                    self.wait(ek, (self.psem[o], self.cnt[o]))
            for sid_, sem_ in self.dma_objs.items():
                self.wait(ek, (sem_, self.dma_cnt[sid_]))

    def op(self, ek, fn, reads=(), writes=(), extra=()):
        self._deps(ek, reads, writes, extra)
        ins = fn(self.engs[ek])
        self.cnt[ek] += 1
        ins.then_inc(self.psem[ek], 1)
        ev = (self.psem[ek], self.cnt[ek])
        self._commit(ev, reads, writes)
        self.ninst += 1
        return ev

    def ops(self, ek, fns, reads=(), writes=(), extra=()):
        self._deps(ek, reads, writes, extra)
        ins = None
        for fn in fns:
            ins = fn(self.engs[ek])
            self.ninst += 1
        self.cnt[ek] += 1
        ins.then_inc(self.psem[ek], 1)
        ev = (self.psem[ek], self.cnt[ek])
        self._commit(ev, reads, writes)
        return ev

    def dma(self, ek, sem, fns, reads=(), writes=(), extra=()):
        self._deps(ek, reads, writes, extra)
        if not isinstance(fns, (list, tuple)):
            fns = [fns]
        sid = id(sem)
        cur = self.dma_cnt.get(sid, 0)
        for f in fns:
            f(self.engs[ek]).then_inc(sem, 16)
            cur += 16
            self.ninst += 1
        self.dma_cnt[sid] = cur
        self.dma_objs[sid] = sem
        ev = (sem, cur)
        self._commit(ev, reads, writes)
        return ev


def run_chains(gens, lag=4, width=2):
    pending = list(gens)
    active = []
    while pending or active:
        if len(active) < width and pending and (not active or active[-1][1] >= lag):
            active.append([pending.pop(0), 0])
        for a in list(active):
            try:
                next(a[0])
                a[1] += 1
            except StopIteration:
                active.remove(a)


def build(debug=None, phases="A0,A1,A1p,A2,B,C"):
    phases = set(phases.split(","))
    debug = debug or ()
    nc = bass.Bass("TRN2", target_bir_lowering=False)

    def din(name, shape, dt=F32):
        return nc.dram_tensor(name, list(shape), dt, kind="ExternalInput").ap()

    x_d = din("x", [L, DM])
    pin_d = din("p", [L, 256])
    w_in_d = din("w_in", [DM, D_IN])
    conv_wT_d = din("conv_wT", [128, 32, 4])
    conv_bT_d = din("conv_bT", [128, 32])
    conv_brow_d = din("conv_brow", [1, 4096])
    dtb_d = din("dt_bias_bc", [128, 32])
    alog_d = din("a_log_bc", [128, 32])
    dsk_d = din("d_skip_bc", [128, 32])
    gmixT_d = din("gmixT", [128, 8])
    gssdT_d = din("gssdT", [128, 16])
    pool_w_d = din("pool_w", [4, 256, 256])
    pscT_d = din("pscT", [128, 8])
    invc_d = din("invc", [128, 4, 16])
    w_out_d = din("w_out", [3072, DM])
    gffn_bc_d = din("gffn_bc", [128, DM])
    rw_d = din("router_w", [DM, NE])
    rb_d = din("rb_bc", [128, NE])
    ebase_d = din("ebase_bc", [128, NE])
    wgu_d = din("w_gate_up", [NE, DM, 2048])
    bguT_d = din("bguT", [128, NE, 16])
    wd_d = din("w_down", [NE, DM, DM])
    bd_d = din("b_down", [NE, DM])
    gpgT_d = din("gpgT", [128, 8])
    wpg_d = din("w_ple_gate", [DM, DM])
    wpp_d = din("w_ple_proj", [256, DM])
    gpn_bc_d = din("gpn_bc", [128, DM])
    gfin_bc_d = din("gfin_bc", [128, DM])
    out_d = nc.dram_tensor("out", [L, DM], F32, kind="ExternalOutput").ap()

    def dscr(name, shape, dt, dbg=False):
        kind = "ExternalOutput" if (name in debug) else "Internal"
        return nc.dram_tensor(name, list(shape), dt, kind=kind).ap()

    ymT_d = dscr("ymT", [24, 128, L], BF16)
    x1_d = dscr("x1d", [L, DM], F32)
    h2_d = dscr("h2d", [L + 1, DM], BF16)
    G_d = dscr("Gd", [L + 1, NE], F32)
    slot_d = dscr("slotd", [SLOT_TAB, 2], I32)
    Y_d = dscr("Yd", [NSLOT + 1, DM], F32)

    with ExitStack() as es:
        E = es.enter_context
        sems = {k: E(nc.semaphore("prog_" + k)) for k in ["pe", "dve", "act", "pool", "sp"]}
        k = K(nc, sems)
        nsem = [0]

        def dsem():
            nsem[0] += 1
            return E(nc.semaphore("d%d" % nsem[0]))

        def sb(es_, name, shape, dt=F32):
            return T(es_.enter_context(nc.sbuf_tensor("s_" + name, list(shape), dt)))

        def ps(es_, name, shape, dt=F32):
            esz = 4 if dt == F32 else 2
            n = 1
            for d_ in shape[1:]:
                n *= d_
            per_bank = 2048 // esz
            nb_ = (n + per_bank - 1) // per_bank
            base = es_.enter_context(nc.psum_tensor("ps_" + name, [128, nb_ * per_bank], dt))
            v = base[:, 0:n]
            if len(shape) == 3:
                v = v.rearrange("p (a b) -> p a b", a=shape[1])
            return T(v)

        def psbank(es_, name, dt=F32):
            per_bank = 2048 // (4 if dt == F32 else 2)
            return es_.enter_context(nc.psum_tensor("ps_" + name, [128, per_bank], dt))

        ident_bf = sb(es, "ident_bf", [128, 128], BF16)
        ident_f = sb(es, "ident_f", [128, 128], F32)
        Mle = sb(es, "Mle", [128, 128], F32)
        Mgt = sb(es, "Mgt", [128, 128], F32)
        Mlt = sb(es, "Mlt", [128, 128], F32)
        ones_f = sb(es, "ones_f", [128, 128], F32)
        dest_all = sb(es, "dest_all", [128, NCH, 4], I32)
        tokid = sb(es, "tokid", [128, NCH, 2], I32)
        zrow = sb(es, "zrow", [1, DM], F32)
        zrow_bf = sb(es, "zrow_bf", [1, DM], BF16)

        def dump(name, t, ap=None):
            if name not in debug:
                return
            a = ap if ap is not None else t.t[:]
            dd = nc.dram_tensor("dbg_" + name, list(a.shape), a.dtype, kind="ExternalOutput").ap()
            k.dma("sp", dsem(), lambda e: e.dma_start(out=dd, in_=a), reads=[t])

        def cst(t, fn):
            k.op("pool", fn, writes=[t])

        cst(ident_bf, lambda e: e.memset(ident_bf.t[:], 1.0))
        cst(ident_bf, lambda e: e.affine_select(out=ident_bf.t[:], in_=ident_bf.t[:], pattern=[[-1, 128]],
                                               compare_op=ALU.is_equal, fill=0.0, base=0, channel_multiplier=1))
        cst(ident_f, lambda e: e.memset(ident_f.t[:], 1.0))
        cst(ident_f, lambda e: e.affine_select(out=ident_f.t[:], in_=ident_f.t[:], pattern=[[-1, 128]],
                                              compare_op=ALU.is_equal, fill=0.0, base=0, channel_multiplier=1))
        cst(Mle, lambda e: e.memset(Mle.t[:], 1.0))
        cst(Mle, lambda e: e.affine_select(out=Mle.t[:], in_=Mle.t[:], pattern=[[1, 128]],
                                          compare_op=ALU.is_ge, fill=0.0, base=0, channel_multiplier=-1))
        cst(Mgt, lambda e: e.memset(Mgt.t[:], 1.0))
        cst(Mgt, lambda e: e.affine_select(out=Mgt.t[:], in_=Mgt.t[:], pattern=[[-1, 128]],
                                          compare_op=ALU.is_gt, fill=0.0, base=0, channel_multiplier=1))
        cst(Mlt, lambda e: e.memset(Mlt.t[:], 1.0))
        cst(Mlt, lambda e: e.affine_select(out=Mlt.t[:], in_=Mlt.t[:], pattern=[[1, 128]],
                                          compare_op=ALU.is_gt, fill=0.0, base=0, channel_multiplier=-1))
        cst(ones_f, lambda e: e.memset(ones_f.t[:], 1.0))
        neghalf_p = sb(es, "neghalf_p", [128, 1], F32)
        cst(neghalf_p, lambda e: e.memset(neghalf_p.t[:], -0.5))
        cst(zrow, lambda e: e.memset(zrow.t[:], 0.0))
        cst(zrow_bf, lambda e: e.memset(zrow_bf.t[:], 0.0))
        cst(tokid, lambda e: e.iota(tokid.t[:], pattern=[[128, NCH], [0, 2]], base=0, channel_multiplier=1))
        cst(dest_all, lambda e: e.memset(dest_all.t[:], 0))

        sem_c = dsem()
        sem_cp = dsem()

        def load_const(es_, name, shape, src, dt=F32, q="sp"):
            t = sb(es_, name, shape, dt)
            k.dma(q, sem_c, lambda e: e.dma_start(out=t.t[:], in_=src), writes=[t])
            return t

        if "A0" in phases:
            with ExitStack() as esA:
                hT = sb(esA, "hT", [128, 8, L], BF16)
                dt_all = sb(esA, "dt_all", [128, NCH, 32], F32)
                a_all = sb(esA, "a_all", [128, NCH, 32], F32)
                e_all = sb(esA, "e_all", [128, NCH, 3, 32], F32)
                gmixT = load_const(esA, "gmixT", [128, 8], gmixT_d)
                gssdT = load_const(esA, "gssdT", [128, 16], gssdT_d)
                conv_wT = load_const(esA, "conv_wT", [128, 32, 4], conv_wT_d)
                conv_bT = load_const(esA, "conv_bT", [128, 32], conv_bT_d)
                dsk = load_const(esA, "dsk", [128, 32], dsk_d)
                pscT = load_const(esA, "pscT", [128, 8], pscT_d)
                invc = load_const(esA, "invc", [128, 4, 16], invc_d)

                with ExitStack() as e0:
                    dtb = load_const(e0, "dtb", [128, 32], dtb_d)
                    alog = load_const(e0, "alog", [128, 32], alog_d)
                    Abc = sb(e0, "Abc", [128, 32], F32)
                    wdt = sb(e0, "wdt", [128, 8, 32], BF16)
                    k.dma("pool", sem_cp, lambda e: e.dma_start(
                        out=wdt.t[:], in_=w_in_d[:, 6144:6176].rearrange("(k p) n -> p k n", p=128)), writes=[wdt])
                    k.op("act", lambda e: e.activation(out=Abc.t[:], in_=alog.t[:], func=AF.Exp), reads=[alog], writes=[Abc])
                    k.op("dve", lambda e: e.tensor_scalar(out=Abc.t[:], in0=Abc.t[:], scalar1=-1.0, scalar2=None, op0=ALU.mult),
                         reads=[Abc], writes=[Abc])
                    xt = [sb(e0, "xt%d" % i, [128, DM], F32) for i in range(2)]
                    xsem = [dsem(), dsem()]
                    junk_l = [sb(e0, "junk%d" % i_, [128, DM], F32) for i_ in range(2)]
                    ss_l = [sb(e0, "ss%d" % i_, [128, 1], F32) for i_ in range(2)]
                    rstd_l = [sb(e0, "rstd%d" % i_, [128, 1], F32) for i_ in range(2)]
                    xn_l = [sb(e0, "xn%d" % i_, [128, DM], BF16) for i_ in range(2)]
                    tpA_l = [ps(e0, "tpA%d" % i_, [128, 8, 128], BF16) for i_ in range(2)]
                    pdt_l = [ps(e0, "pdt%d" % i_, [128, 32], F32) for i_ in range(2)]
                    pcs_l = [ps(e0, "pcs%d" % i_, [128, 3, 32], F32) for i_ in range(2)]
                    dtr_l = [sb(e0, "dtr%d" % i_, [128, 32], F32) for i_ in range(2)]
                    t1_l = [sb(e0, "t1%d" % i_, [128, 32], F32) for i_ in range(2)]
                    t2_l = [sb(e0, "t2%d" % i_, [128, 32], F32) for i_ in range(2)]
                    def a0body(c):
                        xc_ = xt[c % 2]
                        cs = slice(c * 128, (c + 1) * 128)
                        junk = junk_l[c % 2]; ss = ss_l[c % 2]; rstd = rstd_l[c % 2]; xn = xn_l[c % 2]; dtr = dtr_l[c % 2]; t1 = t1_l[c % 2]; t2 = t2_l[c % 2]; tpA = tpA_l[c % 2]; pdt = pdt_l[c % 2]; pcs = pcs_l[c % 2]
                        yield
                        k.dma("sp", xsem[c % 2], lambda e: e.dma_start(out=xc_.t[:], in_=x_d[cs, :]), writes=[xc_])
                        yield
                        k.op("act", lambda e: e.activation(out=junk.t[:], in_=xc_.t[:], func=AF.Square, accum_out=ss.t[:, 0:1]),
                             reads=[xc_], writes=[junk, ss])
                        yield
                        k.op("pool", lambda e: e.tensor_scalar(out=rstd.t[:], in0=ss.t[:], scalar1=1.0 / DM, scalar2=EPS,
                                                               op0=ALU.mult, op1=ALU.add), reads=[ss], writes=[rstd])
                        yield
                        k.op("pool", lambda e: e.tensor_tensor(out=rstd.t[:], in0=rstd.t[:], in1=neghalf_p.t[:], op=ALU.pow),
                             reads=[rstd, neghalf_p], writes=[rstd])
                        yield
                        k.op("act", lambda e: e.activation(out=xn.t[:], in_=xc_.t[:], func=AF.Copy, scale=rstd.t[:, 0:1]),
                             reads=[xc_, rstd], writes=[xn])
                        yield
                        k.ops("pe", [(lambda e, j=j: e.transpose(out=tpA.t[:, j, :], in_=xn.t[:, j * 128:(j + 1) * 128],
                                                                 identity=ident_bf.t[:])) for j in range(8)],
                              reads=[xn, ident_bf], writes=[tpA])
                        yield
                        k.op("dve", lambda e: e.tensor_tensor(out=hT.t[:, :, cs], in0=tpA.t[:],
                                                              in1=gmixT.t[:, :, None].to_broadcast([128, 8, 128]), op=ALU.mult),
                             reads=[tpA, gmixT], writes=[hT])
                        yield
                        k.ops("pe", [(lambda e, j=j: e.matmul(pdt.t[:], lhsT=hT.t[:, j, cs], rhs=wdt.t[:, j, :],
                                                              start=(j == 0), stop=(j == 7))) for j in range(8)],
                              reads=[hT, wdt], writes=[pdt])
                        yield
                        k.op("dve", lambda e: e.tensor_tensor(out=dtr.t[:], in0=pdt.t[:], in1=dtb.t[:], op=ALU.add),
                             reads=[pdt, dtb], writes=[dtr])
                        yield
                        k.op("act", lambda e: e.activation(out=t1.t[:], in_=dtr.t[:], func=AF.Abs),
                             reads=[dtr], writes=[t1])
                        yield
                        k.op("act", lambda e: e.activation(out=t2.t[:], in_=t1.t[:], func=AF.Exp, scale=-1.0), reads=[t1], writes=[t2])
                        yield
                        k.op("act", lambda e: e.activation(out=t1.t[:], in_=t2.t[:], func=AF.Ln, bias=1.0), reads=[t2], writes=[t1])
                        yield
                        k.op("dve", lambda e: e.scalar_tensor_tensor(out=dt_all.t[:, c, :], in0=dtr.t[:], scalar=0.0, in1=t1.t[:],
                                                                     op0=ALU.max, op1=ALU.add),
                             reads=[dtr, t1], writes=[dt_all])
                        yield
                        k.op("dve", lambda e: e.tensor_tensor(out=a_all.t[:, c, :], in0=dt_all.t[:, c, :], in1=Abc.t[:], op=ALU.mult),
                             reads=[dt_all, Abc], writes=[a_all])
                        yield
                        k.ops("pe", [
                            lambda e: e.matmul(pcs.t[:, 0, :], lhsT=Mle.t[:], rhs=a_all.t[:, c, :], start=True, stop=True),
                            lambda e: e.matmul(pcs.t[:, 1, :], lhsT=Mgt.t[:], rhs=a_all.t[:, c, :], start=True, stop=True),
                            lambda e: e.matmul(pcs.t[:, 2, :], lhsT=ones_f.t[:], rhs=a_all.t[:, c, :], start=True, stop=True),
                        ], reads=[a_all, Mle, Mgt, ones_f], writes=[pcs])
                        yield
                        k.op("act", lambda e: e.activation(out=e_all.t[:, c, :, :], in_=pcs.t[:], func=AF.Exp),
                             reads=[pcs], writes=[e_all])
                        yield
                    run_chains([a0body(c) for c in range(NCH)], lag=6)
                    k.barrier()
                dump("hT", hT); dump("dt_all", dt_all); dump("a_all", a_all); dump("e_all", e_all)
                if "A1" in phases:
                    with ExitStack() as e1:
                        wg = [sb(e1, "wg%d" % i, [128, 8, 768], BF16) for i in range(2)]
                        wsem = [dsem(), dsem()]
                        dg = [sb(e1, "dg%d" % i, [128, 4, 4, 128], BF16) for i in range(2)]
                        ust = [sb(e1, "ust%d" % i, [128, 4, 515], BF16) for i in range(2)]
                        xc = [sb(e1, "xc%d" % i, [128, 4, 512], BF16) for i in range(2)]
                        state = sb(e1, "state", [128, 256], F32)
                        state_bf = [sb(e1, "state_bf%d" % i, [128, 256], BF16) for i in range(3)]
                        zstate = sb(e1, "zstate", [128, 256], BF16)
                        ynT = [sb(e1, "ynT%d" % i, [128, 2, 512], BF16) for i in range(2)]
                        ysem = [dsem(), dsem()]
                        sz = [sb(e1, "sz%d" % i, [128, 256], BF16) for i in range(3)]
                        xbtm = [sb(e1, "xbtm%d" % i, [128, 384], BF16) for i in range(3)]
                        xd = [sb(e1, "xd%d" % i, [128, 4, 64], BF16) for i in range(3)]
                        y4 = [sb(e1, "y4_%d" % i, [128, 256], F32) for i in range(3)]
                        xde = [sb(e1, "xde%d" % i, [128, 4, 64], BF16) for i in range(2)]
                        cbm = [sb(e1, "cbm%d" % i, [128, 128], F32) for i in range(2)]
                        lh = [sb(e1, "lh%d" % i, [128, 4, 128], F32) for i in range(2)]
                        Ex = [sb(e1, "Ex%d" % i, [128, 4, 128], F32) for i in range(2)]
                        MT = [sb(e1, "MT%d" % i, [128, 4, 128], BF16) for i in range(2)]
                        y1 = [sb(e1, "y1_%d" % i, [128, 4, 64], F32) for i in range(2)]
                        tD = [sb(e1, "tD%d" % i, [128, 4, 64], F32) for i in range(2)]
                        yb = [sb(e1, "yb%d" % i, [128, 256], BF16) for i in range(2)]
                        junk2 = sb(e1, "junk2", [128, 256], F32)
                        ss2 = [sb(e1, "ss2_%d" % i, [128, 1], F32) for i in range(2)]
                        rs2 = [sb(e1, "rs2_%d" % i, [128, 1], F32) for i in range(2)]
                        p_u = [ps(e1, "p_u%d" % i, [128, 512], F32) for i in range(2)]
                        bk1 = psbank(e1, "bk1", F32)
                        p_z = T(bk1[:, 0:256])
                        p_cb = T(bk1[:, 256:384])
                        p_cb.r = p_z.r
                        p_s = ps(e1, "p_s", [128, 256], F32)
                        p_seg1 = ps(e1, "p_seg", [128, 4, 128], F32)
                        p_seg = [p_seg1, p_seg1]
                        bk4 = psbank(e1, "bk4", F32)
                        p_y = T(bk4[:, 0:256])
                        p_yo = T(bk4[:, 256:512])
                        p_yo.r = p_y.r
                        bk6 = psbank(e1, "bk6", BF16)
                        p_tpx = T(bk6[:, 0:384])
                        bk7 = psbank(e1, "bk7", BF16)
                        p_tpy = T(bk7[:, 0:256])
                        k.op("pool", lambda e: e.memset(zstate.t[:], 0.0), writes=[zstate])
                        thz = [sb(e1, "thz%d" % i, [128, 256], F32) for i in range(2)]
                        thc = [sb(e1, "thc%d" % i, [128, 512], F32) for i in range(2)]
                        neghalf = sb(e1, "neghalf", [128, 1], F32)
                        k.op("pool", lambda e: e.memset(neghalf.t[:], -0.5), writes=[neghalf])
                        ones_row = sb(e1, "ones_row", [1, 512], BF16)
                        k.op("pool", lambda e: e.memset(ones_row.t[:], 1.0), writes=[ones_row])
                        cb_row = sb(e1, "cb_row", [1, 4096], F32)
                        k.dma("sp", sem_c, lambda e: e.dma_start(out=cb_row.t[:], in_=conv_brow_d), writes=[cb_row])
                        hb_row = sb(e1, "hb_row", [1, 4096], BF16)
                        k.op("dve", lambda e: e.tensor_scalar(out=hb_row.t[:], in0=cb_row.t[:], scalar1=0.5, scalar2=None, op0=ALU.mult),
                             reads=[cb_row], writes=[hb_row])

                        def group_setup(g):
                            w = wg[g % 2]
                            srcs = [(0, 2048 + g * 256, 256), (256, 4096 + g * 128, 128),
                                    (384, 5120 + g * 128, 128), (512, g * 256, 256)]
                            k.dma("pool", wsem[g % 2],
                                  [(lambda e, o=o, s=s, n=n: e.dma_start(
                                      out=w.t[:, :, o:o + n],
                                      in_=w_in_d[:, s:s + n].rearrange("(k p) n -> p k n", p=128))) for (o, s, n) in srcs],
                                  writes=[w])
                            d_ = dg[g % 2]
                            chunks = [2 * g, 2 * g + 1, 16 + g, 24 + g]
                            k.ops("pool", [(lambda e, cc=cc, kk=kk: e.tensor_scalar(
                                out=d_.t[:, cc, kk, :], in0=ident_bf.t[:], scalar1=conv_wT.t[:, chunks[cc], kk:kk + 1],
                                scalar2=0.5, op0=ALU.mult, op1=ALU.mult)) for cc in range(4) for kk in range(4)],
                                reads=[ident_bf, conv_wT], writes=[d_])
                            k.op("pool", lambda e: e.tensor_scalar(out=w.t[:, :, 512:768], in0=w.t[:, :, 512:768], scalar1=0.5,
                                                                   scalar2=0.0, op0=ALU.mult, op1=ALU.add), reads=[w], writes=[w])

                        NT = 8 * NCH

                        def dec(n):
                            g = n // NCH
                            c = n % NCH
                            return g, c, c // 4, c % 4

                        def S0a(si):
                            g, sc = si // NSC, si % NSC
                            w = wg[g % 2]
                            us = ust[si % 2]
                            ts = sc * 512
                            if sc == 0:
                                k.op("pool", lambda e: e.memset(us.t[:, :, 0:3], 0.0), writes=[us])
                            for cc in range(4):
                                pu = p_u[cc % 2]
                                k.ops("pe", [(lambda e, j=j: e.matmul(pu.t[:], lhsT=w.t[:, j, cc * 128:(cc + 1) * 128],
                                                                      rhs=hT.t[:, j, ts:ts + 512], start=(j == 0), stop=(j == 7)))
                                             for j in range(8)], reads=[w, hT], writes=[pu])
                                k.op("act", lambda e: e.copy(out=us.t[:, cc, 3:515], in_=pu.t[:]), reads=[pu], writes=[us])
                            if sc + 1 < NSC:
                                un = ust[(si + 1) % 2]
                                k.op("pool", lambda e: e.tensor_copy(out=un.t[:, :, 0:3], in_=us.t[:, :, 512:515]),
                                     reads=[us], writes=[un])

                        def S0b(si):
                            g, sc = si // NSC, si % NSC
                            d_ = dg[g % 2]
                            us = ust[si % 2]
                            xo = xc[si % 2]
                            chunks = [2 * g, 2 * g + 1, 16 + g, 24 + g]
                            for cc in range(4):
                                pu = p_u[cc % 2]
                                ch = chunks[cc]
                                k.ops("pe", [(lambda e, kk=kk: e.matmul(pu.t[:], lhsT=d_.t[:, cc, kk, :],
                                                                        rhs=us.t[:, cc, kk:kk + 512], start=(kk == 0), stop=False))
                                             for kk in range(4)] +
                                      [lambda e: e.matmul(pu.t[:], lhsT=hb_row.t[0:1, ch * 128:(ch + 1) * 128],
                                                          rhs=ones_row.t[0:1, :], start=False, stop=True)],
                                      reads=[d_, us, hb_row, ones_row], writes=[pu])
                                tc_ = thc[cc % 2]
                                k.op("act", lambda e: e.activation(out=tc_.t[:], in_=pu.t[:], func=AF.Tanh),
                                     reads=[pu], writes=[tc_])
                                k.op("dve", lambda e: e.scalar_tensor_tensor(out=xo.t[:, cc, :], in0=tc_.t[:], scalar=1.0, in1=pu.t[:],
                                                                             op0=ALU.add, op1=ALU.mult),
                                     reads=[tc_, pu], writes=[xo])

                        def ctx(n):
                            g, c, sc, q = dec(n)
                            si = g * NSC + sc
                            return dict(g=g, c=c, sc=sc, q=q, si=si, w=wg[g % 2], xo=xc[si % 2],
                                        qs=slice(q * 128, (q + 1) * 128), cs=slice(c * 128, (c + 1) * 128),
                                        g4=slice(g * 4, g * 4 + 4), b3=n % 3, b2=n % 2)

                        def A_pe(n):
                            x_ = ctx(n); w = x_["w"]; xo = x_["xo"]; qs = x_["qs"]; cs = x_["cs"]
                            k.ops("pe", [(lambda e, j=j: e.matmul(p_z.t[:], lhsT=hT.t[:, j, cs], rhs=w.t[:, j, 512:768],
                                                                  start=(j == 0), stop=(j == 7))) for j in range(8)],
                                  reads=[hT, w], writes=[p_z])
                            k.ops("pe", [(lambda e, cc=cc: e.transpose(out=p_tpx.t[:, cc * 128:(cc + 1) * 128],
                                                                       in_=xo.t[:, cc, qs], identity=ident_bf.t[:]))
                                         for cc in range(3)], reads=[xo, ident_bf], writes=[p_tpx])
                            k.op("pe", lambda e: e.matmul(p_cb.t[:], lhsT=xo.t[:, 2, qs], rhs=xo.t[:, 3, qs],
                                                          start=True, stop=True), reads=[xo], writes=[p_cb])

                        def A_act(n):
                            x_ = ctx(n); b3 = x_["b3"]; b2 = x_["b2"]; c = x_["c"]; g = x_["g"]
                            k.op("act", lambda e: e.activation(out=thz[b2].t[:], in_=p_z.t[:], func=AF.Tanh),
                                 reads=[p_z], writes=[thz[b2]])
                            k.op("act", lambda e: e.copy(out=xbtm[b3].t[:], in_=p_tpx.t[:]), reads=[p_tpx], writes=[xbtm[b3]])
                            k.ops("act", [(lambda e, r=r: e.activation(out=lh[b2].t[:, r, :], in_=Mgt.t[:], func=AF.Copy,
                                                                       scale=a_all.t[:, c, g * 4 + r:g * 4 + r + 1]))
                                          for r in range(4)], reads=[Mgt, a_all], writes=[lh[b2]])

                        def A_dve(n):
                            x_ = ctx(n); b3 = x_["b3"]; b2 = x_["b2"]; c = x_["c"]; g4 = x_["g4"]
                            k.op("dve", lambda e: e.scalar_tensor_tensor(out=sz[b3].t[:], in0=thz[b2].t[:], scalar=1.0, in1=p_z.t[:],
                                                                         op0=ALU.add, op1=ALU.mult),
                                 reads=[thz[b2], p_z], writes=[sz[b3]])
                            k.op("dve", lambda e: e.tensor_tensor(
                                out=xd[b3].t[:], in0=xbtm[b3].t[:, 0:256].rearrange("p (r d) -> p r d", r=4),
                                in1=dt_all.t[:, c, g4, None].to_broadcast([128, 4, 64]), op=ALU.mult),
                                reads=[xbtm[b3], dt_all], writes=[xd[b3]])
                            k.op("dve", lambda e: e.tensor_tensor(out=cbm[b2].t[:], in0=p_cb.t[:], in1=Mle.t[:], op=ALU.mult),
                                 reads=[p_cb, Mle], writes=[cbm[b2]])

                        def A_pool(n):
                            x_ = ctx(n); b3 = x_["b3"]; b2 = x_["b2"]; c = x_["c"]; g4 = x_["g4"]
                            k.op("pool", lambda e: e.tensor_tensor(
                                out=xde[b2].t[:], in0=xd[b3].t[:],
                                in1=e_all.t[:, c, 1, g4, None].to_broadcast([128, 4, 64]), op=ALU.mult),
                                reads=[xd[b3], e_all], writes=[xde[b2]])

                        def B_pe(n):
                            x_ = ctx(n); b3 = x_["b3"]; b2 = x_["b2"]
                            k.ops("pe", [(lambda e, r=r: e.matmul(p_seg[b2].t[:, r, :], lhsT=lh[b2].t[:, r, :], rhs=Mle.t[:],
                                                                  start=True, stop=True)) for r in range(4)],
                                  reads=[lh[b2], Mle], writes=[p_seg[b2]])
                            k.op("pe", lambda e: e.matmul(p_s.t[:], lhsT=xbtm[b3].t[:, 256:384],
                                                          rhs=xde[b2].t[:].rearrange("p r d -> p (r d)"), start=True, stop=True),
                                 reads=[xbtm[b3], xde[b2]], writes=[p_s])

                        def B_act(n):
                            x_ = ctx(n); b2 = x_["b2"]
                            k.op("act", lambda e: e.activation(out=Ex[b2].t[:], in_=p_seg[b2].t[:], func=AF.Exp),
                                 reads=[p_seg[b2]], writes=[Ex[b2]])

                        def B_dve(n):
                            x_ = ctx(n); b2 = x_["b2"]; c = x_["c"]; g4 = x_["g4"]
                            k.op("dve", lambda e: e.tensor_tensor(
                                out=MT[b2].t[:], in0=Ex[b2].t[:],
                                in1=cbm[b2].t[:, None, :].to_broadcast([128, 4, 128]), op=ALU.mult),
                                reads=[Ex[b2], cbm[b2]], writes=[MT[b2]])
                            if c == 0:
                                k.op("dve", lambda e: e.tensor_copy(out=state.t[:], in_=p_s.t[:]), reads=[p_s], writes=[state])
                            else:
                                k.op("dve", lambda e: e.tensor_tensor(
                                    out=state.t[:].rearrange("p (r d) -> p r d", r=4),
                                    in0=state.t[:].rearrange("p (r d) -> p r d", r=4),
                                    in1=e_all.t[:, c, 2, g4, None].to_broadcast([128, 4, 64]), op=ALU.mult),
                                    reads=[state, e_all], writes=[state])
                                k.op("dve", lambda e: e.tensor_tensor(out=state.t[:], in0=state.t[:], in1=p_s.t[:], op=ALU.add),
                                     reads=[state, p_s], writes=[state])

                        def B_dve2(n):
                            x_ = ctx(n); b3 = x_["b3"]; b2 = x_["b2"]; g4 = x_["g4"]
                            k.op("dve", lambda e: e.tensor_tensor(
                                out=tD[b2].t[:], in0=xbtm[b3].t[:, 0:256].rearrange("p (r d) -> p r d", r=4),
                                in1=dsk.t[:, g4, None].to_broadcast([128, 4, 64]), op=ALU.mult),
                                reads=[xbtm[b3], dsk], writes=[tD[b2]])

                        def C_act(n):
                            x_ = ctx(n); b3 = x_["b3"]
                            k.op("act", lambda e: e.copy(out=state_bf[b3].t[:], in_=state.t[:]),
                                 reads=[state], writes=[state_bf[b3]])

                        def C_pe(n):
                            x_ = ctx(n); b3 = x_["b3"]; b2 = x_["b2"]; xo = x_["xo"]; qs = x_["qs"]; c = x_["c"]
                            k.ops("pe", [(lambda e, r=r: e.matmul(p_y.t[:, r * 64:(r + 1) * 64], lhsT=MT[b2].t[:, r, :],
                                                                  rhs=xd[b3].t[:, r, :], start=True, stop=True))
                                         for r in range(4)], reads=[MT[b2], xd[b3]], writes=[p_y])
                            st_prev = zstate if c == 0 else state_bf[(n - 1) % 3]
                            k.op("pe", lambda e: e.matmul(p_yo.t[:], lhsT=xo.t[:, 3, qs], rhs=st_prev.t[:],
                                                          start=True, stop=True), reads=[xo, st_prev], writes=[p_yo])

                        def C_dve(n):
                            x_ = ctx(n); b3 = x_["b3"]; b2 = x_["b2"]; c = x_["c"]; g4 = x_["g4"]
                            k.op("dve", lambda e: e.tensor_tensor(
                                out=y1[b2].t[:], in0=p_yo.t[:].rearrange("p (r d) -> p r d", r=4),
                                in1=e_all.t[:, c, 0, g4, None].to_broadcast([128, 4, 64]), op=ALU.mult),
                                reads=[p_yo, e_all], writes=[y1[b2]])
                            k.op("dve", lambda e: e.tensor_tensor(
                                out=y1[b2].t[:], in0=y1[b2].t[:],
                                in1=p_y.t[:].rearrange("p (r d) -> p r d", r=4), op=ALU.add),
                                reads=[y1[b2], p_y], writes=[y1[b2]])
                            k.op("dve", lambda e: e.tensor_tensor(out=y1[b2].t[:], in0=y1[b2].t[:], in1=tD[b2].t[:], op=ALU.add),
                                 reads=[y1[b2], tD[b2]], writes=[y1[b2]])
                            k.op("dve", lambda e: e.tensor_tensor(
                                out=y4[b3].t[:], in0=y1[b2].t[:].rearrange("p r d -> p (r d)"), in1=sz[b3].t[:], op=ALU.mult),
                                reads=[y1[b2], sz[b3]], writes=[y4[b3]])

                        def D_act(n):
                            x_ = ctx(n); b3 = x_["b3"]; b2 = x_["b2"]
                            k.op("act", lambda e: e.activation(out=junk2.t[:], in_=y4[b3].t[:], func=AF.Square,
                                                               accum_out=ss2[b2].t[:, 0:1]),
                                 reads=[y4[b3]], writes=[junk2, ss2[b2]])

                        def D_pool(n):
                            x_ = ctx(n); b2 = x_["b2"]
                            k.op("pool", lambda e: e.tensor_scalar(out=rs2[b2].t[:], in0=ss2[b2].t[:], scalar1=1.0 / 256, scalar2=EPS,
                                                                   op0=ALU.mult, op1=ALU.add), reads=[ss2[b2]], writes=[rs2[b2]])
                            k.op("pool", lambda e: e.tensor_tensor(out=rs2[b2].t[:], in0=rs2[b2].t[:], in1=neghalf.t[:], op=ALU.pow),
                                 reads=[rs2[b2], neghalf], writes=[rs2[b2]])

                        def E_dve(n):
                            x_ = ctx(n); b3 = x_["b3"]; b2 = x_["b2"]
                            k.op("dve", lambda e: e.tensor_scalar(out=yb[b2].t[:], in0=y4[b3].t[:], scalar1=rs2[b2].t[:, 0:1],
                                                                  scalar2=None, op0=ALU.mult),
                                 reads=[y4[b3], rs2[b2]], writes=[yb[b2]])

                        def F_pe(n):
                            x_ = ctx(n); b2 = x_["b2"]
                            k.ops("pe", [(lambda e, h=h: e.transpose(out=p_tpy.t[:, h * 128:(h + 1) * 128],
                                                                     in_=yb[b2].t[:, h * 128:(h + 1) * 128], identity=ident_bf.t[:]))
                                         for h in range(2)], reads=[yb[b2], ident_bf], writes=[p_tpy])

                        def F_act(n):
                            x_ = ctx(n); g = x_["g"]; qs = x_["qs"]; si = x_["si"]; q = x_["q"]; sc = x_["sc"]
                            yo = ynT[si % 2]
                            for h in range(2):
                                k.op("act", lambda e: e.activation(out=yo.t[:, h, qs], in_=p_tpy.t[:, h * 128:(h + 1) * 128], func=AF.Copy,
                                                                   scale=gssdT.t[:, 2 * g + h:2 * g + h + 1]),
                                     reads=[p_tpy, gssdT], writes=[yo])
                            if q == 3:
                                ts = sc * 512
                                k.dma("sp", ysem[si % 2], lambda e: e.dma_start(
                                    out=ymT_d[2 * g:2 * g + 2, :, ts:ts + 512].rearrange("f p t -> p f t"), in_=yo.t[:]),
                                    reads=[yo])

                        def ok(n):
                            return 0 <= n < NT

                        import os as _os
                        if _os.environ.get("A1_NOSKEW"):
                            for n in range(NT):
                                g, c, sc, q = dec(n)
                                if c == 0:
                                    group_setup(g)
                                if q == 0:
                                    S0a(g * NSC + sc)
                                    S0b(g * NSC + sc)
                                for fn in (A_pe, A_act, A_dve, A_pool, B_pe, B_act, B_dve, B_dve2, C_pe, C_act, C_dve, D_act, D_pool, E_dve, F_pe, F_act):
                                    fn(n)
                        else:
                            LAGS = dict(A=0, B=1, C=2, D=3, E=4, F=5)
                            group_setup(0)
                            S0a(0)
                            S0b(0)
                            S0a(1)
                            for t in range(NT + 5):
                                for fn, lag in ((F_pe, 5), (C_pe, 2), (B_pe, 1), (A_pe, 0),
                                                (F_act, 5), (C_act, 2), (B_act, 1), (A_act, 0), (D_act, 3),
                                                (E_dve, 4), (B_dve, 1), (B_dve2, 1), (C_dve, 2), (A_dve, 0),
                                                (D_pool, 3), (A_pool, 0)):
                                    if ok(t - lag):
                                        fn(t - lag)
                                if t % 4 == 1 and t // 4 + 1 < 8 * NSC:
                                    S0b(t // 4 + 1)
                                if t % 4 == 2 and t // 4 + 2 < 8 * NSC:
                                    S0a(t // 4 + 2)
                                if t % NCH == 4 and t // NCH + 1 < 8:
                                    group_setup(t // NCH + 1)
                k.barrier()
                if "A1p" in phases:
                    with ExitStack() as e2:
                        wp = sb(e2, "wp", [128, 8, 1024], BF16)
                        k.dma("pool", sem_cp, lambda e: e.dma_start(
                            out=wp.t[:], in_=w_in_d[:, 6176:7200].rearrange("(k p) n -> p k n", p=128)), writes=[wp])
                        pw = sb(e2, "pw", [128, 4, 2, 256], BF16)
                        k.dma("pool", sem_cp, lambda e: e.dma_start(
                            out=pw.t[:], in_=pool_w_d.rearrange("g (k p) n -> p g k n", p=128)), writes=[pw])
                        pst = [sb(e2, "pst%d" % i, [128, 8, 527], F32) for i in range(2)]
                        sA = sb(e2, "sA", [128, 2, 527], F32)
                        sB = sb(e2, "sB", [128, 2, 527], F32)
                        ypl = [sb(e2, "ypl%d" % i, [128, 2, 512], BF16) for i in range(2)]
                        tmp16 = sb(e2, "tmp16", [128, 2, 16], F32)
                        ypT = [sb(e2, "ypT%d" % i, [128, 2, 512], BF16) for i in range(2)]
                        psem_ = [dsem(), dsem()]
                        p_u2 = [ps(e2, "p_u2_%d" % i, [128, 512], F32) for i in range(2)]
                        p_p = [ps(e2, "p_p%d" % i, [128, 512], F32) for i in range(2)]
                        k.op("pool", lambda e: e.memset(pst[0].t[:, :, 0:15], 0.0), writes=[pst[0]])
                        it = 0
                        for sc in range(NSC):
                            ts = sc * 512
                            cur = pst[sc % 2]
                            nxt = pst[(sc + 1) % 2]
                            for pc in range(8):
                                pu = p_u2[pc % 2]
                                k.ops("pe", [(lambda e, j=j: e.matmul(pu.t[:], lhsT=wp.t[:, j, pc * 128:(pc + 1) * 128],
                                                                      rhs=hT.t[:, j, ts:ts + 512], start=(j == 0), stop=(j == 7)))
                                             for j in range(8)], reads=[wp, hT], writes=[pu])
                                k.op("act", lambda e: e.copy(out=cur.t[:, pc, 15:527], in_=pu.t[:]), reads=[pu], writes=[cur])
                            if sc + 1 < NSC:
                                k.op("pool", lambda e: e.tensor_copy(out=nxt.t[:, :, 0:15], in_=cur.t[:, :, 512:527]),
                                     reads=[cur], writes=[nxt])
                            for pg in range(4):
                                u = cur.t[:, 2 * pg:2 * pg + 2, :]
                                nlev = pg + 1
                                src = u
                                bufs = [sA, sB]
                                eng = "dve"
                                for lv in range(nlev):
                                    sh = 1 << lv
                                    lo = (1 << (lv + 1)) - 1
                                    dst = bufs[lv % 2]
                                    src_t = cur if lv == 0 else bufs[(lv - 1) % 2]
                                    s_ap = src
                                    k.op(eng, lambda e, dst=dst, s_ap=s_ap, lo=lo, sh=sh: e.tensor_tensor(
                                        out=dst.t[:, :, lo:527], in0=s_ap[:, :, lo:527], in1=s_ap[:, :, lo - sh:527 - sh], op=ALU.add),
                                        reads=[src_t], writes=[dst])
                                    src = dst.t[:, :, :]
                                fin = bufs[(nlev - 1) % 2]
                                wv = 1 << nlev
                                yp = ypl[it % 2]
                                k.op(eng, lambda e: e.scalar_tensor_tensor(
                                    out=yp.t[:], in0=fin.t[:, :, 15:527], scalar=1.0 / wv, in1=u[:, :, 15:527],
                                    op0=ALU.mult, op1=ALU.subtract), reads=[fin, cur], writes=[yp])
                                if sc == 0:
                                    k.op(eng, lambda e: e.tensor_tensor(
                                        out=tmp16.t[:], in0=fin.t[:, :, 15:31],
                                        in1=invc.t[:, pg, None, :].to_broadcast([128, 2, 16]), op=ALU.mult),
                                        reads=[fin, invc], writes=[tmp16])
                                    k.op(eng, lambda e: e.tensor_tensor(out=yp.t[:, :, 0:16], in0=tmp16.t[:], in1=u[:, :, 15:31],
                                                                        op=ALU.subtract), reads=[tmp16, cur], writes=[yp])
                                yT_ = ypT[it % 2]
                                for dc in range(2):
                                    pp = p_p[dc]
                                    k.ops("pe", [(lambda e, kc=kc: e.matmul(pp.t[:], lhsT=pw.t[:, pg, kc, dc * 128:(dc + 1) * 128],
                                                                            rhs=yp.t[:, kc, :], start=(kc == 0), stop=(kc == 1)))
                                                 for kc in range(2)], reads=[pw, yp], writes=[pp])
                                    k.op("act", lambda e: e.activation(out=yT_.t[:, dc, :], in_=pp.t[:], func=AF.Copy,
                                                                       scale=pscT.t[:, 2 * pg + dc:2 * pg + dc + 1]),
                                         reads=[pp, pscT], writes=[yT_])
                                k.dma("sp", psem_[it % 2], lambda e: e.dma_start(
                                    out=ymT_d[16 + 2 * pg:16 + 2 * pg + 2, :, ts:ts + 512].rearrange("f p t -> p f t"), in_=yT_.t[:]),
                                    reads=[yT_])
                                it += 1

        k.barrier()
        if "A2" in phases:
            with ExitStack() as e3:
                wout = sb(e3, "wout", [128, 24, DM], BF16)
                k.dma("pool", sem_cp, [(lambda e, i=i: e.dma_start(
                    out=wout.t[:, 6 * i:6 * i + 6, :],
                    in_=w_out_d[768 * i:768 * (i + 1), :].rearrange("(k p) n -> p k n", p=128))) for i in range(4)],
                    writes=[wout])
                gffn_bc = load_const(e3, "gffn_bc", [128, DM], gffn_bc_d)
                rw = load_const(e3, "rw", [128, 8, NE], rw_d.rearrange("(k p) n -> p k n", p=128))
                rb = load_const(e3, "rb", [128, NE], rb_d)
                ebase = load_const(e3, "ebase", [128, NE], ebase_d)
                s4096 = sb(e3, "s4096", [128, 514], I32)
                k.op("pool", lambda e: e.memset(s4096.t[:], L), writes=[s4096])
                ev_init = [
                    k.dma("sp", sem_c, lambda e: e.dma_start(out=slot_d.rearrange("(p f) o -> p (f o)", p=128), in_=s4096.t[:]),
                          reads=[s4096]),
                    k.dma("sp", sem_c, lambda e: e.dma_start(out=h2_d[L:L + 1, :], in_=zrow_bf.t[:]), reads=[zrow_bf]),
                    k.dma("sp", sem_c, lambda e: e.dma_start(out=G_d[L:L + 1, :], in_=zrow.t[:, 0:NE]), reads=[zrow]),
                    k.dma("sp", sem_c, lambda e: e.dma_start(out=Y_d[0:1, :], in_=zrow.t[:]), reads=[zrow]),
                ]
                ym = [sb(e3, "ym%d" % i, [128, 24, 512], BF16) for i in range(2)]
                ymsem = [dsem(), dsem()]
                xt2 = [sb(e3, "xt2_%d" % i, [128, DM], F32) for i in range(2)]
                xsem2 = [dsem(), dsem()]
                x1 = [sb(e3, "x1_%d" % i, [128, DM], F32) for i in range(2)]
                x1sem = [dsem(), dsem()]
                junk3_l = [sb(e3, "junk3%d" % i_, [128, DM], F32) for i_ in range(2)]
                ss3_l = [sb(e3, "ss3%d" % i_, [128, 1], F32) for i_ in range(2)]
                rs3_l = [sb(e3, "rs3%d" % i_, [128, 1], F32) for i_ in range(2)]
                h2f_l = [sb(e3, "h2f%d" % i_, [128, DM], F32) for i_ in range(2)]
                h2b = [sb(e3, "h2b%d" % i, [128, DM], BF16) for i in range(2)]
                h2sem = [dsem(), dsem()]
                h2T_l = [sb(e3, "h2T%d" % i_, [128, 8, 128], F32) for i_ in range(2)]
                lg_l = [sb(e3, "lg%d" % i_, [128, NE], F32) for i_ in range(2)]
                m8_l = [sb(e3, "m8%d" % i_, [128, 8], F32) for i_ in range(2)]
                nv1_l = [sb(e3, "nv1%d" % i_, [128, 1], F32) for i_ in range(2)]
                mask_l = [sb(e3, "mask%d" % i_, [128, NE], F32) for i_ in range(2)]
                ex_l = [sb(e3, "ex%d" % i_, [128, NE], F32) for i_ in range(2)]
                sm_l = [sb(e3, "sm%d" % i_, [128, 1], F32) for i_ in range(2)]
                Gt = [sb(e3, "Gt%d" % i, [128, NE], F32) for i in range(2)]
                gsem = [dsem(), dsem()]
                cnt = sb(e3, "cnt", [128, NE], F32)
                rank_l = [sb(e3, "rank%d" % i_, [128, NE], F32) for i_ in range(2)]
                vld_l = [sb(e3, "vld%d" % i_, [128, NE], F32) for i_ in range(2)]
                val_l = [sb(e3, "val%d" % i_, [128, NE], F32) for i_ in range(2)]
                v8_l = [sb(e3, "v8%d" % i_, [128, 8], F32) for i_ in range(2)]
                scsem = dsem()
                p_o_l = [ps(e3, "p_o%d" % i_, [128, DM], F32) for i_ in range(2)]
                p_tf = ps(e3, "p_tf", [128, 8, 128], F32)
                p_l = ps(e3, "p_l", [128, NE], F32)
                p_r = ps(e3, "p_r", [128, 2, NE], F32)
                k.op("pool", lambda e: e.memset(cnt.t[:], 0.0), writes=[cnt])
                scat_evs = []
                def ym_load(sc):
                    ts = sc * 512
                    ymc = ym[sc % 2]
                    k.dma("sp", ymsem[sc % 2], [(lambda e, i=i: e.dma_start(
                        out=ymc.t[:, 6 * i:6 * i + 6, :],
                        in_=ymT_d[6 * i:6 * i + 6, :, ts:ts + 512].rearrange("f p t -> p f t"))) for i in range(4)],
                        writes=[ymc])

                def a2body(sc, q):
                    ts = sc * 512
                    ymc = ym[sc % 2]
                    if q == 1 and sc + 1 < NSC:
                        ym_load(sc + 1)
                    c = sc * 4 + q
                    b = c % 2
                    qs = slice(q * 128, (q + 1) * 128)
                    cs = slice(c * 128, (c + 1) * 128)
                    p_o = p_o_l[b]
                    junk3 = junk3_l[b]; ss3 = ss3_l[b]; rs3 = rs3_l[b]; h2f = h2f_l[b]; h2T = h2T_l[b]; lg = lg_l[b]; m8 = m8_l[b]; nv1 = nv1_l[b]; mask = mask_l[b]; ex = ex_l[b]; sm = sm_l[b]; rank = rank_l[b]; vld = vld_l[b]; val = val_l[b]; v8 = v8_l[b]
                    yield
                    k.dma("sp", xsem2[b], lambda e: e.dma_start(out=xt2[b].t[:], in_=x_d[cs, :]), writes=[xt2[b]])
                    yield
                    k.ops("pe", [(lambda e, fc=fc, h=h: e.matmul(p_o.t[:, h * 512:(h + 1) * 512], lhsT=ymc.t[:, fc, qs],
                                                                 rhs=wout.t[:, fc, h * 512:(h + 1) * 512],
                                                                 start=(fc == 0), stop=(fc == 23)))
                                 for h in range(2) for fc in range(24)], reads=[ymc, wout], writes=[p_o])
                    yield
                    k.op("dve", lambda e: e.tensor_tensor(out=x1[b].t[:], in0=p_o.t[:], in1=xt2[b].t[:], op=ALU.add),
                         reads=[p_o, xt2[b]], writes=[x1[b]])
                    yield
                    k.dma("sp", x1sem[b], lambda e: e.dma_start(out=x1_d[cs, :], in_=x1[b].t[:]), reads=[x1[b]])
                    yield
                    k.op("act", lambda e: e.activation(out=junk3.t[:], in_=x1[b].t[:], func=AF.Square, accum_out=ss3.t[:, 0:1]),
                         reads=[x1[b]], writes=[junk3, ss3])
                    yield
                    k.op("pool", lambda e: e.tensor_scalar(out=rs3.t[:], in0=ss3.t[:], scalar1=1.0 / DM, scalar2=EPS,
                                                           op0=ALU.mult, op1=ALU.add), reads=[ss3], writes=[rs3])
                    yield
                    k.op("pool", lambda e: e.tensor_tensor(out=rs3.t[:], in0=rs3.t[:], in1=neghalf_p.t[:], op=ALU.pow),
                         reads=[rs3, neghalf_p], writes=[rs3])
                    yield
                    k.op("dve", lambda e: e.scalar_tensor_tensor(out=h2f.t[:], in0=x1[b].t[:], scalar=rs3.t[:, 0:1],
                                                                 in1=gffn_bc.t[:], op0=ALU.mult, op1=ALU.mult),
                         reads=[x1[b], rs3, gffn_bc], writes=[h2f])
                    yield
                    k.op("act", lambda e: e.copy(out=h2b[b].t[:], in_=h2f.t[:]), reads=[h2f], writes=[h2b[b]])
                    yield
                    k.dma("sp", h2sem[b], lambda e: e.dma_start(out=h2_d[cs, :], in_=h2b[b].t[:]), reads=[h2b[b]])
                    yield
                    k.ops("pe", [(lambda e, j=j: e.transpose(out=p_tf.t[:, j, :], in_=h2f.t[:, j * 128:(j + 1) * 128],
                                                             identity=ident_f.t[:])) for j in range(8)],
                          reads=[h2f, ident_f], writes=[p_tf])
                    yield
                    k.op("act", lambda e: e.copy(out=h2T.t[:], in_=p_tf.t[:]), reads=[p_tf], writes=[h2T])
                    yield
                    k.ops("pe", [(lambda e, j=j: e.matmul(p_l.t[:], lhsT=h2T.t[:, j, :], rhs=rw.t[:, j, :],
                                                          start=(j == 0), stop=(j == 7))) for j in range(8)],
                          reads=[h2T, rw], writes=[p_l])
                    yield
                    k.op("dve", lambda e: e.tensor_tensor(out=lg.t[:], in0=p_l.t[:], in1=rb.t[:], op=ALU.add),
                         reads=[p_l, rb], writes=[lg])
                    yield
                    k.op("dve", lambda e: e.max(out=m8.t[:], in_=lg.t[:]), reads=[lg], writes=[m8])
                    yield
                    k.op("dve", lambda e: e.tensor_scalar(out=mask.t[:], in0=lg.t[:], scalar1=m8.t[:, 3:4], scalar2=None,
                                                          op0=ALU.is_ge), reads=[lg, m8], writes=[mask])
                    yield
                    k.op("dve", lambda e: e.tensor_scalar(out=nv1.t[:], in0=m8.t[:, 0:1], scalar1=-1.0, scalar2=None,
                                                          op0=ALU.mult), reads=[m8], writes=[nv1])
                    yield
                    k.op("act", lambda e: e.activation(out=ex.t[:], in_=lg.t[:], func=AF.Exp, bias=nv1.t[:, 0:1]),
                         reads=[lg, nv1], writes=[ex])
                    yield
                    k.op("dve", lambda e: e.tensor_tensor(out=ex.t[:], in0=ex.t[:], in1=mask.t[:], op=ALU.mult),
                         reads=[ex, mask], writes=[ex])
                    yield
                    k.op("dve", lambda e: e.reduce_sum(out=sm.t[:], in_=ex.t[:], axis=AX.X), reads=[ex], writes=[sm])
                    yield
                    k.op("dve", lambda e: e.reciprocal(out=sm.t[:], in_=sm.t[:]), reads=[sm], writes=[sm])
                    yield
                    k.op("dve", lambda e: e.tensor_scalar(out=Gt[b].t[:], in0=ex.t[:], scalar1=sm.t[:, 0:1], scalar2=None,
                                                          op0=ALU.mult), reads=[ex, sm], writes=[Gt[b]])
                    yield
                    k.dma("sp", gsem[b], lambda e: e.dma_start(out=G_d[cs, :], in_=Gt[b].t[:]), reads=[Gt[b]])
                    yield
                    k.ops("pe", [
                        lambda e: e.matmul(p_r.t[:, 0, :], lhsT=Mlt.t[:], rhs=mask.t[:], start=True, stop=True),
                        lambda e: e.matmul(p_r.t[:, 1, :], lhsT=ones_f.t[:], rhs=mask.t[:], start=True, stop=True),
                    ], reads=[Mlt, ones_f, mask], writes=[p_r])
                    yield
                    k.op("dve", lambda e: e.tensor_tensor(out=rank.t[:], in0=p_r.t[:, 0, :], in1=cnt.t[:], op=ALU.add),
                         reads=[p_r, cnt], writes=[rank])
                    yield
                    k.op("dve", lambda e: e.tensor_tensor(out=cnt.t[:], in0=p_r.t[:, 1, :], in1=cnt.t[:], op=ALU.add),
                         reads=[p_r, cnt], writes=[cnt])
                    yield
                    k.op("dve", lambda e: e.tensor_scalar(out=vld.t[:], in0=rank.t[:], scalar1=float(CAP), scalar2=None,
                                                          op0=ALU.is_lt), reads=[rank], writes=[vld])
                    yield
                    k.op("dve", lambda e: e.tensor_tensor(out=vld.t[:], in0=vld.t[:], in1=mask.t[:], op=ALU.mult),
                         reads=[vld, mask], writes=[vld])
                    yield
                    k.op("dve", lambda e: e.tensor_tensor(out=val.t[:], in0=rank.t[:], in1=ebase.t[:], op=ALU.add),
                         reads=[rank, ebase], writes=[val])
                    yield
                    k.op("dve", lambda e: e.tensor_tensor(out=val.t[:], in0=val.t[:], in1=vld.t[:], op=ALU.mult),
                         reads=[val, vld], writes=[val])
                    yield
                    k.op("dve", lambda e: e.max(out=v8.t[:], in_=val.t[:]), reads=[val], writes=[v8])
                    yield
                    k.op("dve", lambda e: e.tensor_copy(out=dest_all.t[:, c, :], in_=v8.t[:, 0:4]), reads=[v8], writes=[dest_all])
                    yield
                    for kk in range(4):
                        scat_evs.append(k.dma("pool", scsem, lambda e: e.indirect_dma_start(
                            out=slot_d[:, :], out_offset=bass.IndirectOffsetOnAxis(ap=dest_all.t[:, c, kk:kk + 1], axis=0),
                            in_=tokid.t[:, c, :], in_offset=None),
                            reads=[dest_all, tokid], extra=ev_init))
                    yield
                ym_load(0)
                run_chains([a2body(sc, q) for sc in range(NSC) for q in range(4)], lag=8)
                a2_done = [x1[0], x1[1], h2b[0], h2b[1], Gt[0], Gt[1]]
                a2_evs = list(scat_evs[-1:])
                for t in a2_done:
                    a2_evs += t.r.rs
        else:
            a2_evs = []

        k.barrier()
        if "B" in phases:
            with ExitStack() as e4:
                bguT = load_const(e4, "bguT", [128, NE, 16], bguT_d)
                wgu = [sb(e4, "wgu%d" % i, [128, 8, 2048], BF16) for i in range(2)]
                wdn = [sb(e4, "wdn%d" % i, [128, 8, DM], BF16) for i in range(2)]
                bdb = [sb(e4, "bdb%d" % i, [128, DM], F32) for i in range(2)]
                wesem = [dsem(), dsem()]
                bdsem = [dsem(), dsem()]
                idx = [sb(e4, "idx%d" % i, [128, NBLK, 2], I32) for i in range(2)]
                isem = [dsem(), dsem()]
                xg = [sb(e4, "xg%d" % i, [128, DM], BF16) for i in range(NBLK)]
                xgsem = [dsem() for _ in range(NBLK)]
                gg = [sb(e4, "gg%d" % i, [128, NBLK, NE], F32) for i in range(2)]
                ggsem = [dsem(), dsem()]
                xgT = sb(e4, "xgT", [128, 8, CAP], BF16)
                actT = sb(e4, "actT", [128, 8, CAP], BF16)
                HW = CAP // 2
                gm = [sb(e4, "gm%d" % i, [128, HW], F32) for i in range(2)]
                sg = [sb(e4, "sg%d" % i, [128, HW], F32) for i in range(2)]
                u1 = [sb(e4, "u1_%d" % i, [128, HW], F32) for i in range(2)]
                yA = [sb(e4, "yA%d" % i, [128, DM], F32) for i in range(2)]
                yB = [sb(e4, "yB%d" % i, [128, DM], F32) for i in range(2)]
                ysem2 = [dsem(), dsem()]
                p_tg2 = [ps(e4, "p_tg%d" % i, [128, 8, 128], BF16) for i in range(2)]
                p_g = [ps(e4, "p_g%d" % i, [128, 512], F32) for i in range(2)]
                p_up = [ps(e4, "p_up%d" % i, [128, 512], F32) for i in range(2)]
                p_dh = [ps(e4, "p_dh%d" % i, [128, 512], F32) for i in range(2)]

                def load_w(e_):
                    s = e_ % 2
                    k.dma("pool", wesem[s],
                          [(lambda e, i=i: e.dma_start(out=wgu[s].t[:, 2 * i:2 * i + 2, :],
                                                       in_=wgu_d[e_, 256 * i:256 * (i + 1), :].rearrange("(k p) n -> p k n", p=128)))
                           for i in range(4)] +
                          [(lambda e, i=i: e.dma_start(out=wdn[s].t[:, 4 * i:4 * i + 4, :],
                                                       in_=wd_d[e_, 512 * i:512 * (i + 1), :].rearrange("(k p) n -> p k n", p=128)))
                           for i in range(2)],
                          writes=[wgu[s], wdn[s]])
                    k.dma("sp", bdsem[s], lambda e: e.dma_start(out=bdb[s].t[:], in_=bd_d[e_:e_ + 1, :].to_broadcast([128, DM])),
                          writes=[bdb[s]])

                def prefetch(e_):
                    s_ = e_ % 2
                    base_ = 1 + e_ * CAP
                    k.dma("sp", isem[s_], [(lambda e, j=j: e.dma_start(out=idx[s_].t[:, j, :],
                                                                       in_=slot_d[base_ + j * 128:base_ + (j + 1) * 128, :]))
                                           for j in range(NBLK)], writes=[idx[s_]], extra=a2_evs)
                    for j in range(NBLK):
                        k.dma("pool", xgsem[j], lambda e: e.indirect_dma_start(
                            out=xg[j].t[:, :], out_offset=None, in_=h2_d[:, :],
                            in_offset=bass.IndirectOffsetOnAxis(ap=idx[s_].t[:, j, 0:1], axis=0)),
                            reads=[idx[s_]], writes=[xg[j]], extra=a2_evs)
                    k.dma("pool", ggsem[s_], [(lambda e, j=j: e.indirect_dma_start(
                        out=gg[s_].t[:, j, :], out_offset=None, in_=G_d[:, :],
                        in_offset=bass.IndirectOffsetOnAxis(ap=idx[s_].t[:, j, 0:1], axis=0))) for j in range(NBLK)],
                        reads=[idx[s_]], writes=[gg[s_]], extra=a2_evs)

                def transposes():
                    for j in range(NBLK):
                        p_tg = p_tg2[j % 2]
                        k.ops("pe", [(lambda e, kk=kk: e.transpose(out=p_tg.t[:, kk, :], in_=xg[j].t[:, kk * 128:(kk + 1) * 128],
                                                                   identity=ident_bf.t[:])) for kk in range(8)],
                              reads=[xg[j], ident_bf], writes=[p_tg])
                        k.op("act", lambda e: e.copy(out=xgT.t[:, :, j * 128:(j + 1) * 128], in_=p_tg.t[:]),
                             reads=[p_tg], writes=[xgT])

                load_w(0)
                prefetch(0)
                transposes()
                for e_ in range(NE):
                    s = e_ % 2
                    base = 1 + e_ * CAP
                    if e_ + 1 < NE:
                        prefetch(e_ + 1)
                        load_w(e_ + 1)
                    for fc in range(8):
                        for h in range(2):
                            hs = slice(h * HW, (h + 1) * HW)
                            k.ops("pe", [(lambda e, kk=kk: e.matmul(p_g[h].t[:, 0:HW], lhsT=wgu[s].t[:, kk, fc * 128:(fc + 1) * 128],
                                                                    rhs=xgT.t[:, kk, hs], start=(kk == 0), stop=(kk == 7)))
                                         for kk in range(8)], reads=[wgu[s], xgT], writes=[p_g[h]])
                            k.ops("pe", [(lambda e, kk=kk: e.matmul(p_up[h].t[:, 0:HW],
                                                                    lhsT=wgu[s].t[:, kk, 1024 + fc * 128:1024 + (fc + 1) * 128],
                                                                    rhs=xgT.t[:, kk, hs], start=(kk == 0), stop=(kk == 7)))
                                         for kk in range(8)], reads=[wgu[s], xgT], writes=[p_up[h]])
                            k.op("dve", lambda e: e.tensor_scalar(out=gm[h].t[:], in0=p_g[h].t[:, 0:HW],
                                                                  scalar1=bguT.t[:, e_, fc:fc + 1], scalar2=7.0,
                                                                  op0=ALU.add, op1=ALU.min), reads=[p_g[h], bguT], writes=[gm[h]])
                            k.op("act", lambda e: e.activation(out=sg[h].t[:], in_=gm[h].t[:], func=AF.Sigmoid, scale=1.702),
                                 reads=[gm[h]], writes=[sg[h]])
                            k.op("dve", lambda e: e.tensor_scalar(out=u1[h].t[:], in0=p_up[h].t[:, 0:HW],
                                                                  scalar1=bguT.t[:, e_, 8 + fc:9 + fc], scalar2=7.0,
                                                                  op0=ALU.add, op1=ALU.min), reads=[p_up[h], bguT], writes=[u1[h]])
                            k.op("dve", lambda e: e.tensor_scalar(out=u1[h].t[:], in0=u1[h].t[:], scalar1=-7.0, scalar2=1.0,
                                                                  op0=ALU.max, op1=ALU.add), reads=[u1[h]], writes=[u1[h]])
                            k.op("dve", lambda e: e.tensor_tensor(out=sg[h].t[:], in0=gm[h].t[:], in1=sg[h].t[:], op=ALU.mult),
                                 reads=[gm[h], sg[h]], writes=[sg[h]])
                            k.op("dve", lambda e: e.tensor_tensor(out=actT.t[:, fc, hs], in0=sg[h].t[:], in1=u1[h].t[:], op=ALU.mult),
                                 reads=[sg[h], u1[h]], writes=[actT])
                    if e_ + 1 < NE:
                        transposes()
                    for j in range(NBLK):
                        js = slice(j * 128, (j + 1) * 128)
                        b = j % 2
                        for h in range(2):
                            k.ops("pe", [(lambda e, kk=kk: e.matmul(p_dh[h].t[:], lhsT=actT.t[:, kk, js],
                                                                    rhs=wdn[s].t[:, kk, h * 512:(h + 1) * 512],
                                                                    start=(kk == 0), stop=(kk == 7)))
                                         for kk in range(8)], reads=[actT, wdn[s]], writes=[p_dh[h]])
                            k.op("dve", lambda e: e.tensor_tensor(out=yA[b].t[:, h * 512:(h + 1) * 512], in0=p_dh[h].t[:],
                                                                  in1=bdb[s].t[:, h * 512:(h + 1) * 512], op=ALU.add),
                                 reads=[p_dh[h], bdb[s]], writes=[yA[b]])
                        k.op("act", lambda e: e.activation(out=yB[b].t[:], in_=yA[b].t[:], func=AF.Copy,
                                                           scale=gg[s].t[:, j, e_:e_ + 1]),
                             reads=[yA[b], gg[s]], writes=[yB[b]])
                        k.dma("sp", ysem2[b], lambda e: e.dma_start(out=Y_d[base + j * 128:base + (j + 1) * 128, :], in_=yB[b].t[:]),
                              reads=[yB[b]])
                b_evs = []
                for t in yB:
                    b_evs += t.r.rs
        else:
            b_evs = []

        k.barrier()
        fin_evs = []
        if "C" in phases:
            with ExitStack() as e5:
                wpg = sb(e5, "wpg", [128, 8, DM], BF16)
                k.dma("pool", sem_cp, [(lambda e, i=i: e.dma_start(
                    out=wpg.t[:, 4 * i:4 * i + 4, :],
                    in_=wpg_d[512 * i:512 * (i + 1), :].rearrange("(k p) n -> p k n", p=128))) for i in range(2)], writes=[wpg])
                wpp = sb(e5, "wpp", [128, 2, DM], BF16)
                k.dma("pool", sem_cp, lambda e: e.dma_start(out=wpp.t[:], in_=wpp_d.rearrange("(k p) n -> p k n", p=128)),
                      writes=[wpp])
                gpgT = load_const(e5, "gpgT", [128, 8], gpgT_d)
                gpn_bc = load_const(e5, "gpn_bc", [128, DM], gpn_bc_d)
                gfin_bc = load_const(e5, "gfin_bc", [128, DM], gfin_bc_d)
                x1c = [sb(e5, "x1c%d" % i, [128, DM], F32) for i in range(2)]
                x1csem = [dsem(), dsem()]
                yk = [[sb(e5, "yk%d_%d" % (i, kk), [128, DM], F32) for kk in range(4)] for i in range(2)]
                yksem = [[dsem() for kk in range(4)] for i in range(2)]
                pt = [sb(e5, "pt%d" % i, [128, 256], F32) for i in range(2)]
                ptsem = [dsem(), dsem()]
                x2_l = [sb(e5, "x2%d" % i_, [128, DM], F32) for i_ in range(2)]
                junk4_l = [sb(e5, "junk4%d" % i_, [128, DM], F32) for i_ in range(2)]
                ssc_l = [sb(e5, "ssc%d" % i_, [128, 1], F32) for i_ in range(2)]
                rsc_l = [sb(e5, "rsc%d" % i_, [128, 1], F32) for i_ in range(2)]
                xnb_l = [sb(e5, "xnb%d" % i_, [128, DM], BF16) for i_ in range(2)]
                xnT_l = [sb(e5, "xnT%d" % i_, [128, 8, 128], BF16) for i_ in range(2)]
                sgate_l = [sb(e5, "sgate%d" % i_, [128, DM], F32) for i_ in range(2)]
                pT_l = [sb(e5, "pT%d" % i_, [128, 2, 128], BF16) for i_ in range(2)]
                sse_l = [sb(e5, "sse%d" % i_, [128, 1], F32) for i_ in range(2)]
                rse_l = [sb(e5, "rse%d" % i_, [128, 1], F32) for i_ in range(2)]
                e1__l = [sb(e5, "e1_%d" % i_, [128, DM], F32) for i_ in range(2)]
                x3_l = [sb(e5, "x3%d" % i_, [128, DM], F32) for i_ in range(2)]
                ssf_l = [sb(e5, "ssf%d" % i_, [128, 1], F32) for i_ in range(2)]
                rsf_l = [sb(e5, "rsf%d" % i_, [128, 1], F32) for i_ in range(2)]
                ot = [sb(e5, "ot%d" % i, [128, DM], F32) for i in range(2)]
                osem = [dsem(), dsem()]
                p_t3 = ps(e5, "p_t3", [128, 8, 128], BF16)
                p_ga = ps(e5, "p_ga", [128, DM], F32)
                p_pt = ps(e5, "p_pt", [128, 2, 128], F32)
                p_e = ps(e5, "p_e", [128, DM], F32)
                def cbody(c):
                    b = c % 2
                    cs = slice(c * 128, (c + 1) * 128)
                    x2 = x2_l[b]; junk4 = junk4_l[b]; ssc = ssc_l[b]; rsc = rsc_l[b]; xnb = xnb_l[b]; xnT = xnT_l[b]; sgate = sgate_l[b]; pT = pT_l[b]; sse = sse_l[b]; rse = rse_l[b]; e1_ = e1__l[b]; x3 = x3_l[b]; ssf = ssf_l[b]; rsf = rsf_l[b]
                    yield
                    k.dma("sp", x1csem[b], lambda e: e.dma_start(out=x1c[b].t[:], in_=x1_d[cs, :]), writes=[x1c[b]], extra=a2_evs)
                    yield
                    k.dma("sp", ptsem[b], lambda e: e.dma_start(out=pt[b].t[:], in_=pin_d[cs, :]), writes=[pt[b]])
                    yield
                    for kk in range(4):
                        k.dma("pool", yksem[b][kk], lambda e: e.indirect_dma_start(
                            out=yk[b][kk].t[:, :], out_offset=None, in_=Y_d[:, :],
                            in_offset=bass.IndirectOffsetOnAxis(ap=dest_all.t[:, c, kk:kk + 1], axis=0)),
                            reads=[dest_all], writes=[yk[b][kk]], extra=b_evs)
                    yield
                    k.op("dve", lambda e: e.tensor_tensor(out=x2.t[:], in0=x1c[b].t[:], in1=yk[b][0].t[:], op=ALU.add),
                         reads=[x1c[b], yk[b][0]], writes=[x2])
                    yield
                    k.op("pool", lambda e: e.tensor_tensor(out=yk[b][1].t[:], in0=yk[b][1].t[:], in1=yk[b][2].t[:], op=ALU.add),
                         reads=[yk[b][1], yk[b][2]], writes=[yk[b][1]])
                    yield
                    k.op("dve", lambda e: e.tensor_tensor(out=x2.t[:], in0=x2.t[:], in1=yk[b][3].t[:], op=ALU.add),
                         reads=[x2, yk[b][3]], writes=[x2])
                    yield
                    k.op("dve", lambda e: e.tensor_tensor(out=x2.t[:], in0=x2.t[:], in1=yk[b][1].t[:], op=ALU.add),
                         reads=[x2, yk[b][1]], writes=[x2])
                    yield
                    k.op("act", lambda e: e.activation(out=junk4.t[:], in_=x2.t[:], func=AF.Square, accum_out=ssc.t[:, 0:1]),
                         reads=[x2], writes=[junk4, ssc])
                    yield
                    k.op("pool", lambda e: e.tensor_scalar(out=rsc.t[:], in0=ssc.t[:], scalar1=1.0 / DM, scalar2=EPS,
                                                           op0=ALU.mult, op1=ALU.add), reads=[ssc], writes=[rsc])
                    yield
                    k.op("pool", lambda e: e.tensor_tensor(out=rsc.t[:], in0=rsc.t[:], in1=neghalf_p.t[:], op=ALU.pow),
                         reads=[rsc, neghalf_p], writes=[rsc])
                    yield
                    k.op("act", lambda e: e.activation(out=xnb.t[:], in_=x2.t[:], func=AF.Copy, scale=rsc.t[:, 0:1]),
                         reads=[x2, rsc], writes=[xnb])
                    yield
                    k.ops("pe", [(lambda e, j=j: e.transpose(out=p_t3.t[:, j, :], in_=xnb.t[:, j * 128:(j + 1) * 128],
                                                             identity=ident_bf.t[:])) for j in range(8)],
                          reads=[xnb, ident_bf], writes=[p_t3])
                    yield
                    k.op("dve", lambda e: e.tensor_tensor(out=xnT.t[:], in0=p_t3.t[:],
                                                          in1=gpgT.t[:, :, None].to_broadcast([128, 8, 128]), op=ALU.mult),
                         reads=[p_t3, gpgT], writes=[xnT])
                    yield
                    k.ops("pe", [(lambda e, j=j, h=h: e.matmul(p_ga.t[:, h * 512:(h + 1) * 512], lhsT=xnT.t[:, j, :],
                                                               rhs=wpg.t[:, j, h * 512:(h + 1) * 512], start=(j == 0), stop=(j == 7)))
                                 for h in range(2) for j in range(8)], reads=[xnT, wpg], writes=[p_ga])
                    yield
                    k.op("act", lambda e: e.activation(out=sgate.t[:], in_=p_ga.t[:], func=AF.Sigmoid), reads=[p_ga], writes=[sgate])
                    yield
                    k.ops("pe", [(lambda e, j=j: e.transpose(out=p_pt.t[:, j, :], in_=pt[b].t[:, j * 128:(j + 1) * 128],
                                                             identity=ident_f.t[:])) for j in range(2)],
                          reads=[pt[b], ident_f], writes=[p_pt])
                    yield
                    k.op("act", lambda e: e.copy(out=pT.t[:], in_=p_pt.t[:]), reads=[p_pt], writes=[pT])
                    yield
                    k.ops("pe", [(lambda e, j=j, h=h: e.matmul(p_e.t[:, h * 512:(h + 1) * 512], lhsT=pT.t[:, j, :],
                                                               rhs=wpp.t[:, j, h * 512:(h + 1) * 512], start=(j == 0), stop=(j == 1)))
                                 for h in range(2) for j in range(2)], reads=[pT, wpp], writes=[p_e])
                    yield
                    k.op("act", lambda e: e.activation(out=junk4.t[:], in_=p_e.t[:], func=AF.Square, accum_out=sse.t[:, 0:1]),
                         reads=[p_e], writes=[junk4, sse])
                    yield
                    k.op("pool", lambda e: e.tensor_scalar(out=rse.t[:], in0=sse.t[:], scalar1=1.0 / DM, scalar2=EPS,
                                                           op0=ALU.mult, op1=ALU.add), reads=[sse], writes=[rse])
                    yield
                    k.op("pool", lambda e: e.tensor_tensor(out=rse.t[:], in0=rse.t[:], in1=neghalf_p.t[:], op=ALU.pow),
                         reads=[rse, neghalf_p], writes=[rse])
                    yield
                    k.op("dve", lambda e: e.scalar_tensor_tensor(out=e1_.t[:], in0=p_e.t[:], scalar=rse.t[:, 0:1], in1=gpn_bc.t[:],
                                                                 op0=ALU.mult, op1=ALU.mult), reads=[p_e, rse, gpn_bc], writes=[e1_])
                    yield
                    k.op("pool", lambda e: e.tensor_tensor(out=e1_.t[:], in0=e1_.t[:], in1=sgate.t[:], op=ALU.mult),
                         reads=[e1_, sgate], writes=[e1_])
                    yield
                    k.op("dve", lambda e: e.tensor_tensor(out=x3.t[:], in0=x2.t[:], in1=e1_.t[:], op=ALU.add),
                         reads=[x2, e1_], writes=[x3])
                    yield
                    k.op("act", lambda e: e.activation(out=junk4.t[:], in_=x3.t[:], func=AF.Square, accum_out=ssf.t[:, 0:1]),
                         reads=[x3], writes=[junk4, ssf])
                    yield
                    k.op("pool", lambda e: e.tensor_scalar(out=rsf.t[:], in0=ssf.t[:], scalar1=1.0 / DM, scalar2=EPS,
                                                           op0=ALU.mult, op1=ALU.add), reads=[ssf], writes=[rsf])
                    yield
                    k.op("pool", lambda e: e.tensor_tensor(out=rsf.t[:], in0=rsf.t[:], in1=neghalf_p.t[:], op=ALU.pow),
                         reads=[rsf, neghalf_p], writes=[rsf])
                    yield
                    k.op("dve", lambda e: e.scalar_tensor_tensor(out=ot[b].t[:], in0=x3.t[:], scalar=rsf.t[:, 0:1], in1=gfin_bc.t[:],
                                                                 op0=ALU.mult, op1=ALU.mult), reads=[x3, rsf, gfin_bc], writes=[ot[b]])
                    yield
                    fin_evs.append(k.dma("sp", osem[b], lambda e: e.dma_start(out=out_d[cs, :], in_=ot[b].t[:]), reads=[ot[b]]))

                    yield
                run_chains([cbody(c) for c in range(NCH)], lag=6)

        tail = list(fin_evs[-2:]) + list(a2_evs) + list(b_evs)
        for sid_ev in tail:
            k.wait("sp", sid_ev)
        for sid_, sem_ in k.dma_objs.items():
            k.wait("sp", (sem_, k.dma_cnt[sid_]))
        build.stats = (k.ninst, k.nwaits, dict(k.cnt))
    return nc


def host_layout(inp):
    f = np.float32

    def colT(v, n):
        return np.ascontiguousarray(np.asarray(v, f).reshape(n, 128).T)

    def bc(v):
        v = np.asarray(v, f).reshape(1, -1)
        return np.ascontiguousarray(np.broadcast_to(v, (128, v.shape[1])))

    cw = np.asarray(inp["conv_w"][0], f)
    conv_wT = np.ascontiguousarray(cw.reshape(4, 32, 128).transpose(2, 1, 0))
    bgu = np.asarray(inp["b_gate_up"][0], f)
    bguT = np.ascontiguousarray(bgu.reshape(NE, 16, 128).transpose(2, 0, 1))
    invc = np.zeros((128, 4, 16), f)
    for gi, w in enumerate((2, 4, 8, 16)):
        invc[:, gi, :] = 1.0 / np.minimum(np.arange(16) + 1, w).astype(f)
    ebase = (1 + np.arange(NE) * CAP).astype(f)
    shared = {
        "w_in": np.ascontiguousarray(inp["w_in"][0], f),
        "conv_wT": conv_wT,
        "conv_bT": colT(inp["conv_b"][0], 32),
        "conv_brow": np.ascontiguousarray(np.asarray(inp["conv_b"][0], f).reshape(1, 4096)),
        "dt_bias_bc": bc(inp["dt_bias"][0]),
        "a_log_bc": bc(inp["a_log"][0]),
        "d_skip_bc": bc(inp["d_skip"][0]),
        "gmixT": colT(inp["mix_norm_g"][0], 8),
        "gssdT": colT(inp["ssd_norm_g"][0], 16),
        "pool_w": np.ascontiguousarray(inp["pool_w"][0], f),
        "pscT": colT(inp["pool_scale"][0], 8),
        "invc": invc,
        "w_out": np.ascontiguousarray(inp["w_out"][0], f),
        "gffn_bc": bc(inp["ffn_norm_g"][0]),
        "router_w": np.ascontiguousarray(inp["router_w"][0], f),
        "rb_bc": bc(inp["router_b"][0]),
        "ebase_bc": bc(ebase),
        "w_gate_up": np.ascontiguousarray(inp["w_gate_up"][0], f),
        "bguT": bguT,
        "w_down": np.ascontiguousarray(inp["w_down"][0], f),
        "b_down": np.ascontiguousarray(inp["b_down"][0], f),
        "gpgT": colT(inp["ple_gate_norm_g"][0], 8),
        "w_ple_gate": np.ascontiguousarray(inp["w_ple_gate"][0], f),
        "w_ple_proj": np.ascontiguousarray(inp["w_ple_proj"][0], f),
        "gpn_bc": bc(inp["ple_norm_g"][0]),
        "gfin_bc": bc(inp["final_norm_g"]),
    }
    return shared


def kernel(**inputs):
    inp = {k_: np.asarray(v) for k_, v in inputs.items()}
    shared = host_layout(inp)
    x = np.asarray(inp["x"], np.float32)
    p = np.asarray(inp["p"], np.float32)[0]
    nb = x.shape[0]
    in_maps = []
    for b in range(nb):
        m = dict(shared)
        m["x"] = np.ascontiguousarray(x[b])
        m["p"] = np.ascontiguousarray(p[b])
        in_maps.append(m)
    nc = build()
    res = run_bass_kernel_spmd(nc, in_maps, core_ids=list(range(nb)))
    return np.stack([np.asarray(r["out"], np.float32) for r in res.results], axis=0)
```

```python
from contextlib import ExitStack
import numpy as np
import concourse.bass as bass
import concourse.mybir as mybir
from concourse.bass_utils import run_bass_kernel_spmd

F32 = mybir.dt.float32
BF16 = mybir.dt.bfloat16
I32 = mybir.dt.int32
AF = mybir.ActivationFunctionType
ALU = mybir.AluOpType
AX = mybir.AxisListType

L = 4096
DM = 1024
NCH = 32
NSC = 8
D_IN = 7200
NE = 32
CAP = 1024
NBLK = CAP // 128
NSLOT = NE * CAP
SLOT_TAB = 128 * 257
EPS = 1e-6


class R:
    __slots__ = ("w", "rs")

    def __init__(self):
        self.w = None
        self.rs = []


class T:
    def __init__(self, t):
        self.t = t
        self.r = R()


class K:
    def __init__(self, nc, sems):
        self.nc = nc
        self.engs = {"pe": nc.tensor, "dve": nc.vector, "act": nc.scalar,
                     "pool": nc.gpsimd, "sp": nc.sync}
        self.psem = sems
        self.cnt = {k: 0 for k in self.engs}
        self.waited = {k: {} for k in self.engs}
        self.dma_cnt = {}
        self.dma_objs = {}
        self.ninst = 0
        self.nwaits = 0

    def wait(self, ek, ev):
        if ev is None:
            return
        sem, val = ev
        sid = id(sem)
        if sid in self.dma_cnt:
            val = max(val, self.dma_cnt[sid])
        w = self.waited[ek]
        if w.get(sid, 0) >= val:
            return
        self.engs[ek].wait_ge(sem, val)
        self.nwaits += 1
        w[sid] = val

    def _deps(self, ek, reads, writes, extra):
        for t in reads:
            self.wait(ek, t.r.w)
        for t in writes:
            self.wait(ek, t.r.w)
            for e in t.r.rs:
                self.wait(ek, e)
        for e in extra:
            self.wait(ek, e)

    def _commit(self, ev, reads, writes):
        for t in reads:
            t.r.rs.append(ev)
        for t in writes:
            t.r.w = ev
            t.r.rs = []

    def barrier(self):
        for ek in self.engs:
            for o in self.engs:
                if self.cnt[o] > 0:
                    self.wait(ek, (self.psem[o], self.cnt[o]))
            for sid_, sem_ in self.dma_objs.items():
                self.wait(ek, (sem_, self.dma_cnt[sid_]))

    def op(self, ek, fn, reads=(), writes=(), extra=()):
        self._deps(ek, reads, writes, extra)
        ins = fn(self.engs[ek])
        self.cnt[ek] += 1
        ins.then_inc(self.psem[ek], 1)
        ev = (self.psem[ek], self.cnt[ek])
        self._commit(ev, reads, writes)
        self.ninst += 1
        return ev

    def ops(self, ek, fns, reads=(), writes=(), extra=()):
        self._deps(ek, reads, writes, extra)
        ins = None
        for fn in fns:
            ins = fn(self.engs[ek])
            self.ninst += 1
        self.cnt[ek] += 1
        ins.then_inc(self.psem[ek], 1)
        ev = (self.psem[ek], self.cnt[ek])
        self._commit(ev, reads, writes)
        return ev

    def dma(self, ek, sem, fns, reads=(), writes=(), extra=()):
        self._deps(ek, reads, writes, extra)
        if not isinstance(fns, (list, tuple)):
            fns = [fns]
        sid = id(sem)
        cur = self.dma_cnt.get(sid, 0)
        for f in fns:
            f(self.engs[ek]).then_inc(sem, 16)
            cur += 16
            self.ninst += 1
        self.dma_cnt[sid] = cur
        self.dma_objs[sid] = sem
        ev = (sem, cur)
        self._commit(ev, reads, writes)
        return ev


def run_chains(gens, lag=4, width=2):
    pending = list(gens)
    active = []
    while pending or active:
        if len(active) < width and pending and (not active or active[-1][1] >= lag):
            active.append([pending.pop(0), 0])
        for a in list(active):
            try:
                next(a[0])
                a[1] += 1
            except StopIteration:
                active.remove(a)


def build(debug=None, phases="A0,A1,A1p,A2,B,C"):
    phases = set(phases.split(","))
    debug = debug or ()
    nc = bass.Bass("TRN2", target_bir_lowering=False)

    def din(name, shape, dt=F32):
        return nc.dram_tensor(name, list(shape), dt, kind="ExternalInput").ap()

    x_d = din("x", [L, DM])
    pin_d = din("p", [L, 256])
    w_in_d = din("w_in", [DM, D_IN])
    conv_wT_d = din("conv_wT", [128, 32, 4])
    conv_bT_d = din("conv_bT", [128, 32])
    conv_brow_d = din("conv_brow", [1, 4096])
    dtb_d = din("dt_bias_bc", [128, 32])
    alog_d = din("a_log_bc", [128, 32])
    dsk_d = din("d_skip_bc", [128, 32])
    gmixT_d = din("gmixT", [128, 8])
    gssdT_d = din("gssdT", [128, 16])
    pool_w_d = din("pool_w", [4, 256, 256])
    pscT_d = din("pscT", [128, 8])
    invc_d = din("invc", [128, 4, 16])
    w_out_d = din("w_out", [3072, DM])
    gffn_bc_d = din("gffn_bc", [128, DM])
    rw_d = din("router_w", [DM, NE])
    rb_d = din("rb_bc", [128, NE])
    ebase_d = din("ebase_bc", [128, NE])
    wgu_d = din("w_gate_up", [NE, DM, 2048])
    bguT_d = din("bguT", [128, NE, 16])
    wd_d = din("w_down", [NE, DM, DM])
    bd_d = din("b_down", [NE, DM])
    gpgT_d = din("gpgT", [128, 8])
    wpg_d = din("w_ple_gate", [DM, DM])
    wpp_d = din("w_ple_proj", [256, DM])
    gpn_bc_d = din("gpn_bc", [128, DM])
    gfin_bc_d = din("gfin_bc", [128, DM])
    out_d = nc.dram_tensor("out", [L, DM], F32, kind="ExternalOutput").ap()

    def dscr(name, shape, dt, dbg=False):
        kind = "ExternalOutput" if (name in debug) else "Internal"
        return nc.dram_tensor(name, list(shape), dt, kind=kind).ap()

    ymT_d = dscr("ymT", [24, 128, L], BF16)
    x1_d = dscr("x1d", [L, DM], F32)
    h2_d = dscr("h2d", [L + 1, DM], BF16)
    G_d = dscr("Gd", [L + 1, NE], F32)
    slot_d = dscr("slotd", [SLOT_TAB, 2], I32)
    Y_d = dscr("Yd", [NSLOT + 1, DM], F32)

    with ExitStack() as es:
        E = es.enter_context
        sems = {k: E(nc.semaphore("prog_" + k)) for k in ["pe", "dve", "act", "pool", "sp"]}
        k = K(nc, sems)
        nsem = [0]

        def dsem():
            nsem[0] += 1
            return E(nc.semaphore("d%d" % nsem[0]))

        def sb(es_, name, shape, dt=F32):
            return T(es_.enter_context(nc.sbuf_tensor("s_" + name, list(shape), dt)))

        def ps(es_, name, shape, dt=F32):
            esz = 4 if dt == F32 else 2
            n = 1
            for d_ in shape[1:]:
                n *= d_
            per_bank = 2048 // esz
            nb_ = (n + per_bank - 1) // per_bank
            base = es_.enter_context(nc.psum_tensor("ps_" + name, [128, nb_ * per_bank], dt))
            v = base[:, 0:n]
            if len(shape) == 3:
                v = v.rearrange("p (a b) -> p a b", a=shape[1])
            return T(v)

        def psbank(es_, name, dt=F32):
            per_bank = 2048 // (4 if dt == F32 else 2)
            return es_.enter_context(nc.psum_tensor("ps_" + name, [128, per_bank], dt))

        ident_bf = sb(es, "ident_bf", [128, 128], BF16)
        ident_f = sb(es, "ident_f", [128, 128], F32)
        Mle = sb(es, "Mle", [128, 128], F32)
        Mgt = sb(es, "Mgt", [128, 128], F32)
        Mlt = sb(es, "Mlt", [128, 128], F32)
        ones_f = sb(es, "ones_f", [128, 128], F32)
        dest_all = sb(es, "dest_all", [128, NCH, 4], I32)
        tokid = sb(es, "tokid", [128, NCH, 2], I32)
        zrow = sb(es, "zrow", [1, DM], F32)
        zrow_bf = sb(es, "zrow_bf", [1, DM], BF16)

        def dump(name, t, ap=None):
            if name not in debug:
                return
            a = ap if ap is not None else t.t[:]
            dd = nc.dram_tensor("dbg_" + name, list(a.shape), a.dtype, kind="ExternalOutput").ap()
            k.dma("sp", dsem(), lambda e: e.dma_start(out=dd, in_=a), reads=[t])

        def cst(t, fn):
            k.op("pool", fn, writes=[t])

        cst(ident_bf, lambda e: e.memset(ident_bf.t[:], 1.0))
        cst(ident_bf, lambda e: e.affine_select(out=ident_bf.t[:], in_=ident_bf.t[:], pattern=[[-1, 128]],
                                               compare_op=ALU.is_equal, fill=0.0, base=0, channel_multiplier=1))
        cst(ident_f, lambda e: e.memset(ident_f.t[:], 1.0))
        cst(ident_f, lambda e: e.affine_select(out=ident_f.t[:], in_=ident_f.t[:], pattern=[[-1, 128]],
                                              compare_op=ALU.is_equal, fill=0.0, base=0, channel_multiplier=1))
        cst(Mle, lambda e: e.memset(Mle.t[:], 1.0))
        cst(Mle, lambda e: e.affine_select(out=Mle.t[:], in_=Mle.t[:], pattern=[[1, 128]],
                                          compare_op=ALU.is_ge, fill=0.0, base=0, channel_multiplier=-1))
        cst(Mgt, lambda e: e.memset(Mgt.t[:], 1.0))
        cst(Mgt, lambda e: e.affine_select(out=Mgt.t[:], in_=Mgt.t[:], pattern=[[-1, 128]],
                                          compare_op=ALU.is_gt, fill=0.0, base=0, channel_multiplier=1))
        cst(Mlt, lambda e: e.memset(Mlt.t[:], 1.0))
        cst(Mlt, lambda e: e.affine_select(out=Mlt.t[:], in_=Mlt.t[:], pattern=[[1, 128]],
                                          compare_op=ALU.is_gt, fill=0.0, base=0, channel_multiplier=-1))
        cst(ones_f, lambda e: e.memset(ones_f.t[:], 1.0))
        neghalf_p = sb(es, "neghalf_p", [128, 1], F32)
        cst(neghalf_p, lambda e: e.memset(neghalf_p.t[:], -0.5))
        cst(zrow, lambda e: e.memset(zrow.t[:], 0.0))
        cst(zrow_bf, lambda e: e.memset(zrow_bf.t[:], 0.0))
        cst(tokid, lambda e: e.iota(tokid.t[:], pattern=[[128, NCH], [0, 2]], base=0, channel_multiplier=1))
        cst(dest_all, lambda e: e.memset(dest_all.t[:], 0))

        sem_c = dsem()
        sem_cp = dsem()

        def load_const(es_, name, shape, src, dt=F32, q="sp"):
            t = sb(es_, name, shape, dt)
            k.dma(q, sem_c, lambda e: e.dma_start(out=t.t[:], in_=src), writes=[t])
            return t

        if "A0" in phases:
            with ExitStack() as esA:
                hT = sb(esA, "hT", [128, 8, L], BF16)
                dt_all = sb(esA, "dt_all", [128, NCH, 32], F32)
                a_all = sb(esA, "a_all", [128, NCH, 32], F32)
                e_all = sb(esA, "e_all", [128, NCH, 3, 32], F32)
                gmixT = load_const(esA, "gmixT", [128, 8], gmixT_d)
                gssdT = load_const(esA, "gssdT", [128, 16], gssdT_d)
                conv_wT = load_const(esA, "conv_wT", [128, 32, 4], conv_wT_d)
                conv_bT = load_const(esA, "conv_bT", [128, 32], conv_bT_d)
                dsk = load_const(esA, "dsk", [128, 32], dsk_d)
                pscT = load_const(esA, "pscT", [128, 8], pscT_d)
                invc = load_const(esA, "invc", [128, 4, 16], invc_d)

                with ExitStack() as e0:
                    dtb = load_const(e0, "dtb", [128, 32], dtb_d)
                    alog = load_const(e0, "alog", [128, 32], alog_d)
                    Abc = sb(e0, "Abc", [128, 32], F32)
                    wdt = sb(e0, "wdt", [128, 8, 32], BF16)
                    k.dma("pool", sem_cp, lambda e: e.dma_start(
                        out=wdt.t[:], in_=w_in_d[:, 6144:6176].rearrange("(k p) n -> p k n", p=128)), writes=[wdt])
                    k.op("act", lambda e: e.activation(out=Abc.t[:], in_=alog.t[:], func=AF.Exp), reads=[alog], writes=[Abc])
                    k.op("dve", lambda e: e.tensor_scalar(out=Abc.t[:], in0=Abc.t[:], scalar1=-1.0, scalar2=None, op0=ALU.mult),
                         reads=[Abc], writes=[Abc])
                    xt = [sb(e0, "xt%d" % i, [128, DM], F32) for i in range(2)]
                    xsem = [dsem(), dsem()]
                    junk_l = [sb(e0, "junk%d" % i_, [128, DM], F32) for i_ in range(2)]
                    ss_l = [sb(e0, "ss%d" % i_, [128, 1], F32) for i_ in range(2)]
                    rstd_l = [sb(e0, "rstd%d" % i_, [128, 1], F32) for i_ in range(2)]
                    xn_l = [sb(e0, "xn%d" % i_, [128, DM], BF16) for i_ in range(2)]
                    tpA_l = [ps(e0, "tpA%d" % i_, [128, 8, 128], BF16) for i_ in range(2)]
                    pdt_l = [ps(e0, "pdt%d" % i_, [128, 32], F32) for i_ in range(2)]
                    pcs_l = [ps(e0, "pcs%d" % i_, [128, 3, 32], F32) for i_ in range(2)]
                    dtr_l = [sb(e0, "dtr%d" % i_, [128, 32], F32) for i_ in range(2)]
                    t1_l = [sb(e0, "t1%d" % i_, [128, 32], F32) for i_ in range(2)]
                    t2_l = [sb(e0, "t2%d" % i_, [128, 32], F32) for i_ in range(2)]
                    def a0body(c):
                        xc_ = xt[c % 2]
                        cs = slice(c * 128, (c + 1) * 128)
                        junk = junk_l[c % 2]; ss = ss_l[c % 2]; rstd = rstd_l[c % 2]; xn = xn_l[c % 2]; dtr = dtr_l[c % 2]; t1 = t1_l[c % 2]; t2 = t2_l[c % 2]; tpA = tpA_l[c % 2]; pdt = pdt_l[c % 2]; pcs = pcs_l[c % 2]
                        yield
                        k.dma("sp", xsem[c % 2], lambda e: e.dma_start(out=xc_.t[:], in_=x_d[cs, :]), writes=[xc_])
                        yield
                        k.op("act", lambda e: e.activation(out=junk.t[:], in_=xc_.t[:], func=AF.Square, accum_out=ss.t[:, 0:1]),
                             reads=[xc_], writes=[junk, ss])
                        yield
                        k.op("pool", lambda e: e.tensor_scalar(out=rstd.t[:], in0=ss.t[:], scalar1=1.0 / DM, scalar2=EPS,
                                                               op0=ALU.mult, op1=ALU.add), reads=[ss], writes=[rstd])
                        yield
                        k.op("pool", lambda e: e.tensor_tensor(out=rstd.t[:], in0=rstd.t[:], in1=neghalf_p.t[:], op=ALU.pow),
                             reads=[rstd, neghalf_p], writes=[rstd])
                        yield
                        k.op("act", lambda e: e.activation(out=xn.t[:], in_=xc_.t[:], func=AF.Copy, scale=rstd.t[:, 0:1]),
                             reads=[xc_, rstd], writes=[xn])
                        yield
                        k.ops("pe", [(lambda e, j=j: e.transpose(out=tpA.t[:, j, :], in_=xn.t[:, j * 128:(j + 1) * 128],
                                                                 identity=ident_bf.t[:])) for j in range(8)],
                              reads=[xn, ident_bf], writes=[tpA])
                        yield
                        k.op("dve", lambda e: e.tensor_tensor(out=hT.t[:, :, cs], in0=tpA.t[:],
                                                              in1=gmixT.t[:, :, None].to_broadcast([128, 8, 128]), op=ALU.mult),
                             reads=[tpA, gmixT], writes=[hT])
                        yield
                        k.ops("pe", [(lambda e, j=j: e.matmul(pdt.t[:], lhsT=hT.t[:, j, cs], rhs=wdt.t[:, j, :],
                                                              start=(j == 0), stop=(j == 7))) for j in range(8)],
                              reads=[hT, wdt], writes=[pdt])
                        yield
                        k.op("dve", lambda e: e.tensor_tensor(out=dtr.t[:], in0=pdt.t[:], in1=dtb.t[:], op=ALU.add),
                             reads=[pdt, dtb], writes=[dtr])
                        yield
                        k.op("act", lambda e: e.activation(out=t1.t[:], in_=dtr.t[:], func=AF.Abs),
                             reads=[dtr], writes=[t1])
                        yield
                        k.op("act", lambda e: e.activation(out=t2.t[:], in_=t1.t[:], func=AF.Exp, scale=-1.0), reads=[t1], writes=[t2])
                        yield
                        k.op("act", lambda e: e.activation(out=t1.t[:], in_=t2.t[:], func=AF.Ln, bias=1.0), reads=[t2], writes=[t1])
                        yield
                        k.op("dve", lambda e: e.scalar_tensor_tensor(out=dt_all.t[:, c, :], in0=dtr.t[:], scalar=0.0, in1=t1.t[:],
                                                                     op0=ALU.max, op1=ALU.add),
                             reads=[dtr, t1], writes=[dt_all])
                        yield
                        k.op("dve", lambda e: e.tensor_tensor(out=a_all.t[:, c, :], in0=dt_all.t[:, c, :], in1=Abc.t[:], op=ALU.mult),
                             reads=[dt_all, Abc], writes=[a_all])
                        yield
                        k.ops("pe", [
                            lambda e: e.matmul(pcs.t[:, 0, :], lhsT=Mle.t[:], rhs=a_all.t[:, c, :], start=True, stop=True),
                            lambda e: e.matmul(pcs.t[:, 1, :], lhsT=Mgt.t[:], rhs=a_all.t[:, c, :], start=True, stop=True),
                            lambda e: e.matmul(pcs.t[:, 2, :], lhsT=ones_f.t[:], rhs=a_all.t[:, c, :], start=True, stop=True),
                        ], reads=[a_all, Mle, Mgt, ones_f], writes=[pcs])
                        yield
                        k.op("act", lambda e: e.activation(out=e_all.t[:, c, :, :], in_=pcs.t[:], func=AF.Exp),
                             reads=[pcs], writes=[e_all])
                        yield
                    run_chains([a0body(c) for c in range(NCH)], lag=6)
                    k.barrier()
                dump("hT", hT); dump("dt_all", dt_all); dump("a_all", a_all); dump("e_all", e_all)
                if "A1" in phases:
                    with ExitStack() as e1:
                        wg = [sb(e1, "wg%d" % i, [128, 8, 768], BF16) for i in range(2)]
                        wsem = [dsem(), dsem()]
                        dg = [sb(e1, "dg%d" % i, [128, 4, 4, 128], BF16) for i in range(2)]
                        ust = [sb(e1, "ust%d" % i, [128, 4, 515], BF16) for i in range(2)]
                        xc = [sb(e1, "xc%d" % i, [128, 4, 512], BF16) for i in range(2)]
                        state = sb(e1, "state", [128, 256], F32)
                        state_bf = [sb(e1, "state_bf%d" % i, [128, 256], BF16) for i in range(3)]
                        zstate = sb(e1, "zstate", [128, 256], BF16)
                        ynT = [sb(e1, "ynT%d" % i, [128, 2, 512], BF16) for i in range(2)]
                        ysem = [dsem(), dsem()]
                        sz = [sb(e1, "sz%d" % i, [128, 256], BF16) for i in range(3)]
                        xbtm = [sb(e1, "xbtm%d" % i, [128, 384], BF16) for i in range(3)]
                        xd = [sb(e1, "xd%d" % i, [128, 4, 64], BF16) for i in range(3)]
                        y4 = [sb(e1, "y4_%d" % i, [128, 256], F32) for i in range(3)]
                        xde = [sb(e1, "xde%d" % i, [128, 4, 64], BF16) for i in range(2)]
                        cbm = [sb(e1, "cbm%d" % i, [128, 128], F32) for i in range(2)]
                        lh = [sb(e1, "lh%d" % i, [128, 4, 128], F32) for i in range(2)]
                        Ex = [sb(e1, "Ex%d" % i, [128, 4, 128], F32) for i in range(2)]
                        MT = [sb(e1, "MT%d" % i, [128, 4, 128], BF16) for i in range(2)]
                        y1 = [sb(e1, "y1_%d" % i, [128, 4, 64], F32) for i in range(2)]
                        tD = [sb(e1, "tD%d" % i, [128, 4, 64], F32) for i in range(2)]
                        yb = [sb(e1, "yb%d" % i, [128, 256], BF16) for i in range(2)]
                        junk2 = sb(e1, "junk2", [128, 256], F32)
                        ss2 = [sb(e1, "ss2_%d" % i, [128, 1], F32) for i in range(2)]
                        rs2 = [sb(e1, "rs2_%d" % i, [128, 1], F32) for i in range(2)]
                        p_u = [ps(e1, "p_u%d" % i, [128, 512], F32) for i in range(2)]
                        bk1 = psbank(e1, "bk1", F32)
                        p_z = T(bk1[:, 0:256])
                        p_cb = T(bk1[:, 256:384])
                        p_cb.r = p_z.r
                        p_s = ps(e1, "p_s", [128, 256], F32)
                        p_seg1 = ps(e1, "p_seg", [128, 4, 128], F32)
                        p_seg = [p_seg1, p_seg1]
                        bk4 = psbank(e1, "bk4", F32)
                        p_y = T(bk4[:, 0:256])
                        p_yo = T(bk4[:, 256:512])
                        p_yo.r = p_y.r
                        bk6 = psbank(e1, "bk6", BF16)
                        p_tpx = T(bk6[:, 0:384])
                        bk7 = psbank(e1, "bk7", BF16)
                        p_tpy = T(bk7[:, 0:256])
                        k.op("pool", lambda e: e.memset(zstate.t[:], 0.0), writes=[zstate])
                        thz = [sb(e1, "thz%d" % i, [128, 256], F32) for i in range(2)]
                        thc = [sb(e1, "thc%d" % i, [128, 512], F32) for i in range(2)]
                        neghalf = sb(e1, "neghalf", [128, 1], F32)
                        k.op("pool", lambda e: e.memset(neghalf.t[:], -0.5), writes=[neghalf])
                        ones_row = sb(e1, "ones_row", [1, 512], BF16)
                        k.op("pool", lambda e: e.memset(ones_row.t[:], 1.0), writes=[ones_row])
                        cb_row = sb(e1, "cb_row", [1, 4096], F32)
                        k.dma("sp", sem_c, lambda e: e.dma_start(out=cb_row.t[:], in_=conv_brow_d), writes=[cb_row])
                        hb_row = sb(e1, "hb_row", [1, 4096], BF16)
                        k.op("dve", lambda e: e.tensor_scalar(out=hb_row.t[:], in0=cb_row.t[:], scalar1=0.5, scalar2=None, op0=ALU.mult),
                             reads=[cb_row], writes=[hb_row])

                        def group_setup(g):
                            w = wg[g % 2]
                            srcs = [(0, 2048 + g * 256, 256), (256, 4096 + g * 128, 128),
                                    (384, 5120 + g * 128, 128), (512, g * 256, 256)]
                            k.dma("pool", wsem[g % 2],
                                  [(lambda e, o=o, s=s, n=n: e.dma_start(
                                      out=w.t[:, :, o:o + n],
                                      in_=w_in_d[:, s:s + n].rearrange("(k p) n -> p k n", p=128))) for (o, s, n) in srcs],
                                  writes=[w])
                            d_ = dg[g % 2]
                            chunks = [2 * g, 2 * g + 1, 16 + g, 24 + g]
                            k.ops("pool", [(lambda e, cc=cc, kk=kk: e.tensor_scalar(
                                out=d_.t[:, cc, kk, :], in0=ident_bf.t[:], scalar1=conv_wT.t[:, chunks[cc], kk:kk + 1],
                                scalar2=0.5, op0=ALU.mult, op1=ALU.mult)) for cc in range(4) for kk in range(4)],
                                reads=[ident_bf, conv_wT], writes=[d_])
                            k.op("pool", lambda e: e.tensor_scalar(out=w.t[:, :, 512:768], in0=w.t[:, :, 512:768], scalar1=0.5,
                                                                   scalar2=0.0, op0=ALU.mult, op1=ALU.add), reads=[w], writes=[w])

                        NT = 8 * NCH

                        def dec(n):
                            g = n // NCH
                            c = n % NCH
                            return g, c, c // 4, c % 4

                        def S0a(si):
                            g, sc = si // NSC, si % NSC
                            w = wg[g % 2]
                            us = ust[si % 2]
                            ts = sc * 512
                            if sc == 0:
                                k.op("pool", lambda e: e.memset(us.t[:, :, 0:3], 0.0), writes=[us])
                            for cc in range(4):
                                pu = p_u[cc % 2]
                                k.ops("pe", [(lambda e, j=j: e.matmul(pu.t[:], lhsT=w.t[:, j, cc * 128:(cc + 1) * 128],
                                                                      rhs=hT.t[:, j, ts:ts + 512], start=(j == 0), stop=(j == 7)))
                                             for j in range(8)], reads=[w, hT], writes=[pu])
                                k.op("act", lambda e: e.copy(out=us.t[:, cc, 3:515], in_=pu.t[:]), reads=[pu], writes=[us])
                            if sc + 1 < NSC:
                                un = ust[(si + 1) % 2]
                                k.op("pool", lambda e: e.tensor_copy(out=un.t[:, :, 0:3], in_=us.t[:, :, 512:515]),
                                     reads=[us], writes=[un])

                        def S0b(si):
                            g, sc = si // NSC, si % NSC
                            d_ = dg[g % 2]
                            us = ust[si % 2]
                            xo = xc[si % 2]
                            chunks = [2 * g, 2 * g + 1, 16 + g, 24 + g]
                            for cc in range(4):
                                pu = p_u[cc % 2]
                                ch = chunks[cc]
                                k.ops("pe", [(lambda e, kk=kk: e.matmul(pu.t[:], lhsT=d_.t[:, cc, kk, :],
                                                                        rhs=us.t[:, cc, kk:kk + 512], start=(kk == 0), stop=False))
                                             for kk in range(4)] +
                                      [lambda e: e.matmul(pu.t[:], lhsT=hb_row.t[0:1, ch * 128:(ch + 1) * 128],
                                                          rhs=ones_row.t[0:1, :], start=False, stop=True)],
                                      reads=[d_, us, hb_row, ones_row], writes=[pu])
                                tc_ = thc[cc % 2]
                                k.op("act", lambda e: e.activation(out=tc_.t[:], in_=pu.t[:], func=AF.Tanh),
                                     reads=[pu], writes=[tc_])
                                k.op("dve", lambda e: e.scalar_tensor_tensor(out=xo.t[:, cc, :], in0=tc_.t[:], scalar=1.0, in1=pu.t[:],
                                                                             op0=ALU.add, op1=ALU.mult),
                                     reads=[tc_, pu], writes=[xo])

                        def ctx(n):
                            g, c, sc, q = dec(n)
                            si = g * NSC + sc
                            return dict(g=g, c=c, sc=sc, q=q, si=si, w=wg[g % 2], xo=xc[si % 2],
                                        qs=slice(q * 128, (q + 1) * 128), cs=slice(c * 128, (c + 1) * 128),
                                        g4=slice(g * 4, g * 4 + 4), b3=n % 3, b2=n % 2)

                        def A_pe(n):
                            x_ = ctx(n); w = x_["w"]; xo = x_["xo"]; qs = x_["qs"]; cs = x_["cs"]
                            k.ops("pe", [(lambda e, j=j: e.matmul(p_z.t[:], lhsT=hT.t[:, j, cs], rhs=w.t[:, j, 512:768],
                                                                  start=(j == 0), stop=(j == 7))) for j in range(8)],
                                  reads=[hT, w], writes=[p_z])
                            k.ops("pe", [(lambda e, cc=cc: e.transpose(out=p_tpx.t[:, cc * 128:(cc + 1) * 128],
                                                                       in_=xo.t[:, cc, qs], identity=ident_bf.t[:]))
                                         for cc in range(3)], reads=[xo, ident_bf], writes=[p_tpx])
                            k.op("pe", lambda e: e.matmul(p_cb.t[:], lhsT=xo.t[:, 2, qs], rhs=xo.t[:, 3, qs],
                                                          start=True, stop=True), reads=[xo], writes=[p_cb])

                        def A_act(n):
                            x_ = ctx(n); b3 = x_["b3"]; b2 = x_["b2"]; c = x_["c"]; g = x_["g"]
                            k.op("act", lambda e: e.activation(out=thz[b2].t[:], in_=p_z.t[:], func=AF.Tanh),
                                 reads=[p_z], writes=[thz[b2]])
                            k.op("act", lambda e: e.copy(out=xbtm[b3].t[:], in_=p_tpx.t[:]), reads=[p_tpx], writes=[xbtm[b3]])
                            k.ops("act", [(lambda e, r=r: e.activation(out=lh[b2].t[:, r, :], in_=Mgt.t[:], func=AF.Copy,
                                                                       scale=a_all.t[:, c, g * 4 + r:g * 4 + r + 1]))
                                          for r in range(4)], reads=[Mgt, a_all], writes=[lh[b2]])

                        def A_dve(n):
                            x_ = ctx(n); b3 = x_["b3"]; b2 = x_["b2"]; c = x_["c"]; g4 = x_["g4"]
                            k.op("dve", lambda e: e.scalar_tensor_tensor(out=sz[b3].t[:], in0=thz[b2].t[:], scalar=1.0, in1=p_z.t[:],
                                                                         op0=ALU.add, op1=ALU.mult),
                                 reads=[thz[b2], p_z], writes=[sz[b3]])
                            k.op("dve", lambda e: e.tensor_tensor(
                                out=xd[b3].t[:], in0=xbtm[b3].t[:, 0:256].rearrange("p (r d) -> p r d", r=4),
                                in1=dt_all.t[:, c, g4, None].to_broadcast([128, 4, 64]), op=ALU.mult),
                                reads=[xbtm[b3], dt_all], writes=[xd[b3]])
                            k.op("dve", lambda e: e.tensor_tensor(out=cbm[b2].t[:], in0=p_cb.t[:], in1=Mle.t[:], op=ALU.mult),
                                 reads=[p_cb, Mle], writes=[cbm[b2]])

                        def A_pool(n):
                            x_ = ctx(n); b3 = x_["b3"]; b2 = x_["b2"]; c = x_["c"]; g4 = x_["g4"]
                            k.op("pool", lambda e: e.tensor_tensor(
                                out=xde[b2].t[:], in0=xd[b3].t[:],
                                in1=e_all.t[:, c, 1, g4, None].to_broadcast([128, 4, 64]), op=ALU.mult),
                                reads=[xd[b3], e_all], writes=[xde[b2]])

                        def B_pe(n):
                            x_ = ctx(n); b3 = x_["b3"]; b2 = x_["b2"]
                            k.ops("pe", [(lambda e, r=r: e.matmul(p_seg[b2].t[:, r, :], lhsT=lh[b2].t[:, r, :], rhs=Mle.t[:],
                                                                  start=True, stop=True)) for r in range(4)],
                                  reads=[lh[b2], Mle], writes=[p_seg[b2]])
                            k.op("pe", lambda e: e.matmul(p_s.t[:], lhsT=xbtm[b3].t[:, 256:384],
                                                          rhs=xde[b2].t[:].rearrange("p r d -> p (r d)"), start=True, stop=True),
                                 reads=[xbtm[b3], xde[b2]], writes=[p_s])

                        def B_act(n):
                            x_ = ctx(n); b2 = x_["b2"]
                            k.op("act", lambda e: e.activation(out=Ex[b2].t[:], in_=p_seg[b2].t[:], func=AF.Exp),
                                 reads=[p_seg[b2]], writes=[Ex[b2]])

                        def B_dve(n):
                            x_ = ctx(n); b2 = x_["b2"]; c = x_["c"]; g4 = x_["g4"]
                            k.op("dve", lambda e: e.tensor_tensor(
                                out=MT[b2].t[:], in0=Ex[b2].t[:],
                                in1=cbm[b2].t[:, None, :].to_broadcast([128, 4, 128]), op=ALU.mult),
                                reads=[Ex[b2], cbm[b2]], writes=[MT[b2]])
                            if c == 0:
                                k.op("dve", lambda e: e.tensor_copy(out=state.t[:], in_=p_s.t[:]), reads=[p_s], writes=[state])
                            else:
                                k.op("dve", lambda e: e.tensor_tensor(
                                    out=state.t[:].rearrange("p (r d) -> p r d", r=4),
                                    in0=state.t[:].rearrange("p (r d) -> p r d", r=4),
                                    in1=e_all.t[:, c, 2, g4, None].to_broadcast([128, 4, 64]), op=ALU.mult),
                                    reads=[state, e_all], writes=[state])
                                k.op("dve", lambda e: e.tensor_tensor(out=state.t[:], in0=state.t[:], in1=p_s.t[:], op=ALU.add),
                                     reads=[state, p_s], writes=[state])

                        def B_dve2(n):
                            x_ = ctx(n); b3 = x_["b3"]; b2 = x_["b2"]; g4 = x_["g4"]
                            k.op("dve", lambda e: e.tensor_tensor(
                                out=tD[b2].t[:], in0=xbtm[b3].t[:, 0:256].rearrange("p (r d) -> p r d", r=4),
                                in1=dsk.t[:, g4, None].to_broadcast([128, 4, 64]), op=ALU.mult),
                                reads=[xbtm[b3], dsk], writes=[tD[b2]])

                        def C_act(n):
                            x_ = ctx(n); b3 = x_["b3"]
                            k.op("act", lambda e: e.copy(out=state_bf[b3].t[:], in_=state.t[:]),
                                 reads=[state], writes=[state_bf[b3]])

                        def C_pe(n):
                            x_ = ctx(n); b3 = x_["b3"]; b2 = x_["b2"]; xo = x_["xo"]; qs = x_["qs"]; c = x_["c"]
                            k.ops("pe", [(lambda e, r=r: e.matmul(p_y.t[:, r * 64:(r + 1) * 64], lhsT=MT[b2].t[:, r, :],
                                                                  rhs=xd[b3].t[:, r, :], start=True, stop=True))
                                         for r in range(4)], reads=[MT[b2], xd[b3]], writes=[p_y])
                            st_prev = zstate if c == 0 else state_bf[(n - 1) % 3]
                            k.op("pe", lambda e: e.matmul(p_yo.t[:], lhsT=xo.t[:, 3, qs], rhs=st_prev.t[:],
                                                          start=True, stop=True), reads=[xo, st_prev], writes=[p_yo])

                        def C_dve(n):
                            x_ = ctx(n); b3 = x_["b3"]; b2 = x_["b2"]; c = x_["c"]; g4 = x_["g4"]
                            k.op("dve", lambda e: e.tensor_tensor(
                                out=y1[b2].t[:], in0=p_yo.t[:].rearrange("p (r d) -> p r d", r=4),
                                in1=e_all.t[:, c, 0, g4, None].to_broadcast([128, 4, 64]), op=ALU.mult),
                                reads=[p_yo, e_all], writes=[y1[b2]])
                            k.op("dve", lambda e: e.tensor_tensor(
                                out=y1[b2].t[:], in0=y1[b2].t[:],
                                in1=p_y.t[:].rearrange("p (r d) -> p r d", r=4), op=ALU.add),
                                reads=[y1[b2], p_y], writes=[y1[b2]])
                            k.op("dve", lambda e: e.tensor_tensor(out=y1[b2].t[:], in0=y1[b2].t[:], in1=tD[b2].t[:], op=ALU.add),
                                 reads=[y1[b2], tD[b2]], writes=[y1[b2]])
                            k.op("dve", lambda e: e.tensor_tensor(
                                out=y4[b3].t[:], in0=y1[b2].t[:].rearrange("p r d -> p (r d)"), in1=sz[b3].t[:], op=ALU.mult),
                                reads=[y1[b2], sz[b3]], writes=[y4[b3]])

                        def D_act(n):
                            x_ = ctx(n); b3 = x_["b3"]; b2 = x_["b2"]
                            k.op("act", lambda e: e.activation(out=junk2.t[:], in_=y4[b3].t[:], func=AF.Square,
                                                               accum_out=ss2[b2].t[:, 0:1]),
                                 reads=[y4[b3]], writes=[junk2, ss2[b2]])

                        def D_pool(n):
                            x_ = ctx(n); b2 = x_["b2"]
                            k.op("pool", lambda e: e.tensor_scalar(out=rs2[b2].t[:], in0=ss2[b2].t[:], scalar1=1.0 / 256, scalar2=EPS,
                                                                   op0=ALU.mult, op1=ALU.add), reads=[ss2[b2]], writes=[rs2[b2]])
                            k.op("pool", lambda e: e.tensor_tensor(out=rs2[b2].t[:], in0=rs2[b2].t[:], in1=neghalf.t[:], op=ALU.pow),
                                 reads=[rs2[b2], neghalf], writes=[rs2[b2]])

                        def E_dve(n):
                            x_ = ctx(n); b3 = x_["b3"]; b2 = x_["b2"]
                            k.op("dve", lambda e: e.tensor_scalar(out=yb[b2].t[:], in0=y4[b3].t[:], scalar1=rs2[b2].t[:, 0:1],
                                                                  scalar2=None, op0=ALU.mult),
                                 reads=[y4[b3], rs2[b2]], writes=[yb[b2]])

                        def F_pe(n):
                            x_ = ctx(n); b2 = x_["b2"]
                            k.ops("pe", [(lambda e, h=h: e.transpose(out=p_tpy.t[:, h * 128:(h + 1) * 128],
                                                                     in_=yb[b2].t[:, h * 128:(h + 1) * 128], identity=ident_bf.t[:]))
                                         for h in range(2)], reads=[yb[b2], ident_bf], writes=[p_tpy])

                        def F_act(n):
                            x_ = ctx(n); g = x_["g"]; qs = x_["qs"]; si = x_["si"]; q = x_["q"]; sc = x_["sc"]
                            yo = ynT[si % 2]
                            for h in range(2):
                                k.op("act", lambda e: e.activation(out=yo.t[:, h, qs], in_=p_tpy.t[:, h * 128:(h + 1) * 128], func=AF.Copy,
                                                                   scale=gssdT.t[:, 2 * g + h:2 * g + h + 1]),
                                     reads=[p_tpy, gssdT], writes=[yo])
                            if q == 3:
                                ts = sc * 512
                                k.dma("sp", ysem[si % 2], lambda e: e.dma_start(
                                    out=ymT_d[2 * g:2 * g + 2, :, ts:ts + 512].rearrange("f p t -> p f t"), in_=yo.t[:]),
                                    reads=[yo])

                        def ok(n):
                            return 0 <= n < NT

                        import os as _os
                        if _os.environ.get("A1_NOSKEW"):
                            for n in range(NT):
                                g, c, sc, q = dec(n)
                                if c == 0:
                                    group_setup(g)
                                if q == 0:
                                    S0a(g * NSC + sc)
                                    S0b(g * NSC + sc)
                                for fn in (A_pe, A_act, A_dve, A_pool, B_pe, B_act, B_dve, B_dve2, C_pe, C_act, C_dve, D_act, D_pool, E_dve, F_pe, F_act):
                                    fn(n)
                        else:
                            LAGS = dict(A=0, B=1, C=2, D=3, E=4, F=5)
                            group_setup(0)
                            S0a(0)
                            S0b(0)
                            S0a(1)
                            for t in range(NT + 5):
                                for fn, lag in ((F_pe, 5), (C_pe, 2), (B_pe, 1), (A_pe, 0),
                                                (F_act, 5), (C_act, 2), (B_act, 1), (A_act, 0), (D_act, 3),
                                                (E_dve, 4), (B_dve, 1), (B_dve2, 1), (C_dve, 2), (A_dve, 0),
                                                (D_pool, 3), (A_pool, 0)):
                                    if ok(t - lag):
                                        fn(t - lag)
                                if t % 4 == 1 and t // 4 + 1 < 8 * NSC:
                                    S0b(t // 4 + 1)
                                if t % 4 == 2 and t // 4 + 2 < 8 * NSC:
                                    S0a(t // 4 + 2)
                                if t % NCH == 4 and t // NCH + 1 < 8:
                                    group_setup(t // NCH + 1)
                k.barrier()
                if "A1p" in phases:
                    with ExitStack() as e2:
                        wp = sb(e2, "wp", [128, 8, 1024], BF16)
                        k.dma("pool", sem_cp, lambda e: e.dma_start(
                            out=wp.t[:], in_=w_in_d[:, 6176:7200].rearrange("(k p) n -> p k n", p=128)), writes=[wp])
                        pw = sb(e2, "pw", [128, 4, 2, 256], BF16)
                        k.dma("pool", sem_cp, lambda e: e.dma_start(
                            out=pw.t[:], in_=pool_w_d.rearrange("g (k p) n -> p g k n", p=128)), writes=[pw])
                        pst = [sb(e2, "pst%d" % i, [128, 8, 527], F32) for i in range(2)]
                        sA = sb(e2, "sA", [128, 2, 527], F32)
                        sB = sb(e2, "sB", [128, 2, 527], F32)
                        ypl = [sb(e2, "ypl%d" % i, [128, 2, 512], BF16) for i in range(2)]
                        tmp16 = sb(e2, "tmp16", [128, 2, 16], F32)
                        ypT = [sb(e2, "ypT%d" % i, [128, 2, 512], BF16) for i in range(2)]
                        psem_ = [dsem(), dsem()]
                        p_u2 = [ps(e2, "p_u2_%d" % i, [128, 512], F32) for i in range(2)]
                        p_p = [ps(e2, "p_p%d" % i, [128, 512], F32) for i in range(2)]
                        k.op("pool", lambda e: e.memset(pst[0].t[:, :, 0:15], 0.0), writes=[pst[0]])
                        it = 0
                        for sc in range(NSC):
                            ts = sc * 512
                            cur = pst[sc % 2]
                            nxt = pst[(sc + 1) % 2]
                            for pc in range(8):
                                pu = p_u2[pc % 2]
                                k.ops("pe", [(lambda e, j=j: e.matmul(pu.t[:], lhsT=wp.t[:, j, pc * 128:(pc + 1) * 128],
                                                                      rhs=hT.t[:, j, ts:ts + 512], start=(j == 0), stop=(j == 7)))
                                             for j in range(8)], reads=[wp, hT], writes=[pu])
                                k.op("act", lambda e: e.copy(out=cur.t[:, pc, 15:527], in_=pu.t[:]), reads=[pu], writes=[cur])
                            if sc + 1 < NSC:
                                k.op("pool", lambda e: e.tensor_copy(out=nxt.t[:, :, 0:15], in_=cur.t[:, :, 512:527]),
                                     reads=[cur], writes=[nxt])
                            for pg in range(4):
                                u = cur.t[:, 2 * pg:2 * pg + 2, :]
                                nlev = pg + 1
                                src = u
                                bufs = [sA, sB]
                                eng = "dve"
                                for lv in range(nlev):
                                    sh = 1 << lv
                                    lo = (1 << (lv + 1)) - 1
                                    dst = bufs[lv % 2]
                                    src_t = cur if lv == 0 else bufs[(lv - 1) % 2]
                                    s_ap = src
                                    k.op(eng, lambda e, dst=dst, s_ap=s_ap, lo=lo, sh=sh: e.tensor_tensor(
                                        out=dst.t[:, :, lo:527], in0=s_ap[:, :, lo:527], in1=s_ap[:, :, lo - sh:527 - sh], op=ALU.add),
                                        reads=[src_t], writes=[dst])
                                    src = dst.t[:, :, :]
                                fin = bufs[(nlev - 1) % 2]
                                wv = 1 << nlev
                                yp = ypl[it % 2]
                                k.op(eng, lambda e: e.scalar_tensor_tensor(
                                    out=yp.t[:], in0=fin.t[:, :, 15:527], scalar=1.0 / wv, in1=u[:, :, 15:527],
                                    op0=ALU.mult, op1=ALU.subtract), reads=[fin, cur], writes=[yp])
                                if sc == 0:
                                    k.op(eng, lambda e: e.tensor_tensor(
                                        out=tmp16.t[:], in0=fin.t[:, :, 15:31],
                                        in1=invc.t[:, pg, None, :].to_broadcast([128, 2, 16]), op=ALU.mult),
                                        reads=[fin, invc], writes=[tmp16])
                                    k.op(eng, lambda e: e.tensor_tensor(out=yp.t[:, :, 0:16], in0=tmp16.t[:], in1=u[:, :, 15:31],
                                                                        op=ALU.subtract), reads=[tmp16, cur], writes=[yp])
                                yT_ = ypT[it % 2]
                                for dc in range(2):
                                    pp = p_p[dc]
                                    k.ops("pe", [(lambda e, kc=kc: e.matmul(pp.t[:], lhsT=pw.t[:, pg, kc, dc * 128:(dc + 1) * 128],
                                                                            rhs=yp.t[:, kc, :], start=(kc == 0), stop=(kc == 1)))
                                                 for kc in range(2)], reads=[pw, yp], writes=[pp])
                                    k.op("act", lambda e: e.activation(out=yT_.t[:, dc, :], in_=pp.t[:], func=AF.Copy,
                                                                       scale=pscT.t[:, 2 * pg + dc:2 * pg + dc + 1]),
                                         reads=[pp, pscT], writes=[yT_])
                                k.dma("sp", psem_[it % 2], lambda e: e.dma_start(
                                    out=ymT_d[16 + 2 * pg:16 + 2 * pg + 2, :, ts:ts + 512].rearrange("f p t -> p f t"), in_=yT_.t[:]),
                                    reads=[yT_])
                                it += 1

        k.barrier()
        if "A2" in phases:
            with ExitStack() as e3:
                wout = sb(e3, "wout", [128, 24, DM], BF16)
                k.dma("pool", sem_cp, [(lambda e, i=i: e.dma_start(
                    out=wout.t[:, 6 * i:6 * i + 6, :],
                    in_=w_out_d[768 * i:768 * (i + 1), :].rearrange("(k p) n -> p k n", p=128))) for i in range(4)],
                    writes=[wout])
                gffn_bc = load_const(e3, "gffn_bc", [128, DM], gffn_bc_d)
                rw = load_const(e3, "rw", [128, 8, NE], rw_d.rearrange("(k p) n -> p k n", p=128))
                rb = load_const(e3, "rb", [128, NE], rb_d)
                ebase = load_const(e3, "ebase", [128, NE], ebase_d)
                s4096 = sb(e3, "s4096", [128, 514], I32)
                k.op("pool", lambda e: e.memset(s4096.t[:], L), writes=[s4096])
                ev_init = [
                    k.dma("sp", sem_c, lambda e: e.dma_start(out=slot_d.rearrange("(p f) o -> p (f o)", p=128), in_=s4096.t[:]),
                          reads=[s4096]),
                    k.dma("sp", sem_c, lambda e: e.dma_start(out=h2_d[L:L + 1, :], in_=zrow_bf.t[:]), reads=[zrow_bf]),
                    k.dma("sp", sem_c, lambda e: e.dma_start(out=G_d[L:L + 1, :], in_=zrow.t[:, 0:NE]), reads=[zrow]),
                    k.dma("sp", sem_c, lambda e: e.dma_start(out=Y_d[0:1, :], in_=zrow.t[:]), reads=[zrow]),
                ]
                ym = [sb(e3, "ym%d" % i, [128, 24, 512], BF16) for i in range(2)]
                ymsem = [dsem(), dsem()]
                xt2 = [sb(e3, "xt2_%d" % i, [128, DM], F32) for i in range(2)]
                xsem2 = [dsem(), dsem()]
                x1 = [sb(e3, "x1_%d" % i, [128, DM], F32) for i in range(2)]
                x1sem = [dsem(), dsem()]
                junk3_l = [sb(e3, "junk3%d" % i_, [128, DM], F32) for i_ in range(2)]
                ss3_l = [sb(e3, "ss3%d" % i_, [128, 1], F32) for i_ in range(2)]
                rs3_l = [sb(e3, "rs3%d" % i_, [128, 1], F32) for i_ in range(2)]
                h2f_l = [sb(e3, "h2f%d" % i_, [128, DM], F32) for i_ in range(2)]
                h2b = [sb(e3, "h2b%d" % i, [128, DM], BF16) for i in range(2)]
                h2sem = [dsem(), dsem()]
                h2T_l = [sb(e3, "h2T%d" % i_, [128, 8, 128], F32) for i_ in range(2)]
                lg_l = [sb(e3, "lg%d" % i_, [128, NE], F32) for i_ in range(2)]
                m8_l = [sb(e3, "m8%d" % i_, [128, 8], F32) for i_ in range(2)]
                nv1_l = [sb(e3, "nv1%d" % i_, [128, 1], F32) for i_ in range(2)]
                mask_l = [sb(e3, "mask%d" % i_, [128, NE], F32) for i_ in range(2)]
                ex_l = [sb(e3, "ex%d" % i_, [128, NE], F32) for i_ in range(2)]
                sm_l = [sb(e3, "sm%d" % i_, [128, 1], F32) for i_ in range(2)]
                Gt = [sb(e3, "Gt%d" % i, [128, NE], F32) for i in range(2)]
                gsem = [dsem(), dsem()]
                cnt = sb(e3, "cnt", [128, NE], F32)
                rank_l = [sb(e3, "rank%d" % i_, [128, NE], F32) for i_ in range(2)]
                vld_l = [sb(e3, "vld%d" % i_, [128, NE], F32) for i_ in range(2)]
                val_l = [sb(e3, "val%d" % i_, [128, NE], F32) for i_ in range(2)]
                v8_l = [sb(e3, "v8%d" % i_, [128, 8], F32) for i_ in range(2)]
                scsem = dsem()
                p_o_l = [ps(e3, "p_o%d" % i_, [128, DM], F32) for i_ in range(2)]
                p_tf = ps(e3, "p_tf", [128, 8, 128], F32)
                p_l = ps(e3, "p_l", [128, NE], F32)
                p_r = ps(e3, "p_r", [128, 2, NE], F32)
                k.op("pool", lambda e: e.memset(cnt.t[:], 0.0), writes=[cnt])
                scat_evs = []
                def ym_load(sc):
                    ts = sc * 512
                    ymc = ym[sc % 2]
                    k.dma("sp", ymsem[sc % 2], [(lambda e, i=i: e.dma_start(
                        out=ymc.t[:, 6 * i:6 * i + 6, :],
                        in_=ymT_d[6 * i:6 * i + 6, :, ts:ts + 512].rearrange("f p t -> p f t"))) for i in range(4)],
                        writes=[ymc])

                def a2body(sc, q):
                    ts = sc * 512
                    ymc = ym[sc % 2]
                    if q == 1 and sc + 1 < NSC:
                        ym_load(sc + 1)
                    c = sc * 4 + q
                    b = c % 2
                    qs = slice(q * 128, (q + 1) * 128)
                    cs = slice(c * 128, (c + 1) * 128)
                    p_o = p_o_l[b]
                    junk3 = junk3_l[b]; ss3 = ss3_l[b]; rs3 = rs3_l[b]; h2f = h2f_l[b]; h2T = h2T_l[b]; lg = lg_l[b]; m8 = m8_l[b]; nv1 = nv1_l[b]; mask = mask_l[b]; ex = ex_l[b]; sm = sm_l[b]; rank = rank_l[b]; vld = vld_l[b]; val = val_l[b]; v8 = v8_l[b]
                    yield
                    k.dma("sp", xsem2[b], lambda e: e.dma_start(out=xt2[b].t[:], in_=x_d[cs, :]), writes=[xt2[b]])
                    yield
                    k.ops("pe", [(lambda e, fc=fc, h=h: e.matmul(p_o.t[:, h * 512:(h + 1) * 512], lhsT=ymc.t[:, fc, qs],
                                                                 rhs=wout.t[:, fc, h * 512:(h + 1) * 512],
                                                                 start=(fc == 0), stop=(fc == 23)))
                                 for h in range(2) for fc in range(24)], reads=[ymc, wout], writes=[p_o])
                    yield
                    k.op("dve", lambda e: e.tensor_tensor(out=x1[b].t[:], in0=p_o.t[:], in1=xt2[b].t[:], op=ALU.add),
                         reads=[p_o, xt2[b]], writes=[x1[b]])
                    yield
                    k.dma("sp", x1sem[b], lambda e: e.dma_start(out=x1_d[cs, :], in_=x1[b].t[:]), reads=[x1[b]])
                    yield
                    k.op("act", lambda e: e.activation(out=junk3.t[:], in_=x1[b].t[:], func=AF.Square, accum_out=ss3.t[:, 0:1]),
                         reads=[x1[b]], writes=[junk3, ss3])
                    yield
                    k.op("pool", lambda e: e.tensor_scalar(out=rs3.t[:], in0=ss3.t[:], scalar1=1.0 / DM, scalar2=EPS,
                                                           op0=ALU.mult, op1=ALU.add), reads=[ss3], writes=[rs3])
                    yield
                    k.op("pool", lambda e: e.tensor_tensor(out=rs3.t[:], in0=rs3.t[:], in1=neghalf_p.t[:], op=ALU.pow),
                         reads=[rs3, neghalf_p], writes=[rs3])
                    yield
                    k.op("dve", lambda e: e.scalar_tensor_tensor(out=h2f.t[:], in0=x1[b].t[:], scalar=rs3.t[:, 0:1],
                                                                 in1=gffn_bc.t[:], op0=ALU.mult, op1=ALU.mult),
                         reads=[x1[b], rs3, gffn_bc], writes=[h2f])
                    yield
                    k.op("act", lambda e: e.copy(out=h2b[b].t[:], in_=h2f.t[:]), reads=[h2f], writes=[h2b[b]])
                    yield
                    k.dma("sp", h2sem[b], lambda e: e.dma_start(out=h2_d[cs, :], in_=h2b[b].t[:]), reads=[h2b[b]])
                    yield
                    k.ops("pe", [(lambda e, j=j: e.transpose(out=p_tf.t[:, j, :], in_=h2f.t[:, j * 128:(j + 1) * 128],
                                                             identity=ident_f.t[:])) for j in range(8)],
                          reads=[h2f, ident_f], writes=[p_tf])
                    yield
                    k.op("act", lambda e: e.copy(out=h2T.t[:], in_=p_tf.t[:]), reads=[p_tf], writes=[h2T])
                    yield
                    k.ops("pe", [(lambda e, j=j: e.matmul(p_l.t[:], lhsT=h2T.t[:, j, :], rhs=rw.t[:, j, :],
                                                          start=(j == 0), stop=(j == 7))) for j in range(8)],
                          reads=[h2T, rw], writes=[p_l])
                    yield
                    k.op("dve", lambda e: e.tensor_tensor(out=lg.t[:], in0=p_l.t[:], in1=rb.t[:], op=ALU.add),
                         reads=[p_l, rb], writes=[lg])
                    yield
                    k.op("dve", lambda e: e.max(out=m8.t[:], in_=lg.t[:]), reads=[lg], writes=[m8])
                    yield
                    k.op("dve", lambda e: e.tensor_scalar(out=mask.t[:], in0=lg.t[:], scalar1=m8.t[:, 3:4], scalar2=None,
                                                          op0=ALU.is_ge), reads=[lg, m8], writes=[mask])
                    yield
                    k.op("dve", lambda e: e.tensor_scalar(out=nv1.t[:], in0=m8.t[:, 0:1], scalar1=-1.0, scalar2=None,
                                                          op0=ALU.mult), reads=[m8], writes=[nv1])
                    yield
                    k.op("act", lambda e: e.activation(out=ex.t[:], in_=lg.t[:], func=AF.Exp, bias=nv1.t[:, 0:1]),
                         reads=[lg, nv1], writes=[ex])
                    yield
                    k.op("dve", lambda e: e.tensor_tensor(out=ex.t[:], in0=ex.t[:], in1=mask.t[:], op=ALU.mult),
                         reads=[ex, mask], writes=[ex])
                    yield
                    k.op("dve", lambda e: e.reduce_sum(out=sm.t[:], in_=ex.t[:], axis=AX.X), reads=[ex], writes=[sm])
                    yield
                    k.op("dve", lambda e: e.reciprocal(out=sm.t[:], in_=sm.t[:]), reads=[sm], writes=[sm])
                    yield
                    k.op("dve", lambda e: e.tensor_scalar(out=Gt[b].t[:], in0=ex.t[:], scalar1=sm.t[:, 0:1], scalar2=None,
                                                          op0=ALU.mult), reads=[ex, sm], writes=[Gt[b]])
                    yield
                    k.dma("sp", gsem[b], lambda e: e.dma_start(out=G_d[cs, :], in_=Gt[b].t[:]), reads=[Gt[b]])
                    yield
                    k.ops("pe", [
                        lambda e: e.matmul(p_r.t[:, 0, :], lhsT=Mlt.t[:], rhs=mask.t[:], start=True, stop=True),
                        lambda e: e.matmul(p_r.t[:, 1, :], lhsT=ones_f.t[:], rhs=mask.t[:], start=True, stop=True),
                    ], reads=[Mlt, ones_f, mask], writes=[p_r])
                    yield
                    k.op("dve", lambda e: e.tensor_tensor(out=rank.t[:], in0=p_r.t[:, 0, :], in1=cnt.t[:], op=ALU.add),
                         reads=[p_r, cnt], writes=[rank])
                    yield
                    k.op("dve", lambda e: e.tensor_tensor(out=cnt.t[:], in0=p_r.t[:, 1, :], in1=cnt.t[:], op=ALU.add),
                         reads=[p_r, cnt], writes=[cnt])
                    yield
                    k.op("dve", lambda e: e.tensor_scalar(out=vld.t[:], in0=rank.t[:], scalar1=float(CAP), scalar2=None,
                                                          op0=ALU.is_lt), reads=[rank], writes=[vld])
                    yield
                    k.op("dve", lambda e: e.tensor_tensor(out=vld.t[:], in0=vld.t[:], in1=mask.t[:], op=ALU.mult),
                         reads=[vld, mask], writes=[vld])
                    yield
                    k.op("dve", lambda e: e.tensor_tensor(out=val.t[:], in0=rank.t[:], in1=ebase.t[:], op=ALU.add),
                         reads=[rank, ebase], writes=[val])
                    yield
                    k.op("dve", lambda e: e.tensor_tensor(out=val.t[:], in0=val.t[:], in1=vld.t[:], op=ALU.mult),
                         reads=[val, vld], writes=[val])
                    yield
                    k.op("dve", lambda e: e.max(out=v8.t[:], in_=val.t[:]), reads=[val], writes=[v8])
                    yield
                    k.op("dve", lambda e: e.tensor_copy(out=dest_all.t[:, c, :], in_=v8.t[:, 0:4]), reads=[v8], writes=[dest_all])
                    yield
                    for kk in range(4):
                        scat_evs.append(k.dma("pool", scsem, lambda e: e.indirect_dma_start(
                            out=slot_d[:, :], out_offset=bass.IndirectOffsetOnAxis(ap=dest_all.t[:, c, kk:kk + 1], axis=0),
                            in_=tokid.t[:, c, :], in_offset=None),
                            reads=[dest_all, tokid], extra=ev_init))
                    yield
                ym_load(0)
                run_chains([a2body(sc, q) for sc in range(NSC) for q in range(4)], lag=8)
                a2_done = [x1[0], x1[1], h2b[0], h2b[1], Gt[0], Gt[1]]
                a2_evs = list(scat_evs[-1:])
                for t in a2_done:
                    a2_evs += t.r.rs
        else:
            a2_evs = []

        k.barrier()
        if "B" in phases:
            with ExitStack() as e4:
                bguT = load_const(e4, "bguT", [128, NE, 16], bguT_d)
                wgu = [sb(e4, "wgu%d" % i, [128, 8, 2048], BF16) for i in range(2)]
                wdn = [sb(e4, "wdn%d" % i, [128, 8, DM], BF16) for i in range(2)]
                bdb = [sb(e4, "bdb%d" % i, [128, DM], F32) for i in range(2)]
                wesem = [dsem(), dsem()]
                bdsem = [dsem(), dsem()]
                idx = [sb(e4, "idx%d" % i, [128, NBLK, 2], I32) for i in range(2)]
                isem = [dsem(), dsem()]
                xg = [sb(e4, "xg%d" % i, [128, DM], BF16) for i in range(NBLK)]
                xgsem = [dsem() for _ in range(NBLK)]
                gg = [sb(e4, "gg%d" % i, [128, NBLK, NE], F32) for i in range(2)]
                ggsem = [dsem(), dsem()]
                xgT = sb(e4, "xgT", [128, 8, CAP], BF16)
                actT = sb(e4, "actT", [128, 8, CAP], BF16)
                HW = CAP // 2
                gm = [sb(e4, "gm%d" % i, [128, HW], F32) for i in range(2)]
                sg = [sb(e4, "sg%d" % i, [128, HW], F32) for i in range(2)]
                u1 = [sb(e4, "u1_%d" % i, [128, HW], F32) for i in range(2)]
                yA = [sb(e4, "yA%d" % i, [128, DM], F32) for i in range(2)]
                yB = [sb(e4, "yB%d" % i, [128, DM], F32) for i in range(2)]
                ysem2 = [dsem(), dsem()]
                p_tg2 = [ps(e4, "p_tg%d" % i, [128, 8, 128], BF16) for i in range(2)]
                p_g = [ps(e4, "p_g%d" % i, [128, 512], F32) for i in range(2)]
                p_up = [ps(e4, "p_up%d" % i, [128, 512], F32) for i in range(2)]
                p_dh = [ps(e4, "p_dh%d" % i, [128, 512], F32) for i in range(2)]

                def load_w(e_):
                    s = e_ % 2
                    k.dma("pool", wesem[s],
                          [(lambda e, i=i: e.dma_start(out=wgu[s].t[:, 2 * i:2 * i + 2, :],
                                                       in_=wgu_d[e_, 256 * i:256 * (i + 1), :].rearrange("(k p) n -> p k n", p=128)))
                           for i in range(4)] +
                          [(lambda e, i=i: e.dma_start(out=wdn[s].t[:, 4 * i:4 * i + 4, :],
                                                       in_=wd_d[e_, 512 * i:512 * (i + 1), :].rearrange("(k p) n -> p k n", p=128)))
                           for i in range(2)],
                          writes=[wgu[s], wdn[s]])
                    k.dma("sp", bdsem[s], lambda e: e.dma_start(out=bdb[s].t[:], in_=bd_d[e_:e_ + 1, :].to_broadcast([128, DM])),
                          writes=[bdb[s]])

                def prefetch(e_):
                    s_ = e_ % 2
                    base_ = 1 + e_ * CAP
                    k.dma("sp", isem[s_], [(lambda e, j=j: e.dma_start(out=idx[s_].t[:, j, :],
                                                                       in_=slot_d[base_ + j * 128:base_ + (j + 1) * 128, :]))
                                           for j in range(NBLK)], writes=[idx[s_]], extra=a2_evs)
                    for j in range(NBLK):
                        k.dma("pool", xgsem[j], lambda e: e.indirect_dma_start(
                            out=xg[j].t[:, :], out_offset=None, in_=h2_d[:, :],
                            in_offset=bass.IndirectOffsetOnAxis(ap=idx[s_].t[:, j, 0:1], axis=0)),
                            reads=[idx[s_]], writes=[xg[j]], extra=a2_evs)
                    k.dma("pool", ggsem[s_], [(lambda e, j=j: e.indirect_dma_start(
                        out=gg[s_].t[:, j, :], out_offset=None, in_=G_d[:, :],
                        in_offset=bass.IndirectOffsetOnAxis(ap=idx[s_].t[:, j, 0:1], axis=0))) for j in range(NBLK)],
                        reads=[idx[s_]], writes=[gg[s_]], extra=a2_evs)

                def transposes():
                    for j in range(NBLK):
                        p_tg = p_tg2[j % 2]
                        k.ops("pe", [(lambda e, kk=kk: e.transpose(out=p_tg.t[:, kk, :], in_=xg[j].t[:, kk * 128:(kk + 1) * 128],
                                                                   identity=ident_bf.t[:])) for kk in range(8)],
                              reads=[xg[j], ident_bf], writes=[p_tg])
                        k.op("act", lambda e: e.copy(out=xgT.t[:, :, j * 128:(j + 1) * 128], in_=p_tg.t[:]),
                             reads=[p_tg], writes=[xgT])

                load_w(0)
                prefetch(0)
                transposes()
                for e_ in range(NE):
                    s = e_ % 2
                    base = 1 + e_ * CAP
                    if e_ + 1 < NE:
                        prefetch(e_ + 1)
                        load_w(e_ + 1)
                    for fc in range(8):
                        for h in range(2):
                            hs = slice(h * HW, (h + 1) * HW)
                            k.ops("pe", [(lambda e, kk=kk: e.matmul(p_g[h].t[:, 0:HW], lhsT=wgu[s].t[:, kk, fc * 128:(fc + 1) * 128],
                                                                    rhs=xgT.t[:, kk, hs], start=(kk == 0), stop=(kk == 7)))
                                         for kk in range(8)], reads=[wgu[s], xgT], writes=[p_g[h]])
                            k.ops("pe", [(lambda e, kk=kk: e.matmul(p_up[h].t[:, 0:HW],
                                                                    lhsT=wgu[s].t[:, kk, 1024 + fc * 128:1024 + (fc + 1) * 128],
                                                                    rhs=xgT.t[:, kk, hs], start=(kk == 0), stop=(kk == 7)))
                                         for kk in range(8)], reads=[wgu[s], xgT], writes=[p_up[h]])
                            k.op("dve", lambda e: e.tensor_scalar(out=gm[h].t[:], in0=p_g[h].t[:, 0:HW],
                                                                  scalar1=bguT.t[:, e_, fc:fc + 1], scalar2=7.0,
                                                                  op0=ALU.add, op1=ALU.min), reads=[p_g[h], bguT], writes=[gm[h]])
                            k.op("act", lambda e: e.activation(out=sg[h].t[:], in_=gm[h].t[:], func=AF.Sigmoid, scale=1.702),
                                 reads=[gm[h]], writes=[sg[h]])
                            k.op("dve", lambda e: e.tensor_scalar(out=u1[h].t[:], in0=p_up[h].t[:, 0:HW],
                                                                  scalar1=bguT.t[:, e_, 8 + fc:9 + fc], scalar2=7.0,
                                                                  op0=ALU.add, op1=ALU.min), reads=[p_up[h], bguT], writes=[u1[h]])
                            k.op("dve", lambda e: e.tensor_scalar(out=u1[h].t[:], in0=u1[h].t[:], scalar1=-7.0, scalar2=1.0,
                                                                  op0=ALU.max, op1=ALU.add), reads=[u1[h]], writes=[u1[h]])
                            k.op("dve", lambda e: e.tensor_tensor(out=sg[h].t[:], in0=gm[h].t[:], in1=sg[h].t[:], op=ALU.mult),
                                 reads=[gm[h], sg[h]], writes=[sg[h]])
                            k.op("dve", lambda e: e.tensor_tensor(out=actT.t[:, fc, hs], in0=sg[h].t[:], in1=u1[h].t[:], op=ALU.mult),
                                 reads=[sg[h], u1[h]], writes=[actT])
                    if e_ + 1 < NE:
                        transposes()
                    for j in range(NBLK):
                        js = slice(j * 128, (j + 1) * 128)
                        b = j % 2
                        for h in range(2):
                            k.ops("pe", [(lambda e, kk=kk: e.matmul(p_dh[h].t[:], lhsT=actT.t[:, kk, js],
                                                                    rhs=wdn[s].t[:, kk, h * 512:(h + 1) * 512],
                                                                    start=(kk == 0), stop=(kk == 7)))
                                         for kk in range(8)], reads=[actT, wdn[s]], writes=[p_dh[h]])
                            k.op("dve", lambda e: e.tensor_tensor(out=yA[b].t[:, h * 512:(h + 1) * 512], in0=p_dh[h].t[:],
                                                                  in1=bdb[s].t[:, h * 512:(h + 1) * 512], op=ALU.add),
                                 reads=[p_dh[h], bdb[s]], writes=[yA[b]])
                        k.op("act", lambda e: e.activation(out=yB[b].t[:], in_=yA[b].t[:], func=AF.Copy,
                                                           scale=gg[s].t[:, j, e_:e_ + 1]),
                             reads=[yA[b], gg[s]], writes=[yB[b]])
                        k.dma("sp", ysem2[b], lambda e: e.dma_start(out=Y_d[base + j * 128:base + (j + 1) * 128, :], in_=yB[b].t[:]),
                              reads=[yB[b]])
                b_evs = []
                for t in yB:
                    b_evs += t.r.rs
        else:
            b_evs = []

        k.barrier()
        fin_evs = []
        if "C" in phases:
            with ExitStack() as e5:
                wpg = sb(e5, "wpg", [128, 8, DM], BF16)
                k.dma("pool", sem_cp, [(lambda e, i=i: e.dma_start(
                    out=wpg.t[:, 4 * i:4 * i + 4, :],
                    in_=wpg_d[512 * i:512 * (i + 1), :].rearrange("(k p) n -> p k n", p=128))) for i in range(2)], writes=[wpg])
                wpp = sb(e5, "wpp", [128, 2, DM], BF16)
                k.dma("pool", sem_cp, lambda e: e.dma_start(out=wpp.t[:], in_=wpp_d.rearrange("(k p) n -> p k n", p=128)),
                      writes=[wpp])
                gpgT = load_const(e5, "gpgT", [128, 8], gpgT_d)
                gpn_bc = load_const(e5, "gpn_bc", [128, DM], gpn_bc_d)
                gfin_bc = load_const(e5, "gfin_bc", [128, DM], gfin_bc_d)
                x1c = [sb(e5, "x1c%d" % i, [128, DM], F32) for i in range(3)]
                x1csem = [dsem(), dsem(), dsem()]
                yk = [[sb(e5, "yk%d_%d" % (i, kk), [128, DM], F32) for kk in range(4)] for i in range(3)]
                yksem = [[dsem() for kk in range(4)] for i in range(3)]
                pt = [sb(e5, "pt%d" % i, [128, 256], F32) for i in range(3)]
                ptsem = [dsem(), dsem(), dsem()]
                x2_l = [sb(e5, "x2%d" % i_, [128, DM], F32) for i_ in range(2)]
                junk4_l = [sb(e5, "junk4%d" % i_, [128, DM], F32) for i_ in range(2)]
                ssc_l = [sb(e5, "ssc%d" % i_, [128, 1], F32) for i_ in range(2)]
                rsc_l = [sb(e5, "rsc%d" % i_, [128, 1], F32) for i_ in range(2)]
                xnb_l = [sb(e5, "xnb%d" % i_, [128, DM], BF16) for i_ in range(2)]
                xnT_l = [sb(e5, "xnT%d" % i_, [128, 8, 128], BF16) for i_ in range(2)]
                sgate_l = [sb(e5, "sgate%d" % i_, [128, DM], F32) for i_ in range(2)]
                pT_l = [sb(e5, "pT%d" % i_, [128, 2, 128], BF16) for i_ in range(2)]
                sse_l = [sb(e5, "sse%d" % i_, [128, 1], F32) for i_ in range(2)]
                rse_l = [sb(e5, "rse%d" % i_, [128, 1], F32) for i_ in range(2)]
                e1__l = [sb(e5, "e1_%d" % i_, [128, DM], F32) for i_ in range(2)]
                x3_l = [sb(e5, "x3%d" % i_, [128, DM], F32) for i_ in range(2)]
                ssf_l = [sb(e5, "ssf%d" % i_, [128, 1], F32) for i_ in range(2)]
                rsf_l = [sb(e5, "rsf%d" % i_, [128, 1], F32) for i_ in range(2)]
                ot = [sb(e5, "ot%d" % i, [128, DM], F32) for i in range(2)]
                osem = [dsem(), dsem()]
                p_t3 = ps(e5, "p_t3", [128, 8, 128], BF16)
                p_ga = ps(e5, "p_ga", [128, DM], F32)
                p_pt = ps(e5, "p_pt", [128, 2, 128], F32)
                p_e = ps(e5, "p_e", [128, DM], F32)
                def cload(c):
                    b3 = c % 3
                    cs = slice(c * 128, (c + 1) * 128)
                    k.dma("sp", x1csem[b3], lambda e: e.dma_start(out=x1c[b3].t[:], in_=x1_d[cs, :]), writes=[x1c[b3]], extra=a2_evs)
                    k.dma("sp", ptsem[b3], lambda e: e.dma_start(out=pt[b3].t[:], in_=pin_d[cs, :]), writes=[pt[b3]])
                    for kk in range(4):
                        k.dma("pool", yksem[b3][kk], lambda e: e.indirect_dma_start(
                            out=yk[b3][kk].t[:, :], out_offset=None, in_=Y_d[:, :],
                            in_offset=bass.IndirectOffsetOnAxis(ap=dest_all.t[:, c, kk:kk + 1], axis=0)),
                            reads=[dest_all], writes=[yk[b3][kk]], extra=b_evs)

                def cbody(c):
                    b = c % 2
                    b3 = c % 3
                    cs = slice(c * 128, (c + 1) * 128)
                    x2 = x2_l[b]; junk4 = junk4_l[b]; ssc = ssc_l[b]; rsc = rsc_l[b]; xnb = xnb_l[b]; xnT = xnT_l[b]; sgate = sgate_l[b]; pT = pT_l[b]; sse = sse_l[b]; rse = rse_l[b]; e1_ = e1__l[b]; x3 = x3_l[b]; ssf = ssf_l[b]; rsf = rsf_l[b]
                    if c == 0:
                        cload(0)
                    if c + 1 < NCH:
                        cload(c + 1)
                    yield
                    k.op("dve", lambda e: e.tensor_tensor(out=x2.t[:], in0=x1c[b3].t[:], in1=yk[b3][0].t[:], op=ALU.add),
                         reads=[x1c[b3], yk[b3][0]], writes=[x2])
                    yield
                    k.op("pool", lambda e: e.tensor_tensor(out=yk[b3][1].t[:], in0=yk[b3][1].t[:], in1=yk[b3][2].t[:], op=ALU.add),
                         reads=[yk[b3][1], yk[b3][2]], writes=[yk[b3][1]])
                    yield
                    k.op("dve", lambda e: e.tensor_tensor(out=x2.t[:], in0=x2.t[:], in1=yk[b3][3].t[:], op=ALU.add),
                         reads=[x2, yk[b3][3]], writes=[x2])
                    yield
                    k.op("dve", lambda e: e.tensor_tensor(out=x2.t[:], in0=x2.t[:], in1=yk[b3][1].t[:], op=ALU.add),
                         reads=[x2, yk[b3][1]], writes=[x2])
                    yield
                    k.op("act", lambda e: e.activation(out=junk4.t[:], in_=x2.t[:], func=AF.Square, accum_out=ssc.t[:, 0:1]),
                         reads=[x2], writes=[junk4, ssc])
                    yield
                    k.op("pool", lambda e: e.tensor_scalar(out=rsc.t[:], in0=ssc.t[:], scalar1=1.0 / DM, scalar2=EPS,
                                                           op0=ALU.mult, op1=ALU.add), reads=[ssc], writes=[rsc])
                    yield
                    k.op("pool", lambda e: e.tensor_tensor(out=rsc.t[:], in0=rsc.t[:], in1=neghalf_p.t[:], op=ALU.pow),
                         reads=[rsc, neghalf_p], writes=[rsc])
                    yield
                    k.op("act", lambda e: e.activation(out=xnb.t[:], in_=x2.t[:], func=AF.Copy, scale=rsc.t[:, 0:1]),
                         reads=[x2, rsc], writes=[xnb])
                    yield
                    k.ops("pe", [(lambda e, j=j: e.transpose(out=p_t3.t[:, j, :], in_=xnb.t[:, j * 128:(j + 1) * 128],
                                                             identity=ident_bf.t[:])) for j in range(8)],
                          reads=[xnb, ident_bf], writes=[p_t3])
                    yield
                    k.op("dve", lambda e: e.tensor_tensor(out=xnT.t[:], in0=p_t3.t[:],
                                                          in1=gpgT.t[:, :, None].to_broadcast([128, 8, 128]), op=ALU.mult),
                         reads=[p_t3, gpgT], writes=[xnT])
                    yield
                    k.ops("pe", [(lambda e, j=j, h=h: e.matmul(p_ga.t[:, h * 512:(h + 1) * 512], lhsT=xnT.t[:, j, :],
                                                               rhs=wpg.t[:, j, h * 512:(h + 1) * 512], start=(j == 0), stop=(j == 7)))
                                 for h in range(2) for j in range(8)], reads=[xnT, wpg], writes=[p_ga])
                    yield
                    k.op("act", lambda e: e.activation(out=sgate.t[:], in_=p_ga.t[:], func=AF.Sigmoid), reads=[p_ga], writes=[sgate])
                    yield
                    k.ops("pe", [(lambda e, j=j: e.transpose(out=p_pt.t[:, j, :], in_=pt[b3].t[:, j * 128:(j + 1) * 128],
                                                             identity=ident_f.t[:])) for j in range(2)],
                          reads=[pt[b3], ident_f], writes=[p_pt])
                    yield
                    k.op("act", lambda e: e.copy(out=pT.t[:], in_=p_pt.t[:]), reads=[p_pt], writes=[pT])
                    yield
                    k.ops("pe", [(lambda e, j=j, h=h: e.matmul(p_e.t[:, h * 512:(h + 1) * 512], lhsT=pT.t[:, j, :],
                                                               rhs=wpp.t[:, j, h * 512:(h + 1) * 512], start=(j == 0), stop=(j == 1)))
                                 for h in range(2) for j in range(2)], reads=[pT, wpp], writes=[p_e])
                    yield
                    k.op("act", lambda e: e.activation(out=junk4.t[:], in_=p_e.t[:], func=AF.Square, accum_out=sse.t[:, 0:1]),
                         reads=[p_e], writes=[junk4, sse])
                    yield
                    k.op("pool", lambda e: e.tensor_scalar(out=rse.t[:], in0=sse.t[:], scalar1=1.0 / DM, scalar2=EPS,
                                                           op0=ALU.mult, op1=ALU.add), reads=[sse], writes=[rse])
                    yield
                    k.op("pool", lambda e: e.tensor_tensor(out=rse.t[:], in0=rse.t[:], in1=neghalf_p.t[:], op=ALU.pow),
                         reads=[rse, neghalf_p], writes=[rse])
                    yield
                    k.op("dve", lambda e: e.scalar_tensor_tensor(out=e1_.t[:], in0=p_e.t[:], scalar=rse.t[:, 0:1], in1=gpn_bc.t[:],
                                                                 op0=ALU.mult, op1=ALU.mult), reads=[p_e, rse, gpn_bc], writes=[e1_])
                    yield
                    k.op("pool", lambda e: e.tensor_tensor(out=e1_.t[:], in0=e1_.t[:], in1=sgate.t[:], op=ALU.mult),
                         reads=[e1_, sgate], writes=[e1_])
                    yield
                    k.op("dve", lambda e: e.tensor_tensor(out=x3.t[:], in0=x2.t[:], in1=e1_.t[:], op=ALU.add),
                         reads=[x2, e1_], writes=[x3])
                    yield
                    k.op("act", lambda e: e.activation(out=junk4.t[:], in_=x3.t[:], func=AF.Square, accum_out=ssf.t[:, 0:1]),
                         reads=[x3], writes=[junk4, ssf])
                    yield
                    k.op("pool", lambda e: e.tensor_scalar(out=rsf.t[:], in0=ssf.t[:], scalar1=1.0 / DM, scalar2=EPS,
                                                           op0=ALU.mult, op1=ALU.add), reads=[ssf], writes=[rsf])
                    yield
                    k.op("pool", lambda e: e.tensor_tensor(out=rsf.t[:], in0=rsf.t[:], in1=neghalf_p.t[:], op=ALU.pow),
                         reads=[rsf, neghalf_p], writes=[rsf])
                    yield
                    k.op("dve", lambda e: e.scalar_tensor_tensor(out=ot[b].t[:], in0=x3.t[:], scalar=rsf.t[:, 0:1], in1=gfin_bc.t[:],
                                                                 op0=ALU.mult, op1=ALU.mult), reads=[x3, rsf, gfin_bc], writes=[ot[b]])
                    yield
                    fin_evs.append(k.dma("sp", osem[b], lambda e: e.dma_start(out=out_d[cs, :], in_=ot[b].t[:]), reads=[ot[b]]))

                    yield
                run_chains([cbody(c) for c in range(NCH)], lag=6)

        tail = list(fin_evs[-2:]) + list(a2_evs) + list(b_evs)
        for sid_ev in tail:
            k.wait("sp", sid_ev)
        for sid_, sem_ in k.dma_objs.items():
            k.wait("sp", (sem_, k.dma_cnt[sid_]))
        build.stats = (k.ninst, k.nwaits, dict(k.cnt))
    return nc


def host_layout(inp):
    f = np.float32

    def colT(v, n):
        return np.ascontiguousarray(np.asarray(v, f).reshape(n, 128).T)

    def bc(v):
        v = np.asarray(v, f).reshape(1, -1)
        return np.ascontiguousarray(np.broadcast_to(v, (128, v.shape[1])))

    cw = np.asarray(inp["conv_w"][0], f)
    conv_wT = np.ascontiguousarray(cw.reshape(4, 32, 128).transpose(2, 1, 0))
    bgu = np.asarray(inp["b_gate_up"][0], f)
    bguT = np.ascontiguousarray(bgu.reshape(NE, 16, 128).transpose(2, 0, 1))
    invc = np.zeros((128, 4, 16), f)
    for gi, w in enumerate((2, 4, 8, 16)):
        invc[:, gi, :] = 1.0 / np.minimum(np.arange(16) + 1, w).astype(f)
    ebase = (1 + np.arange(NE) * CAP).astype(f)
    shared = {
        "w_in": np.ascontiguousarray(inp["w_in"][0], f),
        "conv_wT": conv_wT,
        "conv_bT": colT(inp["conv_b"][0], 32),
        "conv_brow": np.ascontiguousarray(np.asarray(inp["conv_b"][0], f).reshape(1, 4096)),
        "dt_bias_bc": bc(inp["dt_bias"][0]),
        "a_log_bc": bc(inp["a_log"][0]),
        "d_skip_bc": bc(inp["d_skip"][0]),
        "gmixT": colT(inp["mix_norm_g"][0], 8),
        "gssdT": colT(inp["ssd_norm_g"][0], 16),
        "pool_w": np.ascontiguousarray(inp["pool_w"][0], f),
        "pscT": colT(inp["pool_scale"][0], 8),
        "invc": invc,
        "w_out": np.ascontiguousarray(inp["w_out"][0], f),
        "gffn_bc": bc(inp["ffn_norm_g"][0]),
        "router_w": np.ascontiguousarray(inp["router_w"][0], f),
        "rb_bc": bc(inp["router_b"][0]),
        "ebase_bc": bc(ebase),
        "w_gate_up": np.ascontiguousarray(inp["w_gate_up"][0], f),
        "bguT": bguT,
        "w_down": np.ascontiguousarray(inp["w_down"][0], f),
        "b_down": np.ascontiguousarray(inp["b_down"][0], f),
        "gpgT": colT(inp["ple_gate_norm_g"][0], 8),
        "w_ple_gate": np.ascontiguousarray(inp["w_ple_gate"][0], f),
        "w_ple_proj": np.ascontiguousarray(inp["w_ple_proj"][0], f),
        "gpn_bc": bc(inp["ple_norm_g"][0]),
        "gfin_bc": bc(inp["final_norm_g"]),
    }
    return shared


def kernel(**inputs):
    inp = {k_: np.asarray(v) for k_, v in inputs.items()}
    shared = host_layout(inp)
    x = np.asarray(inp["x"], np.float32)
    p = np.asarray(inp["p"], np.float32)[0]
    nb = x.shape[0]
    in_maps = []
    for b in range(nb):
        m = dict(shared)
        m["x"] = np.ascontiguousarray(x[b])
        m["p"] = np.ascontiguousarray(p[b])
        in_maps.append(m)
    nc = build()
    res = run_bass_kernel_spmd(nc, in_maps, core_ids=list(range(nb)))
    return np.stack([np.asarray(r["out"], np.float32) for r in res.results], axis=0)
```

```python
from contextlib import ExitStack
import numpy as np
import concourse.bass as bass
import concourse.mybir as mybir
from concourse.bass_utils import run_bass_kernel_spmd

F32 = mybir.dt.float32
BF16 = mybir.dt.bfloat16
I32 = mybir.dt.int32
AF = mybir.ActivationFunctionType
ALU = mybir.AluOpType
AX = mybir.AxisListType

L = 4096
DM = 1024
NCH = 32
NSC = 8
D_IN = 7200
NE = 32
CAP = 1024
NBLK = CAP // 128
NSLOT = NE * CAP
SLOT_TAB = 128 * 257
EPS = 1e-6


class R:
    __slots__ = ("w", "rs")

    def __init__(self):
        self.w = None
        self.rs = []


class T:
    def __init__(self, t):
        self.t = t
        self.r = R()


class K:
    def __init__(self, nc, sems):
        self.nc = nc
        self.engs = {"pe": nc.tensor, "dve": nc.vector, "act": nc.scalar,
                     "pool": nc.gpsimd, "sp": nc.sync}
        self.psem = sems
        self.cnt = {k: 0 for k in self.engs}
        self.waited = {k: {} for k in self.engs}
        self.dma_cnt = {}
        self.dma_objs = {}
        self.ninst = 0
        self.nwaits = 0

    def wait(self, ek, ev):
        if ev is None:
            return
        sem, val = ev
        sid = id(sem)
        if sid in self.dma_cnt:
            val = max(val, self.dma_cnt[sid])
        w = self.waited[ek]
        if w.get(sid, 0) >= val:
            return
        self.engs[ek].wait_ge(sem, val)
        self.nwaits += 1
        w[sid] = val

    def _deps(self, ek, reads, writes, extra):
        for t in reads:
            self.wait(ek, t.r.w)
        for t in writes:
            self.wait(ek, t.r.w)
            for e in t.r.rs:
                self.wait(ek, e)
        for e in extra:
            self.wait(ek, e)

    def _commit(self, ev, reads, writes):
        for t in reads:
            t.r.rs.append(ev)
        for t in writes:
            t.r.w = ev
            t.r.rs = []

    def barrier(self):
        for ek in self.engs:
            for o in self.engs:
                if self.cnt[o] > 0:
                    self.wait(ek, (self.psem[o], self.cnt[o]))
            for sid_, sem_ in self.dma_objs.items():
                self.wait(ek, (sem_, self.dma_cnt[sid_]))

    def op(self, ek, fn, reads=(), writes=(), extra=()):
        self._deps(ek, reads, writes, extra)
        ins = fn(self.engs[ek])
        self.cnt[ek] += 1
        ins.then_inc(self.psem[ek], 1)
        ev = (self.psem[ek], self.cnt[ek])
        self._commit(ev, reads, writes)
        self.ninst += 1
        return ev

    def ops(self, ek, fns, reads=(), writes=(), extra=()):
        self._deps(ek, reads, writes, extra)
        ins = None
        for fn in fns:
            ins = fn(self.engs[ek])
            self.ninst += 1
        self.cnt[ek] += 1
        ins.then_inc(self.psem[ek], 1)
        ev = (self.psem[ek], self.cnt[ek])
        self._commit(ev, reads, writes)
        return ev

    def dma(self, ek, sem, fns, reads=(), writes=(), extra=()):
        self._deps(ek, reads, writes, extra)
        if not isinstance(fns, (list, tuple)):
            fns = [fns]
        sid = id(sem)
        cur = self.dma_cnt.get(sid, 0)
        for f in fns:
            f(self.engs[ek]).then_inc(sem, 16)
            cur += 16
            self.ninst += 1
        self.dma_cnt[sid] = cur
        self.dma_objs[sid] = sem
        ev = (sem, cur)
        self._commit(ev, reads, writes)
        return ev


def run_chains(gens, lag=4, width=2):
    pending = list(gens)
    active = []
    while pending or active:
        if len(active) < width and pending and (not active or active[-1][1] >= lag):
            active.append([pending.pop(0), 0])
        for a in list(active):
            try:
                next(a[0])
                a[1] += 1
            except StopIteration:
                active.remove(a)


def build(debug=None, phases="A0,A1,A1p,A2,B,C"):
    phases = set(phases.split(","))
    debug = debug or ()
    nc = bass.Bass("TRN2", target_bir_lowering=False)

    def din(name, shape, dt=F32):
        return nc.dram_tensor(name, list(shape), dt, kind="ExternalInput").ap()

    x_d = din("x", [L, DM])
    pin_d = din("p", [L, 256])
    w_in_d = din("w_in", [DM, D_IN])
    conv_wT_d = din("conv_wT", [128, 32, 4])
    conv_bT_d = din("conv_bT", [128, 32])
    conv_brow_d = din("conv_brow", [1, 4096])
    dtb_d = din("dt_bias_bc", [128, 32])
    alog_d = din("a_log_bc", [128, 32])
    dsk_d = din("d_skip_bc", [128, 32])
    gmixT_d = din("gmixT", [128, 8])
    gssdT_d = din("gssdT", [128, 16])
    pool_w_d = din("pool_w", [4, 256, 256])
    pscT_d = din("pscT", [128, 8])
    invc_d = din("invc", [128, 4, 16])
    w_out_d = din("w_out", [3072, DM])
    gffn_bc_d = din("gffn_bc", [128, DM])
    rw_d = din("router_w", [DM, NE])
    rb_d = din("rb_bc", [128, NE])
    ebase_d = din("ebase_bc", [128, NE])
    wgu_d = din("w_gate_up", [NE, DM, 2048])
    bguT_d = din("bguT", [128, NE, 16])
    wd_d = din("w_down", [NE, DM, DM])
    bd_d = din("b_down", [NE, DM])
    gpgT_d = din("gpgT", [128, 8])
    wpg_d = din("w_ple_gate", [DM, DM])
    wpp_d = din("w_ple_proj", [256, DM])
    gpn_bc_d = din("gpn_bc", [128, DM])
    gfin_bc_d = din("gfin_bc", [128, DM])
    out_d = nc.dram_tensor("out", [L, DM], F32, kind="ExternalOutput").ap()

    def dscr(name, shape, dt, dbg=False):
        kind = "ExternalOutput" if (name in debug) else "Internal"
        return nc.dram_tensor(name, list(shape), dt, kind=kind).ap()

    ymT_d = dscr("ymT", [24, 128, L], BF16)
    x1_d = dscr("x1d", [L, DM], F32)
    h2_d = dscr("h2d", [L + 1, DM], BF16)
    G_d = dscr("Gd", [L + 1, NE], F32)
    slot_d = dscr("slotd", [SLOT_TAB, 2], I32)
    Y_d = dscr("Yd", [NSLOT + 1, DM], F32)

    with ExitStack() as es:
        E = es.enter_context
        sems = {k: E(nc.semaphore("prog_" + k)) for k in ["pe", "dve", "act", "pool", "sp"]}
        k = K(nc, sems)
        nsem = [0]

        def dsem():
            nsem[0] += 1
            return E(nc.semaphore("d%d" % nsem[0]))

        def sb(es_, name, shape, dt=F32):
            return T(es_.enter_context(nc.sbuf_tensor("s_" + name, list(shape), dt)))

        def ps(es_, name, shape, dt=F32):
            esz = 4 if dt == F32 else 2
            n = 1
            for d_ in shape[1:]:
                n *= d_
            per_bank = 2048 // esz
            nb_ = (n + per_bank - 1) // per_bank
            base = es_.enter_context(nc.psum_tensor("ps_" + name, [128, nb_ * per_bank], dt))
            v = base[:, 0:n]
            if len(shape) == 3:
                v = v.rearrange("p (a b) -> p a b", a=shape[1])
            return T(v)

        def psbank(es_, name, dt=F32):
            per_bank = 2048 // (4 if dt == F32 else 2)
            return es_.enter_context(nc.psum_tensor("ps_" + name, [128, per_bank], dt))

        ident_bf = sb(es, "ident_bf", [128, 128], BF16)
        ident_f = sb(es, "ident_f", [128, 128], F32)
        Mle = sb(es, "Mle", [128, 128], F32)
        Mgt = sb(es, "Mgt", [128, 128], F32)
        Mlt = sb(es, "Mlt", [128, 128], F32)
        ones_f = sb(es, "ones_f", [128, 128], F32)
        dest_all = sb(es, "dest_all", [128, NCH, 4], I32)
        tokid = sb(es, "tokid", [128, NCH, 2], I32)
        zrow = sb(es, "zrow", [1, DM], F32)
        zrow_bf = sb(es, "zrow_bf", [1, DM], BF16)

        def dump(name, t, ap=None):
            if name not in debug:
                return
            a = ap if ap is not None else t.t[:]
            dd = nc.dram_tensor("dbg_" + name, list(a.shape), a.dtype, kind="ExternalOutput").ap()
            k.dma("sp", dsem(), lambda e: e.dma_start(out=dd, in_=a), reads=[t])

        def cst(t, fn):
            k.op("pool", fn, writes=[t])

        cst(ident_bf, lambda e: e.memset(ident_bf.t[:], 1.0))
        cst(ident_bf, lambda e: e.affine_select(out=ident_bf.t[:], in_=ident_bf.t[:], pattern=[[-1, 128]],
                                               compare_op=ALU.is_equal, fill=0.0, base=0, channel_multiplier=1))
        cst(ident_f, lambda e: e.memset(ident_f.t[:], 1.0))
        cst(ident_f, lambda e: e.affine_select(out=ident_f.t[:], in_=ident_f.t[:], pattern=[[-1, 128]],
                                              compare_op=ALU.is_equal, fill=0.0, base=0, channel_multiplier=1))
        cst(Mle, lambda e: e.memset(Mle.t[:], 1.0))
        cst(Mle, lambda e: e.affine_select(out=Mle.t[:], in_=Mle.t[:], pattern=[[1, 128]],
                                          compare_op=ALU.is_ge, fill=0.0, base=0, channel_multiplier=-1))
        cst(Mgt, lambda e: e.memset(Mgt.t[:], 1.0))
        cst(Mgt, lambda e: e.affine_select(out=Mgt.t[:], in_=Mgt.t[:], pattern=[[-1, 128]],
                                          compare_op=ALU.is_gt, fill=0.0, base=0, channel_multiplier=1))
        cst(Mlt, lambda e: e.memset(Mlt.t[:], 1.0))
        cst(Mlt, lambda e: e.affine_select(out=Mlt.t[:], in_=Mlt.t[:], pattern=[[1, 128]],
                                          compare_op=ALU.is_gt, fill=0.0, base=0, channel_multiplier=-1))
        cst(ones_f, lambda e: e.memset(ones_f.t[:], 1.0))
        neghalf_p = sb(es, "neghalf_p", [128, 1], F32)
        cst(neghalf_p, lambda e: e.memset(neghalf_p.t[:], -0.5))
        cst(zrow, lambda e: e.memset(zrow.t[:], 0.0))
        cst(zrow_bf, lambda e: e.memset(zrow_bf.t[:], 0.0))
        cst(tokid, lambda e: e.iota(tokid.t[:], pattern=[[128, NCH], [0, 2]], base=0, channel_multiplier=1))
        cst(dest_all, lambda e: e.memset(dest_all.t[:], 0))

        sem_c = dsem()
        sem_cp = dsem()

        def load_const(es_, name, shape, src, dt=F32, q="sp"):
            t = sb(es_, name, shape, dt)
            k.dma(q, sem_c, lambda e: e.dma_start(out=t.t[:], in_=src), writes=[t])
            return t

        if "A0" in phases:
            with ExitStack() as esA:
                hT = sb(esA, "hT", [128, 8, L], BF16)
                dt_all = sb(esA, "dt_all", [128, NCH, 32], F32)
                a_all = sb(esA, "a_all", [128, NCH, 32], F32)
                e_all = sb(esA, "e_all", [128, NCH, 3, 32], F32)
                gmixT = load_const(esA, "gmixT", [128, 8], gmixT_d)
                gssdT = load_const(esA, "gssdT", [128, 16], gssdT_d)
                conv_wT = load_const(esA, "conv_wT", [128, 32, 4], conv_wT_d)
                conv_bT = load_const(esA, "conv_bT", [128, 32], conv_bT_d)
                dsk = load_const(esA, "dsk", [128, 32], dsk_d)
                pscT = load_const(esA, "pscT", [128, 8], pscT_d)
                invc = load_const(esA, "invc", [128, 4, 16], invc_d)

                with ExitStack() as e0:
                    dtb = load_const(e0, "dtb", [128, 32], dtb_d)
                    alog = load_const(e0, "alog", [128, 32], alog_d)
                    Abc = sb(e0, "Abc", [128, 32], F32)
                    wdt = sb(e0, "wdt", [128, 8, 32], BF16)
                    k.dma("pool", sem_cp, lambda e: e.dma_start(
                        out=wdt.t[:], in_=w_in_d[:, 6144:6176].rearrange("(k p) n -> p k n", p=128)), writes=[wdt])
                    k.op("act", lambda e: e.activation(out=Abc.t[:], in_=alog.t[:], func=AF.Exp), reads=[alog], writes=[Abc])
                    k.op("dve", lambda e: e.tensor_scalar(out=Abc.t[:], in0=Abc.t[:], scalar1=-1.0, scalar2=None, op0=ALU.mult),
                         reads=[Abc], writes=[Abc])
                    xt = [sb(e0, "xt%d" % i, [128, DM], F32) for i in range(2)]
                    xsem = [dsem(), dsem()]
                    junk_l = [sb(e0, "junk%d" % i_, [128, DM], F32) for i_ in range(2)]
                    ss_l = [sb(e0, "ss%d" % i_, [128, 1], F32) for i_ in range(2)]
                    rstd_l = [sb(e0, "rstd%d" % i_, [128, 1], F32) for i_ in range(2)]
                    xn_l = [sb(e0, "xn%d" % i_, [128, DM], BF16) for i_ in range(2)]
                    tpA_l = [ps(e0, "tpA%d" % i_, [128, 8, 128], BF16) for i_ in range(2)]
                    pdt_l = [ps(e0, "pdt%d" % i_, [128, 32], F32) for i_ in range(2)]
                    pcs_l = [ps(e0, "pcs%d" % i_, [128, 3, 32], F32) for i_ in range(2)]
                    dtr_l = [sb(e0, "dtr%d" % i_, [128, 32], F32) for i_ in range(2)]
                    t1_l = [sb(e0, "t1%d" % i_, [128, 32], F32) for i_ in range(2)]
                    t2_l = [sb(e0, "t2%d" % i_, [128, 32], F32) for i_ in range(2)]
                    def a0body(c):
                        xc_ = xt[c % 2]
                        cs = slice(c * 128, (c + 1) * 128)
                        junk = junk_l[c % 2]; ss = ss_l[c % 2]; rstd = rstd_l[c % 2]; xn = xn_l[c % 2]; dtr = dtr_l[c % 2]; t1 = t1_l[c % 2]; t2 = t2_l[c % 2]; tpA = tpA_l[c % 2]; pdt = pdt_l[c % 2]; pcs = pcs_l[c % 2]
                        yield
                        k.dma("sp", xsem[c % 2], lambda e: e.dma_start(out=xc_.t[:], in_=x_d[cs, :]), writes=[xc_])
                        yield
                        k.op("act", lambda e: e.activation(out=junk.t[:], in_=xc_.t[:], func=AF.Square, accum_out=ss.t[:, 0:1]),
                             reads=[xc_], writes=[junk, ss])
                        yield
                        k.op("pool", lambda e: e.tensor_scalar(out=rstd.t[:], in0=ss.t[:], scalar1=1.0 / DM, scalar2=EPS,
                                                               op0=ALU.mult, op1=ALU.add), reads=[ss], writes=[rstd])
                        yield
                        k.op("pool", lambda e: e.tensor_tensor(out=rstd.t[:], in0=rstd.t[:], in1=neghalf_p.t[:], op=ALU.pow),
                             reads=[rstd, neghalf_p], writes=[rstd])
                        yield
                        k.op("act", lambda e: e.activation(out=xn.t[:], in_=xc_.t[:], func=AF.Copy, scale=rstd.t[:, 0:1]),
                             reads=[xc_, rstd], writes=[xn])
                        yield
                        k.ops("pe", [(lambda e, j=j: e.transpose(out=tpA.t[:, j, :], in_=xn.t[:, j * 128:(j + 1) * 128],
                                                                 identity=ident_bf.t[:])) for j in range(8)],
                              reads=[xn, ident_bf], writes=[tpA])
                        yield
                        k.op("dve", lambda e: e.tensor_tensor(out=hT.t[:, :, cs], in0=tpA.t[:],
                                                              in1=gmixT.t[:, :, None].to_broadcast([128, 8, 128]), op=ALU.mult),
                             reads=[tpA, gmixT], writes=[hT])
                        yield
                        k.ops("pe", [(lambda e, j=j: e.matmul(pdt.t[:], lhsT=hT.t[:, j, cs], rhs=wdt.t[:, j, :],
                                                              start=(j == 0), stop=(j == 7))) for j in range(8)],
                              reads=[hT, wdt], writes=[pdt])
                        yield
                        k.op("dve", lambda e: e.tensor_tensor(out=dtr.t[:], in0=pdt.t[:], in1=dtb.t[:], op=ALU.add),
                             reads=[pdt, dtb], writes=[dtr])
                        yield
                        k.op("act", lambda e: e.activation(out=t1.t[:], in_=dtr.t[:], func=AF.Abs),
                             reads=[dtr], writes=[t1])
                        yield
                        k.op("act", lambda e: e.activation(out=t2.t[:], in_=t1.t[:], func=AF.Exp, scale=-1.0), reads=[t1], writes=[t2])
                        yield
                        k.op("act", lambda e: e.activation(out=t1.t[:], in_=t2.t[:], func=AF.Ln, bias=1.0), reads=[t2], writes=[t1])
                        yield
                        k.op("dve", lambda e: e.scalar_tensor_tensor(out=dt_all.t[:, c, :], in0=dtr.t[:], scalar=0.0, in1=t1.t[:],
                                                                     op0=ALU.max, op1=ALU.add),
                             reads=[dtr, t1], writes=[dt_all])
                        yield
                        k.op("dve", lambda e: e.tensor_tensor(out=a_all.t[:, c, :], in0=dt_all.t[:, c, :], in1=Abc.t[:], op=ALU.mult),
                             reads=[dt_all, Abc], writes=[a_all])
                        yield
                        k.ops("pe", [
                            lambda e: e.matmul(pcs.t[:, 0, :], lhsT=Mle.t[:], rhs=a_all.t[:, c, :], start=True, stop=True),
                            lambda e: e.matmul(pcs.t[:, 1, :], lhsT=Mgt.t[:], rhs=a_all.t[:, c, :], start=True, stop=True),
                            lambda e: e.matmul(pcs.t[:, 2, :], lhsT=ones_f.t[:], rhs=a_all.t[:, c, :], start=True, stop=True),
                        ], reads=[a_all, Mle, Mgt, ones_f], writes=[pcs])
                        yield
                        k.op("act", lambda e: e.activation(out=e_all.t[:, c, :, :], in_=pcs.t[:], func=AF.Exp),
                             reads=[pcs], writes=[e_all])
                        yield
                    run_chains([a0body(c) for c in range(NCH)], lag=6)
                    k.barrier()
                dump("hT", hT); dump("dt_all", dt_all); dump("a_all", a_all); dump("e_all", e_all)
                if "A1" in phases:
                    with ExitStack() as e1:
                        wg = [sb(e1, "wg%d" % i, [128, 8, 768], BF16) for i in range(2)]
                        wsem = [dsem(), dsem()]
                        dg = [sb(e1, "dg%d" % i, [128, 4, 4, 128], BF16) for i in range(2)]
                        ust = [sb(e1, "ust%d" % i, [128, 4, 515], BF16) for i in range(2)]
                        xc = [sb(e1, "xc%d" % i, [128, 4, 512], BF16) for i in range(2)]
                        state = sb(e1, "state", [128, 256], F32)
                        state_bf = [sb(e1, "state_bf%d" % i, [128, 256], BF16) for i in range(3)]
                        zstate = sb(e1, "zstate", [128, 256], BF16)
                        ynT = [sb(e1, "ynT%d" % i, [128, 2, 512], BF16) for i in range(2)]
                        ysem = [dsem(), dsem()]
                        sz = [sb(e1, "sz%d" % i, [128, 256], BF16) for i in range(3)]
                        xbtm = [sb(e1, "xbtm%d" % i, [128, 384], BF16) for i in range(3)]
                        xd = [sb(e1, "xd%d" % i, [128, 4, 64], BF16) for i in range(3)]
                        y4 = [sb(e1, "y4_%d" % i, [128, 256], F32) for i in range(3)]
                        xde = [sb(e1, "xde%d" % i, [128, 4, 64], BF16) for i in range(2)]
                        cbm = [sb(e1, "cbm%d" % i, [128, 128], F32) for i in range(2)]
                        lh = [sb(e1, "lh%d" % i, [128, 4, 128], F32) for i in range(2)]
                        Ex = [sb(e1, "Ex%d" % i, [128, 4, 128], F32) for i in range(2)]
                        MT = [sb(e1, "MT%d" % i, [128, 4, 128], BF16) for i in range(2)]
                        y1 = [sb(e1, "y1_%d" % i, [128, 4, 64], F32) for i in range(2)]
                        tD = [sb(e1, "tD%d" % i, [128, 4, 64], F32) for i in range(2)]
                        yb = [sb(e1, "yb%d" % i, [128, 256], BF16) for i in range(2)]
                        junk2 = sb(e1, "junk2", [128, 256], F32)
                        ss2 = [sb(e1, "ss2_%d" % i, [128, 1], F32) for i in range(2)]
                        rs2 = [sb(e1, "rs2_%d" % i, [128, 1], F32) for i in range(2)]
                        p_u = [ps(e1, "p_u%d" % i, [128, 512], F32) for i in range(2)]
                        bk1 = psbank(e1, "bk1", F32)
                        p_z = T(bk1[:, 0:256])
                        p_cb = T(bk1[:, 256:384])
                        p_cb.r = p_z.r
                        p_s = ps(e1, "p_s", [128, 256], F32)
                        p_seg1 = ps(e1, "p_seg", [128, 4, 128], F32)
                        p_seg = [p_seg1, p_seg1]
                        bk4 = psbank(e1, "bk4", F32)
                        p_y = T(bk4[:, 0:256])
                        p_yo = T(bk4[:, 256:512])
                        p_yo.r = p_y.r
                        bk6 = psbank(e1, "bk6", BF16)
                        p_tpx = T(bk6[:, 0:384])
                        bk7 = psbank(e1, "bk7", BF16)
                        p_tpy = T(bk7[:, 0:256])
                        k.op("pool", lambda e: e.memset(zstate.t[:], 0.0), writes=[zstate])
                        thz = [sb(e1, "thz%d" % i, [128, 256], F32) for i in range(2)]
                        thc = [sb(e1, "thc%d" % i, [128, 512], F32) for i in range(2)]
                        neghalf = sb(e1, "neghalf", [128, 1], F32)
                        k.op("pool", lambda e: e.memset(neghalf.t[:], -0.5), writes=[neghalf])
                        ones_row = sb(e1, "ones_row", [1, 512], BF16)
                        k.op("pool", lambda e: e.memset(ones_row.t[:], 1.0), writes=[ones_row])
                        cb_row = sb(e1, "cb_row", [1, 4096], F32)
                        k.dma("sp", sem_c, lambda e: e.dma_start(out=cb_row.t[:], in_=conv_brow_d), writes=[cb_row])
                        hb_row = sb(e1, "hb_row", [1, 4096], BF16)
                        k.op("dve", lambda e: e.tensor_scalar(out=hb_row.t[:], in0=cb_row.t[:], scalar1=0.5, scalar2=None, op0=ALU.mult),
                             reads=[cb_row], writes=[hb_row])

                        def group_setup(g):
                            w = wg[g % 2]
                            srcs = [(0, 2048 + g * 256, 256), (256, 4096 + g * 128, 128),
                                    (384, 5120 + g * 128, 128), (512, g * 256, 256)]
                            k.dma("pool", wsem[g % 2],
                                  [(lambda e, o=o, s=s, n=n: e.dma_start(
                                      out=w.t[:, :, o:o + n],
                                      in_=w_in_d[:, s:s + n].rearrange("(k p) n -> p k n", p=128))) for (o, s, n) in srcs],
                                  writes=[w])
                            d_ = dg[g % 2]
                            chunks = [2 * g, 2 * g + 1, 16 + g, 24 + g]
                            k.ops("pool", [(lambda e, cc=cc, kk=kk: e.tensor_scalar(
                                out=d_.t[:, cc, kk, :], in0=ident_bf.t[:], scalar1=conv_wT.t[:, chunks[cc], kk:kk + 1],
                                scalar2=0.5, op0=ALU.mult, op1=ALU.mult)) for cc in range(4) for kk in range(4)],
                                reads=[ident_bf, conv_wT], writes=[d_])
                            k.op("pool", lambda e: e.tensor_scalar(out=w.t[:, :, 512:768], in0=w.t[:, :, 512:768], scalar1=0.5,
                                                                   scalar2=0.0, op0=ALU.mult, op1=ALU.add), reads=[w], writes=[w])

                        NT = 8 * NCH

                        def dec(n):
                            g = n // NCH
                            c = n % NCH
                            return g, c, c // 4, c % 4

                        def S0a(si):
                            g, sc = si // NSC, si % NSC
                            w = wg[g % 2]
                            us = ust[si % 2]
                            ts = sc * 512
                            if sc == 0:
                                k.op("pool", lambda e: e.memset(us.t[:, :, 0:3], 0.0), writes=[us])
                            for cc in range(4):
                                pu = p_u[cc % 2]
                                k.ops("pe", [(lambda e, j=j: e.matmul(pu.t[:], lhsT=w.t[:, j, cc * 128:(cc + 1) * 128],
                                                                      rhs=hT.t[:, j, ts:ts + 512], start=(j == 0), stop=(j == 7)))
                                             for j in range(8)], reads=[w, hT], writes=[pu])
                                k.op("act", lambda e: e.copy(out=us.t[:, cc, 3:515], in_=pu.t[:]), reads=[pu], writes=[us])
                            if sc + 1 < NSC:
                                un = ust[(si + 1) % 2]
                                k.op("pool", lambda e: e.tensor_copy(out=un.t[:, :, 0:3], in_=us.t[:, :, 512:515]),
                                     reads=[us], writes=[un])

                        def S0b(si):
                            g, sc = si // NSC, si % NSC
                            d_ = dg[g % 2]
                            us = ust[si % 2]
                            xo = xc[si % 2]
                            chunks = [2 * g, 2 * g + 1, 16 + g, 24 + g]
                            for cc in range(4):
                                pu = p_u[cc % 2]
                                ch = chunks[cc]
                                k.ops("pe", [(lambda e, kk=kk: e.matmul(pu.t[:], lhsT=d_.t[:, cc, kk, :],
                                                                        rhs=us.t[:, cc, kk:kk + 512], start=(kk == 0), stop=False))
                                             for kk in range(4)] +
                                      [lambda e: e.matmul(pu.t[:], lhsT=hb_row.t[0:1, ch * 128:(ch + 1) * 128],
                                                          rhs=ones_row.t[0:1, :], start=False, stop=True)],
                                      reads=[d_, us, hb_row, ones_row], writes=[pu])
                                tc_ = thc[cc % 2]
                                k.op("act", lambda e: e.activation(out=tc_.t[:], in_=pu.t[:], func=AF.Tanh),
                                     reads=[pu], writes=[tc_])
                                k.op("dve", lambda e: e.scalar_tensor_tensor(out=xo.t[:, cc, :], in0=tc_.t[:], scalar=1.0, in1=pu.t[:],
                                                                             op0=ALU.add, op1=ALU.mult),
                                     reads=[tc_, pu], writes=[xo])

                        def ctx(n):
                            g, c, sc, q = dec(n)
                            si = g * NSC + sc
                            return dict(g=g, c=c, sc=sc, q=q, si=si, w=wg[g % 2], xo=xc[si % 2],
                                        qs=slice(q * 128, (q + 1) * 128), cs=slice(c * 128, (c + 1) * 128),
                                        g4=slice(g * 4, g * 4 + 4), b3=n % 3, b2=n % 2)

                        def A_pe(n):
                            x_ = ctx(n); w = x_["w"]; xo = x_["xo"]; qs = x_["qs"]; cs = x_["cs"]
                            k.ops("pe", [(lambda e, j=j: e.matmul(p_z.t[:], lhsT=hT.t[:, j, cs], rhs=w.t[:, j, 512:768],
                                                                  start=(j == 0), stop=(j == 7))) for j in range(8)],
                                  reads=[hT, w], writes=[p_z])
                            k.ops("pe", [(lambda e, cc=cc: e.transpose(out=p_tpx.t[:, cc * 128:(cc + 1) * 128],
                                                                       in_=xo.t[:, cc, qs], identity=ident_bf.t[:]))
                                         for cc in range(3)], reads=[xo, ident_bf], writes=[p_tpx])
                            k.op("pe", lambda e: e.matmul(p_cb.t[:], lhsT=xo.t[:, 2, qs], rhs=xo.t[:, 3, qs],
                                                          start=True, stop=True), reads=[xo], writes=[p_cb])

                        def A_act(n):
                            x_ = ctx(n); b3 = x_["b3"]; b2 = x_["b2"]; c = x_["c"]; g = x_["g"]
                            k.op("act", lambda e: e.activation(out=thz[b2].t[:], in_=p_z.t[:], func=AF.Tanh),
                                 reads=[p_z], writes=[thz[b2]])
                            k.op("act", lambda e: e.copy(out=xbtm[b3].t[:], in_=p_tpx.t[:]), reads=[p_tpx], writes=[xbtm[b3]])
                            k.ops("act", [(lambda e, r=r: e.activation(out=lh[b2].t[:, r, :], in_=Mgt.t[:], func=AF.Copy,
                                                                       scale=a_all.t[:, c, g * 4 + r:g * 4 + r + 1]))
                                          for r in range(4)], reads=[Mgt, a_all], writes=[lh[b2]])

                        def A_dve(n):
                            x_ = ctx(n); b3 = x_["b3"]; b2 = x_["b2"]; c = x_["c"]; g4 = x_["g4"]
                            k.op("dve", lambda e: e.scalar_tensor_tensor(out=sz[b3].t[:], in0=thz[b2].t[:], scalar=1.0, in1=p_z.t[:],
                                                                         op0=ALU.add, op1=ALU.mult),
                                 reads=[thz[b2], p_z], writes=[sz[b3]])
                            k.op("dve", lambda e: e.tensor_tensor(
                                out=xd[b3].t[:], in0=xbtm[b3].t[:, 0:256].rearrange("p (r d) -> p r d", r=4),
                                in1=dt_all.t[:, c, g4, None].to_broadcast([128, 4, 64]), op=ALU.mult),
                                reads=[xbtm[b3], dt_all], writes=[xd[b3]])
                            k.op("dve", lambda e: e.tensor_tensor(out=cbm[b2].t[:], in0=p_cb.t[:], in1=Mle.t[:], op=ALU.mult),
                                 reads=[p_cb, Mle], writes=[cbm[b2]])

                        def A_pool(n):
                            x_ = ctx(n); b3 = x_["b3"]; b2 = x_["b2"]; c = x_["c"]; g4 = x_["g4"]
                            k.op("pool", lambda e: e.tensor_tensor(
                                out=xde[b2].t[:], in0=xd[b3].t[:],
                                in1=e_all.t[:, c, 1, g4, None].to_broadcast([128, 4, 64]), op=ALU.mult),
                                reads=[xd[b3], e_all], writes=[xde[b2]])

                        def B_pe(n):
                            x_ = ctx(n); b3 = x_["b3"]; b2 = x_["b2"]
                            k.ops("pe", [(lambda e, r=r: e.matmul(p_seg[b2].t[:, r, :], lhsT=lh[b2].t[:, r, :], rhs=Mle.t[:],
                                                                  start=True, stop=True)) for r in range(4)],
                                  reads=[lh[b2], Mle], writes=[p_seg[b2]])
                            k.op("pe", lambda e: e.matmul(p_s.t[:], lhsT=xbtm[b3].t[:, 256:384],
                                                          rhs=xde[b2].t[:].rearrange("p r d -> p (r d)"), start=True, stop=True),
                                 reads=[xbtm[b3], xde[b2]], writes=[p_s])

                        def B_act(n):
                            x_ = ctx(n); b2 = x_["b2"]
                            k.op("act", lambda e: e.activation(out=Ex[b2].t[:], in_=p_seg[b2].t[:], func=AF.Exp),
                                 reads=[p_seg[b2]], writes=[Ex[b2]])

                        def B_dve(n):
                            x_ = ctx(n); b2 = x_["b2"]; c = x_["c"]; g4 = x_["g4"]
                            k.op("dve", lambda e: e.tensor_tensor(
                                out=MT[b2].t[:], in0=Ex[b2].t[:],
                                in1=cbm[b2].t[:, None, :].to_broadcast([128, 4, 128]), op=ALU.mult),
                                reads=[Ex[b2], cbm[b2]], writes=[MT[b2]])
                            if c == 0:
                                k.op("dve", lambda e: e.tensor_copy(out=state.t[:], in_=p_s.t[:]), reads=[p_s], writes=[state])
                            else:
                                k.op("dve", lambda e: e.tensor_tensor(
                                    out=state.t[:].rearrange("p (r d) -> p r d", r=4),
                                    in0=state.t[:].rearrange("p (r d) -> p r d", r=4),
                                    in1=e_all.t[:, c, 2, g4, None].to_broadcast([128, 4, 64]), op=ALU.mult),
                                    reads=[state, e_all], writes=[state])
                                k.op("dve", lambda e: e.tensor_tensor(out=state.t[:], in0=state.t[:], in1=p_s.t[:], op=ALU.add),
                                     reads=[state, p_s], writes=[state])

                        def B_dve2(n):
                            x_ = ctx(n); b3 = x_["b3"]; b2 = x_["b2"]; g4 = x_["g4"]
                            k.op("dve", lambda e: e.tensor_tensor(
                                out=tD[b2].t[:], in0=xbtm[b3].t[:, 0:256].rearrange("p (r d) -> p r d", r=4),
                                in1=dsk.t[:, g4, None].to_broadcast([128, 4, 64]), op=ALU.mult),
                                reads=[xbtm[b3], dsk], writes=[tD[b2]])

                        def C_act(n):
                            x_ = ctx(n); b3 = x_["b3"]
                            k.op("act", lambda e: e.copy(out=state_bf[b3].t[:], in_=state.t[:]),
                                 reads=[state], writes=[state_bf[b3]])

                        def C_pe(n):
                            x_ = ctx(n); b3 = x_["b3"]; b2 = x_["b2"]; xo = x_["xo"]; qs = x_["qs"]; c = x_["c"]
                            k.ops("pe", [(lambda e, r=r: e.matmul(p_y.t[:, r * 64:(r + 1) * 64], lhsT=MT[b2].t[:, r, :],
                                                                  rhs=xd[b3].t[:, r, :], start=True, stop=True))
                                         for r in range(4)], reads=[MT[b2], xd[b3]], writes=[p_y])
                            st_prev = zstate if c == 0 else state_bf[(n - 1) % 3]
                            k.op("pe", lambda e: e.matmul(p_yo.t[:], lhsT=xo.t[:, 3, qs], rhs=st_prev.t[:],
                                                          start=True, stop=True), reads=[xo, st_prev], writes=[p_yo])

                        def C_dve(n):
                            x_ = ctx(n); b3 = x_["b3"]; b2 = x_["b2"]; c = x_["c"]; g4 = x_["g4"]
                            k.op("dve", lambda e: e.tensor_tensor(
                                out=y1[b2].t[:], in0=p_yo.t[:].rearrange("p (r d) -> p r d", r=4),
                                in1=e_all.t[:, c, 0, g4, None].to_broadcast([128, 4, 64]), op=ALU.mult),
                                reads=[p_yo, e_all], writes=[y1[b2]])
                            k.op("dve", lambda e: e.tensor_tensor(
                                out=y1[b2].t[:], in0=y1[b2].t[:],
                                in1=p_y.t[:].rearrange("p (r d) -> p r d", r=4), op=ALU.add),
                                reads=[y1[b2], p_y], writes=[y1[b2]])
                            k.op("dve", lambda e: e.tensor_tensor(out=y1[b2].t[:], in0=y1[b2].t[:], in1=tD[b2].t[:], op=ALU.add),
                                 reads=[y1[b2], tD[b2]], writes=[y1[b2]])
                            k.op("dve", lambda e: e.tensor_tensor(
                                out=y4[b3].t[:], in0=y1[b2].t[:].rearrange("p r d -> p (r d)"), in1=sz[b3].t[:], op=ALU.mult),
                                reads=[y1[b2], sz[b3]], writes=[y4[b3]])

                        def D_act(n):
                            x_ = ctx(n); b3 = x_["b3"]; b2 = x_["b2"]
                            k.op("act", lambda e: e.activation(out=junk2.t[:], in_=y4[b3].t[:], func=AF.Square,
                                                               accum_out=ss2[b2].t[:, 0:1]),
                                 reads=[y4[b3]], writes=[junk2, ss2[b2]])

                        def D_pool(n):
                            x_ = ctx(n); b2 = x_["b2"]
                            k.op("pool", lambda e: e.tensor_scalar(out=rs2[b2].t[:], in0=ss2[b2].t[:], scalar1=1.0 / 256, scalar2=EPS,
                                                                   op0=ALU.mult, op1=ALU.add), reads=[ss2[b2]], writes=[rs2[b2]])
                            k.op("pool", lambda e: e.tensor_tensor(out=rs2[b2].t[:], in0=rs2[b2].t[:], in1=neghalf.t[:], op=ALU.pow),
                                 reads=[rs2[b2], neghalf], writes=[rs2[b2]])

                        def E_dve(n):
                            x_ = ctx(n); b3 = x_["b3"]; b2 = x_["b2"]
                            k.op("dve", lambda e: e.tensor_scalar(out=yb[b2].t[:], in0=y4[b3].t[:], scalar1=rs2[b2].t[:, 0:1],
                                                                  scalar2=None, op0=ALU.mult),
                                 reads=[y4[b3], rs2[b2]], writes=[yb[b2]])

                        def F_pe(n):
                            x_ = ctx(n); b2 = x_["b2"]
                            k.ops("pe", [(lambda e, h=h: e.transpose(out=p_tpy.t[:, h * 128:(h + 1) * 128],
                                                                     in_=yb[b2].t[:, h * 128:(h + 1) * 128], identity=ident_bf.t[:]))
                                         for h in range(2)], reads=[yb[b2], ident_bf], writes=[p_tpy])

                        def F_act(n):
                            x_ = ctx(n); g = x_["g"]; qs = x_["qs"]; si = x_["si"]; q = x_["q"]; sc = x_["sc"]
                            yo = ynT[si % 2]
                            for h in range(2):
                                k.op("act", lambda e: e.activation(out=yo.t[:, h, qs], in_=p_tpy.t[:, h * 128:(h + 1) * 128], func=AF.Copy,
                                                                   scale=gssdT.t[:, 2 * g + h:2 * g + h + 1]),
                                     reads=[p_tpy, gssdT], writes=[yo])
                            if q == 3:
                                ts = sc * 512
                                k.dma("sp", ysem[si % 2], lambda e: e.dma_start(
                                    out=ymT_d[2 * g:2 * g + 2, :, ts:ts + 512].rearrange("f p t -> p f t"), in_=yo.t[:]),
                                    reads=[yo])

                        def ok(n):
                            return 0 <= n < NT

                        import os as _os
                        if _os.environ.get("A1_NOSKEW"):
                            for n in range(NT):
                                g, c, sc, q = dec(n)
                                if c == 0:
                                    group_setup(g)
                                if q == 0:
                                    S0a(g * NSC + sc)
                                    S0b(g * NSC + sc)
                                for fn in (A_pe, A_act, A_dve, A_pool, B_pe, B_act, B_dve, B_dve2, C_pe, C_act, C_dve, D_act, D_pool, E_dve, F_pe, F_act):
                                    fn(n)
                        else:
                            LAGS = dict(A=0, B=1, C=2, D=3, E=4, F=5)
                            group_setup(0)
                            S0a(0)
                            S0b(0)
                            S0a(1)
                            for t in range(NT + 5):
                                for fn, lag in ((F_pe, 5), (C_pe, 2), (B_pe, 1), (A_pe, 0),
                                                (F_act, 5), (C_act, 2), (B_act, 1), (A_act, 0), (D_act, 3),
                                                (E_dve, 4), (B_dve, 1), (B_dve2, 1), (C_dve, 2), (A_dve, 0),
                                                (D_pool, 3), (A_pool, 0)):
                                    if ok(t - lag):
                                        fn(t - lag)
                                if t % 4 == 1 and t // 4 + 1 < 8 * NSC:
                                    S0b(t // 4 + 1)
                                if t % 4 == 2 and t // 4 + 2 < 8 * NSC:
                                    S0a(t // 4 + 2)
                                if t % NCH == 4 and t // NCH + 1 < 8:
                                    group_setup(t // NCH + 1)
                k.barrier()
                if "A1p" in phases:
                    with ExitStack() as e2:
                        wp = sb(e2, "wp", [128, 8, 1024], BF16)
                        k.dma("pool", sem_cp, lambda e: e.dma_start(
                            out=wp.t[:], in_=w_in_d[:, 6176:7200].rearrange("(k p) n -> p k n", p=128)), writes=[wp])
                        pw = sb(e2, "pw", [128, 4, 2, 256], BF16)
                        k.dma("pool", sem_cp, lambda e: e.dma_start(
                            out=pw.t[:], in_=pool_w_d.rearrange("g (k p) n -> p g k n", p=128)), writes=[pw])
                        pst = [sb(e2, "pst%d" % i, [128, 8, 527], F32) for i in range(2)]
                        sA = sb(e2, "sA", [128, 2, 527], F32)
                        sB = sb(e2, "sB", [128, 2, 527], F32)
                        ypl = [sb(e2, "ypl%d" % i, [128, 2, 512], BF16) for i in range(2)]
                        tmp16 = sb(e2, "tmp16", [128, 2, 16], F32)
                        ypT = [sb(e2, "ypT%d" % i, [128, 2, 512], BF16) for i in range(2)]
                        psem_ = [dsem(), dsem()]
                        p_u2 = [ps(e2, "p_u2_%d" % i, [128, 512], F32) for i in range(2)]
                        p_p = [ps(e2, "p_p%d" % i, [128, 512], F32) for i in range(2)]
                        k.op("pool", lambda e: e.memset(pst[0].t[:, :, 0:15], 0.0), writes=[pst[0]])
                        it = 0
                        for sc in range(NSC):
                            ts = sc * 512
                            cur = pst[sc % 2]
                            nxt = pst[(sc + 1) % 2]
                            for pc in range(8):
                                pu = p_u2[pc % 2]
                                k.ops("pe", [(lambda e, j=j: e.matmul(pu.t[:], lhsT=wp.t[:, j, pc * 128:(pc + 1) * 128],
                                                                      rhs=hT.t[:, j, ts:ts + 512], start=(j == 0), stop=(j == 7)))
                                             for j in range(8)], reads=[wp, hT], writes=[pu])
                                k.op("act", lambda e: e.copy(out=cur.t[:, pc, 15:527], in_=pu.t[:]), reads=[pu], writes=[cur])
                            if sc + 1 < NSC:
                                k.op("pool", lambda e: e.tensor_copy(out=nxt.t[:, :, 0:15], in_=cur.t[:, :, 512:527]),
                                     reads=[cur], writes=[nxt])
                            for pg in range(4):
                                u = cur.t[:, 2 * pg:2 * pg + 2, :]
                                nlev = pg + 1
                                src = u
                                bufs = [sA, sB]
                                eng = "dve"
                                for lv in range(nlev):
                                    sh = 1 << lv
                                    lo = (1 << (lv + 1)) - 1
                                    dst = bufs[lv % 2]
                                    src_t = cur if lv == 0 else bufs[(lv - 1) % 2]
                                    s_ap = src
                                    k.op(eng, lambda e, dst=dst, s_ap=s_ap, lo=lo, sh=sh: e.tensor_tensor(
                                        out=dst.t[:, :, lo:527], in0=s_ap[:, :, lo:527], in1=s_ap[:, :, lo - sh:527 - sh], op=ALU.add),
                                        reads=[src_t], writes=[dst])
                                    src = dst.t[:, :, :]
                                fin = bufs[(nlev - 1) % 2]
                                wv = 1 << nlev
                                yp = ypl[it % 2]
                                k.op(eng, lambda e: e.scalar_tensor_tensor(
                                    out=yp.t[:], in0=fin.t[:, :, 15:527], scalar=1.0 / wv, in1=u[:, :, 15:527],
                                    op0=ALU.mult, op1=ALU.subtract), reads=[fin, cur], writes=[yp])
                                if sc == 0:
                                    k.op(eng, lambda e: e.tensor_tensor(
                                        out=tmp16.t[:], in0=fin.t[:, :, 15:31],
                                        in1=invc.t[:, pg, None, :].to_broadcast([128, 2, 16]), op=ALU.mult),
                                        reads=[fin, invc], writes=[tmp16])
                                    k.op(eng, lambda e: e.tensor_tensor(out=yp.t[:, :, 0:16], in0=tmp16.t[:], in1=u[:, :, 15:31],
                                                                        op=ALU.subtract), reads=[tmp16, cur], writes=[yp])
                                yT_ = ypT[it % 2]
                                for dc in range(2):
                                    pp = p_p[dc]
                                    k.ops("pe", [(lambda e, kc=kc: e.matmul(pp.t[:], lhsT=pw.t[:, pg, kc, dc * 128:(dc + 1) * 128],
                                                                            rhs=yp.t[:, kc, :], start=(kc == 0), stop=(kc == 1)))
                                                 for kc in range(2)], reads=[pw, yp], writes=[pp])
                                    k.op("act", lambda e: e.activation(out=yT_.t[:, dc, :], in_=pp.t[:], func=AF.Copy,
                                                                       scale=pscT.t[:, 2 * pg + dc:2 * pg + dc + 1]),
                                         reads=[pp, pscT], writes=[yT_])
                                k.dma("sp", psem_[it % 2], lambda e: e.dma_start(
                                    out=ymT_d[16 + 2 * pg:16 + 2 * pg + 2, :, ts:ts + 512].rearrange("f p t -> p f t"), in_=yT_.t[:]),
                                    reads=[yT_])
                                it += 1

        k.barrier()
        if "A2" in phases:
            with ExitStack() as e3:
                wout = sb(e3, "wout", [128, 24, DM], BF16)
                k.dma("pool", sem_cp, [(lambda e, i=i: e.dma_start(
                    out=wout.t[:, 6 * i:6 * i + 6, :],
                    in_=w_out_d[768 * i:768 * (i + 1), :].rearrange("(k p) n -> p k n", p=128))) for i in range(4)],
                    writes=[wout])
                gffn_bc = load_const(e3, "gffn_bc", [128, DM], gffn_bc_d)
                rw = load_const(e3, "rw", [128, 8, NE], rw_d.rearrange("(k p) n -> p k n", p=128))
                rb = load_const(e3, "rb", [128, NE], rb_d)
                ebase = load_const(e3, "ebase", [128, NE], ebase_d)
                s4096 = sb(e3, "s4096", [128, 514], I32)
                k.op("pool", lambda e: e.memset(s4096.t[:], L), writes=[s4096])
                ev_init = [
                    k.dma("sp", sem_c, lambda e: e.dma_start(out=slot_d.rearrange("(p f) o -> p (f o)", p=128), in_=s4096.t[:]),
                          reads=[s4096]),
                    k.dma("sp", sem_c, lambda e: e.dma_start(out=h2_d[L:L + 1, :], in_=zrow_bf.t[:]), reads=[zrow_bf]),
                    k.dma("sp", sem_c, lambda e: e.dma_start(out=G_d[L:L + 1, :], in_=zrow.t[:, 0:NE]), reads=[zrow]),
                    k.dma("sp", sem_c, lambda e: e.dma_start(out=Y_d[0:1, :], in_=zrow.t[:]), reads=[zrow]),
                ]
                ym = [sb(e3, "ym%d" % i, [128, 24, 512], BF16) for i in range(2)]
                ymsem = [dsem(), dsem()]
                xt2 = [sb(e3, "xt2_%d" % i, [128, DM], F32) for i in range(2)]
                xsem2 = [dsem(), dsem()]
                x1 = [sb(e3, "x1_%d" % i, [128, DM], F32) for i in range(2)]
                x1sem = [dsem(), dsem()]
                junk3_l = [sb(e3, "junk3%d" % i_, [128, DM], F32) for i_ in range(2)]
                ss3_l = [sb(e3, "ss3%d" % i_, [128, 1], F32) for i_ in range(2)]
                rs3_l = [sb(e3, "rs3%d" % i_, [128, 1], F32) for i_ in range(2)]
                h2f_l = [sb(e3, "h2f%d" % i_, [128, DM], F32) for i_ in range(2)]
                h2b = [sb(e3, "h2b%d" % i, [128, DM], BF16) for i in range(2)]
                h2sem = [dsem(), dsem()]
                h2T_l = [sb(e3, "h2T%d" % i_, [128, 8, 128], F32) for i_ in range(2)]
                lg_l = [sb(e3, "lg%d" % i_, [128, NE], F32) for i_ in range(2)]
                m8_l = [sb(e3, "m8%d" % i_, [128, 8], F32) for i_ in range(2)]
                nv1_l = [sb(e3, "nv1%d" % i_, [128, 1], F32) for i_ in range(2)]
                mask_l = [sb(e3, "mask%d" % i_, [128, NE], F32) for i_ in range(2)]
                ex_l = [sb(e3, "ex%d" % i_, [128, NE], F32) for i_ in range(2)]
                sm_l = [sb(e3, "sm%d" % i_, [128, 1], F32) for i_ in range(2)]
                Gt = [sb(e3, "Gt%d" % i, [128, NE], F32) for i in range(2)]
                gsem = [dsem(), dsem()]
                cnt = sb(e3, "cnt", [128, NE], F32)
                rank_l = [sb(e3, "rank%d" % i_, [128, NE], F32) for i_ in range(2)]
                vld_l = [sb(e3, "vld%d" % i_, [128, NE], F32) for i_ in range(2)]
                val_l = [sb(e3, "val%d" % i_, [128, NE], F32) for i_ in range(2)]
                v8_l = [sb(e3, "v8%d" % i_, [128, 8], F32) for i_ in range(2)]
                scsem = dsem()
                p_o_l = [ps(e3, "p_o%d" % i_, [128, DM], F32) for i_ in range(2)]
                p_tf = ps(e3, "p_tf", [128, 8, 128], F32)
                p_l = ps(e3, "p_l", [128, NE], F32)
                p_r = ps(e3, "p_r", [128, 2, NE], F32)
                k.op("pool", lambda e: e.memset(cnt.t[:], 0.0), writes=[cnt])
                scat_evs = []
                def ym_load(sc):
                    ts = sc * 512
                    ymc = ym[sc % 2]
                    k.dma("sp", ymsem[sc % 2], [(lambda e, i=i: e.dma_start(
                        out=ymc.t[:, 6 * i:6 * i + 6, :],
                        in_=ymT_d[6 * i:6 * i + 6, :, ts:ts + 512].rearrange("f p t -> p f t"))) for i in range(4)],
                        writes=[ymc])

                def a2body(sc, q):
                    ts = sc * 512
                    ymc = ym[sc % 2]
                    if q == 1 and sc + 1 < NSC:
                        ym_load(sc + 1)
                    c = sc * 4 + q
                    b = c % 2
                    qs = slice(q * 128, (q + 1) * 128)
                    cs = slice(c * 128, (c + 1) * 128)
                    p_o = p_o_l[b]
                    junk3 = junk3_l[b]; ss3 = ss3_l[b]; rs3 = rs3_l[b]; h2f = h2f_l[b]; h2T = h2T_l[b]; lg = lg_l[b]; m8 = m8_l[b]; nv1 = nv1_l[b]; mask = mask_l[b]; ex = ex_l[b]; sm = sm_l[b]; rank = rank_l[b]; vld = vld_l[b]; val = val_l[b]; v8 = v8_l[b]
                    yield
                    k.dma("sp", xsem2[b], lambda e: e.dma_start(out=xt2[b].t[:], in_=x_d[cs, :]), writes=[xt2[b]])
                    yield
                    k.ops("pe", [(lambda e, fc=fc, h=h: e.matmul(p_o.t[:, h * 512:(h + 1) * 512], lhsT=ymc.t[:, fc, qs],
                                                                 rhs=wout.t[:, fc, h * 512:(h + 1) * 512],
                                                                 start=(fc == 0), stop=(fc == 23)))
                                 for h in range(2) for fc in range(24)], reads=[ymc, wout], writes=[p_o])
                    yield
                    k.op("dve", lambda e: e.tensor_tensor(out=x1[b].t[:], in0=p_o.t[:], in1=xt2[b].t[:], op=ALU.add),
                         reads=[p_o, xt2[b]], writes=[x1[b]])
                    yield
                    k.dma("sp", x1sem[b], lambda e: e.dma_start(out=x1_d[cs, :], in_=x1[b].t[:]), reads=[x1[b]])
                    yield
                    k.op("act", lambda e: e.activation(out=junk3.t[:], in_=x1[b].t[:], func=AF.Square, accum_out=ss3.t[:, 0:1]),
                         reads=[x1[b]], writes=[junk3, ss3])
                    yield
                    k.op("pool", lambda e: e.tensor_scalar(out=rs3.t[:], in0=ss3.t[:], scalar1=1.0 / DM, scalar2=EPS,
                                                           op0=ALU.mult, op1=ALU.add), reads=[ss3], writes=[rs3])
                    yield
                    k.op("pool", lambda e: e.tensor_tensor(out=rs3.t[:], in0=rs3.t[:], in1=neghalf_p.t[:], op=ALU.pow),
                         reads=[rs3, neghalf_p], writes=[rs3])
                    yield
                    k.op("dve", lambda e: e.scalar_tensor_tensor(out=h2f.t[:], in0=x1[b].t[:], scalar=rs3.t[:, 0:1],
                                                                 in1=gffn_bc.t[:], op0=ALU.mult, op1=ALU.mult),
                         reads=[x1[b], rs3, gffn_bc], writes=[h2f])
                    yield
                    k.op("act", lambda e: e.copy(out=h2b[b].t[:], in_=h2f.t[:]), reads=[h2f], writes=[h2b[b]])
                    yield
                    k.dma("sp", h2sem[b], lambda e: e.dma_start(out=h2_d[cs, :], in_=h2b[b].t[:]), reads=[h2b[b]])
                    yield
                    k.ops("pe", [(lambda e, j=j: e.transpose(out=p_tf.t[:, j, :], in_=h2f.t[:, j * 128:(j + 1) * 128],
                                                             identity=ident_f.t[:])) for j in range(8)],
                          reads=[h2f, ident_f], writes=[p_tf])
                    yield
                    k.op("act", lambda e: e.copy(out=h2T.t[:], in_=p_tf.t[:]), reads=[p_tf], writes=[h2T])
                    yield
                    k.ops("pe", [(lambda e, j=j: e.matmul(p_l.t[:], lhsT=h2T.t[:, j, :], rhs=rw.t[:, j, :],
                                                          start=(j == 0), stop=(j == 7))) for j in range(8)],
                          reads=[h2T, rw], writes=[p_l])
                    yield
                    k.op("dve", lambda e: e.tensor_tensor(out=lg.t[:], in0=p_l.t[:], in1=rb.t[:], op=ALU.add),
                         reads=[p_l, rb], writes=[lg])
                    yield
                    k.op("dve", lambda e: e.max(out=m8.t[:], in_=lg.t[:]), reads=[lg], writes=[m8])
                    yield
                    k.op("dve", lambda e: e.tensor_scalar(out=mask.t[:], in0=lg.t[:], scalar1=m8.t[:, 3:4], scalar2=None,
                                                          op0=ALU.is_ge), reads=[lg, m8], writes=[mask])
                    yield
                    k.op("dve", lambda e: e.tensor_scalar(out=nv1.t[:], in0=m8.t[:, 0:1], scalar1=-1.0, scalar2=None,
                                                          op0=ALU.mult), reads=[m8], writes=[nv1])
                    yield
                    k.op("act", lambda e: e.activation(out=ex.t[:], in_=lg.t[:], func=AF.Exp, bias=nv1.t[:, 0:1]),
                         reads=[lg, nv1], writes=[ex])
                    yield
                    k.op("dve", lambda e: e.tensor_tensor(out=ex.t[:], in0=ex.t[:], in1=mask.t[:], op=ALU.mult),
                         reads=[ex, mask], writes=[ex])
                    yield
                    k.op("dve", lambda e: e.reduce_sum(out=sm.t[:], in_=ex.t[:], axis=AX.X), reads=[ex], writes=[sm])
                    yield
                    k.op("dve", lambda e: e.reciprocal(out=sm.t[:], in_=sm.t[:]), reads=[sm], writes=[sm])
                    yield
                    k.op("dve", lambda e: e.tensor_scalar(out=Gt[b].t[:], in0=ex.t[:], scalar1=sm.t[:, 0:1], scalar2=None,
                                                          op0=ALU.mult), reads=[ex, sm], writes=[Gt[b]])
                    yield
                    k.dma("sp", gsem[b], lambda e: e.dma_start(out=G_d[cs, :], in_=Gt[b].t[:]), reads=[Gt[b]])
                    yield
                    k.ops("pe", [
                        lambda e: e.matmul(p_r.t[:, 0, :], lhsT=Mlt.t[:], rhs=mask.t[:], start=True, stop=True),
                        lambda e: e.matmul(p_r.t[:, 1, :], lhsT=ones_f.t[:], rhs=mask.t[:], start=True, stop=True),
                    ], reads=[Mlt, ones_f, mask], writes=[p_r])
                    yield
                    k.op("dve", lambda e: e.tensor_tensor(out=rank.t[:], in0=p_r.t[:, 0, :], in1=cnt.t[:], op=ALU.add),
                         reads=[p_r, cnt], writes=[rank])
                    yield
                    k.op("dve", lambda e: e.tensor_tensor(out=cnt.t[:], in0=p_r.t[:, 1, :], in1=cnt.t[:], op=ALU.add),
                         reads=[p_r, cnt], writes=[cnt])
                    yield
                    k.op("dve", lambda e: e.tensor_scalar(out=vld.t[:], in0=rank.t[:], scalar1=float(CAP), scalar2=None,
                                                          op0=ALU.is_lt), reads=[rank], writes=[vld])
                    yield
                    k.op("dve", lambda e: e.tensor_tensor(out=vld.t[:], in0=vld.t[:], in1=mask.t[:], op=ALU.mult),
                         reads=[vld, mask], writes=[vld])
                    yield
                    k.op("dve", lambda e: e.tensor_tensor(out=val.t[:], in0=rank.t[:], in1=ebase.t[:], op=ALU.add),
                         reads=[rank, ebase], writes=[val])
                    yield
                    k.op("dve", lambda e: e.tensor_tensor(out=val.t[:], in0=val.t[:], in1=vld.t[:], op=ALU.mult),
                         reads=[val, vld], writes=[val])
                    yield
                    k.op("dve", lambda e: e.max(out=v8.t[:], in_=val.t[:]), reads=[val], writes=[v8])
                    yield
                    k.op("dve", lambda e: e.tensor_copy(out=dest_all.t[:, c, :], in_=v8.t[:, 0:4]), reads=[v8], writes=[dest_all])
                    yield
                    for kk in range(4):
                        scat_evs.append(k.dma("pool", scsem, lambda e: e.indirect_dma_start(
                            out=slot_d[:, :], out_offset=bass.IndirectOffsetOnAxis(ap=dest_all.t[:, c, kk:kk + 1], axis=0),
                            in_=tokid.t[:, c, :], in_offset=None),
                            reads=[dest_all, tokid], extra=ev_init))
                    yield
                ym_load(0)
                run_chains([a2body(sc, q) for sc in range(NSC) for q in range(4)], lag=8)
                a2_done = [x1[0], x1[1], h2b[0], h2b[1], Gt[0], Gt[1]]
                a2_evs = list(scat_evs[-1:])
                for t in a2_done:
                    a2_evs += t.r.rs
        else:
            a2_evs = []

        k.barrier()
        if "B" in phases:
            with ExitStack() as e4:
                bguT = load_const(e4, "bguT", [128, NE, 16], bguT_d)
                wgu = [sb(e4, "wgu%d" % i, [128, 8, 2048], BF16) for i in range(2)]
                wdn = [sb(e4, "wdn%d" % i, [128, 8, DM], BF16) for i in range(2)]
                bdb = [sb(e4, "bdb%d" % i, [128, DM], F32) for i in range(2)]
                wesem = [dsem(), dsem()]
                bdsem = [dsem(), dsem()]
                idx = [sb(e4, "idx%d" % i, [128, NBLK, 2], I32) for i in range(2)]
                isem = [dsem(), dsem()]
                xg = [sb(e4, "xg%d" % i, [128, DM], BF16) for i in range(NBLK)]
                xgsem = [dsem() for _ in range(NBLK)]
                gg = [sb(e4, "gg%d" % i, [128, NBLK, NE], F32) for i in range(2)]
                ggsem = [dsem(), dsem()]
                xgT = sb(e4, "xgT", [128, 8, CAP], BF16)
                actT = sb(e4, "actT", [128, 8, CAP], BF16)
                HW = CAP // 2
                gm = [sb(e4, "gm%d" % i, [128, HW], F32) for i in range(2)]
                sg = [sb(e4, "sg%d" % i, [128, HW], F32) for i in range(2)]
                u1 = [sb(e4, "u1_%d" % i, [128, HW], F32) for i in range(2)]
                yA = [sb(e4, "yA%d" % i, [128, DM], F32) for i in range(2)]
                yB = [sb(e4, "yB%d" % i, [128, DM], F32) for i in range(2)]
                ysem2 = [dsem(), dsem()]
                p_tg2 = [ps(e4, "p_tg%d" % i, [128, 8, 128], BF16) for i in range(2)]
                p_g = [ps(e4, "p_g%d" % i, [128, 512], F32) for i in range(2)]
                p_up = [ps(e4, "p_up%d" % i, [128, 512], F32) for i in range(2)]
                p_dh = [ps(e4, "p_dh%d" % i, [128, 512], F32) for i in range(2)]

                def load_w(e_):
                    s = e_ % 2
                    k.dma("pool", wesem[s],
                          [(lambda e, i=i: e.dma_start(out=wgu[s].t[:, 2 * i:2 * i + 2, :],
                                                       in_=wgu_d[e_, 256 * i:256 * (i + 1), :].rearrange("(k p) n -> p k n", p=128)))
                           for i in range(4)] +
                          [(lambda e, i=i: e.dma_start(out=wdn[s].t[:, 4 * i:4 * i + 4, :],
                                                       in_=wd_d[e_, 512 * i:512 * (i + 1), :].rearrange("(k p) n -> p k n", p=128)))
                           for i in range(2)],
                          writes=[wgu[s], wdn[s]])
                    k.dma("sp", bdsem[s], lambda e: e.dma_start(out=bdb[s].t[:], in_=bd_d[e_:e_ + 1, :].to_broadcast([128, DM])),
                          writes=[bdb[s]])

                def prefetch(e_):
                    s_ = e_ % 2
                    base_ = 1 + e_ * CAP
                    k.dma("sp", isem[s_], [(lambda e, j=j: e.dma_start(out=idx[s_].t[:, j, :],
                                                                       in_=slot_d[base_ + j * 128:base_ + (j + 1) * 128, :]))
                                           for j in range(NBLK)], writes=[idx[s_]], extra=a2_evs)
                    for j in range(NBLK):
                        k.dma("pool", xgsem[j], lambda e: e.indirect_dma_start(
                            out=xg[j].t[:, :], out_offset=None, in_=h2_d[:, :],
                            in_offset=bass.IndirectOffsetOnAxis(ap=idx[s_].t[:, j, 0:1], axis=0)),
                            reads=[idx[s_]], writes=[xg[j]], extra=a2_evs)
                    k.dma("pool", ggsem[s_], [(lambda e, j=j: e.indirect_dma_start(
                        out=gg[s_].t[:, j, :], out_offset=None, in_=G_d[:, :],
                        in_offset=bass.IndirectOffsetOnAxis(ap=idx[s_].t[:, j, 0:1], axis=0))) for j in range(NBLK)],
                        reads=[idx[s_]], writes=[gg[s_]], extra=a2_evs)

                def transposes():
                    for j in range(NBLK):
                        p_tg = p_tg2[j % 2]
                        k.ops("pe", [(lambda e, kk=kk: e.transpose(out=p_tg.t[:, kk, :], in_=xg[j].t[:, kk * 128:(kk + 1) * 128],
                                                                   identity=ident_bf.t[:])) for kk in range(8)],
                              reads=[xg[j], ident_bf], writes=[p_tg])
                        k.op("act", lambda e: e.copy(out=xgT.t[:, :, j * 128:(j + 1) * 128], in_=p_tg.t[:]),
                             reads=[p_tg], writes=[xgT])

                load_w(0)
                prefetch(0)
                transposes()
                for e_ in range(NE):
                    s = e_ % 2
                    base = 1 + e_ * CAP
                    if e_ + 1 < NE:
                        prefetch(e_ + 1)
                        load_w(e_ + 1)
                    for fc in range(8):
                        for h in range(2):
                            hs = slice(h * HW, (h + 1) * HW)
                            k.ops("pe", [(lambda e, kk=kk: e.matmul(p_g[h].t[:, 0:HW], lhsT=wgu[s].t[:, kk, fc * 128:(fc + 1) * 128],
                                                                    rhs=xgT.t[:, kk, hs], start=(kk == 0), stop=(kk == 7)))
                                         for kk in range(8)], reads=[wgu[s], xgT], writes=[p_g[h]])
                            k.ops("pe", [(lambda e, kk=kk: e.matmul(p_up[h].t[:, 0:HW],
                                                                    lhsT=wgu[s].t[:, kk, 1024 + fc * 128:1024 + (fc + 1) * 128],
                                                                    rhs=xgT.t[:, kk, hs], start=(kk == 0), stop=(kk == 7)))
                                         for kk in range(8)], reads=[wgu[s], xgT], writes=[p_up[h]])
                            k.op("dve", lambda e: e.tensor_scalar(out=gm[h].t[:], in0=p_g[h].t[:, 0:HW],
                                                                  scalar1=bguT.t[:, e_, fc:fc + 1], scalar2=7.0,
                                                                  op0=ALU.add, op1=ALU.min), reads=[p_g[h], bguT], writes=[gm[h]])
                            k.op("act", lambda e: e.activation(out=sg[h].t[:], in_=gm[h].t[:], func=AF.Sigmoid, scale=1.702),
                                 reads=[gm[h]], writes=[sg[h]])
                            k.op("dve", lambda e: e.tensor_scalar(out=u1[h].t[:], in0=p_up[h].t[:, 0:HW],
                                                                  scalar1=bguT.t[:, e_, 8 + fc:9 + fc], scalar2=7.0,
                                                                  op0=ALU.add, op1=ALU.min), reads=[p_up[h], bguT], writes=[u1[h]])
                            k.op("dve", lambda e: e.tensor_scalar(out=u1[h].t[:], in0=u1[h].t[:], scalar1=-7.0, scalar2=1.0,
                                                                  op0=ALU.max, op1=ALU.add), reads=[u1[h]], writes=[u1[h]])
                            k.op("dve", lambda e: e.tensor_tensor(out=sg[h].t[:], in0=gm[h].t[:], in1=sg[h].t[:], op=ALU.mult),
                                 reads=[gm[h], sg[h]], writes=[sg[h]])
                            k.op("dve", lambda e: e.tensor_tensor(out=actT.t[:, fc, hs], in0=sg[h].t[:], in1=u1[h].t[:], op=ALU.mult),
                                 reads=[sg[h], u1[h]], writes=[actT])
                    if e_ + 1 < NE:
                        transposes()
                    for j in range(NBLK):
                        js = slice(j * 128, (j + 1) * 128)
                        b = j % 2
                        for h in range(2):
                            k.ops("pe", [(lambda e, kk=kk: e.matmul(p_dh[h].t[:], lhsT=actT.t[:, kk, js],
                                                                    rhs=wdn[s].t[:, kk, h * 512:(h + 1) * 512],
                                                                    start=(kk == 0), stop=(kk == 7)))
                                         for kk in range(8)], reads=[actT, wdn[s]], writes=[p_dh[h]])
                            k.op("dve", lambda e: e.tensor_tensor(out=yA[b].t[:, h * 512:(h + 1) * 512], in0=p_dh[h].t[:],
                                                                  in1=bdb[s].t[:, h * 512:(h + 1) * 512], op=ALU.add),
                                 reads=[p_dh[h], bdb[s]], writes=[yA[b]])
                        k.op("act", lambda e: e.activation(out=yB[b].t[:], in_=yA[b].t[:], func=AF.Copy,
                                                           scale=gg[s].t[:, j, e_:e_ + 1]),
                             reads=[yA[b], gg[s]], writes=[yB[b]])
                        k.dma("sp", ysem2[b], lambda e: e.dma_start(out=Y_d[base + j * 128:base + (j + 1) * 128, :], in_=yB[b].t[:]),
                              reads=[yB[b]])
                b_evs = []
                for t in yB:
                    b_evs += t.r.rs
        else:
            b_evs = []

        k.barrier()
        fin_evs = []
        if "C" in phases:
            with ExitStack() as e5:
                wpg = sb(e5, "wpg", [128, 8, DM], BF16)
                k.dma("pool", sem_cp, [(lambda e, i=i: e.dma_start(
                    out=wpg.t[:, 4 * i:4 * i + 4, :],
                    in_=wpg_d[512 * i:512 * (i + 1), :].rearrange("(k p) n -> p k n", p=128))) for i in range(2)], writes=[wpg])
                wpp = sb(e5, "wpp", [128, 2, DM], BF16)
                k.dma("pool", sem_cp, lambda e: e.dma_start(out=wpp.t[:], in_=wpp_d.rearrange("(k p) n -> p k n", p=128)),
                      writes=[wpp])
                gpgT = load_const(e5, "gpgT", [128, 8], gpgT_d)
                gpn_bc = load_const(e5, "gpn_bc", [128, DM], gpn_bc_d)
                gfin_bc = load_const(e5, "gfin_bc", [128, DM], gfin_bc_d)
                x1c = [sb(e5, "x1c%d" % i, [128, DM], F32) for i in range(3)]
                x1csem = [dsem(), dsem(), dsem()]
                yk = [[sb(e5, "yk%d_%d" % (i, kk), [128, DM], F32) for kk in range(4)] for i in range(3)]
                yksem = [[dsem() for kk in range(4)] for i in range(3)]
                pt = [sb(e5, "pt%d" % i, [128, 256], F32) for i in range(3)]
                ptsem = [dsem(), dsem(), dsem()]
                x2_l = [sb(e5, "x2%d" % i_, [128, DM], F32) for i_ in range(2)]
                junk4_l = [sb(e5, "junk4%d" % i_, [128, DM], F32) for i_ in range(2)]
                ssc_l = [sb(e5, "ssc%d" % i_, [128, 1], F32) for i_ in range(2)]
                rsc_l = [sb(e5, "rsc%d" % i_, [128, 1], F32) for i_ in range(2)]
                xnb_l = [sb(e5, "xnb%d" % i_, [128, DM], BF16) for i_ in range(2)]
                xnT_l = [sb(e5, "xnT%d" % i_, [128, 8, 128], BF16) for i_ in range(2)]
                sgate_l = [sb(e5, "sgate%d" % i_, [128, DM], F32) for i_ in range(2)]
                pT_l = [sb(e5, "pT%d" % i_, [128, 2, 128], BF16) for i_ in range(2)]
                sse_l = [sb(e5, "sse%d" % i_, [128, 1], F32) for i_ in range(2)]
                rse_l = [sb(e5, "rse%d" % i_, [128, 1], F32) for i_ in range(2)]
                e1__l = [sb(e5, "e1_%d" % i_, [128, DM], F32) for i_ in range(2)]
                x3_l = [sb(e5, "x3%d" % i_, [128, DM], F32) for i_ in range(2)]
                ssf_l = [sb(e5, "ssf%d" % i_, [128, 1], F32) for i_ in range(2)]
                rsf_l = [sb(e5, "rsf%d" % i_, [128, 1], F32) for i_ in range(2)]
                ot = [sb(e5, "ot%d" % i, [128, DM], F32) for i in range(2)]
                osem = [dsem(), dsem()]
                p_t3 = ps(e5, "p_t3", [128, 8, 128], BF16)
                p_ga = ps(e5, "p_ga", [128, DM], F32)
                p_pt = ps(e5, "p_pt", [128, 2, 128], F32)
                p_e = ps(e5, "p_e", [128, DM], F32)
                def cload(c):
                    b3 = c % 3
                    cs = slice(c * 128, (c + 1) * 128)
                    k.dma("sp", x1csem[b3], lambda e: e.dma_start(out=x1c[b3].t[:], in_=x1_d[cs, :]), writes=[x1c[b3]], extra=a2_evs)
                    k.dma("sp", ptsem[b3], lambda e: e.dma_start(out=pt[b3].t[:], in_=pin_d[cs, :]), writes=[pt[b3]])
                    for kk in range(4):
                        k.dma("pool", yksem[b3][kk], lambda e: e.indirect_dma_start(
                            out=yk[b3][kk].t[:, :], out_offset=None, in_=Y_d[:, :],
                            in_offset=bass.IndirectOffsetOnAxis(ap=dest_all.t[:, c, kk:kk + 1], axis=0)),
                            reads=[dest_all], writes=[yk[b3][kk]], extra=b_evs)

                def cbody(c):
                    b = c % 2
                    b3 = c % 3
                    cs = slice(c * 128, (c + 1) * 128)
                    x2 = x2_l[b]; junk4 = junk4_l[b]; ssc = ssc_l[b]; rsc = rsc_l[b]; xnb = xnb_l[b]; xnT = xnT_l[b]; sgate = sgate_l[b]; pT = pT_l[b]; sse = sse_l[b]; rse = rse_l[b]; e1_ = e1__l[b]; x3 = x3_l[b]; ssf = ssf_l[b]; rsf = rsf_l[b]
                    if c == 0:
                        cload(0)
                    if c + 1 < NCH:
                        cload(c + 1)
                    yield
                    k.op("dve", lambda e: e.tensor_tensor(out=x2.t[:], in0=x1c[b3].t[:], in1=yk[b3][0].t[:], op=ALU.add),
                         reads=[x1c[b3], yk[b3][0]], writes=[x2])
                    yield
                    k.op("dve", lambda e: e.tensor_tensor(out=yk[b3][1].t[:], in0=yk[b3][1].t[:], in1=yk[b3][2].t[:], op=ALU.add),
                         reads=[yk[b3][1], yk[b3][2]], writes=[yk[b3][1]])
                    yield
                    k.op("dve", lambda e: e.tensor_tensor(out=x2.t[:], in0=x2.t[:], in1=yk[b3][3].t[:], op=ALU.add),
                         reads=[x2, yk[b3][3]], writes=[x2])
                    yield
                    k.op("dve", lambda e: e.tensor_tensor(out=x2.t[:], in0=x2.t[:], in1=yk[b3][1].t[:], op=ALU.add),
                         reads=[x2, yk[b3][1]], writes=[x2])
                    yield
                    k.op("act", lambda e: e.activation(out=junk4.t[:], in_=x2.t[:], func=AF.Square, accum_out=ssc.t[:, 0:1]),
                         reads=[x2], writes=[junk4, ssc])
                    yield
                    k.op("pool", lambda e: e.tensor_scalar(out=rsc.t[:], in0=ssc.t[:], scalar1=1.0 / DM, scalar2=EPS,
                                                           op0=ALU.mult, op1=ALU.add), reads=[ssc], writes=[rsc])
                    yield
                    k.op("pool", lambda e: e.tensor_tensor(out=rsc.t[:], in0=rsc.t[:], in1=neghalf_p.t[:], op=ALU.pow),
                         reads=[rsc, neghalf_p], writes=[rsc])
                    yield
                    k.op("act", lambda e: e.activation(out=xnb.t[:], in_=x2.t[:], func=AF.Copy, scale=rsc.t[:, 0:1]),
                         reads=[x2, rsc], writes=[xnb])
                    yield
                    k.ops("pe", [(lambda e, j=j: e.transpose(out=p_t3.t[:, j, :], in_=xnb.t[:, j * 128:(j + 1) * 128],
                                                             identity=ident_bf.t[:])) for j in range(8)],
                          reads=[xnb, ident_bf], writes=[p_t3])
                    yield
                    k.op("dve", lambda e: e.tensor_tensor(out=xnT.t[:], in0=p_t3.t[:],
                                                          in1=gpgT.t[:, :, None].to_broadcast([128, 8, 128]), op=ALU.mult),
                         reads=[p_t3, gpgT], writes=[xnT])
                    yield
                    k.ops("pe", [(lambda e, j=j, h=h: e.matmul(p_ga.t[:, h * 512:(h + 1) * 512], lhsT=xnT.t[:, j, :],
                                                               rhs=wpg.t[:, j, h * 512:(h + 1) * 512], start=(j == 0), stop=(j == 7)))
                                 for h in range(2) for j in range(8)], reads=[xnT, wpg], writes=[p_ga])
                    yield
                    k.op("act", lambda e: e.activation(out=sgate.t[:], in_=p_ga.t[:], func=AF.Sigmoid), reads=[p_ga], writes=[sgate])
                    yield
                    k.ops("pe", [(lambda e, j=j: e.transpose(out=p_pt.t[:, j, :], in_=pt[b3].t[:, j * 128:(j + 1) * 128],
                                                             identity=ident_f.t[:])) for j in range(2)],
                          reads=[pt[b3], ident_f], writes=[p_pt])
                    yield
                    k.op("act", lambda e: e.copy(out=pT.t[:], in_=p_pt.t[:]), reads=[p_pt], writes=[pT])
                    yield
                    k.ops("pe", [(lambda e, j=j, h=h: e.matmul(p_e.t[:, h * 512:(h + 1) * 512], lhsT=pT.t[:, j, :],
                                                               rhs=wpp.t[:, j, h * 512:(h + 1) * 512], start=(j == 0), stop=(j == 1)))
                                 for h in range(2) for j in range(2)], reads=[pT, wpp], writes=[p_e])
                    yield
                    k.op("act", lambda e: e.activation(out=junk4.t[:], in_=p_e.t[:], func=AF.Square, accum_out=sse.t[:, 0:1]),
                         reads=[p_e], writes=[junk4, sse])
                    yield
                    k.op("pool", lambda e: e.tensor_scalar(out=rse.t[:], in0=sse.t[:], scalar1=1.0 / DM, scalar2=EPS,
                                                           op0=ALU.mult, op1=ALU.add), reads=[sse], writes=[rse])
                    yield
                    k.op("pool", lambda e: e.tensor_tensor(out=rse.t[:], in0=rse.t[:], in1=neghalf_p.t[:], op=ALU.pow),
                         reads=[rse, neghalf_p], writes=[rse])
                    yield
                    k.op("dve", lambda e: e.scalar_tensor_tensor(out=e1_.t[:], in0=p_e.t[:], scalar=rse.t[:, 0:1], in1=gpn_bc.t[:],
                                                                 op0=ALU.mult, op1=ALU.mult), reads=[p_e, rse, gpn_bc], writes=[e1_])
                    yield
                    k.op("dve", lambda e: e.tensor_tensor(out=e1_.t[:], in0=e1_.t[:], in1=sgate.t[:], op=ALU.mult),
                         reads=[e1_, sgate], writes=[e1_])
                    yield
                    k.op("dve", lambda e: e.tensor_tensor(out=x3.t[:], in0=x2.t[:], in1=e1_.t[:], op=ALU.add),
                         reads=[x2, e1_], writes=[x3])
                    yield
                    k.op("act", lambda e: e.activation(out=junk4.t[:], in_=x3.t[:], func=AF.Square, accum_out=ssf.t[:, 0:1]),
                         reads=[x3], writes=[junk4, ssf])
                    yield
                    k.op("pool", lambda e: e.tensor_scalar(out=rsf.t[:], in0=ssf.t[:], scalar1=1.0 / DM, scalar2=EPS,
                                                           op0=ALU.mult, op1=ALU.add), reads=[ssf], writes=[rsf])
                    yield
                    k.op("pool", lambda e: e.tensor_tensor(out=rsf.t[:], in0=rsf.t[:], in1=neghalf_p.t[:], op=ALU.pow),
                         reads=[rsf, neghalf_p], writes=[rsf])
                    yield
                    k.op("dve", lambda e: e.scalar_tensor_tensor(out=ot[b].t[:], in0=x3.t[:], scalar=rsf.t[:, 0:1], in1=gfin_bc.t[:],
                                                                 op0=ALU.mult, op1=ALU.mult), reads=[x3, rsf, gfin_bc], writes=[ot[b]])
                    yield
                    fin_evs.append(k.dma("sp", osem[b], lambda e: e.dma_start(out=out_d[cs, :], in_=ot[b].t[:]), reads=[ot[b]]))

                    yield
                run_chains([cbody(c) for c in range(NCH)], lag=6)

        tail = list(fin_evs[-2:]) + list(a2_evs) + list(b_evs)
        for sid_ev in tail:
            k.wait("sp", sid_ev)
        for sid_, sem_ in k.dma_objs.items():
            k.wait("sp", (sem_, k.dma_cnt[sid_]))
        build.stats = (k.ninst, k.nwaits, dict(k.cnt))
    return nc


def host_layout(inp):
    f = np.float32

    def colT(v, n):
        return np.ascontiguousarray(np.asarray(v, f).reshape(n, 128).T)

    def bc(v):
        v = np.asarray(v, f).reshape(1, -1)
        return np.ascontiguousarray(np.broadcast_to(v, (128, v.shape[1])))

    cw = np.asarray(inp["conv_w"][0], f)
    conv_wT = np.ascontiguousarray(cw.reshape(4, 32, 128).transpose(2, 1, 0))
    bgu = np.asarray(inp["b_gate_up"][0], f)
    bguT = np.ascontiguousarray(bgu.reshape(NE, 16, 128).transpose(2, 0, 1))
    invc = np.zeros((128, 4, 16), f)
    for gi, w in enumerate((2, 4, 8, 16)):
        invc[:, gi, :] = 1.0 / np.minimum(np.arange(16) + 1, w).astype(f)
    ebase = (1 + np.arange(NE) * CAP).astype(f)
    shared = {
        "w_in": np.ascontiguousarray(inp["w_in"][0], f),
        "conv_wT": conv_wT,
        "conv_bT": colT(inp["conv_b"][0], 32),
        "conv_brow": np.ascontiguousarray(np.asarray(inp["conv_b"][0], f).reshape(1, 4096)),
        "dt_bias_bc": bc(inp["dt_bias"][0]),
        "a_log_bc": bc(inp["a_log"][0]),
        "d_skip_bc": bc(inp["d_skip"][0]),
        "gmixT": colT(inp["mix_norm_g"][0], 8),
        "gssdT": colT(inp["ssd_norm_g"][0], 16),
        "pool_w": np.ascontiguousarray(inp["pool_w"][0], f),
        "pscT": colT(inp["pool_scale"][0], 8),
        "invc": invc,
        "w_out": np.ascontiguousarray(inp["w_out"][0], f),
        "gffn_bc": bc(inp["ffn_norm_g"][0]),
        "router_w": np.ascontiguousarray(inp["router_w"][0], f),
        "rb_bc": bc(inp["router_b"][0]),
        "ebase_bc": bc(ebase),
        "w_gate_up": np.ascontiguousarray(inp["w_gate_up"][0], f),
        "bguT": bguT,
        "w_down": np.ascontiguousarray(inp["w_down"][0], f),
        "b_down": np.ascontiguousarray(inp["b_down"][0], f),
        "gpgT": colT(inp["ple_gate_norm_g"][0], 8),
        "w_ple_gate": np.ascontiguousarray(inp["w_ple_gate"][0], f),
        "w_ple_proj": np.ascontiguousarray(inp["w_ple_proj"][0], f),
        "gpn_bc": bc(inp["ple_norm_g"][0]),
        "gfin_bc": bc(inp["final_norm_g"]),
    }
    return shared


def kernel(**inputs):
    inp = {k_: np.asarray(v) for k_, v in inputs.items()}
    shared = host_layout(inp)
    x = np.asarray(inp["x"], np.float32)
    p = np.asarray(inp["p"], np.float32)[0]
    nb = x.shape[0]
    in_maps = []
    for b in range(nb):
        m = dict(shared)
        m["x"] = np.ascontiguousarray(x[b])
        m["p"] = np.ascontiguousarray(p[b])
        in_maps.append(m)
    nc = build()
    res = run_bass_kernel_spmd(nc, in_maps, core_ids=list(range(nb)))
    return np.stack([np.asarray(r["out"], np.float32) for r in res.results], axis=0)
```
